# Optimizing a Trainium2 kernel written in Bass

```python
import jax, jax.numpy as jnp
from jax import lax
import numpy as np

D_MODEL = 1024
BATCH = 2
SEQ = 16384
DEPTH = 2

GRID_W = 64
CTX_LEN = 256
EPS = 1e-6

N_HEADS = 8
N_KV_HEADS = 2
HEAD_DIM = 64
GQA_GROUP = N_HEADS // N_KV_HEADS
WINDOW = 128
BLOCK = 128
ROPE_BASE = 10000.0
Q_DIM = N_HEADS * HEAD_DIM
KV_DIM = N_KV_HEADS * HEAD_DIM

FNET_GROUPS = 4
FNET_GROUP_DIM = 128
FNET_WIDTH = FNET_GROUPS * FNET_GROUP_DIM

CONV_DIM = 512
CONV_K = 3

N_BRANCH = 3

Q_OFF = 0
K_OFF = Q_OFF + Q_DIM
V_OFF = K_OFF + KV_DIM
F_OFF = V_OFF + KV_DIM
CX_OFF = F_OFF + FNET_WIDTH
CB_OFF = CX_OFF + CONV_DIM
CC_OFF = CB_OFF + CONV_DIM
GATE_OFF = CC_OFF + CONV_DIM
IN_DIM = GATE_OFF + N_BRANCH * D_MODEL

D_FF = 2816
N_EXPERTS = 8
TOP_K = 2
D_EXPERT = 3584
N_DENSE = (DEPTH + 1) // 2
N_MOE = DEPTH // 2

kernel_name = "hybrid_gated_parallel_dit_block"


def rms_norm(x, g):
    xf = x.astype(jnp.float32)
    y = xf * lax.rsqrt(jnp.mean(xf * xf, axis=-1, keepdims=True) + EPS)
    return (y * g.astype(jnp.float32)).astype(x.dtype)


def axial_rope_tables(n_tokens):
    rows = n_tokens // GRID_W
    row = jnp.repeat(jnp.arange(rows), GRID_W).astype(jnp.float32)
    col = jnp.tile(jnp.arange(GRID_W), rows).astype(jnp.float32)
    half = HEAD_DIM // 2
    inv_freq = 1.0 / (ROPE_BASE ** (jnp.arange(0, half, 2, dtype=jnp.float32) / half))
    ang = jnp.concatenate([row[:, None] * inv_freq, col[:, None] * inv_freq], axis=-1)
    return jnp.cos(ang), jnp.sin(ang)


def apply_rope(x, cos, sin):
    xf = x.astype(jnp.float32).reshape(x.shape[:-1] + (HEAD_DIM // 2, 2))
    x0, x1 = xf[..., 0], xf[..., 1]
    cs = cos[None, :, None, :]
    sn = sin[None, :, None, :]
    out = jnp.stack([x0 * cs - x1 * sn, x0 * sn + x1 * cs], axis=-1).reshape(x.shape)
    return out.astype(x.dtype)


def latent_window_attention(q, k, v, kc, vc, sink):
    B, S = q.shape[0], q.shape[1]
    nb = S // BLOCK
    scale = HEAD_DIM ** -0.5
    qb = q.reshape(B, nb, BLOCK, N_KV_HEADS, GQA_GROUP, HEAD_DIM).transpose(1, 0, 2, 3, 4, 5)
    pad = ((0, 0), (BLOCK, BLOCK), (0, 0), (0, 0))
    kp = jnp.pad(k, pad)
    vp = jnp.pad(v, pad)
    sink_l = sink.astype(jnp.float32).reshape(1, N_KV_HEADS, GQA_GROUP, 1, 1)
    rel = jnp.arange(3 * BLOCK)[None, :] - BLOCK - jnp.arange(BLOCK)[:, None]
    band = jnp.abs(rel) <= WINDOW

    def block_fn(args):
        i, qi = args
        start = i * BLOCK
        ki = lax.dynamic_slice_in_dim(kp, start, 3 * BLOCK, axis=1)
        vi = lax.dynamic_slice_in_dim(vp, start, 3 * BLOCK, axis=1)
        kpos = start - BLOCK + jnp.arange(3 * BLOCK)
        mask = band & ((kpos >= 0) & (kpos < S))[None, :]
        s_loc = jnp.einsum('bqhgd,bkhd->bhgqk', qi, ki, preferred_element_type=jnp.float32) * scale
        s_loc = jnp.where(mask, s_loc, -jnp.inf)
        s_ctx = jnp.einsum('bqhgd,bkhd->bhgqk', qi, kc, preferred_element_type=jnp.float32) * scale
        s_sink = jnp.broadcast_to(sink_l, s_ctx.shape[:-1] + (1,))
        p = jax.nn.softmax(jnp.concatenate([s_loc, s_ctx, s_sink], axis=-1), axis=-1)
        p_loc = p[..., :3 * BLOCK].astype(v.dtype)
        p_ctx = p[..., 3 * BLOCK:3 * BLOCK + kc.shape[1]].astype(vc.dtype)
        return (jnp.einsum('bhgqk,bkhd->bqhgd', p_loc, vi)
                + jnp.einsum('bhgqk,bkhd->bqhgd', p_ctx, vc))

    out = lax.map(block_fn, (jnp.arange(nb), qb))
    return out.transpose(1, 0, 2, 3, 4, 5).reshape(B, S, Q_DIM)


def context_attention(qc, kc, vc, sink):
    B, L = qc.shape[0], qc.shape[1]
    scale = HEAD_DIM ** -0.5
    qg = qc.reshape(B, L, N_KV_HEADS, GQA_GROUP, HEAD_DIM)
    s = jnp.einsum('bqhgd,bkhd->bhgqk', qg, kc, preferred_element_type=jnp.float32) * scale
    s_sink = jnp.broadcast_to(sink.astype(jnp.float32).reshape(1, N_KV_HEADS, GQA_GROUP, 1, 1), s.shape[:-1] + (1,))
    p = jax.nn.softmax(jnp.concatenate([s, s_sink], axis=-1), axis=-1)
    o = jnp.einsum('bhgqk,bkhd->bqhgd', p[..., :L].astype(vc.dtype), vc)
    return o.reshape(B, L, Q_DIM)


def fourier_mix(u):
    B, N = u.shape[0], u.shape[1]
    ug = u.astype(jnp.float32).reshape(B, N, FNET_GROUPS, FNET_GROUP_DIM)
    f = jnp.fft.fft2(ug, axes=(1, 3), norm="ortho").real
    return f.reshape(B, N, FNET_WIDTH).astype(u.dtype)


def short_conv_mix(xin, bg, cg, w_conv):
    u = cg * xin
    up = jnp.pad(u, ((0, 0), (1, 1), (0, 0)))
    y = up[:, :-2] * w_conv[0] + up[:, 1:-1] * w_conv[1] + up[:, 2:] * w_conv[2]
    return bg * y


def merge_branches(z, attn, w_conv, w_attn_o, w_fnet, w_conv_out, w_o):
    y_attn = attn @ w_attn_o
    y_fnet = fourier_mix(z[..., F_OFF:F_OFF + FNET_WIDTH]) @ w_fnet
    y_conv = short_conv_mix(z[..., CX_OFF:CX_OFF + CONV_DIM], z[..., CB_OFF:CB_OFF + CONV_DIM],
                            z[..., CC_OFF:CC_OFF + CONV_DIM], w_conv) @ w_conv_out
    gates = jax.nn.sigmoid(z[..., GATE_OFF:].astype(jnp.float32)).astype(z.dtype)
    gates = gates.reshape(z.shape[:-1] + (N_BRANCH, D_MODEL))
    merged = gates[..., 0, :] * y_attn + gates[..., 1, :] * y_fnet + gates[..., 2, :] * y_conv
    return merged @ w_o


def swiglu(h, w_gate, w_up, w_down):
    return (jax.nn.silu(h @ w_gate) * (h @ w_up)) @ w_down


def moe_swiglu(h, w_router, b_router, w_gate, w_up, w_down):
    logits = jnp.einsum('bsd,de->bse', h, w_router, preferred_element_type=jnp.float32) + b_router.astype(jnp.float32)
    top_val, top_idx = lax.top_k(logits, TOP_K)
    top_w = jax.nn.softmax(top_val, axis=-1)
    gates = jnp.sum(jax.nn.one_hot(top_idx, N_EXPERTS, dtype=jnp.float32) * top_w[..., None], axis=-2)
    gates = gates.astype(h.dtype)
    out = gates[..., 0:1] * swiglu(h, w_gate[0], w_up[0], w_down[0])
    for e in range(1, N_EXPERTS):
        out = out + gates[..., e:e + 1] * swiglu(h, w_gate[e], w_up[e], w_down[e])
    return out


def setup_inputs(seed: int = 0) -> dict:
    key = jax.random.key(seed)
    ks = jax.random.split(key, 32)
    D = D_MODEL
    nrm = lambda k, shape, s: jax.random.normal(k, shape, jnp.float32) * s
    gain = lambda k: 1.0 + nrm(k, (DEPTH, D), 0.05)
    return {
        "x": nrm(ks[0], (BATCH, SEQ, D), 1.0),
        "c": nrm(ks[1], (BATCH, D), 1.0),
        "ctx": nrm(ks[2], (BATCH, CTX_LEN, D), 1.0),
        "c_ctx": nrm(ks[3], (D,), 1.0),
        "w_mod": nrm(ks[4], (DEPTH, D, 6 * D), 0.5 * D ** -0.5),
        "b_mod": nrm(ks[5], (DEPTH, 6 * D), 0.02),
        "g_pre_mix": gain(ks[6]),
        "g_post_mix": gain(ks[7]),
        "g_pre_ffn": gain(ks[8]),
        "g_post_ffn": gain(ks[9]),
        "w_in": nrm(ks[10], (DEPTH, D, IN_DIM), D ** -0.5),
        "attn_sink": nrm(ks[11], (DEPTH, N_HEADS), 0.5),
        "w_conv": nrm(ks[12], (DEPTH, CONV_K, CONV_DIM), CONV_K ** -0.5),
        "w_attn_o": nrm(ks[13], (DEPTH, Q_DIM, D), Q_DIM ** -0.5),
        "w_fnet": nrm(ks[14], (DEPTH, FNET_WIDTH, D), FNET_WIDTH ** -0.5),
        "w_conv_out": nrm(ks[15], (DEPTH, CONV_DIM, D), CONV_DIM ** -0.5),
        "w_o": nrm(ks[16], (DEPTH, D, D), D ** -0.5),
        "w_ff_gate": nrm(ks[17], (N_DENSE, D, D_FF), D ** -0.5),
        "w_ff_up": nrm(ks[18], (N_DENSE, D, D_FF), D ** -0.5),
        "w_ff_down": nrm(ks[19], (N_DENSE, D_FF, D), D_FF ** -0.5),
        "w_router": nrm(ks[20], (N_MOE, D, N_EXPERTS), D ** -0.5),
        "b_router": nrm(ks[21], (N_MOE, N_EXPERTS), 0.01),
        "w_exp_gate": nrm(ks[22], (N_MOE, N_EXPERTS, D, D_EXPERT), D ** -0.5),
        "w_exp_up": nrm(ks[23], (N_MOE, N_EXPERTS, D, D_EXPERT), D ** -0.5),
        "w_exp_down": nrm(ks[24], (N_MOE, N_EXPERTS, D_EXPERT, D), D_EXPERT ** -0.5),
    }


def reference(x, c, ctx, c_ctx, w_mod, b_mod, g_pre_mix, g_post_mix, g_pre_ffn, g_post_ffn,
              w_in, attn_sink, w_conv, w_attn_o, w_fnet, w_conv_out, w_o,
              w_ff_gate, w_ff_up, w_ff_down, w_router, b_router, w_exp_gate, w_exp_up, w_exp_down):
    B, S = x.shape[0], x.shape[1]
    L = ctx.shape[1]
    D = D_MODEL
    cos, sin = axial_rope_tables(S)
    hctx = ctx
    for l in range(DEPTH):
        last = l == DEPTH - 1
        mod = jax.nn.silu(c) @ w_mod[l] + b_mod[l]
        sh1, sc1, ga1, sh2, sc2, ga2 = [m[:, None, :] for m in jnp.split(mod, 6, axis=-1)]
        if last:
            cmod = jax.nn.silu(c_ctx) @ w_mod[l][:, :2 * D] + b_mod[l][:2 * D]
            csh1, csc1 = jnp.split(cmod, 2, axis=-1)
        else:
            cmod = jax.nn.silu(c_ctx) @ w_mod[l] + b_mod[l]
            csh1, csc1, cga1, csh2, csc2, cga2 = jnp.split(cmod, 6, axis=-1)

        hc = rms_norm(hctx, g_pre_mix[l]) * (1 + csc1) + csh1
        if last:
            zc_kv = hc @ w_in[l][:, K_OFF:F_OFF]
        else:
            zc = hc @ w_in[l]
            zc_kv = zc[..., K_OFF:F_OFF]
        kc = zc_kv[..., :KV_DIM].reshape(B, L, N_KV_HEADS, HEAD_DIM)
        vc = zc_kv[..., KV_DIM:].reshape(B, L, N_KV_HEADS, HEAD_DIM)

        h = rms_norm(x, g_pre_mix[l]) * (1 + sc1) + sh1
        z = h @ w_in[l]
        q = apply_rope(z[..., Q_OFF:K_OFF].reshape(B, S, N_HEADS, HEAD_DIM), cos, sin)
        k = apply_rope(z[..., K_OFF:V_OFF].reshape(B, S, N_KV_HEADS, HEAD_DIM), cos, sin)
        v = z[..., V_OFF:F_OFF].reshape(B, S, N_KV_HEADS, HEAD_DIM)
        attn = latent_window_attention(q, k, v, kc, vc, attn_sink[l])
        mix = merge_branches(z, attn, w_conv[l], w_attn_o[l], w_fnet[l], w_conv_out[l], w_o[l])
        x = x + ga1 * rms_norm(mix, g_post_mix[l])

        h2 = rms_norm(x, g_pre_ffn[l]) * (1 + sc2) + sh2
        if l % 2 == 0:
            f = swiglu(h2, w_ff_gate[l // 2], w_ff_up[l // 2], w_ff_down[l // 2])
        else:
            f = moe_swiglu(h2, w_router[l // 2], b_router[l // 2], w_exp_gate[l // 2],
                           w_exp_up[l // 2], w_exp_down[l // 2])
        x = x + ga2 * rms_norm(f, g_post_ffn[l])

        if not last:
            qc = zc[..., Q_OFF:K_OFF].reshape(B, L, N_HEADS, HEAD_DIM)
            attn_c = context_attention(qc, kc, vc, attn_sink[l])
            mix_c = merge_branches(zc, attn_c, w_conv[l], w_attn_o[l], w_fnet[l], w_conv_out[l], w_o[l])
            hctx = hctx + cga1 * rms_norm(mix_c, g_post_mix[l])
            hc2 = rms_norm(hctx, g_pre_ffn[l]) * (1 + csc2) + csh2
            if l % 2 == 0:
                fc = swiglu(hc2, w_ff_gate[l // 2], w_ff_up[l // 2], w_ff_down[l // 2])
            else:
                fc = moe_swiglu(hc2, w_router[l // 2], b_router[l // 2], w_exp_gate[l // 2],
                                w_exp_up[l // 2], w_exp_down[l // 2])
            hctx = hctx + cga2 * rms_norm(fc, g_post_ffn[l])
    return x
```

```python
import contextlib
import math
import numpy as np
import ml_dtypes
import concourse.bass as bass
import concourse.mybir as mybir
from concourse.bass_utils import run_bass_kernel_spmd

F32 = mybir.dt.float32
BF16 = mybir.dt.bfloat16
AF = mybir.ActivationFunctionType
ALU = mybir.AluOpType
AX = mybir.AxisListType

ENGS = ("pe", "act", "dve", "pool", "sp")
SEM_EPOCH = 30000
SAME_ENGINE_SYNC = True
DEBUG_SCRATCH = False

D = 1024
SEQ = 16384
NB = 2
TOK = 4096
NTILE = 32
CTX = 256
Q_OFF, K_OFF, V_OFF, F_OFF, CX_OFF, CB_OFF, CC_OFF, GATE_OFF = 0, 512, 640, 768, 1280, 1792, 2304, 2816
IN_DIM = 5888
D_FF = 2816
NEXP = 8
D_EXP = 3584
EPS = 1e-6
PA_F, PA_K, PA_V, PA_CX, PA_CC = 0, 512, 640, 768, 1280
PA_W = 1792
PM_Q, PM_CB, PM_G = 1792, 2304, 2816


class Buf:
    __slots__ = ("name", "last_write", "reads", "dkey")

    def __init__(self, name):
        self.name = name
        self.last_write = None
        self.reads = {}
        self.dkey = None


class Tile:
    __slots__ = ("ap", "b")

    def __init__(self, ap, b):
        self.ap = ap
        self.b = b


class _Rec:
    def __init__(self):
        self.calls = []

    def __getattr__(self, name):
        def f(*a, **kw):
            self.calls.append((name, a, kw))
            return None
        return f


class Prog:
    def __init__(self, nc):
        self.nc = nc
        self.stack = contextlib.ExitStack()
        self.q = {e: [] for e in ENGS}
        self.cnt = {e: 0 for e in ENGS}
        self.epoch = {e: 0 for e in ENGS}
        self.pending = {e: False for e in ENGS}
        self.last_tok = {e: None for e in ENGS}
        self.dcnt = {}
        self.waited = {e: {} for e in ENGS}
        self.semkeys = []
        self.sems = {}
        self.nbuf = 0
        self.out_tokens = []
        self.free_dkeys = {"sp": [], "pool": [], "act": []}
        self.dma_bufs = []

    def sbuf(self, name, shape, dtype):
        return self.stack.enter_context(self.nc.sbuf_tensor(name, list(shape), dtype))

    def psum(self, name, shape, dtype=F32):
        return self.stack.enter_context(self.nc.psum_tensor(name, list(shape), dtype))

    def buf(self, name=None):
        self.nbuf += 1
        return Buf(name or f"b{self.nbuf}")

    def _semkey(self, k):
        if k not in self.sems:
            self.sems[k] = None
            self.semkeys.append(k)
        return k

    @staticmethod
    def _deps(reads, writes):
        deps = []
        for b in reads:
            if b.last_write is not None:
                deps.append(b.last_write)
        for b in writes:
            if b.last_write is not None:
                deps.append(b.last_write)
            deps.extend(b.reads.items())
        return deps

    def _emit_waits(self, eng, deps):
        need = {}
        for (k, v) in deps:
            if k[0] == eng and (eng == "pe" or not SAME_ENGINE_SYNC):
                continue
            if v > need.get(k, 0):
                need[k] = v
        w = self.waited[eng]
        for k, v in need.items():
            if w.get(k, 0) >= v:
                continue
            w[k] = v
            self._semkey(k)
            self.q[eng].append(("wait", k, v))

    @staticmethod
    def _mark(tok, reads, writes):
        k, v = tok
        for b in reads:
            if b.reads.get(k, 0) < v:
                b.reads[k] = v
        for b in writes:
            b.last_write = tok
            b.reads = {}

    def op(self, eng, fn, reads=(), writes=(), signal=True):
        reads = [t.b if isinstance(t, Tile) else t for t in reads]
        writes = [t.b if isinstance(t, Tile) else t for t in writes]
        self._emit_waits(eng, self._deps(reads, writes))
        if self.cnt[eng] >= SEM_EPOCH and signal and not self.pending[eng]:
            self.epoch[eng] += 1
            self.cnt[eng] = 0
        self.pending[eng] = not signal
        key = (eng, self.epoch[eng])
        self._semkey(key)
        if signal:
            self.cnt[eng] += 1
            tok = (key, self.cnt[eng])
        else:
            tok = (key, self.cnt[eng] + 1)
        rec = _Rec()
        fn(rec)
        assert len(rec.calls) == 1
        self.q[eng].append(("op", rec.calls[0], key if signal else None))
        self._mark(tok, reads, writes)
        self.last_tok[eng] = tok
        return tok

    def dma(self, qeng, out, in_, reads=(), writes=(), is_output=False):
        reads = [t.b if isinstance(t, Tile) else t for t in reads]
        writes = [t.b if isinstance(t, Tile) else t for t in writes]
        self._emit_waits(qeng, self._deps(reads, writes))
        sb = writes[0] if writes else reads[0]
        if sb.dkey is None or sb.dkey[2] != qeng or self.dcnt.get(sb.dkey, 0) >= SEM_EPOCH:
            fl = self.free_dkeys[qeng]
            while fl and self.dcnt.get(fl[-1], 0) >= SEM_EPOCH:
                fl.pop()
            if fl:
                sb.dkey = fl.pop()
            else:
                self.nbuf += 1
                sb.dkey = ("dma", self.nbuf, qeng)
            self.dma_bufs.append(sb)
        k = sb.dkey
        self._semkey(k)
        self.dcnt[k] = self.dcnt.get(k, 0) + 16
        tok = (k, self.dcnt[k])
        self.q[qeng].append(("dma", (out, in_), k))
        self._mark(tok, reads, writes)
        if is_output:
            self.out_tokens.append(tok)
        return tok

    def cc_allgather(self, out, in_, groups, reads=(), writes=()):
        reads = [t.b if isinstance(t, Tile) else t for t in reads]
        writes = [t.b if isinstance(t, Tile) else t for t in writes]
        self._emit_waits("pool", self._deps(reads, writes))
        self.nbuf += 1
        k = ("cc", self.nbuf)
        self._semkey(k)
        self.dcnt[k] = 1
        tok = (k, 1)
        self.q["pool"].append(("cc", (out, in_, groups), k))
        self._mark(tok, reads, writes)
        return tok

    def barrier(self):
        toks = [t for t in self.last_tok.values() if t is not None]
        toks += [(k, v) for k, v in self.dcnt.items()]
        for e in ENGS:
            self._emit_waits(e, [t for t in toks if not (t[0][0] == e and e in ("pe", "sp", "pool"))])
        seen = set()
        for k in list(self.dcnt.keys()):
            if k[0] == "dma" and k not in seen and all(k not in fl for fl in self.free_dkeys.values()):
                seen.add(k)
                self.free_dkeys[k[2]].append(k)
        for b in self.dma_bufs:
            b.dkey = None
        self.dma_bufs = []

    def finish(self):
        self._emit_waits("sp", self.out_tokens)
        nc = self.nc
        for i, k in enumerate(self.semkeys):
            self.sems[k] = self.stack.enter_context(nc.semaphore(f"s{i}_{k[0]}"))
        sems = self.sems
        q = self.q

        def replay(eng_name):
            def body(e):
                for item in q[eng_name]:
                    if item[0] == "wait":
                        e.wait_ge(sems[item[1]], item[2])
                    elif item[0] == "op":
                        nm, a_, kw_ = item[1]
                        ins = getattr(e, nm)(*a_, **kw_)
                        if item[2] is not None:
                            ins.then_inc(sems[item[2]], 1)
                    elif item[0] == "cc":
                        o, i_, grp = item[1]
                        e.collective_compute("AllGather", ALU.bypass, replica_groups=grp, ins=[i_.opt()], outs=[o.opt()]).then_inc(sems[item[2]], 1)
                    else:
                        o, i_ = item[1]
                        e.dma_start(out=o, in_=i_).then_inc(sems[item[2]], 16)
            return body

        with nc.Block() as block:
            block.tensor(replay("pe"))
            block.scalar(replay("act"))
            block.vector(replay("dve"))
            block.gpsimd(replay("pool"))
            block.sync(replay("sp"))
        self.stack.close()

    def stats(self):
        return {e: len(self.q[e]) for e in ENGS}, len(self.semkeys)


def _prod(s):
    r = 1
    for x in s:
        r *= x
    return r


class Arena:
    def __init__(self, P, nwords):
        self.P = P
        self.t = P.sbuf("arena", [128, nwords], F32)
        self.nwords = nwords
        self.top = 0
        self.peak = 0

    def alloc(self, name, shape, dtype):
        n = _prod(shape)
        nbytes = n * (4 if dtype == F32 else 2)
        words = ((nbytes + 31) // 32) * 8
        off = self.top
        self.top += words
        self.peak = max(self.peak, self.top)
        assert self.top <= self.nwords, f"arena overflow at {name}: {self.top} > {self.nwords}"
        v = self.t[:, off:off + words]
        if dtype != F32:
            v = v.bitcast(dtype)
        v = v[:, :n]
        if len(shape) == 2:
            v = v.rearrange("p (a b) -> p a b", a=shape[0])
        elif len(shape) == 3:
            v = v.rearrange("p (a b c) -> p a b c", a=shape[0], b=shape[1])
        elif len(shape) == 4:
            v = v.rearrange("p (a b c d) -> p a b c d", a=shape[0], b=shape[1], c=shape[2])
        return Tile(v, self.P.buf(name))

    def ring(self, name, n, shape, dtype):
        return Ring([self.alloc(f"{name}{i}", shape, dtype) for i in range(n)])

    def mark(self):
        return self.top

    def release(self, m):
        self.top = m


class Ring:
    def __init__(self, tiles):
        self.tiles = tiles
        self.i = 0

    def next(self):
        t = self.tiles[self.i % len(self.tiles)]
        self.i += 1
        return t


class Builder:
    def __init__(self, kind, layer):
        self.kind = kind
        self.layer = layer
        self.last = layer == 1
        self.sfx = ""
        self.nc = bass.Bass("TRN2", target_bir_lowering=False)
        self.P = Prog(self.nc)
        self.din = {}
        self.A = Arena(self.P, 52000)
        banks = [self.P.psum(f"ps{i}", [128, 512], F32) for i in range(8)]
        self.ps = Ring([Tile(b[:], self.P.buf(f"ps{i}")) for i, b in enumerate(banks)])
        self.pending = []

    def inp(self, name, shape, dtype=F32):
        if name in self.din:
            return self.din[name]
        t = self.nc.dram_tensor(name, list(shape), dtype, kind="ExternalInput").ap()
        self.din[name] = t
        return t

    def outp(self, name, shape, dtype=F32):
        return self.nc.dram_tensor(name, list(shape), dtype, kind="ExternalOutput").ap()

    def scratch(self, name, shape, dtype):
        kind = "ExternalOutput" if DEBUG_SCRATCH else "Internal"
        return self.nc.dram_tensor(name, list(shape), dtype, kind=kind).ap()

    def dl(self, name):
        return self.din[name + self.sfx]

    def load(self, tile, src, q="sp", dep=None):
        self.P.dma(q, tile.ap, src, reads=([dep] if dep is not None else []), writes=[tile])

    def const_tile(self, name, shape, dtype, src, q=None):
        t = self.A.alloc(name, shape, dtype)
        self.load(t, src, q or ("pool" if dtype != F32 else "sp"))
        return t

    def ps_bf16(self, bank, shape):
        v = bank.ap.bitcast(BF16)
        if len(shape) == 2:
            return v.rearrange("p (a b) -> p a b", a=shape[0])
        return v

    def emit_mod(self):
        P, A, nc = self.P, self.A, self.nc
        ccols = self.din["ccols"] if "ccols" in self.din else self.inp("ccols", [128, 8, 2])
        wmod = self.inp("wmod" + self.sfx, [D, 6 * D])
        bcols = self.inp("bcols" + self.sfx, [128, 48])
        brow = self.inp("brow" + self.sfx, [128, 2, D])
        gpm = self.inp("gpm_c" + self.sfx, [128, 8])
        gpf = self.inp("gpf_c" + self.sfx, [128, 8])
        gqm = self.inp("gqm_r" + self.sfx, [128, D])
        gqf = self.inp("gqf_r" + self.sfx, [128, D])
        self.alloc_mod_tiles()
        m0 = A.mark()
        cc = A.alloc("cc", [8, 2], F32)
        self.load(cc, ccols)
        bc = A.alloc("bc", [48], F32)
        self.load(bc, bcols)
        br = A.alloc("br", [2, D], F32)
        self.load(br, brow)
        gq = [A.alloc("gqm", [D], F32), A.alloc("gqf", [D], F32)]
        self.load(gq[0], gqm)
        self.load(gq[1], gqf)
        gp = [A.alloc("gpm", [8], F32), A.alloc("gpf", [8], F32)]
        self.load(gp[0], gpm)
        self.load(gp[1], gpf)
        sc = A.alloc("silu_c", [8, 2], F32)
        P.op("act", lambda e: e.activation(out=sc.ap, in_=cc.ap, func=AF.Silu), reads=[cc], writes=[sc])
        srep = A.alloc("srep", [8, 2, 128], F32)
        P.op("dve", lambda e: e.tensor_copy(out=srep.ap, in_=sc.ap.unsqueeze(3).broadcast_to([128, 8, 2, 128])),
             reads=[sc], writes=[srep])
        wring = A.ring("wm", 2, [8, 512], F32)
        modr = A.alloc("modr", [48, 2], F32)
        P.op("dve", lambda e: e.memset(modr.ap, 0.0), writes=[modr])
        for part in range(6):
            for hf in range(2):
                wm = wring.next()
                c0 = part * 1024 + hf * 512
                self.load(wm, wmod[:, c0:c0 + 512].rearrange("(k p) n -> p k n", p=128))
                if part in (2, 5):
                    gi = 0 if part == 2 else 1
                    for s in range(2):
                        pr = self.ps.next()
                        for k in range(8):
                            P.op("pe", lambda e, k=k, s=s, pr=pr, wm=wm: e.matmul(
                                out=pr.ap, lhsT=srep.ap[:, k, s, :], rhs=wm.ap[:, k, :], start=(k == 0), stop=(k == 7)),
                                reads=[srep, wm], writes=[pr], signal=(k == 7))
                        dst = self.gaG[s][gi]
                        cs = slice(hf * 512, hf * 512 + 512)
                        P.op("dve", lambda e, pr=pr, dst=dst, cs=cs, gi=gi: e.tensor_tensor(
                            out=dst.ap[:, cs], in0=pr.ap, in1=br.ap[:, gi, cs], op=ALU.add), reads=[pr, br], writes=[dst])
                        P.op("dve", lambda e, dst=dst, cs=cs, gi=gi: e.tensor_tensor(
                            out=dst.ap[:, cs], in0=dst.ap[:, cs], in1=gq[gi].ap[:, cs], op=ALU.mult),
                            reads=[dst, gq[gi]], writes=[dst])
                else:
                    for s in range(2):
                        pcb = self.ps.next()
                        pcbv = pcb.ap.rearrange("p (m n) -> p m n", m=4)
                        for m4 in range(4):
                            for k in range(8):
                                P.op("pe", lambda e, k=k, s=s, m4=m4, wm=wm, pcbv=pcbv: e.matmul(
                                    out=pcbv[:, m4, :], lhsT=wm.ap[:, k, m4 * 128:(m4 + 1) * 128], rhs=srep.ap[:, k, s, :],
                                    start=(k == 0), stop=(k == 7)), reads=[wm, srep], writes=[pcb],
                                    signal=(k == 7 and m4 == 3))
                        m_0 = part * 8 + hf * 4
                        P.op("dve", lambda e, s=s, m_0=m_0, pcbv=pcbv: e.tensor_copy(out=modr.ap[:, m_0:m_0 + 4, s], in_=pcbv[:, :, 0]),
                             reads=[pcb], writes=[modr])
        modc = A.alloc("modc", [48, 2], F32)
        self.dbg_modc = modc
        P.op("dve", lambda e: e.tensor_tensor(out=modc.ap, in0=modr.ap, in1=bc.ap.unsqueeze(2).broadcast_to([128, 48, 2]),
                                              op=ALU.add), reads=[modr, bc], writes=[modc])
        for s in range(2):
            for sub in range(2):
                shp, scp = (0, 1) if sub == 0 else (3, 4)
                P.op("dve", lambda e, s=s, sub=sub, shp=shp: e.tensor_copy(
                    out=self.shA.ap[:, s, sub, :], in_=modc.ap[:, shp * 8:shp * 8 + 8, s]), reads=[modc], writes=[self.shA])
                P.op("dve", lambda e, s=s, sub=sub, scp=scp: e.scalar_tensor_tensor(
                    out=self.scA.ap[:, s, sub, :], in0=modc.ap[:, scp * 8:scp * 8 + 8, s], scalar=1.0,
                    in1=gp[sub].ap, op0=ALU.add, op1=ALU.mult), reads=[modc, gp[sub]], writes=[self.scA])
        if getattr(self, "dbg_out", None) is not None:
            P.dma("sp", self.dbg_out, modc.ap.rearrange("p a b -> p (a b)"), reads=[modc], writes=[P.buf("dbgo")], is_output=True)
        P.barrier()
        A.release(m0)

    def emit_consts(self):
        A = self.A
        ident = self.inp("ident", [128, 128])
        self.ident = self.const_tile("ident", [128], BF16, ident)
        if self.kind == "main":
            self.rt = self.const_tile("rt", [128], BF16, self.inp("rt", [128, 128]))
            masks = self.inp("masks", [128, 4, 128])
            self.masks = self.const_tile("masks", [4, 128], BF16, masks)
            self.valid = self.const_tile("valid", [2], F32, self.inp("valid", [128, 2]))
            if self.sfx == "":
                self.emit_layer_consts()
            self.cbsb = self.const_tile("cbsb", [2, 128], BF16, self.inp("cbsb", [128, 2, 128]))

    def alloc_mod_tiles(self):
        A = self.A
        if not hasattr(self, "scA"):
            self.scA = A.alloc("scA", [2, 2, 8], F32)
            self.shA = A.alloc("shA", [2, 2, 8], F32)
            self.gaG = [[A.alloc(f"gaG{s}{g}", [D], F32) for g in range(2)] for s in range(2)]

    def emit_layer_consts(self):
        A = self.A
        es = self.const_tile("esink_src", [8], F32, self.inp("sink_b" + self.sfx, [128, 8]))
        self.esink = A.alloc("esink", [8], F32)
        self.P.op("act", lambda e: e.activation(out=self.esink.ap, in_=es.ap, func=AF.Exp), reads=[es], writes=[self.esink])
        self.wconv = self.const_tile("wconv", [4, 3], F32, self.inp("wconv_c" + self.sfx, [128, 4, 3]))

    def make_hT_dep(self, tiles_src, hT, s, sub, rings, dep):
        return self.make_hT(tiles_src, hT, s, sub, rings, dep)

    def make_hT(self, tiles_src, hT, s, sub, rings, dep=None, defer=False):
        for i, src in enumerate(tiles_src):
            fn = (lambda i=i, src=src: self._hT_tile(i, src, hT, s, sub, rings, dep))
            if defer:
                self.pending.append(fn)
            else:
                fn()

    def tick(self, n=1):
        for _ in range(n):
            if self.pending:
                self.pending.pop(0)()

    def flush(self):
        while self.pending:
            self.pending.pop(0)()

    def _hT_tile(self, i, src, hT, s, sub, rings, dep):
        P = self.P
        xr, xnr, sqr, smr = rings
        xt = xr.next()
        if isinstance(src, tuple):
            self.load(xt, src[0], dep=src[1])
        else:
            self.load(xt, src, dep=dep)
        sq = sqr.next()
        sm = smr.next()
        P.op("act", lambda e: e.activation(out=sq.ap, in_=xt.ap, func=AF.Square, accum_out=sm.ap[:, 0:1]), reads=[xt], writes=[sq, sm])
        P.op("act", lambda e: e.activation(out=sm.ap[:, 1:2], in_=sm.ap[:, 0:1], func=AF.Sqrt, scale=1.0 / D, bias=EPS), reads=[sm], writes=[sm])
        P.op("dve", lambda e: e.reciprocal(out=sm.ap[:, 1:2], in_=sm.ap[:, 1:2]), reads=[sm], writes=[sm])
        xn = xnr.next()
        P.op("dve", lambda e: e.tensor_scalar(out=xn.ap, in0=xt.ap, scalar1=sm.ap[:, 1:2], scalar2=None, op0=ALU.mult), reads=[xt, sm], writes=[xn])
        pt = self.ps.next()
        ptv = self.ps_bf16(pt, [8, 128])
        for k in range(8):
            P.op("pe", lambda e, k=k: e.transpose(out=ptv[:, k, :], in_=xn.ap[:, k * 128:(k + 1) * 128], identity=self.ident.ap),
                 reads=[xn, self.ident], writes=[pt], signal=(k == 7))
        for k in range(4):
            P.op("act", lambda e, k=k: e.activation(
                out=hT.ap[:, k, i * 128:(i + 1) * 128], in_=ptv[:, k, :], func=AF.Identity,
                bias=self.shA.ap[:, s, sub, k:k + 1], scale=self.scA.ap[:, s, sub, k:k + 1]),
                reads=[pt, self.shA, self.scA], writes=[hT])
        for k in range(4, 8):
            P.op("dve", lambda e, k=k: e.scalar_tensor_tensor(
                out=hT.ap[:, k, i * 128:(i + 1) * 128], in0=ptv[:, k, :], scalar=self.scA.ap[:, s, sub, k:k + 1],
                in1=self.shA.ap[:, s, sub, k:k + 1].broadcast_to([128, 128]), op0=ALU.mult, op1=ALU.add),
                reads=[pt, self.shA, self.scA], writes=[hT])

    def norm_rings(self, nx=4):
        A = self.A
        return (A.ring("xt", nx, [D], F32), A.ring("xn", 2, [D], BF16), A.ring("sq", 1, [D], BF16), A.ring("sm", 4, [2], F32))

    def proj_fm(self, w, col0, hT, ntok, nk=8):
        P = self.P
        pr = self.ps.next()
        for k in range(nk):
            P.op("pe", lambda e, k=k, pr=pr: e.matmul(out=pr.ap[:, :ntok], lhsT=w.ap[:, k, col0:col0 + 128], rhs=hT.ap[:, k, :ntok],
                                                   start=(k == 0), stop=(k == nk - 1)), reads=[w, hT], writes=[pr], signal=(k == nk - 1))
        return pr

    def rope(self, pr, raw, dst_ap, dst_tile, cosT, sinT, ntok, tmp):
        P = self.P
        P.op("act", lambda e: e.copy(out=raw.ap[:, :ntok], in_=pr.ap[:, :ntok]), reads=[pr], writes=[raw])
        p2 = self.ps.next()
        P.op("pe", lambda e: e.matmul(out=p2.ap[:, :ntok], lhsT=self.rt.ap, rhs=raw.ap[:, :ntok], start=True, stop=True),
             reads=[self.rt, raw], writes=[p2])
        t1, t2 = tmp
        P.op("dve", lambda e: e.tensor_tensor(out=t1.ap[:, :ntok], in0=pr.ap[:, :ntok], in1=cosT, op=ALU.mult), reads=[pr, self.ropeb, raw], writes=[t1])
        P.op("dve", lambda e: e.tensor_tensor(out=t2.ap[:, :ntok], in0=p2.ap[:, :ntok], in1=sinT, op=ALU.mult), reads=[p2, self.ropeb], writes=[t2])
        P.op("dve", lambda e: e.tensor_tensor(out=dst_ap, in0=t1.ap[:, :ntok], in1=t2.ap[:, :ntok], op=ALU.add), reads=[t1, t2], writes=[dst_tile])

    def emit_phaseA(self, xh, want_zf, zf_out=None):
        P, A = self.P, self.A
        main = self.kind == "main"
        winP = self.dl("winP")
        m0 = A.mark()
        ncolA = PA_W if main else 512
        wA = A.alloc("wA", [8, ncolA], BF16)
        self.load(wA, winP[:, 0:ncolA].rearrange("(k p) n -> p k n", p=128), "pool")
        rings = self.norm_rings(4)
        hTr = A.ring("hT", 2, [8, 512], BF16)
        zfr = A.ring("zfs", 2, [512], BF16)
        if main:
            ropeC = self.din["ropeC"]
            ropeS = self.din["ropeS"]
            rope_r = A.ring("ropeCS", 2, [2, 512], F32)
            kraw = A.alloc("kraw", [512], BF16)
            tmp = (A.alloc("rt1", [512], F32), A.alloc("rt2", [512], F32))
            cxs = A.ring("cxs", 2, [512], F32)
            ust = A.ring("ust", 2, [4, 512], BF16)
        blocks = []
        if main:
            blocks.append((-1, 1))
        for bi in range(8):
            blocks.append((bi * 4, 4))
        if main:
            blocks.append((32, 1))
        def _mk(bidx, defer):
            t0_, nt_ = blocks[bidx]
            hT_ = hTr.next()
            if xh is None:
                srcs_ = [self.xtile(t0_ + i) for i in range(nt_)]
            else:
                srcs_ = [xh[(t0_ + 1 + i) * 128:(t0_ + 2 + i) * 128, :] for i in range(nt_)]
            self.make_hT(srcs_, hT_, 0, 0, rings, defer=defer)
            return hT_
        hT_next = _mk(0, False)
        for bidx, (t0, nt) in enumerate(blocks):
            ntok = nt * 128
            halo = nt == 1
            self.flush()
            hT = hT_next
            if bidx + 1 < len(blocks):
                hT_next = _mk(bidx + 1, True)
            if want_zf and not halo:
                for i in range(nt):
                    pr = self.ps.next()
                    for k in range(8):
                        P.op("pe", lambda e, k=k, i=i, pr=pr, hT=hT: e.matmul(out=pr.ap, lhsT=hT.ap[:, k, i * 128:(i + 1) * 128],
                                                                     rhs=wA.ap[:, k, PA_F:PA_F + 512], start=(k == 0), stop=(k == 7)),
                             reads=[hT, wA], writes=[pr], signal=(k == 7))
                    zs = zfr.next()
                    P.op("act", lambda e, pr=pr, zs=zs: e.copy(out=zs.ap, in_=pr.ap), reads=[pr], writes=[zs])
                    tg = t0 + i
                    if i % 2 == 1:
                        self.tick()
                    if isinstance(zf_out, list):
                        for g_ in range(4):
                            P.dma("pool", zf_out[g_][tg * 128:(tg + 1) * 128, :], zs.ap[:, g_ * 128:(g_ + 1) * 128], reads=[zs], writes=[self.zf_b])
                    else:
                        P.dma("sp", zf_out[:, tg * 128:(tg + 1) * 128, :].rearrange("g t c -> t g c"),
                              zs.ap.rearrange("p (g c) -> p g c", g=4), reads=[zs], writes=[self.zf_b], is_output=(self.kind == "pre"))
            if not main:
                continue
            c0 = (t0 + 1) * 128
            rp = rope_r.next()
            P.dma("sp", rp.ap[:, 0, :ntok], ropeC[:, c0:c0 + ntok], writes=[rp])
            P.dma("sp", rp.ap[:, 1, :ntok], ropeS[:, c0:c0 + ntok], writes=[rp])
            self.ropeb = rp
            pr = self.proj_fm(wA, PA_K, hT, ntok)
            self.rope(pr, kraw, self.kT.ap[:, c0:c0 + ntok], self.kT, rp.ap[:, 0, :ntok], rp.ap[:, 1, :ntok], ntok, tmp)
            pv = self.ps.next()
            pvv = pv.ap.rearrange("p (i c) -> p i c", i=4)
            for i in range(nt):
                for k in range(8):
                    P.op("pe", lambda e, k=k, i=i, hT=hT: e.matmul(out=pvv[:, i, :], lhsT=hT.ap[:, k, i * 128:(i + 1) * 128],
                                                                 rhs=wA.ap[:, k, PA_V:PA_V + 128], start=(k == 0), stop=(k == 7)),
                         reads=[hT, wA], writes=[pv], signal=(k == 7 and i == nt - 1))
            P.op("act", lambda e, t0=t0, nt=nt: e.copy(
                out=self.vaug.ap[:, t0 + 1:t0 + 1 + nt, :, 0:64],
                in_=pvv[:, 0:nt, :].rearrange("p i (g d) -> p i g d", g=2)), reads=[pv], writes=[self.vaug])
            self.tick()
            us = ust.next()
            for m in range(4):
                if m == 2:
                    self.tick()
                px = self.proj_fm(wA, PA_CX + m * 128, hT, ntok)
                cx = cxs.next()
                P.op("act", lambda e, px=px, cx=cx: e.copy(out=cx.ap[:, :ntok], in_=px.ap[:, :ntok]), reads=[px], writes=[cx])
                pc = self.proj_fm(wA, PA_CC + m * 128, hT, ntok)
                if halo:
                    vi = 0 if t0 < 0 else 1
                    P.op("dve", lambda e, m=m, pc=pc, cx=cx, us=us, vi=vi: e.scalar_tensor_tensor(
                        out=us.ap[:, m, :ntok], in0=cx.ap[:, :ntok], scalar=self.valid.ap[:, vi:vi + 1], in1=pc.ap[:, :ntok],
                        op0=ALU.mult, op1=ALU.mult), reads=[cx, pc, self.valid], writes=[us])
                else:
                    P.op("dve", lambda e, m=m, pc=pc, cx=cx, us=us: e.tensor_tensor(
                        out=us.ap[:, m, :ntok], in0=cx.ap[:, :ntok], in1=pc.ap[:, :ntok], op=ALU.mult), reads=[cx, pc], writes=[us])
            P.dma("pool", self.u_d[:, :, c0:c0 + ntok], us.ap[:, :, :ntok], reads=[us], writes=[self.u_b])
        self.flush()
        P.barrier()
        A.release(m0)

    def emit_fft(self, zfg):
        P, A = self.P, self.A
        m0 = A.mark()
        t1 = self.const_tile("t1", [2, 2, 64], BF16, self.inp("t1", [128, 2, 2, 64]))
        etab_d = self.inp("etab", [128, 128, 96])
        et = A.alloc("etab", [128, 96], BF16)
        for q4 in range(4):
            P.dma("pool", et.ap[:, q4 * 32:(q4 + 1) * 32, :], etab_d[:, q4 * 32:(q4 + 1) * 32, :], writes=[et])
        Ur = A.ring("U", 1, [128, 128], BF16)
        Y = A.alloc("Y", [128, 2, 64], BF16)
        G = A.alloc("G", [2, 4096], BF16)
        FTs = A.ring("FTs", 2, [4096], BF16)
        scale = 1.0 / math.sqrt(SEQ * 128.0)
        ev = 0
        for g in range(4):
            U = Ur.next()
            for r in range(4):
                zsrc_ = zfg(r, g) if callable(zfg) else zfg[r, g]
                P.dma("sp", U.ap[r * 32:(r + 1) * 32, :, :], zsrc_.rearrange("(th tl) c -> th tl c", tl=128),
                      reads=([self.zfg_b] if getattr(self, "zfg_b", None) is not None else []), writes=[U])
            for hh in range(2):
                for c4 in range(32):
                    pr = self.ps.next()
                    prv = pr.ap.rearrange("p (c x) -> p c x", c=4)
                    for ci in range(4):
                        c = c4 * 4 + ci
                        P.op("pe", lambda e, c=c, ci=ci, hh=hh, prv=prv: e.matmul(
                            out=prv[:, ci, :], lhsT=U.ap[:, :, c], rhs=t1.ap[:, hh, :, :].rearrange("p r k -> p (r k)"),
                            start=True, stop=True), reads=[U, t1], writes=[pr], signal=(ci == 3))
                    eng = "act" if ev % 2 == 0 else "dve"
                    ev += 1
                    dst = Y.ap[:, c4 * 4:(c4 + 1) * 4, :, :].rearrange("p c r k -> p c (r k)")
                    if eng == "act":
                        P.op("act", lambda e, dst=dst, prv=prv: e.copy(out=dst, in_=prv), reads=[pr], writes=[Y])
                    else:
                        P.op("dve", lambda e, dst=dst, prv=prv: e.tensor_copy(out=dst, in_=prv), reads=[pr], writes=[Y])
                for k8 in range(8):
                    pr = self.ps.next()
                    prv = pr.ap.rearrange("p (k r x) -> p k r x", k=8, r=2)
                    for ki in range(8):
                        k1l = k8 * 8 + ki
                        k1 = hh * 64 + k1l
                        P.op("pe", lambda e, k1=k1, k1l=k1l, ki=ki, prv=prv: e.matmul(
                            out=prv[:, ki, :, :].rearrange("p r x -> p (r x)"), lhsT=Y.ap[:, :, 0, k1l], rhs=et.ap[:, k1, 32:96],
                            start=True, stop=False), reads=[Y, et], writes=[pr], signal=False)
                        P.op("pe", lambda e, k1=k1, k1l=k1l, ki=ki, prv=prv: e.matmul(
                            out=prv[:, ki, :, :].rearrange("p r x -> p (r x)"), lhsT=Y.ap[:, :, 1, k1l], rhs=et.ap[:, k1, 0:64],
                            start=False, stop=True), reads=[Y, et], writes=[pr], signal=(ki == 7))
                    k10 = hh * 64 + k8 * 8
                    for ri in range(2):
                        dst = G.ap[:, ri, :].rearrange("p (k2 k1) -> p k1 k2", k1=128)[:, k10:k10 + 8, :]
                        if ri == 0:
                            P.op("act", lambda e, dst=dst, prv=prv, ri=ri: e.copy(out=dst, in_=prv[:, :, ri, :]), reads=[pr], writes=[G])
                        else:
                            P.op("dve", lambda e, dst=dst, prv=prv, ri=ri: e.tensor_copy(out=dst, in_=prv[:, :, ri, :]), reads=[pr], writes=[G])
            ft = FTs.next()
            for cb in range(8):
                pr = self.ps.next()
                cs = slice(cb * 512, (cb + 1) * 512)
                P.op("pe", lambda e, pr=pr, cs=cs: e.matmul(out=pr.ap, lhsT=self.cbsb.ap[:, 0, :], rhs=G.ap[:, 0, cs], start=True, stop=False),
                     reads=[self.cbsb, G], writes=[pr], signal=False)
                P.op("pe", lambda e, pr=pr, cs=cs: e.matmul(out=pr.ap, lhsT=self.cbsb.ap[:, 1, :], rhs=G.ap[:, 1, cs], start=False, stop=True),
                     reads=[self.cbsb, G], writes=[pr])
                P.op("act", lambda e, pr=pr, cs=cs, ft=ft: e.activation(out=ft.ap[:, cs], in_=pr.ap, func=AF.Copy, scale=scale), reads=[pr], writes=[ft])
            P.dma("sp", self.ft_d[:, g, :], ft.ap, reads=[ft], writes=[self.ft_b])
        P.barrier()
        A.release(m0)

    def attention_tile(self, qT, qcol0, keyblocks, attnT, acol0, bufs):
        P = self.P
        pTr, atok_r, den_r = bufs
        nkb = len(keyblocks)
        pT = pTr.next()
        for kb, (kap, kt, vap, vt, mi) in enumerate(keyblocks):
            for g in range(2):
                pr = self.ps.next()
                P.op("pe", lambda e, g=g, kap=kap, pr=pr: e.matmul(
                    out=pr.ap.rearrange("p (h q) -> p h q", h=4), lhsT=kap[g * 64:(g + 1) * 64, :],
                    rhs=qT.ap[g * 64:(g + 1) * 64, :, qcol0:qcol0 + 128], start=True, stop=True),
                    reads=[kt, qT], writes=[pr])
                P.op("act", lambda e, g=g, kb=kb, pr=pr, pT=pT: e.activation(
                    out=pT.ap[:, kb, g, :, :], in_=pr.ap.rearrange("p (h q) -> p h q", h=4), func=AF.Exp, scale=0.125),
                    reads=[pr], writes=[pT])
            if mi is not None:
                P.op("dve", lambda e, kb=kb, mi=mi, pT=pT: e.tensor_tensor(
                    out=pT.ap[:, kb, :, :, :].rearrange("p g h q -> p (g h) q"),
                    in0=pT.ap[:, kb, :, :, :].rearrange("p g h q -> p (g h) q"),
                    in1=self.masks.ap[:, mi, :].unsqueeze(1).broadcast_to([128, 8, 128]), op=ALU.mult),
                    reads=[pT, self.masks], writes=[pT])
        atok = atok_r.next()
        den = den_r.next()
        for b2 in range(2):
            po = self.ps.next()
            pov = po.ap[:, 0:260].rearrange("p (h x) -> p h x", h=4)
            for hh in range(4):
                for kb, (kap, kt, vap, vt, mi) in enumerate(keyblocks):
                    P.op("pe", lambda e, hh=hh, kb=kb, vap=vap, pov=pov, b2=b2, pT=pT: e.matmul(
                        out=pov[:, hh, :], lhsT=pT.ap[:, kb, b2, hh, :], rhs=vap[:, b2, :], start=(kb == 0), stop=(kb == nkb - 1)),
                        reads=[pT, vt], writes=[po], signal=(hh == 3 and kb == nkb - 1))
            hs = slice(b2 * 4, b2 * 4 + 4)
            P.op("dve", lambda e, pov=pov, hs=hs, den=den: e.tensor_tensor(out=den.ap[:, 0, hs], in0=pov[:, :, 64], in1=self.esink.ap[:, hs], op=ALU.add),
                 reads=[po, self.esink], writes=[den])
            P.op("dve", lambda e, hs=hs, den=den: e.reciprocal(out=den.ap[:, 1, hs], in_=den.ap[:, 0, hs]), reads=[den], writes=[den])
            P.op("dve", lambda e, pov=pov, hs=hs, den=den, atok=atok: e.tensor_tensor(
                out=atok.ap[:, hs, :], in0=pov[:, :, 0:64], in1=den.ap[:, 1, hs].unsqueeze(2).broadcast_to([128, 4, 64]), op=ALU.mult),
                reads=[po, den], writes=[atok])
        pt = self.ps.next()
        ptv = self.ps_bf16(pt, [8, 128])
        av = atok.ap.rearrange("p h d -> p (h d)")
        for kc in range(4):
            P.op("pe", lambda e, kc=kc, ptv=ptv: e.transpose(out=ptv[:, kc, :], in_=av[:, kc * 128:(kc + 1) * 128], identity=self.ident.ap),
                 reads=[atok, self.ident], writes=[pt], signal=(kc == 3))
        P.op("act", lambda e, ptv=ptv: e.copy(out=attnT.ap[:, 0:4, acol0:acol0 + 128], in_=ptv[:, 0:4, :]), reads=[pt], writes=[attnT])

    def mix_block(self, hT, ntok, qT, attnT, uap, u_tile, FTap, ft_tile, wr, work, x_tiles_src, x_out_dst, s, xout_buf, is_out=False):
        P = self.P
        winP = self.dl("winP")
        (yconvT, gsb_r, tmp_r, mergedT, mixt_r, sm_r, xres_r, sq_r) = work
        wcb = wr.next()
        self.load(wcb, winP[:, PM_CB:PM_CB + 512].rearrange("(k p) n -> p k n", p=128), "pool")
        for m in range(4):
            pb = self.proj_fm(wcb, m * 128, hT, ntok)
            t = tmp_r.next()
            P.op("dve", lambda e, m=m, t=t: e.tensor_scalar(out=t.ap[:, :ntok], in0=uap[:, m, 0:ntok], scalar1=self.wconv.ap[:, m, 0:1], scalar2=None, op0=ALU.mult),
                 reads=[u_tile, self.wconv], writes=[t])
            P.op("dve", lambda e, m=m, t=t: e.scalar_tensor_tensor(out=t.ap[:, :ntok], in0=uap[:, m, 1:ntok + 1], scalar=self.wconv.ap[:, m, 1:2],
                                                                 in1=t.ap[:, :ntok], op0=ALU.mult, op1=ALU.add), reads=[u_tile, self.wconv, t], writes=[t])
            P.op("dve", lambda e, m=m, t=t: e.scalar_tensor_tensor(out=t.ap[:, :ntok], in0=uap[:, m, 2:ntok + 2], scalar=self.wconv.ap[:, m, 2:3],
                                                                 in1=t.ap[:, :ntok], op0=ALU.mult, op1=ALU.add), reads=[u_tile, self.wconv, t], writes=[t])
            P.op("dve", lambda e, m=m, t=t, pb=pb: e.tensor_tensor(out=yconvT.ap[:, m, :ntok], in0=t.ap[:, :ntok], in1=pb.ap[:, :ntok], op=ALU.mult),
                 reads=[t, pb], writes=[yconvT])
        self.tick()
        if getattr(self, "mix_stop", None) == "conv":
            return
        wbr = self.wbr
        srcs = [(attnT.ap, attnT), (FTap, ft_tile), (yconvT.ap, yconvT)]
        for m in range(8):
            if m in (2, 5):
                self.tick()
            wg = wr.next()
            P.dma("pool", wg.ap[:, :, 0:384], winP[:, PM_G + m * 384:PM_G + (m + 1) * 384].rearrange("(k p) n -> p k n", p=128), writes=[wg])
            acc = None
            for r in range(3):
                pg = self.proj_fm(wg, r * 128, hT, ntok)
                gs = gsb_r.next()
                P.op("act", lambda e, pg=pg, gs=gs: e.activation(out=gs.ap[:, :ntok], in_=pg.ap[:, :ntok], func=AF.Sigmoid), reads=[pg], writes=[gs])
                sap, st = srcs[r]
                py = self.ps.next()
                for kc in range(4):
                    P.op("pe", lambda e, kc=kc, r=r, m=m, py=py, sap=sap: e.matmul(
                        out=py.ap[:, :ntok], lhsT=wbr[r].ap[:, kc, m * 128:(m + 1) * 128], rhs=sap[:, kc, 0:ntok], start=(kc == 0), stop=(kc == 3)),
                        reads=[wbr[r], st], writes=[py], signal=(kc == 3))
                if r == 0:
                    acc = tmp_r.next()
                    P.op("dve", lambda e, gs=gs, py=py, acc=acc: e.tensor_tensor(out=acc.ap[:, :ntok], in0=gs.ap[:, :ntok], in1=py.ap[:, :ntok], op=ALU.mult),
                         reads=[gs, py], writes=[acc])
                else:
                    t = tmp_r.next()
                    P.op("dve", lambda e, gs=gs, py=py, t=t: e.tensor_tensor(out=t.ap[:, :ntok], in0=gs.ap[:, :ntok], in1=py.ap[:, :ntok], op=ALU.mult),
                         reads=[gs, py], writes=[t])
                    if r == 1:
                        P.op("dve", lambda e, t=t, acc=acc: e.tensor_tensor(out=acc.ap[:, :ntok], in0=acc.ap[:, :ntok], in1=t.ap[:, :ntok], op=ALU.add),
                             reads=[acc, t], writes=[acc])
                    else:
                        P.op("dve", lambda e, t=t, acc=acc, m=m: e.tensor_tensor(out=mergedT.ap[:, m, :ntok], in0=acc.ap[:, :ntok], in1=t.ap[:, :ntok], op=ALU.add),
                             reads=[acc, t], writes=[mergedT])
        if getattr(self, "mix_stop", None) == "merge":
            return
        wo = self.wo2
        self.post_residual(lambda i, hf: (mergedT, [(mergedT.ap[:, k, i * 128:(i + 1) * 128], wo[hf].ap[:, k, :]) for k in range(8)], [mergedT, wo[hf]]),
                           ntok // 128, x_tiles_src, x_out_dst, self.gaG[s][0], (mixt_r, sm_r, xres_r, sq_r), xout_buf, is_out)

    def post_residual(self, mm_fn, nt, x_tiles_src, x_out_dst, gaG, rings, xout_buf, is_out, from_sbuf=None):
        P = self.P
        mixt_r, sm_r, xres_r, sq_r = rings
        for i in range(nt):
            xres = xres_r.next()
            xs = x_tiles_src[i]
            if isinstance(xs, tuple):
                self.load(xres, xs[0], dep=xs[1])
            else:
                self.load(xres, xs)
            sm = sm_r.next()
            mt = mixt_r.next()
            for hf in range(2):
                cs = slice(hf * 512, (hf + 1) * 512)
                if from_sbuf is None:
                    _, pairs, rd = mm_fn(i, hf)
                    pr = self.ps.next()
                    n = len(pairs)
                    for j, (l, r) in enumerate(pairs):
                        P.op("pe", lambda e, l=l, r=r, j=j, n=n, pr=pr: e.matmul(out=pr.ap, lhsT=l, rhs=r, start=(j == 0), stop=(j == n - 1)),
                             reads=rd, writes=[pr], signal=(j == n - 1))
                    src_ap, src_t = pr.ap, pr
                else:
                    src_ap, src_t = from_sbuf(i)[0][:, cs], from_sbuf(i)[1]
                sq = sq_r.next()
                P.op("act", lambda e, src_ap=src_ap, sq=sq, sm=sm, hf=hf: e.activation(out=sq.ap[:, 0:512], in_=src_ap, func=AF.Square, accum_out=sm.ap[:, hf:hf + 1]),
                     reads=[src_t], writes=[sq, sm])
                P.op("dve", lambda e, src_ap=src_ap, mt=mt, cs=cs: e.tensor_tensor(out=mt.ap[:, cs], in0=src_ap, in1=gaG.ap[:, cs], op=ALU.mult),
                     reads=[src_t, gaG, sq], writes=[mt])
            lvl = getattr(self, "pr_stop", 9)
            if lvl <= 1:
                continue
            P.op("dve", lambda e, sm=sm: e.tensor_tensor(out=sm.ap[:, 2:3], in0=sm.ap[:, 0:1], in1=sm.ap[:, 1:2], op=ALU.add), reads=[sm], writes=[sm])
            P.op("act", lambda e, sm=sm: e.activation(out=sm.ap[:, 3:4], in_=sm.ap[:, 2:3], func=AF.Sqrt, scale=1.0 / D, bias=EPS), reads=[sm], writes=[sm])
            P.op("dve", lambda e, sm=sm: e.reciprocal(out=sm.ap[:, 3:4], in_=sm.ap[:, 3:4]), reads=[sm], writes=[sm])
            if lvl <= 2:
                continue
            P.op("dve", lambda e, sm=sm, mt=mt, xres=xres: e.scalar_tensor_tensor(out=xres.ap, in0=mt.ap, scalar=sm.ap[:, 3:4], in1=xres.ap, op0=ALU.mult, op1=ALU.add),
                 reads=[mt, sm, xres], writes=[xres])
            if lvl <= 3:
                continue
            P.dma("pool", x_out_dst[i], xres.ap, reads=[xres], writes=[xout_buf], is_output=is_out)

    def ffn_dense_block(self, h2T, ntok, wr, work, x_tiles_src, x_out_dst, s, xout_buf, is_out):
        P = self.P
        actT, sg_r, rings = work
        wg_d, wu_d, wd_d = self.dl("wfg"), self.dl("wfu"), self.dl("wfd")
        for j in range(6):
            w = 512 if j < 5 else 256
            wg = wr.next()
            wu = wr.next()
            P.dma("pool", wg.ap[:, :, 0:w], wg_d[:, j * 512:j * 512 + w].rearrange("(k p) n -> p k n", p=128), writes=[wg])
            P.dma("pool", wu.ap[:, :, 0:w], wu_d[:, j * 512:j * 512 + w].rearrange("(k p) n -> p k n", p=128), writes=[wu])
            for mm in range(w // 128):
                pg = self.proj_fm(wg, mm * 128, h2T, ntok)
                pu = self.proj_fm(wu, mm * 128, h2T, ntok)
                sg = sg_r.next()
                P.op("act", lambda e, pg=pg, sg=sg: e.activation(out=sg.ap[:, :ntok], in_=pg.ap[:, :ntok], func=AF.Silu), reads=[pg], writes=[sg])
                kc = j * 4 + mm
                P.op("dve", lambda e, sg=sg, pu=pu, kc=kc: e.tensor_tensor(out=actT.ap[:, kc, :ntok], in0=sg.ap[:, :ntok], in1=pu.ap[:, :ntok], op=ALU.mult),
                     reads=[sg, pu], writes=[actT])
            self.tick()
        nt = ntok // 128
        halves = []
        for hf in range(2):
            slots = []
            for sl in range(3):
                k0 = sl * 8
                nk = min(8, 22 - k0)
                wd = wr.next()
                P.dma("pool", wd.ap[:, 0:nk, :], wd_d[k0 * 128:(k0 + nk) * 128, hf * 512:(hf + 1) * 512].rearrange("(k p) n -> p k n", p=128), writes=[wd])
                slots.append((wd, k0, nk))
            halves.append(slots)

        def mm_fn(i, hf):
            pairs = []
            rd = [actT]
            for (wd, k0, nk) in halves[hf]:
                rd.append(wd)
                for k in range(nk):
                    pairs.append((actT.ap[:, k0 + k, i * 128:(i + 1) * 128], wd.ap[:, k, :]))
            return None, pairs, rd
        self.post_residual(mm_fn, nt, x_tiles_src, x_out_dst, self.gaG[s][1], rings, xout_buf, is_out)

    def ffn_moe_block(self, h2T, wr, work, x_tiles_src, x_out_dst, xout_buf):
        P, A = self.P, self.A
        ntok = 1024
        nt = 8
        acc, act_r, sg_r, gates, lg_r, rings, wrt, brt = work
        weg, weu, wed = self.dl("weg"), self.dl("weu"), self.dl("wed")
        for i in range(nt):
            pl = self.ps.next()
            for k in range(8):
                P.op("pe", lambda e, k=k, i=i, pl=pl: e.matmul(out=pl.ap[:, 0:8], lhsT=h2T.ap[:, k, i * 128:(i + 1) * 128], rhs=wrt.ap[:, k, :],
                                                           start=(k == 0), stop=(k == 7)), reads=[h2T, wrt], writes=[pl], signal=(k == 7))
            lg = lg_r.next()
            L, M1, L2, M2 = (lg.ap[:, j, :] for j in range(4))
            sm = lg.ap[:, 4, :]
            P.op("dve", lambda e, pl=pl, L=L: e.tensor_tensor(out=L, in0=pl.ap[:, 0:8], in1=brt.ap, op=ALU.add), reads=[pl, brt], writes=[lg])
            P.op("dve", lambda e, L=L, sm=sm: e.tensor_reduce(out=sm[:, 0:1], in_=L, axis=AX.X, op=ALU.max), reads=[lg], writes=[lg])
            P.op("dve", lambda e, L=L, M1=M1, sm=sm: e.tensor_scalar(out=M1, in0=L, scalar1=sm[:, 0:1], scalar2=None, op0=ALU.is_ge), reads=[lg], writes=[lg])
            P.op("dve", lambda e, L=L, M1=M1, L2=L2: e.scalar_tensor_tensor(out=L2, in0=M1, scalar=-1e30, in1=L, op0=ALU.mult, op1=ALU.add), reads=[lg], writes=[lg])
            P.op("dve", lambda e, L2=L2, sm=sm: e.tensor_reduce(out=sm[:, 1:2], in_=L2, axis=AX.X, op=ALU.max), reads=[lg], writes=[lg])
            P.op("dve", lambda e, L2=L2, M2=M2, sm=sm: e.tensor_scalar(out=M2, in0=L2, scalar1=sm[:, 1:2], scalar2=None, op0=ALU.is_ge), reads=[lg], writes=[lg])
            P.op("dve", lambda e, sm=sm: e.tensor_tensor(out=sm[:, 2:3], in0=sm[:, 1:2], in1=sm[:, 0:1], op=ALU.subtract), reads=[lg], writes=[lg])
            P.op("act", lambda e, sm=sm: e.activation(out=sm[:, 3:4], in_=sm[:, 2:3], func=AF.Exp), reads=[lg], writes=[lg])
            P.op("dve", lambda e, sm=sm: e.tensor_scalar(out=sm[:, 4:5], in0=sm[:, 3:4], scalar1=1.0, scalar2=None, op0=ALU.add), reads=[lg], writes=[lg])
            P.op("dve", lambda e, sm=sm: e.reciprocal(out=sm[:, 5:6], in_=sm[:, 4:5]), reads=[lg], writes=[lg])
            P.op("dve", lambda e, sm=sm: e.tensor_tensor(out=sm[:, 6:7], in0=sm[:, 3:4], in1=sm[:, 5:6], op=ALU.mult), reads=[lg], writes=[lg])
            P.op("dve", lambda e, M1=M1, sm=sm, i=i: e.tensor_scalar(out=gates.ap[:, i, :], in0=M1, scalar1=sm[:, 5:6], scalar2=None, op0=ALU.mult), reads=[lg], writes=[gates])
            P.op("dve", lambda e, M2=M2, sm=sm, i=i: e.scalar_tensor_tensor(out=gates.ap[:, i, :], in0=M2, scalar=sm[:, 6:7], in1=gates.ap[:, i, :], op0=ALU.mult, op1=ALU.add),
                 reads=[lg, gates], writes=[gates])
        first = True
        for ex in range(NEXP):
            for j in range(7):
                wg = wr.next()
                wu = wr.next()
                wd = wr.next()
                self.load(wg, weg[ex, :, j * 512:(j + 1) * 512].rearrange("(k p) n -> p k n", p=128), "pool")
                self.load(wu, weu[ex, :, j * 512:(j + 1) * 512].rearrange("(k p) n -> p k n", p=128), "pool")
                wdv = wd.ap.rearrange("p a b -> p (a b)").rearrange("p (k n) -> p k n", k=4)
                P.dma("pool", wdv, wed[ex, j * 512:(j + 1) * 512, :].rearrange("(k p) n -> p k n", p=128), writes=[wd])
                at = act_r.next()
                for mm in range(4):
                    for th in range(2):
                        ts = slice(th * 512, (th + 1) * 512)
                        pg = self.ps.next()
                        pu = self.ps.next()
                        for k in range(8):
                            P.op("pe", lambda e, k=k, mm=mm, ts=ts, pg=pg, wg=wg: e.matmul(out=pg.ap, lhsT=wg.ap[:, k, mm * 128:(mm + 1) * 128], rhs=h2T.ap[:, k, ts],
                                                                                  start=(k == 0), stop=(k == 7)), reads=[wg, h2T], writes=[pg], signal=(k == 7))
                        for k in range(8):
                            P.op("pe", lambda e, k=k, mm=mm, ts=ts, pu=pu, wu=wu: e.matmul(out=pu.ap, lhsT=wu.ap[:, k, mm * 128:(mm + 1) * 128], rhs=h2T.ap[:, k, ts],
                                                                                  start=(k == 0), stop=(k == 7)), reads=[wu, h2T], writes=[pu], signal=(k == 7))
                        sg = sg_r.next()
                        P.op("act", lambda e, pg=pg, sg=sg: e.activation(out=sg.ap, in_=pg.ap, func=AF.Silu), reads=[pg], writes=[sg])
                        P.op("dve", lambda e, sg=sg, pu=pu, at=at, mm=mm, ts=ts: e.tensor_tensor(out=at.ap[:, mm, ts], in0=sg.ap, in1=pu.ap, op=ALU.mult),
                             reads=[sg, pu], writes=[at])
                for i in range(nt):
                    for hf in range(2):
                        cs = slice(hf * 512, (hf + 1) * 512)
                        pd = self.ps.next()
                        for mm in range(4):
                            rhs = wdv[:, mm, cs]
                            P.op("pe", lambda e, mm=mm, i=i, pd=pd, at=at, rhs=rhs: e.matmul(out=pd.ap, lhsT=at.ap[:, mm, i * 128:(i + 1) * 128], rhs=rhs,
                                                                                    start=(mm == 0), stop=(mm == 3)), reads=[at, wd], writes=[pd], signal=(mm == 3))
                        if first:
                            P.op("dve", lambda e, pd=pd, i=i, cs=cs, ex=ex: e.tensor_scalar(out=acc.ap[:, i, cs], in0=pd.ap, scalar1=gates.ap[:, i, ex:ex + 1], scalar2=None, op0=ALU.mult),
                                 reads=[pd, gates], writes=[acc])
                        else:
                            P.op("dve", lambda e, pd=pd, i=i, cs=cs, ex=ex: e.scalar_tensor_tensor(out=acc.ap[:, i, cs], in0=pd.ap, scalar=gates.ap[:, i, ex:ex + 1], in1=acc.ap[:, i, cs],
                                                                                          op0=ALU.mult, op1=ALU.add), reads=[pd, gates, acc], writes=[acc])
                first = False
                self.tick()
        self.flush()
        self.post_residual(None, nt, x_tiles_src, x_out_dst, self.gaG[0][1], rings, xout_buf, True,
                           from_sbuf=lambda i: (acc.ap[:, i, :], acc))


def _decl_common(B, layer, main):
    B.inp("winP", [D, IN_DIM])
    if main:
        for nm, shp in (("wao", [512, D]), ("wfn", [512, D]), ("wco", [512, D]), ("wo", [D, D])):
            B.inp(nm, shp)
        B.inp("ropeC", [128, 34 * 128])
        B.inp("ropeS", [128, 34 * 128])
        if layer == 0:
            B.inp("wfg", [D, D_FF])
            B.inp("wfu", [D, D_FF])
            B.inp("wfd", [D_FF, D])
            B.inp("t256", [128, 2, 2, 256])
        else:
            B.inp("wrt", [128, 8, NEXP])
            B.inp("brt", [128, NEXP])
            B.inp("weg", [NEXP, D, D_EXP])
            B.inp("weu", [NEXP, D, D_EXP])
            B.inp("wed", [NEXP, D_EXP, D])


def build_pre(layer):
    B = Builder("pre", layer)
    P, A = B.P, B.A
    _decl_common(B, layer, False)
    xh = B.inp("xh", [34 * 128, D])
    zf = B.outp("zf", [4, TOK, 128], BF16)
    B.zf_b = P.buf("zf_d")
    B.emit_consts()
    B.emit_mod()
    B.emit_phaseA(xh, True, zf)
    P.finish()
    return B


def build_main(layer, stop_after=None, mix_stop=None):
    B = Builder("main", layer)
    B.mix_stop = mix_stop
    if isinstance(mix_stop, str) and mix_stop.startswith("pr"):
        B.pr_stop = int(mix_stop[2:])
    P, A, nc = B.P, B.A, B.nc
    last = layer == 1
    _decl_common(B, layer, True)
    xh = B.inp("xh", [34 * 128, D])
    hctx = B.inp("hctx", [CTX, D])
    zfg = B.inp("zfg", [4, 4, TOK, 128], BF16)
    xo = B.outp("xo", [TOK, D])
    if not last:
        hco = B.outp("hco", [CTX, D])
    B.u_d = B.scratch("u_d", [128, 4, 34 * 128], BF16)
    B.u_b = P.buf("u_d")
    B.ft_d = B.scratch("ft_d", [128, 4, TOK], BF16)
    B.ft_b = P.buf("ft_d")
    xmid = B.scratch("xmid_d", [TOK, D], F32)
    xmid_b = P.buf("xmid_d")
    xo_b = P.buf("xo")
    winP = B.din["winP"]
    B.emit_consts()
    B.emit_mod()
    m_mix = A.mark()
    B.kT = A.alloc("kT", [34 * 128], BF16)
    B.vaug = A.alloc("vaug", [34, 2, 65], BF16)
    P.op("dve", lambda e: e.memset(B.vaug.ap, 1.0), writes=[B.vaug])
    kcT = A.alloc("kcT", [CTX], BF16)
    vcaug = A.alloc("vcaug", [2, 2, 65], BF16)
    P.op("dve", lambda e: e.memset(vcaug.ap, 1.0), writes=[vcaug])
    B.wbr = []
    for nm in ("wao", "wfn", "wco"):
        w = A.alloc(nm, [4, D], BF16)
        B.load(w, B.dl(nm).rearrange("(k p) n -> p k n", p=128), "pool")
        B.wbr.append(w)
    B.wo2 = []
    for hf in range(2):
        w = A.alloc(f"wo{hf}", [8, 512], BF16)
        B.load(w, B.dl("wo")[:, hf * 512:(hf + 1) * 512].rearrange("(k p) n -> p k n", p=128), "pool")
        B.wo2.append(w)

    if stop_after == "mod":
        P.finish()
        return B
    m0 = A.mark()
    rings = B.norm_rings(2)
    hcT = A.alloc("hcT", [8, 256], BF16)
    B.make_hT([hctx[i * 128:(i + 1) * 128, :] for i in range(2)], hcT, 1, 0, rings)
    ncolA = PA_W
    wA = A.alloc("wAc", [8, ncolA], BF16)
    B.load(wA, winP[:, 0:ncolA].rearrange("(k p) n -> p k n", p=128), "pool")
    pr = B.proj_fm(wA, PA_K, hcT, CTX)
    P.op("act", lambda e: e.copy(out=kcT.ap, in_=pr.ap[:, :CTX]), reads=[pr], writes=[kcT])
    pv = B.ps.next()
    pvv = pv.ap.rearrange("p (i c) -> p i c", i=4)
    for i in range(2):
        for k in range(8):
            P.op("pe", lambda e, k=k, i=i: e.matmul(out=pvv[:, i, :], lhsT=hcT.ap[:, k, i * 128:(i + 1) * 128], rhs=wA.ap[:, k, PA_V:PA_V + 128],
                                                  start=(k == 0), stop=(k == 7)), reads=[hcT, wA], writes=[pv], signal=(k == 7 and i == 1))
    P.op("act", lambda e: e.copy(out=vcaug.ap[:, :, :, 0:64], in_=pvv[:, 0:2, :].rearrange("p i (g d) -> p i g d", g=2)), reads=[pv], writes=[vcaug])
    ctxkb = [(kcT.ap[:, i * 128:(i + 1) * 128], kcT, vcaug.ap[:, i, :, :], vcaug, None) for i in range(2)]
    if stop_after == "ctx_kv":
        P.finish()
        return B
    if not last:
        hmid = B.scratch("hmid_d", [CTX, D], F32)
        hmid_b = P.buf("hmid_d")
        hco_b = P.buf("hco")
        zcf = A.alloc("zcf", [2, 512], BF16)
        for i in range(2):
            pz = B.ps.next()
            for k in range(8):
                P.op("pe", lambda e, k=k, i=i, pz=pz: e.matmul(out=pz.ap, lhsT=hcT.ap[:, k, i * 128:(i + 1) * 128], rhs=wA.ap[:, k, PA_F:PA_F + 512],
                                                           start=(k == 0), stop=(k == 7)), reads=[hcT, wA], writes=[pz], signal=(k == 7))
            P.op("act", lambda e, i=i, pz=pz: e.copy(out=zcf.ap[:, i, :], in_=pz.ap), reads=[pz], writes=[zcf])
        t256 = B.const_tile("t256", [2, 2, 256], BF16, B.din["t256"])
        Gc = A.alloc("Gc", [2, 256], BF16)
        FTc = A.alloc("FTc", [4, 256], BF16)
        scl = 1.0 / math.sqrt(256.0 * 128.0)
        for g in range(4):
            pg_ = B.ps.next()
            for i in range(2):
                P.op("pe", lambda e, g=g, i=i, pg_=pg_: e.matmul(out=pg_.ap, lhsT=zcf.ap[:, i, g * 128:(g + 1) * 128],
                                                             rhs=t256.ap[:, i, :, :].rearrange("p r k -> p (r k)"), start=(i == 0), stop=(i == 1)),
                     reads=[zcf, t256], writes=[pg_], signal=(i == 1))
            P.op("act", lambda e, pg_=pg_: e.copy(out=Gc.ap.rearrange("p r k -> p (r k)"), in_=pg_.ap), reads=[pg_], writes=[Gc])
            pf = B.ps.next()
            P.op("pe", lambda e, pf=pf: e.matmul(out=pf.ap[:, 0:256], lhsT=B.cbsb.ap[:, 0, :], rhs=Gc.ap[:, 0, :], start=True, stop=False),
                 reads=[B.cbsb, Gc], writes=[pf], signal=False)
            P.op("pe", lambda e, pf=pf: e.matmul(out=pf.ap[:, 0:256], lhsT=B.cbsb.ap[:, 1, :], rhs=Gc.ap[:, 1, :], start=False, stop=True),
                 reads=[B.cbsb, Gc], writes=[pf])
            P.op("act", lambda e, g=g, pf=pf: e.activation(out=FTc.ap[:, g, :], in_=pf.ap[:, 0:256], func=AF.Copy, scale=scl), reads=[pf], writes=[FTc])
        if stop_after == "ctx_fft":
            P.finish()
            return B
        ucT = A.alloc("ucT", [4, 258], BF16)
        P.op("dve", lambda e: e.memset(ucT.ap, 0.0), writes=[ucT])
        cxs = A.alloc("cxs_c", [512], F32)
        for m in range(4):
            px = B.proj_fm(wA, PA_CX + m * 128, hcT, CTX)
            P.op("act", lambda e, px=px: e.copy(out=cxs.ap[:, :CTX], in_=px.ap[:, :CTX]), reads=[px], writes=[cxs])
            pc = B.proj_fm(wA, PA_CC + m * 128, hcT, CTX)
            P.op("dve", lambda e, m=m, pc=pc: e.tensor_tensor(out=ucT.ap[:, m, 1:257], in0=cxs.ap[:, :CTX], in1=pc.ap[:, :CTX], op=ALU.mult),
                 reads=[cxs, pc], writes=[ucT])
        wr = A.ring("wrc", 3, [8, 512], BF16)
        wq = wr.next()
        B.load(wq, winP[:, PM_Q:PM_Q + 512].rearrange("(k p) n -> p k n", p=128), "pool")
        qcT = A.alloc("qcT", [4, 256], BF16)
        for m in range(4):
            pq = B.proj_fm(wq, m * 128, hcT, CTX)
            P.op("act", lambda e, m=m, pq=pq: e.copy(out=qcT.ap[:, m, :], in_=pq.ap[:, :CTX]), reads=[pq], writes=[qcT])
        attnTc = A.alloc("attnTc", [4, 256], BF16)
        abufs = (A.ring("pTc", 1, [2, 2, 4, 128], BF16), A.ring("atokc", 1, [8, 64], BF16), A.ring("denc", 1, [2, 8], F32))
        for t in range(2):
            B.attention_tile(qcT, t * 128, ctxkb, attnTc, t * 128, abufs)
        if stop_after == "ctx_attn":
            dq = B.outp("dbg_qcT", [128, 4 * 256], BF16)
            da = B.outp("dbg_attnTc", [128, 4 * 256], BF16)
            dk = B.outp("dbg_kcT", [128, 256], BF16)
            dv = B.outp("dbg_vc", [128, 2 * 2 * 65], BF16)
            ob = P.buf("dbgo")
            P.dma("sp", dq, qcT.ap.rearrange("p a b -> p (a b)"), reads=[qcT], writes=[ob], is_output=True)
            P.dma("sp", da, attnTc.ap.rearrange("p a b -> p (a b)"), reads=[attnTc], writes=[ob], is_output=True)
            P.dma("sp", dk, kcT.ap, reads=[kcT], writes=[ob], is_output=True)
            P.dma("sp", dv, vcaug.ap.rearrange("p a b c -> p (a b c)"), reads=[vcaug], writes=[ob], is_output=True)
            P.finish()
            return B
        work = (A.alloc("yconvTc", [4, 256], BF16), A.ring("gsbc", 2, [512], BF16), A.ring("tmpc", 3, [512], F32), A.alloc("mergedTc", [8, 256], BF16),
                A.ring("mixtc", 1, [D], F32), A.ring("smc", 2, [4], F32), A.ring("xresc", 2, [D], F32), A.ring("sqc", 1, [512], BF16))
        B.mix_block(hcT, CTX, qcT, attnTc, ucT.ap, ucT, FTc.ap, FTc, wr, work,
                    [hctx[i * 128:(i + 1) * 128, :] for i in range(2)], [hmid[i * 128:(i + 1) * 128, :] for i in range(2)], 1, hmid_b)
        if stop_after == "ctx_mix":
            ob = P.buf("dbgo2")
            for nm_, t_, a_, n_ in (("dbg_FTc", FTc, 4, 256), ("dbg_ucT", ucT, 4, 258), ("dbg_yconv", work[0], 4, 256), ("dbg_merged", work[3], 8, 256), ("dbg_attnTc", attnTc, 4, 256)):
                do = B.outp(nm_, [128, a_, n_], BF16)
                P.dma("sp", do, t_.ap[:, :, 0:n_], reads=[t_], writes=[ob], is_output=True)
            P.finish()
            return B
        P.barrier()
        A.release(m0)
        rings = B.norm_rings(2)
        h2c = A.alloc("h2c", [8, 256], BF16)
        B.make_hT_dep([hmid[i * 128:(i + 1) * 128, :] for i in range(2)], h2c, 1, 1, rings, hmid_b)
        wr = A.ring("wrf", 8, [8, 512], BF16)
        work = (A.alloc("actTc", [22, 256], BF16), A.ring("sgc", 2, [512], BF16),
                (A.ring("mixtf", 1, [D], F32), A.ring("smf", 2, [4], F32), A.ring("xresf", 1, [D], F32), A.ring("sqf", 1, [512], BF16)))
        B.ffn_dense_block(h2c, CTX, wr, work, [(hmid[i * 128:(i + 1) * 128, :], hmid_b) for i in range(2)],
                          [hco[i * 128:(i + 1) * 128, :] for i in range(2)], 1, hco_b, True)
    P.barrier()
    A.release(m0)

    if stop_after == "ctx":
        P.finish()
        return B
    B.emit_phaseA(xh, False)
    if stop_after == "A":
        P.finish()
        return B
    B.emit_fft(zfg)
    if stop_after == "fft":
        P.finish()
        return B

    m0 = A.mark()
    rings = B.norm_rings(3)
    hTr = A.ring("hTm", 1, [8, 512], BF16)
    rope_r = A.ring("ropeM", 1, [2, 512], F32)
    qraw = A.alloc("qraw", [512], BF16)
    rtmp = (A.alloc("rt1m", [512], F32), A.alloc("rt2m", [512], F32))
    qT = A.alloc("qT", [4, 512], BF16)
    attnT = A.alloc("attnT", [4, 512], BF16)
    abufs = (A.ring("pT", 2, [5, 2, 4, 128], BF16), A.ring("atok", 2, [8, 64], BF16), A.ring("den", 2, [2, 8], F32))
    ubr = A.ring("ub", 1, [4, 514], BF16)
    ftr = A.ring("ftb", 1, [4, 512], BF16)
    wr = A.ring("wrm", 3, [8, 512], BF16)
    work = (A.alloc("yconvT", [4, 512], BF16), A.ring("gsb", 2, [512], BF16), A.ring("tmpm", 3, [512], F32), A.alloc("mergedT", [8, 512], BF16),
            A.ring("mixt", 1, [D], F32), A.ring("smm", 2, [4], F32), A.ring("xres", 1, [D], F32), A.ring("sqm", 1, [512], BF16))
    for bi in range(8):
        c0 = (4 * bi + 1) * 128
        hT = hTr.next()
        own = [xh[(4 * bi + 1 + i) * 128:(4 * bi + 2 + i) * 128, :] for i in range(4)]
        B.make_hT(own, hT, 0, 0, rings)
        rp = rope_r.next()
        P.dma("sp", rp.ap[:, 0, :], B.din["ropeC"][:, c0:c0 + 512], writes=[rp])
        P.dma("sp", rp.ap[:, 1, :], B.din["ropeS"][:, c0:c0 + 512], writes=[rp])
        B.ropeb = rp
        wq = wr.next()
        B.load(wq, winP[:, PM_Q:PM_Q + 512].rearrange("(k p) n -> p k n", p=128), "pool")
        for m in range(4):
            pq = B.proj_fm(wq, m * 128, hT, 512)
            B.rope(pq, qraw, qT.ap[:, m, :], qT, rp.ap[:, 0, :], rp.ap[:, 1, :], 512, rtmp)
        for t in range(4):
            T = 4 * bi + t
            kbs = []
            for d_, mi in ((0, 2 if T == 0 else 0), (1, None), (2, 3 if T == 31 else 1)):
                sl = T + d_
                kbs.append((B.kT.ap[:, sl * 128:(sl + 1) * 128], B.kT, B.vaug.ap[:, sl, :, :], B.vaug, mi))
            kbs += ctxkb
            B.attention_tile(qT, t * 128, kbs, attnT, t * 128, abufs)
        ub = ubr.next()
        P.dma("sp", ub.ap, B.u_d[:, :, c0 - 1:c0 + 513], reads=[B.u_b], writes=[ub])
        ftb = ftr.next()
        P.dma("sp", ftb.ap, B.ft_d[:, :, bi * 512:(bi + 1) * 512], reads=[B.ft_b], writes=[ftb])
        B.mix_block(hT, 512, qT, attnT, ub.ap, ub, ftb.ap, ftb, wr, work, own,
                    [xmid[(4 * bi + i) * 128:(4 * bi + i + 1) * 128, :] for i in range(4)], 0, xmid_b)
    P.barrier()
    A.release(m0)

    if stop_after == "mix":
        P.finish()
        return B
    A.release(m_mix)
    m0 = A.mark()
    if not last:
        rings = B.norm_rings(4)
        h2r = A.ring("h2T", 1, [8, 512], BF16)
        wr = A.ring("wrf2", 8, [8, 512], BF16)
        work = (A.alloc("actT", [22, 512], BF16), A.ring("sg", 2, [512], BF16),
                (A.ring("mixtF", 1, [D], F32), A.ring("smF", 2, [4], F32), A.ring("xresF", 2, [D], F32), A.ring("sqF", 1, [512], BF16)))
        for bi in range(8):
            h2T = h2r.next()
            src = [xmid[(4 * bi + i) * 128:(4 * bi + i + 1) * 128, :] for i in range(4)]
            B.make_hT_dep(src, h2T, 0, 1, rings, xmid_b)
            B.ffn_dense_block(h2T, 512, wr, work, [(a, xmid_b) for a in src],
                              [xo[(4 * bi + i) * 128:(4 * bi + i + 1) * 128, :] for i in range(4)], 0, xo_b, True)
    else:
        rings = B.norm_rings(4)
        h2r = A.ring("h2T", 1, [8, 1024], BF16)
        wr = A.ring("wre", 6, [8, 512], BF16)
        wrt = B.const_tile("wrt", [8, NEXP], BF16, B.din["wrt"])
        brt = B.const_tile("brt", [NEXP], F32, B.din["brt"])
        work = (A.alloc("acc", [8, D], F32), A.ring("actc", 2, [4, 1024], BF16), A.ring("sge", 2, [512], BF16), A.alloc("gates", [8, NEXP], F32),
                A.ring("lg", 2, [5, 8], F32),
                (A.ring("mixtE", 1, [D], F32), A.ring("smE", 2, [4], F32), A.ring("xresE", 2, [D], F32), A.ring("sqE", 1, [512], BF16)), wrt, brt)
        for bi in range(4):
            h2T = h2r.next()
            src = [xmid[(8 * bi + i) * 128:(8 * bi + i + 1) * 128, :] for i in range(8)]
            B.make_hT_dep(src, h2T, 0, 1, rings, xmid_b)
            B.ffn_moe_block(h2T, wr, work, [(a, xmid_b) for a in src], [xo[(8 * bi + i) * 128:(8 * bi + i + 1) * 128, :] for i in range(8)], xo_b)
    P.finish()
    return B


def _w_in_perm():
    cols = []
    cols += list(range(F_OFF, F_OFF + 512))
    cols += list(range(K_OFF, K_OFF + 128))
    cols += list(range(V_OFF, V_OFF + 128))
    cols += list(range(CX_OFF, CX_OFF + 512))
    cols += list(range(CC_OFF, CC_OFF + 512))
    for m in range(4):
        cols += list(range(Q_OFF + m * 64, Q_OFF + (m + 1) * 64))
        cols += list(range(Q_OFF + (4 + m) * 64, Q_OFF + (5 + m) * 64))
    cols += list(range(CB_OFF, CB_OFF + 512))
    for m in range(8):
        for r in range(3):
            cols += list(range(GATE_OFF + r * 1024 + m * 128, GATE_OFF + r * 1024 + (m + 1) * 128))
    assert len(cols) == IN_DIM and len(set(cols)) == IN_DIM
    return np.asarray(cols)


def _colform(v, n):
    return np.ascontiguousarray(np.asarray(v, np.float32).reshape(n, 128).T)


_CONST_CACHE = {}


def _static_tables():
    if "t" in _CONST_CACHE:
        return _CONST_CACHE["t"]
    t = {}
    t["ident"] = np.eye(128, dtype=np.float32)
    rt = np.zeros((128, 128), np.float32)
    for i in range(64):
        rt[2 * i + 1, 2 * i] = -1.0
        rt[2 * i, 2 * i + 1] = 1.0
    t["rt"] = rt
    n = np.arange(128, dtype=np.float64)
    k1 = np.arange(128, dtype=np.float64)
    ang = 2 * np.pi * np.outer(n, k1) / 128.0
    t1 = np.stack([np.cos(ang), -np.sin(ang)], axis=1).reshape(128, 2, 2, 64)
    t["t1"] = np.ascontiguousarray(t1.transpose(0, 2, 1, 3)).astype(np.float32)
    angc = 2 * np.pi * np.outer(n, n) / 128.0
    t["cbsb"] = np.ascontiguousarray(np.stack([np.cos(angc), np.sin(angc)], axis=1)).astype(np.float32)
    nn = np.arange(256, dtype=np.float64)
    a256 = 2 * np.pi * np.outer(nn, nn) / 256.0
    t256 = np.stack([np.cos(a256), -np.sin(a256)], axis=1)
    t["t256"] = np.ascontiguousarray(t256.reshape(2, 128, 2, 256).transpose(1, 0, 2, 3)).astype(np.float32)
    kk = np.arange(128)[:, None]
    qq = np.arange(128)[None, :]
    prev = (kk >= qq).astype(np.float32)
    nxt = (kk <= qq).astype(np.float32)
    half = 32
    inv_freq = 1.0 / (10000.0 ** (np.arange(0, half, 2, dtype=np.float64) / half))
    for j in range(4):
        m = np.stack([prev, nxt, prev * (1.0 if j != 0 else 0.0), nxt * (1.0 if j != 3 else 0.0)], axis=1)
        t[("masks", j)] = np.ascontiguousarray(m).astype(np.float32)
        t[("valid", j)] = np.tile(np.asarray([[1.0 if j != 0 else 0.0, 1.0 if j != 3 else 0.0]], np.float32), (128, 1))
        pos = TOK * j - 128 + np.arange(34 * 128)
        row = (pos // 64).astype(np.float64)
        col = (pos % 64).astype(np.float64)
        angp = np.concatenate([row[:, None] * inv_freq[None, :], col[:, None] * inv_freq[None, :]], axis=1)
        d = np.arange(128) % 64
        pi_ = d // 2
        t[("ropeC", j)] = np.ascontiguousarray(np.cos(angp)[:, pi_].T).astype(np.float32)
        t[("ropeS", j)] = np.ascontiguousarray(np.sin(angp)[:, pi_].T).astype(np.float32)
        n2 = np.arange(128, dtype=np.float64)[:, None, None]
        k1_ = np.arange(128, dtype=np.float64)[None, :, None]
        k2_ = (32 * j + np.arange(32, dtype=np.float64))[None, None, :]
        th = 2 * np.pi * n2 * (k1_ + 128.0 * k2_) / float(SEQ)
        et = np.concatenate([np.sin(th), np.cos(th), -np.sin(th)], axis=2)
        t[("etab", j)] = np.ascontiguousarray(et).astype(np.float32)
    _CONST_CACHE["t"] = t
    return t


def _layer_common(inp, l):
    perm = _w_in_perm()
    c = {}
    c["winP"] = np.ascontiguousarray(np.asarray(inp["w_in"][l], np.float32)[:, perm])
    c["wmod"] = np.ascontiguousarray(np.asarray(inp["w_mod"][l], np.float32))
    bm = np.asarray(inp["b_mod"][l], np.float32)
    c["bcols"] = _colform(bm, 48)
    c["brow"] = np.ascontiguousarray(np.broadcast_to(np.stack([bm[2 * D:3 * D], bm[5 * D:6 * D]])[None], (128, 2, D))).astype(np.float32)
    c["gpm_c"] = _colform(inp["g_pre_mix"][l], 8)
    c["gpf_c"] = _colform(inp["g_pre_ffn"][l], 8)
    c["gqm_r"] = np.ascontiguousarray(np.broadcast_to(np.asarray(inp["g_post_mix"][l], np.float32)[None], (128, D)))
    c["gqf_r"] = np.ascontiguousarray(np.broadcast_to(np.asarray(inp["g_post_ffn"][l], np.float32)[None], (128, D)))
    c["wao"] = np.ascontiguousarray(np.asarray(inp["w_attn_o"][l], np.float32))
    c["wfn"] = np.ascontiguousarray(np.asarray(inp["w_fnet"][l], np.float32))
    c["wco"] = np.ascontiguousarray(np.asarray(inp["w_conv_out"][l], np.float32))
    c["wo"] = np.ascontiguousarray(np.asarray(inp["w_o"][l], np.float32))
    c["sink_b"] = np.ascontiguousarray(np.broadcast_to(np.asarray(inp["attn_sink"][l], np.float32)[None], (128, 8)))
    wc = np.asarray(inp["w_conv"][l], np.float32)
    c["wconv_c"] = np.ascontiguousarray(wc.reshape(3, 4, 128).transpose(2, 1, 0))
    if l == 0:
        c["wfg"] = np.ascontiguousarray(np.asarray(inp["w_ff_gate"][0], np.float32))
        c["wfu"] = np.ascontiguousarray(np.asarray(inp["w_ff_up"][0], np.float32))
        c["wfd"] = np.ascontiguousarray(np.asarray(inp["w_ff_down"][0], np.float32))
    else:
        wr = np.asarray(inp["w_router"][0], np.float32)
        c["wrt"] = np.ascontiguousarray(wr.reshape(8, 128, NEXP).transpose(1, 0, 2))
        c["brt"] = np.ascontiguousarray(np.broadcast_to(np.asarray(inp["b_router"][0], np.float32)[None], (128, NEXP)))
        c["weg"] = np.ascontiguousarray(np.asarray(inp["w_exp_gate"][0], np.float32))
        c["weu"] = np.ascontiguousarray(np.asarray(inp["w_exp_up"][0], np.float32))
        c["wed"] = np.ascontiguousarray(np.asarray(inp["w_exp_down"][0], np.float32))
    return c


def _ccols(inp, b):
    cb = np.asarray(inp["c"][b], np.float32)
    cc = np.asarray(inp["c_ctx"], np.float32)
    return np.ascontiguousarray(np.stack([_colform(cb, 8), _colform(cc, 8)], axis=2))


def _xh(xfull, cid):
    b, j = cid // 4, cid % 4
    out = np.zeros((34 * 128, D), np.float32)
    lo = TOK * j - 128
    hi = TOK * (j + 1) + 128
    s0, s1 = max(lo, 0), min(hi, SEQ)
    out[s0 - lo:s1 - lo] = xfull[b, s0:s1]
    return out


_PROG_CACHE = {}


def _get_prog(kind, layer):
    key = (kind, layer)
    if key not in _PROG_CACHE:
        _PROG_CACHE[key] = build_pre(layer) if kind == "pre" else build_main(layer)
    return _PROG_CACHE[key]


def _run(B, maps):
    names = set(B.din.keys())
    in_maps = [{k: v for k, v in m.items() if k in names} for m in maps]
    for m in in_maps:
        missing = names - set(m.keys())
        assert not missing, missing
    res = run_bass_kernel_spmd(B.nc, in_maps, core_ids=list(range(8)))
    return res.results


def kernel_unfused(**inputs):
    tabs = _static_tables()
    x = np.asarray(inputs["x"], np.float32)
    hctx = [np.ascontiguousarray(np.asarray(inputs["ctx"][b], np.float32)) for b in range(NB)]
    for l in range(2):
        com = _layer_common(inputs, l)
        maps = []
        for cid in range(8):
            b, j = cid // 4, cid % 4
            m = dict(com)
            for nm in ("ident", "rt", "t1", "cbsb", "t256"):
                m[nm] = tabs[nm]
            for nm in ("masks", "valid", "ropeC", "ropeS", "etab"):
                m[nm] = tabs[(nm, j)]
            m["ccols"] = _ccols(inputs, b)
            m["xh"] = _xh(x, cid)
            m["hctx"] = hctx[b]
            maps.append(m)
        r = _run(_get_prog("pre", l), maps)
        for b in range(NB):
            zfg = np.ascontiguousarray(np.stack([np.asarray(r[4 * b + j]["zf"]) for j in range(4)]))
            for j in range(4):
                maps[4 * b + j]["zfg"] = zfg
        r = _run(_get_prog("main", l), maps)
        xn = np.empty_like(x)
        for cid in range(8):
            b, j = cid // 4, cid % 4
            xn[b, TOK * j:TOK * (j + 1)] = np.asarray(r[cid]["xo"], np.float32)
        x = xn
        if l == 0:
            hctx = [np.ascontiguousarray(np.asarray(r[4 * b]["hco"], np.float32)) for b in range(NB)]
    return x


GROUPS = [[0, 1, 2, 3], [4, 5, 6, 7]]


def _decl_layer(B, l):
    sfx = str(l)
    B.inp("winP" + sfx, [D, IN_DIM])
    for nm, shp in (("wao", [512, D]), ("wfn", [512, D]), ("wco", [512, D]), ("wo", [D, D])):
        B.inp(nm + sfx, shp)
    if l == 0:
        B.inp("wfg0", [D, D_FF])
        B.inp("wfu0", [D, D_FF])
        B.inp("wfd0", [D_FF, D])
    else:
        B.inp("wrt1", [128, 8, NEXP])
        B.inp("brt1", [128, NEXP])
        B.inp("weg1", [NEXP, D, D_EXP])
        B.inp("weu1", [NEXP, D, D_EXP])
        B.inp("wed1", [NEXP, D_EXP, D])


def build_fused(sim_cc=False, stop=None):
    B = Builder("main", 0)
    P, A, nc = B.P, B.A, B.nc
    B.sfx = "0"
    for l in range(2):
        if l == 1 and stop is not None and stop != "l1mix":
            continue
        _decl_layer(B, l)
    B.inp("ropeC", [128, 34 * 128])
    B.inp("ropeS", [128, 34 * 128])
    B.inp("t256", [128, 2, 2, 256])
    xh = B.inp("xh", [34 * 128, D])
    hctx_in = B.inp("hctx", [CTX, D])
    selh = B.inp("selh", [128, 8])
    xo = B.outp("xo", [TOK, D])
    xo_b = P.buf("xo")
    B.u_d = B.scratch("u_d", [128, 4, 34 * 128], BF16)
    B.u_b = P.buf("u_d")
    B.ft_d = B.scratch("ft_d", [128, 4, TOK], BF16)
    B.ft_b = P.buf("ft_d")
    xmid = B.scratch("xmid_d", [TOK, D], F32)
    xmid_b = P.buf("xmid_d")
    x1 = B.scratch("x1_d", [TOK, D], F32)
    x1_b = P.buf("x1_d")
    hmid = B.scratch("hmid_d", [CTX, D], F32)
    hmid_b = P.buf("hmid_d")
    hc1 = B.scratch("hc1_d", [CTX, D], F32)
    hc1_b = P.buf("hc1_d")
    xhalo = B.scratch("xhalo_d", [256, D], F32)
    xhalo_b = P.buf("xhalo_d")
    zf_l = [nc.dram_tensor(f"zf_cc{g}", [TOK, 128], BF16).ap() for g in range(4)]
    zfg_l = [nc.dram_tensor(f"zfg_cc{g}", [4 * TOK, 128], BF16).ap() for g in range(4)]
    xb_src = nc.dram_tensor("xb_cc", [256, D], F32).ap()
    xb_all = nc.dram_tensor("xball_cc", [4 * 256, D], F32).ap()
    B.zf_b = P.buf("zf_cc")
    zfg_b = P.buf("zfg_cc")
    xb_b = P.buf("xb_cc")
    xball_b = P.buf("xball_cc")
    def zsrc(r, g):
        return zfg_l[g][r * TOK:(r + 1) * TOK, :]

    B.emit_consts()
    B.alloc_mod_tiles()
    m_top = A.mark()
    for l in range(2):
        last = l == 1
        B.layer = l
        B.last = last
        B.sfx = str(l)
        winP = B.dl("winP")
        B.emit_layer_consts()
        B.emit_mod()
        m_mix = A.mark()
        B.kT = A.alloc("kT", [34 * 128], BF16)
        B.vaug = A.alloc("vaug", [34, 2, 65], BF16)
        P.op("dve", lambda e: e.memset(B.vaug.ap, 1.0), writes=[B.vaug])
        kcT = A.alloc("kcT", [CTX], BF16)
        vcaug = A.alloc("vcaug", [2, 2, 65], BF16)
        P.op("dve", lambda e: e.memset(vcaug.ap, 1.0), writes=[vcaug])
        B.wbr = []
        for nm in ("wao", "wfn", "wco"):
            w = A.alloc(nm, [4, D], BF16)
            B.load(w, B.dl(nm).rearrange("(k p) n -> p k n", p=128), "pool")
            B.wbr.append(w)
        B.wo2 = []
        for hf in range(2):
            w = A.alloc(f"wo{hf}", [8, 512], BF16)
            B.load(w, B.dl("wo")[:, hf * 512:(hf + 1) * 512].rearrange("(k p) n -> p k n", p=128), "pool")
            B.wo2.append(w)

        if l == 0:
            def xtile(t):
                return xh[(t + 1) * 128:(t + 2) * 128, :]
            hsrc = [hctx_in[i * 128:(i + 1) * 128, :] for i in range(2)]
        else:
            def xtile(t):
                if t < 0:
                    return (xhalo[0:128, :], xhalo_b)
                if t >= NTILE:
                    return (xhalo[128:256, :], xhalo_b)
                return (x1[t * 128:(t + 1) * 128, :], x1_b)
            hsrc = [(hc1[i * 128:(i + 1) * 128, :], hc1_b) for i in range(2)]
        B.xtile = xtile

        B.emit_phaseA(None, True, zf_l)
        m0 = A.mark()
        wA = A.alloc("wAc", [8, PA_W], BF16)
        B.load(wA, winP[:, 0:PA_W].rearrange("(k p) n -> p k n", p=128), "pool")
        if not last:
            wr_c = A.ring("wrc", 3, [8, 512], BF16)
            wq_c = wr_c.next()
            B.load(wq_c, winP[:, PM_Q:PM_Q + 512].rearrange("(k p) n -> p k n", p=128), "pool")
        for g in range(4):
            if sim_cc:
                for r in range(4):
                    P.dma("sp", zfg_l[g][r * TOK:(r + 1) * TOK, :], zf_l[g], reads=[B.zf_b], writes=[zfg_b])
            else:
                P.cc_allgather(zfg_l[g], zf_l[g], GROUPS, reads=[B.zf_b], writes=[zfg_b])

        rings = B.norm_rings(2)
        hcT = A.alloc("hcT", [8, 256], BF16)
        B.make_hT(hsrc, hcT, 1, 0, rings)
        pr = B.proj_fm(wA, PA_K, hcT, CTX)
        P.op("act", lambda e: e.copy(out=kcT.ap, in_=pr.ap[:, :CTX]), reads=[pr], writes=[kcT])
        pv = B.ps.next()
        pvv = pv.ap.rearrange("p (i c) -> p i c", i=4)
        for i in range(2):
            for k in range(8):
                P.op("pe", lambda e, k=k, i=i: e.matmul(out=pvv[:, i, :], lhsT=hcT.ap[:, k, i * 128:(i + 1) * 128], rhs=wA.ap[:, k, PA_V:PA_V + 128],
                                                      start=(k == 0), stop=(k == 7)), reads=[hcT, wA], writes=[pv], signal=(k == 7 and i == 1))
        P.op("act", lambda e: e.copy(out=vcaug.ap[:, :, :, 0:64], in_=pvv[:, 0:2, :].rearrange("p i (g d) -> p i g d", g=2)), reads=[pv], writes=[vcaug])
        ctxkb = [(kcT.ap[:, i * 128:(i + 1) * 128], kcT, vcaug.ap[:, i, :, :], vcaug, None) for i in range(2)]
        if not last:
            zcf = A.alloc("zcf", [2, 512], BF16)
            for i in range(2):
                pz = B.ps.next()
                for k in range(8):
                    P.op("pe", lambda e, k=k, i=i, pz=pz: e.matmul(out=pz.ap, lhsT=hcT.ap[:, k, i * 128:(i + 1) * 128], rhs=wA.ap[:, k, PA_F:PA_F + 512],
                                                               start=(k == 0), stop=(k == 7)), reads=[hcT, wA], writes=[pz], signal=(k == 7))
                P.op("act", lambda e, i=i, pz=pz: e.copy(out=zcf.ap[:, i, :], in_=pz.ap), reads=[pz], writes=[zcf])
            t256 = B.const_tile("t256", [2, 2, 256], BF16, B.din["t256"])
            Gc = A.alloc("Gc", [2, 256], BF16)
            FTc = A.alloc("FTc", [4, 256], BF16)
            scl = 1.0 / math.sqrt(256.0 * 128.0)
            for g in range(4):
                pg_ = B.ps.next()
                for i in range(2):
                    P.op("pe", lambda e, g=g, i=i, pg_=pg_: e.matmul(out=pg_.ap, lhsT=zcf.ap[:, i, g * 128:(g + 1) * 128],
                                                                 rhs=t256.ap[:, i, :, :].rearrange("p r k -> p (r k)"), start=(i == 0), stop=(i == 1)),
                         reads=[zcf, t256], writes=[pg_], signal=(i == 1))
                P.op("act", lambda e, pg_=pg_: e.copy(out=Gc.ap.rearrange("p r k -> p (r k)"), in_=pg_.ap), reads=[pg_], writes=[Gc])
                pf = B.ps.next()
                P.op("pe", lambda e, pf=pf: e.matmul(out=pf.ap[:, 0:256], lhsT=B.cbsb.ap[:, 0, :], rhs=Gc.ap[:, 0, :], start=True, stop=False),
                     reads=[B.cbsb, Gc], writes=[pf], signal=False)
                P.op("pe", lambda e, pf=pf: e.matmul(out=pf.ap[:, 0:256], lhsT=B.cbsb.ap[:, 1, :], rhs=Gc.ap[:, 1, :], start=False, stop=True),
                     reads=[B.cbsb, Gc], writes=[pf])
                P.op("act", lambda e, g=g, pf=pf: e.activation(out=FTc.ap[:, g, :], in_=pf.ap[:, 0:256], func=AF.Copy, scale=scl), reads=[pf], writes=[FTc])
            ucT = A.alloc("ucT", [4, 258], BF16)
            P.op("dve", lambda e: e.memset(ucT.ap, 0.0), writes=[ucT])
            cxs = A.alloc("cxs_c", [512], F32)
            for m in range(4):
                px = B.proj_fm(wA, PA_CX + m * 128, hcT, CTX)
                P.op("act", lambda e, px=px: e.copy(out=cxs.ap[:, :CTX], in_=px.ap[:, :CTX]), reads=[px], writes=[cxs])
                pc = B.proj_fm(wA, PA_CC + m * 128, hcT, CTX)
                P.op("dve", lambda e, m=m, pc=pc: e.tensor_tensor(out=ucT.ap[:, m, 1:257], in0=cxs.ap[:, :CTX], in1=pc.ap[:, :CTX], op=ALU.mult),
                     reads=[cxs, pc], writes=[ucT])
            wr = wr_c
            wq = wq_c
            qcT = A.alloc("qcT", [4, 256], BF16)
            for m in range(4):
                pq = B.proj_fm(wq, m * 128, hcT, CTX)
                P.op("act", lambda e, m=m, pq=pq: e.copy(out=qcT.ap[:, m, :], in_=pq.ap[:, :CTX]), reads=[pq], writes=[qcT])
            attnTc = A.alloc("attnTc", [4, 256], BF16)
            abufs = (A.ring("pTc", 1, [2, 2, 4, 128], BF16), A.ring("atokc", 1, [8, 64], BF16), A.ring("denc", 1, [2, 8], F32))
            for t in range(2):
                B.attention_tile(qcT, t * 128, ctxkb, attnTc, t * 128, abufs)
            work = (A.alloc("yconvTc", [4, 256], BF16), A.ring("gsbc", 2, [512], BF16), A.ring("tmpc", 3, [512], F32), A.alloc("mergedTc", [8, 256], BF16),
                    A.ring("mixtc", 1, [D], F32), A.ring("smc", 2, [4], F32), A.ring("xresc", 1, [D], F32), A.ring("sqc", 1, [512], BF16))
            B.mix_block(hcT, CTX, qcT, attnTc, ucT.ap, ucT, FTc.ap, FTc, wr, work, hsrc,
                        [hmid[i * 128:(i + 1) * 128, :] for i in range(2)], 1, hmid_b)
            P.barrier()
            A.release(m0)
            rings = B.norm_rings(2)
            h2c = A.alloc("h2c", [8, 256], BF16)
            B.make_hT([(hmid[i * 128:(i + 1) * 128, :], hmid_b) for i in range(2)], h2c, 1, 1, rings)
            wr = A.ring("wrf", 8, [8, 512], BF16)
            work = (A.alloc("actTc", [22, 256], BF16), A.ring("sgc", 2, [512], BF16),
                    (A.ring("mixtf", 1, [D], F32), A.ring("smf", 2, [4], F32), A.ring("xresf", 1, [D], F32), A.ring("sqf", 1, [512], BF16)))
            B.ffn_dense_block(h2c, CTX, wr, work, [(hmid[i * 128:(i + 1) * 128, :], hmid_b) for i in range(2)],
                              [hc1[i * 128:(i + 1) * 128, :] for i in range(2)], 1, hc1_b, False)
        P.barrier()
        A.release(m0)

        B.zfg_b = zfg_b
        B.emit_fft(zsrc)
        if stop == "l0fft":
            P.dma("sp", xo[0:128, :], xh[128:256, :], reads=[B.ft_b], writes=[xo_b], is_output=True)
            P.finish()
            return B

        m0 = A.mark()
        rings = B.norm_rings(2)
        hTr = A.ring("hTm", 2, [8, 512], BF16)
        rope_r = A.ring("ropeM", 1, [2, 512], F32)
        qraw = A.alloc("qraw", [512], BF16)
        tmpm_ring = A.ring("tmpm", 3, [512], F32)
        rtmp = (tmpm_ring.tiles[0], tmpm_ring.tiles[1])
        qT = A.alloc("qT", [4, 512], BF16)
        attnT = A.alloc("attnT", [4, 512], BF16)
        abufs = (A.ring("pT", 2, [5, 2, 4, 128], BF16), A.ring("atok", 2, [8, 64], BF16), A.ring("den", 2, [2, 8], F32))
        ubr = A.ring("ub", 1, [4, 514], BF16)
        ftr = A.ring("ftb", 1, [4, 512], BF16)
        wr = A.ring("wrm", 3, [8, 512], BF16)
        work = (A.alloc("yconvT", [4, 512], BF16), A.ring("gsb", 2, [512], BF16), tmpm_ring, A.alloc("mergedT", [8, 512], BF16),
                A.ring("mixt", 1, [D], F32), A.ring("smm", 2, [4], F32), A.ring("xres", 1, [D], F32), A.ring("sqm", 1, [512], BF16))

        def _mkm(bi_, defer):
            h_ = hTr.next()
            own_ = [xtile(4 * bi_ + i) for i in range(4)]
            B.make_hT(own_, h_, 0, 0, rings, defer=defer)
            return h_, own_
        nxtm = _mkm(0, False)
        for bi in range(8):
            c0 = (4 * bi + 1) * 128
            B.flush()
            hT, own = nxtm
            if bi + 1 < 8:
                nxtm = _mkm(bi + 1, True)
            rp = rope_r.next()
            P.dma("sp", rp.ap[:, 0, :], B.din["ropeC"][:, c0:c0 + 512], writes=[rp])
            P.dma("sp", rp.ap[:, 1, :], B.din["ropeS"][:, c0:c0 + 512], writes=[rp])
            B.ropeb = rp
            wq = wr.next()
            B.load(wq, winP[:, PM_Q:PM_Q + 512].rearrange("(k p) n -> p k n", p=128), "pool")
            for m in range(4):
                pq = B.proj_fm(wq, m * 128, hT, 512)
                B.rope(pq, qraw, qT.ap[:, m, :], qT, rp.ap[:, 0, :], rp.ap[:, 1, :], 512, rtmp)
            for t in range(4):
                T = 4 * bi + t
                kbs = []
                for d_, mi in ((0, 2 if T == 0 else 0), (1, None), (2, 3 if T == 31 else 1)):
                    sl = T + d_
                    kbs.append((B.kT.ap[:, sl * 128:(sl + 1) * 128], B.kT, B.vaug.ap[:, sl, :, :], B.vaug, mi))
                kbs += ctxkb
                B.attention_tile(qT, t * 128, kbs, attnT, t * 128, abufs)
                if t in (0, 2):
                    B.tick()
            ub = ubr.next()
            P.dma("sp", ub.ap, B.u_d[:, :, c0 - 1:c0 + 513], reads=[B.u_b], writes=[ub])
            ftb = ftr.next()
            P.dma("sp", ftb.ap, B.ft_d[:, :, bi * 512:(bi + 1) * 512], reads=[B.ft_b], writes=[ftb])
            B.mix_block(hT, 512, qT, attnT, ub.ap, ub, ftb.ap, ftb, wr, work, own,
                        [xmid[(4 * bi + i) * 128:(4 * bi + i + 1) * 128, :] for i in range(4)], 0, xmid_b)
        P.barrier()
        A.release(m_mix)

        m0 = A.mark()
        if not last:
            rings = B.norm_rings(4)
            h2r = A.ring("h2T", 2, [8, 512], BF16)
            wr = A.ring("wrf2", 8, [8, 512], BF16)
            work = (A.alloc("actT", [22, 512], BF16), A.ring("sg", 2, [512], BF16),
                    (A.ring("mixtF", 1, [D], F32), A.ring("smF", 2, [4], F32), A.ring("xresF", 2, [D], F32), A.ring("sqF", 1, [512], BF16)))
            def _mk2(bi_, defer):
                h_ = h2r.next()
                src_ = [(xmid[(4 * bi_ + i) * 128:(4 * bi_ + i + 1) * 128, :], xmid_b) for i in range(4)]
                B.make_hT(src_, h_, 0, 1, rings, defer=defer)
                return h_, src_
            nxt = _mk2(0, False)
            for bi in range(8):
                B.flush()
                h2T, src = nxt
                if bi + 1 < 8:
                    nxt = _mk2(bi + 1, True)
                B.ffn_dense_block(h2T, 512, wr, work, src,
                                  [x1[(4 * bi + i) * 128:(4 * bi + i + 1) * 128, :] for i in range(4)], 0, x1_b, False)
            P.dma("sp", xb_src[0:128, :], x1[0:128, :], reads=[x1_b], writes=[xb_b])
            P.dma("sp", xb_src[128:256, :], x1[TOK - 128:TOK, :], reads=[x1_b], writes=[xb_b])
            if sim_cc:
                for r in range(4):
                    P.dma("sp", xb_all[r * 256:(r + 1) * 256, :], xb_src, reads=[xb_b], writes=[xball_b])
            else:
                P.cc_allgather(xb_all, xb_src, GROUPS, reads=[xb_b], writes=[xball_b])
            P.barrier()
            A.release(m0)
            m0 = A.mark()
            sel = B.const_tile("selh", [8], F32, selh)
            hal = A.alloc("hal", [2, D], F32)
            gat = A.ring("gat", 2, [D], F32)
            for side in range(2):
                for r in range(4):
                    gt = gat.next()
                    row0 = r * 256 + (128 if side == 0 else 0)
                    P.dma("sp", gt.ap, xb_all[row0:row0 + 128, :], reads=[xball_b], writes=[gt])
                    sc_ap = sel.ap[:, side * 4 + r:side * 4 + r + 1]
                    if r == 0:
                        P.op("dve", lambda e, gt=gt, side=side, sc_ap=sc_ap: e.tensor_scalar(out=hal.ap[:, side, :], in0=gt.ap, scalar1=sc_ap, scalar2=None, op0=ALU.mult),
                             reads=[gt, sel], writes=[hal])
                    else:
                        P.op("dve", lambda e, gt=gt, side=side, sc_ap=sc_ap: e.scalar_tensor_tensor(out=hal.ap[:, side, :], in0=gt.ap, scalar=sc_ap, in1=hal.ap[:, side, :],
                                                                                              op0=ALU.mult, op1=ALU.add), reads=[gt, sel, hal], writes=[hal])
                P.dma("sp", xhalo[side * 128:(side + 1) * 128, :], hal.ap[:, side, :], reads=[hal], writes=[xhalo_b])
        else:
            rings = B.norm_rings(4)
            h2r = A.ring("h2T", 2, [8, 1024], BF16)
            wr = A.ring("wre", 6, [8, 512], BF16)
            wrt = B.const_tile("wrt", [8, NEXP], BF16, B.dl("wrt"))
            brt = B.const_tile("brt", [NEXP], F32, B.dl("brt"))
            work = (A.alloc("acc", [8, D], F32), A.ring("actc", 2, [4, 1024], BF16), A.ring("sge", 2, [512], BF16), A.alloc("gates", [8, NEXP], F32),
                    A.ring("lg", 2, [5, 8], F32),
                    (A.ring("mixtE", 1, [D], F32), A.ring("smE", 2, [4], F32), A.ring("xresE", 2, [D], F32), A.ring("sqE", 1, [512], BF16)), wrt, brt)
            def _mk3(bi_, defer):
                h_ = h2r.next()
                src_ = [(xmid[(8 * bi_ + i) * 128:(8 * bi_ + i + 1) * 128, :], xmid_b) for i in range(8)]
                B.make_hT(src_, h_, 0, 1, rings, defer=defer)
                return h_, src_
            nxt = _mk3(0, False)
            for bi in range(4):
                B.flush()
                h2T, src = nxt
                if bi + 1 < 4:
                    nxt = _mk3(bi + 1, True)
                B.ffn_moe_block(h2T, wr, work, src, [xo[(8 * bi + i) * 128:(8 * bi + i + 1) * 128, :] for i in range(8)], xo_b)
        P.barrier()
        A.release(m_top)
        if stop == "l0" and l == 0:
            P.dma("sp", xo[0:256, :], xhalo, reads=[xhalo_b], writes=[xo_b], is_output=True)
            P.dma("sp", xo[256:TOK, :], x1[256:TOK, :], reads=[x1_b], writes=[xo_b], is_output=True)
            P.finish()
            return B
    P.finish()
    return B


_FUSED = {}


def kernel(**inputs):
    tabs = _static_tables()
    x = np.asarray(inputs["x"], np.float32)
    com = {}
    for l in range(2):
        for k, v in _layer_common(inputs, l).items():
            com[k + str(l)] = v
    maps = []
    for cid in range(8):
        b, j = cid // 4, cid % 4
        m = dict(com)
        for nm in ("ident", "rt", "t1", "cbsb", "t256"):
            m[nm] = tabs[nm]
        for nm in ("masks", "valid", "ropeC", "ropeS", "etab"):
            m[nm] = tabs[(nm, j)]
        m["ccols"] = _ccols(inputs, b)
        m["xh"] = _xh(x, cid)
        m["hctx"] = np.ascontiguousarray(np.asarray(inputs["ctx"][b], np.float32))
        sel = np.zeros((128, 8), np.float32)
        if j - 1 >= 0:
            sel[:, j - 1] = 1.0
        if j + 1 <= 3:
            sel[:, 4 + j + 1] = 1.0
        m["selh"] = sel
        maps.append(m)
    if "B" not in _FUSED:
        _FUSED["B"] = build_fused()
    r = _run(_FUSED["B"], maps)
    out = np.empty_like(x)
    for cid in range(8):
        b, j = cid // 4, cid % 4
        out[b, TOK * j:TOK * (j + 1)] = np.asarray(r[cid]["xo"], np.float32)
    return out
```

```python
import contextlib
import math
import numpy as np
import ml_dtypes
import concourse.bass as bass
import concourse.mybir as mybir
from concourse.bass_utils import run_bass_kernel_spmd

F32 = mybir.dt.float32
BF16 = mybir.dt.bfloat16
AF = mybir.ActivationFunctionType
ALU = mybir.AluOpType
AX = mybir.AxisListType

ENGS = ("pe", "act", "dve", "pool", "sp")
SEM_EPOCH = 30000
SAME_ENGINE_SYNC = True
DEBUG_SCRATCH = False

D = 1024
SEQ = 16384
NB = 2
TOK = 4096
NTILE = 32
CTX = 256
Q_OFF, K_OFF, V_OFF, F_OFF, CX_OFF, CB_OFF, CC_OFF, GATE_OFF = 0, 512, 640, 768, 1280, 1792, 2304, 2816
IN_DIM = 5888
D_FF = 2816
NEXP = 8
D_EXP = 3584
EPS = 1e-6
PA_F, PA_K, PA_V, PA_CX, PA_CC = 0, 512, 640, 768, 1280
PA_W = 1792
PM_Q, PM_CB, PM_G = 1792, 2304, 2816


class Buf:
    __slots__ = ("name", "last_write", "reads", "dkey")

    def __init__(self, name):
        self.name = name
        self.last_write = None
        self.reads = {}
        self.dkey = None


class Tile:
    __slots__ = ("ap", "b")

    def __init__(self, ap, b):
        self.ap = ap
        self.b = b


class _Rec:
    def __init__(self):
        self.calls = []

    def __getattr__(self, name):
        def f(*a, **kw):
            self.calls.append((name, a, kw))
            return None
        return f


class Prog:
    def __init__(self, nc):
        self.nc = nc
        self.stack = contextlib.ExitStack()
        self.q = {e: [] for e in ENGS}
        self.cnt = {e: 0 for e in ENGS}
        self.epoch = {e: 0 for e in ENGS}
        self.pending = {e: False for e in ENGS}
        self.last_tok = {e: None for e in ENGS}
        self.dcnt = {}
        self.waited = {e: {} for e in ENGS}
        self.semkeys = []
        self.sems = {}
        self.nbuf = 0
        self.out_tokens = []
        self.free_dkeys = {"sp": [], "pool": [], "act": []}
        self.dma_bufs = []

    def sbuf(self, name, shape, dtype):
        return self.stack.enter_context(self.nc.sbuf_tensor(name, list(shape), dtype))

    def psum(self, name, shape, dtype=F32):
        return self.stack.enter_context(self.nc.psum_tensor(name, list(shape), dtype))

    def buf(self, name=None):
        self.nbuf += 1
        return Buf(name or f"b{self.nbuf}")

    def _semkey(self, k):
        if k not in self.sems:
            self.sems[k] = None
            self.semkeys.append(k)
        return k

    @staticmethod
    def _deps(reads, writes):
        deps = []
        for b in reads:
            if b.last_write is not None:
                deps.append(b.last_write)
        for b in writes:
            if b.last_write is not None:
                deps.append(b.last_write)
            deps.extend(b.reads.items())
        return deps

    def _emit_waits(self, eng, deps):
        need = {}
        for (k, v) in deps:
            if k[0] == eng and (eng == "pe" or not SAME_ENGINE_SYNC):
                continue
            if v > need.get(k, 0):
                need[k] = v
        w = self.waited[eng]
        for k, v in need.items():
            if w.get(k, 0) >= v:
                continue
            w[k] = v
            self._semkey(k)
            self.q[eng].append(("wait", k, v))

    @staticmethod
    def _mark(tok, reads, writes):
        k, v = tok
        for b in reads:
            if b.reads.get(k, 0) < v:
                b.reads[k] = v
        for b in writes:
            b.last_write = tok
            b.reads = {}

    def op(self, eng, fn, reads=(), writes=(), signal=True):
        reads = [t.b if isinstance(t, Tile) else t for t in reads]
        writes = [t.b if isinstance(t, Tile) else t for t in writes]
        self._emit_waits(eng, self._deps(reads, writes))
        if self.cnt[eng] >= SEM_EPOCH and signal and not self.pending[eng]:
            self.epoch[eng] += 1
            self.cnt[eng] = 0
        self.pending[eng] = not signal
        key = (eng, self.epoch[eng])
        self._semkey(key)
        if signal:
            self.cnt[eng] += 1
            tok = (key, self.cnt[eng])
        else:
            tok = (key, self.cnt[eng] + 1)
        rec = _Rec()
        fn(rec)
        assert len(rec.calls) == 1
        self.q[eng].append(("op", rec.calls[0], key if signal else None))
        self._mark(tok, reads, writes)
        self.last_tok[eng] = tok
        return tok

    def dma(self, qeng, out, in_, reads=(), writes=(), is_output=False):
        reads = [t.b if isinstance(t, Tile) else t for t in reads]
        writes = [t.b if isinstance(t, Tile) else t for t in writes]
        self._emit_waits(qeng, self._deps(reads, writes))
        sb = writes[0] if writes else reads[0]
        if sb.dkey is None or sb.dkey[2] != qeng or self.dcnt.get(sb.dkey, 0) >= SEM_EPOCH:
            fl = self.free_dkeys[qeng]
            while fl and self.dcnt.get(fl[-1], 0) >= SEM_EPOCH:
                fl.pop()
            if fl:
                sb.dkey = fl.pop()
            else:
                self.nbuf += 1
                sb.dkey = ("dma", self.nbuf, qeng)
            self.dma_bufs.append(sb)
        k = sb.dkey
        self._semkey(k)
        self.dcnt[k] = self.dcnt.get(k, 0) + 16
        tok = (k, self.dcnt[k])
        self.q[qeng].append(("dma", (out, in_), k))
        self._mark(tok, reads, writes)
        if is_output:
            self.out_tokens.append(tok)
        return tok

    def cc_allgather(self, out, in_, groups, reads=(), writes=()):
        reads = [t.b if isinstance(t, Tile) else t for t in reads]
        writes = [t.b if isinstance(t, Tile) else t for t in writes]
        self._emit_waits("pool", self._deps(reads, writes))
        self.nbuf += 1
        k = ("cc", self.nbuf)
        self._semkey(k)
        self.dcnt[k] = 1
        tok = (k, 1)
        self.q["pool"].append(("cc", (out, in_, groups), k))
        self._mark(tok, reads, writes)
        return tok

    def barrier(self):
        toks = [t for t in self.last_tok.values() if t is not None]
        toks += [(k, v) for k, v in self.dcnt.items()]
        for e in ENGS:
            self._emit_waits(e, [t for t in toks if not (t[0][0] == e and e in ("pe", "sp", "pool"))])
        seen = set()
        for k in list(self.dcnt.keys()):
            if k[0] == "dma" and k not in seen and all(k not in fl for fl in self.free_dkeys.values()):
                seen.add(k)
                self.free_dkeys[k[2]].append(k)
        for b in self.dma_bufs:
            b.dkey = None
        self.dma_bufs = []

    def finish(self):
        self._emit_waits("sp", self.out_tokens)
        nc = self.nc
        for i, k in enumerate(self.semkeys):
            self.sems[k] = self.stack.enter_context(nc.semaphore(f"s{i}_{k[0]}"))
        sems = self.sems
        q = self.q

        def replay(eng_name):
            def body(e):
                for item in q[eng_name]:
                    if item[0] == "wait":
                        e.wait_ge(sems[item[1]], item[2])
                    elif item[0] == "op":
                        nm, a_, kw_ = item[1]
                        ins = getattr(e, nm)(*a_, **kw_)
                        if item[2] is not None:
                            ins.then_inc(sems[item[2]], 1)
                    elif item[0] == "cc":
                        o, i_, grp = item[1]
                        e.collective_compute("AllGather", ALU.bypass, replica_groups=grp, ins=[i_.opt()], outs=[o.opt()]).then_inc(sems[item[2]], 1)
                    else:
                        o, i_ = item[1]
                        e.dma_start(out=o, in_=i_).then_inc(sems[item[2]], 16)
            return body

        with nc.Block() as block:
            block.tensor(replay("pe"))
            block.scalar(replay("act"))
            block.vector(replay("dve"))
            block.gpsimd(replay("pool"))
            block.sync(replay("sp"))
        self.stack.close()

    def stats(self):
        return {e: len(self.q[e]) for e in ENGS}, len(self.semkeys)


def _prod(s):
    r = 1
    for x in s:
        r *= x
    return r


class Arena:
    def __init__(self, P, nwords):
        self.P = P
        self.t = P.sbuf("arena", [128, nwords], F32)
        self.nwords = nwords
        self.top = 0
        self.peak = 0

    def alloc(self, name, shape, dtype):
        n = _prod(shape)
        nbytes = n * (4 if dtype == F32 else 2)
        words = ((nbytes + 31) // 32) * 8
        off = self.top
        self.top += words
        self.peak = max(self.peak, self.top)
        assert self.top <= self.nwords, f"arena overflow at {name}: {self.top} > {self.nwords}"
        v = self.t[:, off:off + words]
        if dtype != F32:
            v = v.bitcast(dtype)
        v = v[:, :n]
        if len(shape) == 2:
            v = v.rearrange("p (a b) -> p a b", a=shape[0])
        elif len(shape) == 3:
            v = v.rearrange("p (a b c) -> p a b c", a=shape[0], b=shape[1])
        elif len(shape) == 4:
            v = v.rearrange("p (a b c d) -> p a b c d", a=shape[0], b=shape[1], c=shape[2])
        return Tile(v, self.P.buf(name))

    def ring(self, name, n, shape, dtype):
        return Ring([self.alloc(f"{name}{i}", shape, dtype) for i in range(n)])

    def mark(self):
        return self.top

    def release(self, m):
        self.top = m


class Ring:
    def __init__(self, tiles):
        self.tiles = tiles
        self.i = 0

    def next(self):
        t = self.tiles[self.i % len(self.tiles)]
        self.i += 1
        return t


class Builder:
    def __init__(self, kind, layer):
        self.kind = kind
        self.layer = layer
        self.last = layer == 1
        self.sfx = ""
        self.nc = bass.Bass("TRN2", target_bir_lowering=False)
        self.P = Prog(self.nc)
        self.din = {}
        self.A = Arena(self.P, 52000)
        banks = [self.P.psum(f"ps{i}", [128, 512], F32) for i in range(8)]
        self.ps = Ring([Tile(b[:], self.P.buf(f"ps{i}")) for i, b in enumerate(banks)])
        self.pending = []

    def inp(self, name, shape, dtype=F32):
        if name in self.din:
            return self.din[name]
        t = self.nc.dram_tensor(name, list(shape), dtype, kind="ExternalInput").ap()
        self.din[name] = t
        return t

    def outp(self, name, shape, dtype=F32):
        return self.nc.dram_tensor(name, list(shape), dtype, kind="ExternalOutput").ap()

    def scratch(self, name, shape, dtype):
        kind = "ExternalOutput" if DEBUG_SCRATCH else "Internal"
        return self.nc.dram_tensor(name, list(shape), dtype, kind=kind).ap()

    def dl(self, name):
        return self.din[name + self.sfx]

    def load(self, tile, src, q="sp", dep=None):
        self.P.dma(q, tile.ap, src, reads=([dep] if dep is not None else []), writes=[tile])

    def const_tile(self, name, shape, dtype, src, q=None):
        t = self.A.alloc(name, shape, dtype)
        self.load(t, src, q or ("pool" if dtype != F32 else "sp"))
        return t

    def ps_bf16(self, bank, shape):
        v = bank.ap.bitcast(BF16)
        if len(shape) == 2:
            return v.rearrange("p (a b) -> p a b", a=shape[0])
        return v

    def emit_mod(self):
        P, A, nc = self.P, self.A, self.nc
        ccols = self.din["ccols"] if "ccols" in self.din else self.inp("ccols", [128, 8, 2])
        wmod = self.inp("wmod" + self.sfx, [D, 6 * D])
        bcols = self.inp("bcols" + self.sfx, [128, 48])
        brow = self.inp("brow" + self.sfx, [128, 2, D])
        gpm = self.inp("gpm_c" + self.sfx, [128, 8])
        gpf = self.inp("gpf_c" + self.sfx, [128, 8])
        gqm = self.inp("gqm_r" + self.sfx, [128, D])
        gqf = self.inp("gqf_r" + self.sfx, [128, D])
        self.alloc_mod_tiles()
        m0 = A.mark()
        cc = A.alloc("cc", [8, 2], F32)
        self.load(cc, ccols)
        bc = A.alloc("bc", [48], F32)
        self.load(bc, bcols)
        br = A.alloc("br", [2, D], F32)
        self.load(br, brow)
        gq = [A.alloc("gqm", [D], F32), A.alloc("gqf", [D], F32)]
        self.load(gq[0], gqm)
        self.load(gq[1], gqf)
        gp = [A.alloc("gpm", [8], F32), A.alloc("gpf", [8], F32)]
        self.load(gp[0], gpm)
        self.load(gp[1], gpf)
        sc = A.alloc("silu_c", [8, 2], F32)
        P.op("act", lambda e: e.activation(out=sc.ap, in_=cc.ap, func=AF.Silu), reads=[cc], writes=[sc])
        srep = A.alloc("srep", [8, 2, 128], F32)
        P.op("dve", lambda e: e.tensor_copy(out=srep.ap, in_=sc.ap.unsqueeze(3).broadcast_to([128, 8, 2, 128])),
             reads=[sc], writes=[srep])
        wring = A.ring("wm", 2, [8, 512], F32)
        modr = A.alloc("modr", [48, 2], F32)
        P.op("dve", lambda e: e.memset(modr.ap, 0.0), writes=[modr])
        for part in range(6):
            for hf in range(2):
                wm = wring.next()
                c0 = part * 1024 + hf * 512
                self.load(wm, wmod[:, c0:c0 + 512].rearrange("(k p) n -> p k n", p=128))
                if part in (2, 5):
                    gi = 0 if part == 2 else 1
                    for s in range(2):
                        pr = self.ps.next()
                        for k in range(8):
                            P.op("pe", lambda e, k=k, s=s, pr=pr, wm=wm: e.matmul(
                                out=pr.ap, lhsT=srep.ap[:, k, s, :], rhs=wm.ap[:, k, :], start=(k == 0), stop=(k == 7)),
                                reads=[srep, wm], writes=[pr], signal=(k == 7))
                        dst = self.gaG[s][gi]
                        cs = slice(hf * 512, hf * 512 + 512)
                        P.op("dve", lambda e, pr=pr, dst=dst, cs=cs, gi=gi: e.tensor_tensor(
                            out=dst.ap[:, cs], in0=pr.ap, in1=br.ap[:, gi, cs], op=ALU.add), reads=[pr, br], writes=[dst])
                        P.op("dve", lambda e, dst=dst, cs=cs, gi=gi: e.tensor_tensor(
                            out=dst.ap[:, cs], in0=dst.ap[:, cs], in1=gq[gi].ap[:, cs], op=ALU.mult),
                            reads=[dst, gq[gi]], writes=[dst])
                else:
                    for s in range(2):
                        pcb = self.ps.next()
                        pcbv = pcb.ap.rearrange("p (m n) -> p m n", m=4)
                        for m4 in range(4):
                            for k in range(8):
                                P.op("pe", lambda e, k=k, s=s, m4=m4, wm=wm, pcbv=pcbv: e.matmul(
                                    out=pcbv[:, m4, :], lhsT=wm.ap[:, k, m4 * 128:(m4 + 1) * 128], rhs=srep.ap[:, k, s, :],
                                    start=(k == 0), stop=(k == 7)), reads=[wm, srep], writes=[pcb],
                                    signal=(k == 7 and m4 == 3))
                        m_0 = part * 8 + hf * 4
                        P.op("dve", lambda e, s=s, m_0=m_0, pcbv=pcbv: e.tensor_copy(out=modr.ap[:, m_0:m_0 + 4, s], in_=pcbv[:, :, 0]),
                             reads=[pcb], writes=[modr])
        modc = A.alloc("modc", [48, 2], F32)
        self.dbg_modc = modc
        P.op("dve", lambda e: e.tensor_tensor(out=modc.ap, in0=modr.ap, in1=bc.ap.unsqueeze(2).broadcast_to([128, 48, 2]),
                                              op=ALU.add), reads=[modr, bc], writes=[modc])
        for s in range(2):
            for sub in range(2):
                shp, scp = (0, 1) if sub == 0 else (3, 4)
                P.op("dve", lambda e, s=s, sub=sub, shp=shp: e.tensor_copy(
                    out=self.shA.ap[:, s, sub, :], in_=modc.ap[:, shp * 8:shp * 8 + 8, s]), reads=[modc], writes=[self.shA])
                P.op("dve", lambda e, s=s, sub=sub, scp=scp: e.scalar_tensor_tensor(
                    out=self.scA.ap[:, s, sub, :], in0=modc.ap[:, scp * 8:scp * 8 + 8, s], scalar=1.0,
                    in1=gp[sub].ap, op0=ALU.add, op1=ALU.mult), reads=[modc, gp[sub]], writes=[self.scA])
        if getattr(self, "dbg_out", None) is not None:
            P.dma("sp", self.dbg_out, modc.ap.rearrange("p a b -> p (a b)"), reads=[modc], writes=[P.buf("dbgo")], is_output=True)
        P.barrier()
        A.release(m0)

    def emit_consts(self):
        A = self.A
        ident = self.inp("ident", [128, 128])
        self.ident = self.const_tile("ident", [128], BF16, ident)
        if self.kind == "main":
            self.rt = self.const_tile("rt", [128], BF16, self.inp("rt", [128, 128]))
            masks = self.inp("masks", [128, 4, 128])
            self.masks = self.const_tile("masks", [4, 128], BF16, masks)
            self.valid = self.const_tile("valid", [2], F32, self.inp("valid", [128, 2]))
            if self.sfx == "":
                self.emit_layer_consts()
            self.cbsb = self.const_tile("cbsb", [2, 128], BF16, self.inp("cbsb", [128, 2, 128]))

    def alloc_mod_tiles(self):
        A = self.A
        if not hasattr(self, "scA"):
            self.scA = A.alloc("scA", [2, 2, 8], F32)
            self.shA = A.alloc("shA", [2, 2, 8], F32)
            self.gaG = [[A.alloc(f"gaG{s}{g}", [D], F32) for g in range(2)] for s in range(2)]

    def emit_layer_consts(self):
        A = self.A
        es = self.const_tile("esink_src", [8], F32, self.inp("sink_b" + self.sfx, [128, 8]))
        self.esink = A.alloc("esink", [8], F32)
        self.P.op("act", lambda e: e.activation(out=self.esink.ap, in_=es.ap, func=AF.Exp), reads=[es], writes=[self.esink])
        self.wconv = self.const_tile("wconv", [4, 3], F32, self.inp("wconv_c" + self.sfx, [128, 4, 3]))

    def make_hT_dep(self, tiles_src, hT, s, sub, rings, dep):
        return self.make_hT(tiles_src, hT, s, sub, rings, dep)

    def make_hT(self, tiles_src, hT, s, sub, rings, dep=None, defer=False):
        for i, src in enumerate(tiles_src):
            fn = (lambda i=i, src=src: self._hT_tile(i, src, hT, s, sub, rings, dep))
            if defer:
                self.pending.append(fn)
            else:
                fn()

    def tick(self, n=1):
        for _ in range(n):
            if self.pending:
                self.pending.pop(0)()

    def flush(self):
        while self.pending:
            self.pending.pop(0)()

    def _hT_tile(self, i, src, hT, s, sub, rings, dep):
        P = self.P
        xr, xnr, sqr, smr = rings
        xt = xr.next()
        if isinstance(src, tuple):
            self.load(xt, src[0], dep=src[1])
        else:
            self.load(xt, src, dep=dep)
        sq = sqr.next()
        sm = smr.next()
        P.op("act", lambda e: e.activation(out=sq.ap, in_=xt.ap, func=AF.Square, accum_out=sm.ap[:, 0:1]), reads=[xt], writes=[sq, sm])
        P.op("act", lambda e: e.activation(out=sm.ap[:, 1:2], in_=sm.ap[:, 0:1], func=AF.Sqrt, scale=1.0 / D, bias=EPS), reads=[sm], writes=[sm])
        P.op("dve", lambda e: e.reciprocal(out=sm.ap[:, 1:2], in_=sm.ap[:, 1:2]), reads=[sm], writes=[sm])
        xn = xnr.next()
        P.op("dve", lambda e: e.tensor_scalar(out=xn.ap, in0=xt.ap, scalar1=sm.ap[:, 1:2], scalar2=None, op0=ALU.mult), reads=[xt, sm], writes=[xn])
        pt = self.ps.next()
        ptv = self.ps_bf16(pt, [8, 128])
        for k in range(8):
            P.op("pe", lambda e, k=k: e.transpose(out=ptv[:, k, :], in_=xn.ap[:, k * 128:(k + 1) * 128], identity=self.ident.ap),
                 reads=[xn, self.ident], writes=[pt], signal=(k == 7))
        for k in range(4):
            P.op("act", lambda e, k=k: e.activation(
                out=hT.ap[:, k, i * 128:(i + 1) * 128], in_=ptv[:, k, :], func=AF.Identity,
                bias=self.shA.ap[:, s, sub, k:k + 1], scale=self.scA.ap[:, s, sub, k:k + 1]),
                reads=[pt, self.shA, self.scA], writes=[hT])
        for k in range(4, 8):
            P.op("dve", lambda e, k=k: e.scalar_tensor_tensor(
                out=hT.ap[:, k, i * 128:(i + 1) * 128], in0=ptv[:, k, :], scalar=self.scA.ap[:, s, sub, k:k + 1],
                in1=self.shA.ap[:, s, sub, k:k + 1].broadcast_to([128, 128]), op0=ALU.mult, op1=ALU.add),
                reads=[pt, self.shA, self.scA], writes=[hT])

    def norm_rings(self, nx=4):
        A = self.A
        return (A.ring("xt", nx, [D], F32), A.ring("xn", 2, [D], BF16), A.ring("sq", 1, [D], BF16), A.ring("sm", 4, [2], F32))

    def proj_fm(self, w, col0, hT, ntok, nk=8):
        P = self.P
        pr = self.ps.next()
        for k in range(nk):
            P.op("pe", lambda e, k=k, pr=pr: e.matmul(out=pr.ap[:, :ntok], lhsT=w.ap[:, k, col0:col0 + 128], rhs=hT.ap[:, k, :ntok],
                                                   start=(k == 0), stop=(k == nk - 1)), reads=[w, hT], writes=[pr], signal=(k == nk - 1))
        return pr

    def rope(self, pr, raw, dst_ap, dst_tile, cosT, sinT, ntok, tmp):
        P = self.P
        P.op("act", lambda e: e.copy(out=raw.ap[:, :ntok], in_=pr.ap[:, :ntok]), reads=[pr], writes=[raw])
        p2 = self.ps.next()
        P.op("pe", lambda e: e.matmul(out=p2.ap[:, :ntok], lhsT=self.rt.ap, rhs=raw.ap[:, :ntok], start=True, stop=True),
             reads=[self.rt, raw], writes=[p2])
        t1, t2 = tmp
        P.op("dve", lambda e: e.tensor_tensor(out=t1.ap[:, :ntok], in0=pr.ap[:, :ntok], in1=cosT, op=ALU.mult), reads=[pr, self.ropeb, raw], writes=[t1])
        P.op("dve", lambda e: e.tensor_tensor(out=t2.ap[:, :ntok], in0=p2.ap[:, :ntok], in1=sinT, op=ALU.mult), reads=[p2, self.ropeb], writes=[t2])
        P.op("dve", lambda e: e.tensor_tensor(out=dst_ap, in0=t1.ap[:, :ntok], in1=t2.ap[:, :ntok], op=ALU.add), reads=[t1, t2], writes=[dst_tile])

    def emit_phaseA(self, xh, want_zf, zf_out=None):
        P, A = self.P, self.A
        main = self.kind == "main"
        winP = self.dl("winP")
        m0 = A.mark()
        ncolA = PA_W if main else 512
        wA = A.alloc("wA", [8, ncolA], BF16)
        self.load(wA, winP[:, 0:ncolA].rearrange("(k p) n -> p k n", p=128), "pool")
        rings = self.norm_rings(4)
        hTr = A.ring("hT", 2, [8, 512], BF16)
        zfr = A.ring("zfs", 2, [512], BF16)
        if main:
            ropeC = self.din["ropeC"]
            ropeS = self.din["ropeS"]
            rope_r = A.ring("ropeCS", 2, [2, 512], F32)
            kraw = A.alloc("kraw", [512], BF16)
            tmp = (A.alloc("rt1", [512], F32), A.alloc("rt2", [512], F32))
            cxs = A.ring("cxs", 2, [512], F32)
            ust = A.ring("ust", 2, [4, 512], BF16)
        blocks = []
        if main:
            blocks.append((-1, 1))
        for bi in range(8):
            blocks.append((bi * 4, 4))
        if main:
            blocks.append((32, 1))
        def _mk(bidx, defer):
            t0_, nt_ = blocks[bidx]
            hT_ = hTr.next()
            if xh is None:
                srcs_ = [self.xtile(t0_ + i) for i in range(nt_)]
            else:
                srcs_ = [xh[(t0_ + 1 + i) * 128:(t0_ + 2 + i) * 128, :] for i in range(nt_)]
            self.make_hT(srcs_, hT_, 0, 0, rings, defer=defer)
            return hT_
        hT_next = _mk(0, False)
        for bidx, (t0, nt) in enumerate(blocks):
            ntok = nt * 128
            halo = nt == 1
            self.flush()
            hT = hT_next
            if bidx + 1 < len(blocks):
                hT_next = _mk(bidx + 1, True)
            if want_zf and not halo:
                for i in range(nt):
                    pr = self.ps.next()
                    for k in range(8):
                        P.op("pe", lambda e, k=k, i=i, pr=pr, hT=hT: e.matmul(out=pr.ap, lhsT=hT.ap[:, k, i * 128:(i + 1) * 128],
                                                                     rhs=wA.ap[:, k, PA_F:PA_F + 512], start=(k == 0), stop=(k == 7)),
                             reads=[hT, wA], writes=[pr], signal=(k == 7))
                    zs = zfr.next()
                    P.op("act", lambda e, pr=pr, zs=zs: e.copy(out=zs.ap, in_=pr.ap), reads=[pr], writes=[zs])
                    tg = t0 + i
                    if i % 2 == 1:
                        self.tick()
                    if isinstance(zf_out, list):
                        for g_ in range(4):
                            P.dma("pool", zf_out[g_][tg * 128:(tg + 1) * 128, :], zs.ap[:, g_ * 128:(g_ + 1) * 128], reads=[zs], writes=[self.zf_b])
                    else:
                        P.dma("sp", zf_out[:, tg * 128:(tg + 1) * 128, :].rearrange("g t c -> t g c"),
                              zs.ap.rearrange("p (g c) -> p g c", g=4), reads=[zs], writes=[self.zf_b], is_output=(self.kind == "pre"))
            if not main:
                continue
            c0 = (t0 + 1) * 128
            rp = rope_r.next()
            P.dma("sp", rp.ap[:, 0, :ntok], ropeC[:, c0:c0 + ntok], writes=[rp])
            P.dma("sp", rp.ap[:, 1, :ntok], ropeS[:, c0:c0 + ntok], writes=[rp])
            self.ropeb = rp
            pr = self.proj_fm(wA, PA_K, hT, ntok)
            self.rope(pr, kraw, self.kT.ap[:, c0:c0 + ntok], self.kT, rp.ap[:, 0, :ntok], rp.ap[:, 1, :ntok], ntok, tmp)
            pv = self.ps.next()
            pvv = pv.ap.rearrange("p (i c) -> p i c", i=4)
            for i in range(nt):
                for k in range(8):
                    P.op("pe", lambda e, k=k, i=i, hT=hT: e.matmul(out=pvv[:, i, :], lhsT=hT.ap[:, k, i * 128:(i + 1) * 128],
                                                                 rhs=wA.ap[:, k, PA_V:PA_V + 128], start=(k == 0), stop=(k == 7)),
                         reads=[hT, wA], writes=[pv], signal=(k == 7 and i == nt - 1))
            P.op("act", lambda e, t0=t0, nt=nt: e.copy(
                out=self.vaug.ap[:, t0 + 1:t0 + 1 + nt, :, 0:64],
                in_=pvv[:, 0:nt, :].rearrange("p i (g d) -> p i g d", g=2)), reads=[pv], writes=[self.vaug])
            self.tick()
            us = ust.next()
            for m in range(4):
                if m == 2:
                    self.tick()
                px = self.proj_fm(wA, PA_CX + m * 128, hT, ntok)
                cx = cxs.next()
                P.op("act", lambda e, px=px, cx=cx: e.copy(out=cx.ap[:, :ntok], in_=px.ap[:, :ntok]), reads=[px], writes=[cx])
                pc = self.proj_fm(wA, PA_CC + m * 128, hT, ntok)
                if halo:
                    vi = 0 if t0 < 0 else 1
                    P.op("dve", lambda e, m=m, pc=pc, cx=cx, us=us, vi=vi: e.scalar_tensor_tensor(
                        out=us.ap[:, m, :ntok], in0=cx.ap[:, :ntok], scalar=self.valid.ap[:, vi:vi + 1], in1=pc.ap[:, :ntok],
                        op0=ALU.mult, op1=ALU.mult), reads=[cx, pc, self.valid], writes=[us])
                else:
                    P.op("dve", lambda e, m=m, pc=pc, cx=cx, us=us: e.tensor_tensor(
                        out=us.ap[:, m, :ntok], in0=cx.ap[:, :ntok], in1=pc.ap[:, :ntok], op=ALU.mult), reads=[cx, pc], writes=[us])
            P.dma("pool", self.u_d[:, :, c0:c0 + ntok], us.ap[:, :, :ntok], reads=[us], writes=[self.u_b])
        self.flush()
        P.barrier()
        A.release(m0)

    def emit_fft(self, zfg):
        P, A = self.P, self.A
        m0 = A.mark()
        t1 = self.const_tile("t1", [2, 2, 64], BF16, self.inp("t1", [128, 2, 2, 64]))
        etab_d = self.inp("etab", [128, 128, 96])
        et = A.alloc("etab", [128, 96], BF16)
        for q4 in range(4):
            P.dma("pool", et.ap[:, q4 * 32:(q4 + 1) * 32, :], etab_d[:, q4 * 32:(q4 + 1) * 32, :], writes=[et])
        Ur = A.ring("U", 1, [128, 128], BF16)
        Y = A.alloc("Y", [128, 2, 64], BF16)
        G = A.alloc("G", [2, 4096], BF16)
        FTs = A.ring("FTs", 2, [4096], BF16)
        scale = 1.0 / math.sqrt(SEQ * 128.0)
        ev = 0
        for g in range(4):
            U = Ur.next()
            for r in range(4):
                zsrc_ = zfg(r, g) if callable(zfg) else zfg[r, g]
                P.dma("sp", U.ap[r * 32:(r + 1) * 32, :, :], zsrc_.rearrange("(th tl) c -> th tl c", tl=128),
                      reads=([self.zfg_b] if getattr(self, "zfg_b", None) is not None else []), writes=[U])
            for hh in range(2):
                for c4 in range(32):
                    pr = self.ps.next()
                    prv = pr.ap.rearrange("p (c x) -> p c x", c=4)
                    for ci in range(4):
                        c = c4 * 4 + ci
                        P.op("pe", lambda e, c=c, ci=ci, hh=hh, prv=prv: e.matmul(
                            out=prv[:, ci, :], lhsT=U.ap[:, :, c], rhs=t1.ap[:, hh, :, :].rearrange("p r k -> p (r k)"),
                            start=True, stop=True), reads=[U, t1], writes=[pr], signal=(ci == 3))
                    eng = "act" if ev % 2 == 0 else "dve"
                    ev += 1
                    dst = Y.ap[:, c4 * 4:(c4 + 1) * 4, :, :].rearrange("p c r k -> p c (r k)")
                    if eng == "act":
                        P.op("act", lambda e, dst=dst, prv=prv: e.copy(out=dst, in_=prv), reads=[pr], writes=[Y])
                    else:
                        P.op("dve", lambda e, dst=dst, prv=prv: e.tensor_copy(out=dst, in_=prv), reads=[pr], writes=[Y])
                for k8 in range(8):
                    pr = self.ps.next()
                    prv = pr.ap.rearrange("p (k r x) -> p k r x", k=8, r=2)
                    for ki in range(8):
                        k1l = k8 * 8 + ki
                        k1 = hh * 64 + k1l
                        P.op("pe", lambda e, k1=k1, k1l=k1l, ki=ki, prv=prv: e.matmul(
                            out=prv[:, ki, :, :].rearrange("p r x -> p (r x)"), lhsT=Y.ap[:, :, 0, k1l], rhs=et.ap[:, k1, 32:96],
                            start=True, stop=False), reads=[Y, et], writes=[pr], signal=False)
                        P.op("pe", lambda e, k1=k1, k1l=k1l, ki=ki, prv=prv: e.matmul(
                            out=prv[:, ki, :, :].rearrange("p r x -> p (r x)"), lhsT=Y.ap[:, :, 1, k1l], rhs=et.ap[:, k1, 0:64],
                            start=False, stop=True), reads=[Y, et], writes=[pr], signal=(ki == 7))
                    k10 = hh * 64 + k8 * 8
                    for ri in range(2):
                        dst = G.ap[:, ri, :].rearrange("p (k2 k1) -> p k1 k2", k1=128)[:, k10:k10 + 8, :]
                        if ri == 0:
                            P.op("act", lambda e, dst=dst, prv=prv, ri=ri: e.copy(out=dst, in_=prv[:, :, ri, :]), reads=[pr], writes=[G])
                        else:
                            P.op("dve", lambda e, dst=dst, prv=prv, ri=ri: e.tensor_copy(out=dst, in_=prv[:, :, ri, :]), reads=[pr], writes=[G])
            ft = FTs.next()
            for cb in range(8):
                pr = self.ps.next()
                cs = slice(cb * 512, (cb + 1) * 512)
                P.op("pe", lambda e, pr=pr, cs=cs: e.matmul(out=pr.ap, lhsT=self.cbsb.ap[:, 0, :], rhs=G.ap[:, 0, cs], start=True, stop=False),
                     reads=[self.cbsb, G], writes=[pr], signal=False)
                P.op("pe", lambda e, pr=pr, cs=cs: e.matmul(out=pr.ap, lhsT=self.cbsb.ap[:, 1, :], rhs=G.ap[:, 1, cs], start=False, stop=True),
                     reads=[self.cbsb, G], writes=[pr])
                P.op("act", lambda e, pr=pr, cs=cs, ft=ft: e.activation(out=ft.ap[:, cs], in_=pr.ap, func=AF.Copy, scale=scale), reads=[pr], writes=[ft])
            P.dma("sp", self.ft_d[:, g, :], ft.ap, reads=[ft], writes=[self.ft_b])
        P.barrier()
        A.release(m0)

    def attention_tile(self, qT, qcol0, keyblocks, attnT, acol0, bufs):
        st = self.attention_scores(qT, qcol0, keyblocks, bufs)
        self.attention_pv(st, keyblocks, attnT, acol0, bufs)

    def attention_scores(self, qT, qcol0, keyblocks, bufs):
        P = self.P
        pTr, atok_r, den_r = bufs
        pT = pTr.next()
        for kb, (kap, kt, vap, vt, mi) in enumerate(keyblocks):
            for g in range(2):
                pr = self.ps.next()
                P.op("pe", lambda e, g=g, kap=kap, pr=pr: e.matmul(
                    out=pr.ap.rearrange("p (h q) -> p h q", h=4), lhsT=kap[g * 64:(g + 1) * 64, :],
                    rhs=qT.ap[g * 64:(g + 1) * 64, :, qcol0:qcol0 + 128], start=True, stop=True),
                    reads=[kt, qT], writes=[pr])
                P.op("act", lambda e, g=g, kb=kb, pr=pr, pT=pT: e.activation(
                    out=pT.ap[:, kb, g, :, :], in_=pr.ap.rearrange("p (h q) -> p h q", h=4), func=AF.Exp, scale=0.125),
                    reads=[pr], writes=[pT])
            if mi is not None:
                P.op("dve", lambda e, kb=kb, mi=mi, pT=pT: e.tensor_tensor(
                    out=pT.ap[:, kb, :, :, :].rearrange("p g h q -> p (g h) q"),
                    in0=pT.ap[:, kb, :, :, :].rearrange("p g h q -> p (g h) q"),
                    in1=self.masks.ap[:, mi, :].unsqueeze(1).broadcast_to([128, 8, 128]), op=ALU.mult),
                    reads=[pT, self.masks], writes=[pT])
        return pT

    def attention_pv(self, pT, keyblocks, attnT, acol0, bufs):
        P = self.P
        pTr, atok_r, den_r = bufs
        nkb = len(keyblocks)
        atok = atok_r.next()
        den = den_r.next()
        for b2 in range(2):
            po = self.ps.next()
            pov = po.ap[:, 0:260].rearrange("p (h x) -> p h x", h=4)
            for hh in range(4):
                for kb, (kap, kt, vap, vt, mi) in enumerate(keyblocks):
                    P.op("pe", lambda e, hh=hh, kb=kb, vap=vap, pov=pov, b2=b2, pT=pT: e.matmul(
                        out=pov[:, hh, :], lhsT=pT.ap[:, kb, b2, hh, :], rhs=vap[:, b2, :], start=(kb == 0), stop=(kb == nkb - 1)),
                        reads=[pT, vt], writes=[po], signal=(hh == 3 and kb == nkb - 1))
            hs = slice(b2 * 4, b2 * 4 + 4)
            P.op("dve", lambda e, pov=pov, hs=hs, den=den: e.tensor_tensor(out=den.ap[:, 0, hs], in0=pov[:, :, 64], in1=self.esink.ap[:, hs], op=ALU.add),
                 reads=[po, self.esink], writes=[den])
            P.op("dve", lambda e, hs=hs, den=den: e.reciprocal(out=den.ap[:, 1, hs], in_=den.ap[:, 0, hs]), reads=[den], writes=[den])
            P.op("dve", lambda e, pov=pov, hs=hs, den=den, atok=atok: e.tensor_tensor(
                out=atok.ap[:, hs, :], in0=pov[:, :, 0:64], in1=den.ap[:, 1, hs].unsqueeze(2).broadcast_to([128, 4, 64]), op=ALU.mult),
                reads=[po, den], writes=[atok])
        pt = self.ps.next()
        ptv = self.ps_bf16(pt, [8, 128])
        av = atok.ap.rearrange("p h d -> p (h d)")
        for kc in range(4):
            P.op("pe", lambda e, kc=kc, ptv=ptv: e.transpose(out=ptv[:, kc, :], in_=av[:, kc * 128:(kc + 1) * 128], identity=self.ident.ap),
                 reads=[atok, self.ident], writes=[pt], signal=(kc == 3))
        P.op("act", lambda e, ptv=ptv: e.copy(out=attnT.ap[:, 0:4, acol0:acol0 + 128], in_=ptv[:, 0:4, :]), reads=[pt], writes=[attnT])

    def mix_block(self, hT, ntok, qT, attnT, uap, u_tile, FTap, ft_tile, wr, work, x_tiles_src, x_out_dst, s, xout_buf, is_out=False):
        P = self.P
        winP = self.dl("winP")
        (yconvT, gsb_r, tmp_r, mergedT, mixt_r, sm_r, xres_r, sq_r) = work
        wcb = wr.next()
        self.load(wcb, winP[:, PM_CB:PM_CB + 512].rearrange("(k p) n -> p k n", p=128), "pool")
        for m in range(4):
            pb = self.proj_fm(wcb, m * 128, hT, ntok)
            t = tmp_r.next()
            P.op("dve", lambda e, m=m, t=t: e.tensor_scalar(out=t.ap[:, :ntok], in0=uap[:, m, 0:ntok], scalar1=self.wconv.ap[:, m, 0:1], scalar2=None, op0=ALU.mult),
                 reads=[u_tile, self.wconv], writes=[t])
            P.op("dve", lambda e, m=m, t=t: e.scalar_tensor_tensor(out=t.ap[:, :ntok], in0=uap[:, m, 1:ntok + 1], scalar=self.wconv.ap[:, m, 1:2],
                                                                 in1=t.ap[:, :ntok], op0=ALU.mult, op1=ALU.add), reads=[u_tile, self.wconv, t], writes=[t])
            P.op("dve", lambda e, m=m, t=t: e.scalar_tensor_tensor(out=t.ap[:, :ntok], in0=uap[:, m, 2:ntok + 2], scalar=self.wconv.ap[:, m, 2:3],
                                                                 in1=t.ap[:, :ntok], op0=ALU.mult, op1=ALU.add), reads=[u_tile, self.wconv, t], writes=[t])
            P.op("dve", lambda e, m=m, t=t, pb=pb: e.tensor_tensor(out=yconvT.ap[:, m, :ntok], in0=t.ap[:, :ntok], in1=pb.ap[:, :ntok], op=ALU.mult),
                 reads=[t, pb], writes=[yconvT])
        self.tick()
        if getattr(self, "mix_stop", None) == "conv":
            return
        wbr = self.wbr
        srcs = [(attnT.ap, attnT), (FTap, ft_tile), (yconvT.ap, yconvT)]
        for m in range(8):
            if m in (2, 5):
                self.tick()
            wg = wr.next()
            P.dma("pool", wg.ap[:, :, 0:384], winP[:, PM_G + m * 384:PM_G + (m + 1) * 384].rearrange("(k p) n -> p k n", p=128), writes=[wg])
            acc = None
            for r in range(3):
                pg = self.proj_fm(wg, r * 128, hT, ntok)
                gs = gsb_r.next()
                P.op("act", lambda e, pg=pg, gs=gs: e.activation(out=gs.ap[:, :ntok], in_=pg.ap[:, :ntok], func=AF.Sigmoid), reads=[pg], writes=[gs])
                sap, st = srcs[r]
                py = self.ps.next()
                for kc in range(4):
                    P.op("pe", lambda e, kc=kc, r=r, m=m, py=py, sap=sap: e.matmul(
                        out=py.ap[:, :ntok], lhsT=wbr[r].ap[:, kc, m * 128:(m + 1) * 128], rhs=sap[:, kc, 0:ntok], start=(kc == 0), stop=(kc == 3)),
                        reads=[wbr[r], st], writes=[py], signal=(kc == 3))
                if r == 0:
                    acc = tmp_r.next()
                    P.op("dve", lambda e, gs=gs, py=py, acc=acc: e.tensor_tensor(out=acc.ap[:, :ntok], in0=gs.ap[:, :ntok], in1=py.ap[:, :ntok], op=ALU.mult),
                         reads=[gs, py], writes=[acc])
                else:
                    t = tmp_r.next()
                    P.op("dve", lambda e, gs=gs, py=py, t=t: e.tensor_tensor(out=t.ap[:, :ntok], in0=gs.ap[:, :ntok], in1=py.ap[:, :ntok], op=ALU.mult),
                         reads=[gs, py], writes=[t])
                    if r == 1:
                        P.op("dve", lambda e, t=t, acc=acc: e.tensor_tensor(out=acc.ap[:, :ntok], in0=acc.ap[:, :ntok], in1=t.ap[:, :ntok], op=ALU.add),
                             reads=[acc, t], writes=[acc])
                    else:
                        P.op("dve", lambda e, t=t, acc=acc, m=m: e.tensor_tensor(out=mergedT.ap[:, m, :ntok], in0=acc.ap[:, :ntok], in1=t.ap[:, :ntok], op=ALU.add),
                             reads=[acc, t], writes=[mergedT])
        if getattr(self, "mix_stop", None) == "merge":
            return
        wo = self.wo2
        self.post_residual(lambda i, hf: (mergedT, [(mergedT.ap[:, k, i * 128:(i + 1) * 128], wo[hf].ap[:, k, :]) for k in range(8)], [mergedT, wo[hf]]),
                           ntok // 128, x_tiles_src, x_out_dst, self.gaG[s][0], (mixt_r, sm_r, xres_r, sq_r), xout_buf, is_out)

    def post_residual(self, mm_fn, nt, x_tiles_src, x_out_dst, gaG, rings, xout_buf, is_out, from_sbuf=None):
        P = self.P
        mixt_r, sm_r, xres_r, sq_r = rings
        for i in range(nt):
            xres = xres_r.next()
            xs = x_tiles_src[i]
            if isinstance(xs, tuple):
                self.load(xres, xs[0], dep=xs[1])
            else:
                self.load(xres, xs)
            sm = sm_r.next()
            mt = mixt_r.next()
            for hf in range(2):
                cs = slice(hf * 512, (hf + 1) * 512)
                if from_sbuf is None:
                    _, pairs, rd = mm_fn(i, hf)
                    pr = self.ps.next()
                    n = len(pairs)
                    for j, (l, r) in enumerate(pairs):
                        P.op("pe", lambda e, l=l, r=r, j=j, n=n, pr=pr: e.matmul(out=pr.ap, lhsT=l, rhs=r, start=(j == 0), stop=(j == n - 1)),
                             reads=rd, writes=[pr], signal=(j == n - 1))
                    src_ap, src_t = pr.ap, pr
                else:
                    src_ap, src_t = from_sbuf(i)[0][:, cs], from_sbuf(i)[1]
                sq = sq_r.next()
                P.op("act", lambda e, src_ap=src_ap, sq=sq, sm=sm, hf=hf: e.activation(out=sq.ap[:, 0:512], in_=src_ap, func=AF.Square, accum_out=sm.ap[:, hf:hf + 1]),
                     reads=[src_t], writes=[sq, sm])
                P.op("dve", lambda e, src_ap=src_ap, mt=mt, cs=cs: e.tensor_tensor(out=mt.ap[:, cs], in0=src_ap, in1=gaG.ap[:, cs], op=ALU.mult),
                     reads=[src_t, gaG, sq], writes=[mt])
            lvl = getattr(self, "pr_stop", 9)
            if lvl <= 1:
                continue
            P.op("dve", lambda e, sm=sm: e.tensor_tensor(out=sm.ap[:, 2:3], in0=sm.ap[:, 0:1], in1=sm.ap[:, 1:2], op=ALU.add), reads=[sm], writes=[sm])
            P.op("act", lambda e, sm=sm: e.activation(out=sm.ap[:, 3:4], in_=sm.ap[:, 2:3], func=AF.Sqrt, scale=1.0 / D, bias=EPS), reads=[sm], writes=[sm])
            P.op("dve", lambda e, sm=sm: e.reciprocal(out=sm.ap[:, 3:4], in_=sm.ap[:, 3:4]), reads=[sm], writes=[sm])
            if lvl <= 2:
                continue
            P.op("dve", lambda e, sm=sm, mt=mt, xres=xres: e.scalar_tensor_tensor(out=xres.ap, in0=mt.ap, scalar=sm.ap[:, 3:4], in1=xres.ap, op0=ALU.mult, op1=ALU.add),
                 reads=[mt, sm, xres], writes=[xres])
            if lvl <= 3:
                continue
            P.dma("sp", x_out_dst[i], xres.ap, reads=[xres], writes=[xout_buf], is_output=is_out)

    def ffn_dense_block(self, h2T, ntok, wr, work, x_tiles_src, x_out_dst, s, xout_buf, is_out):
        P = self.P
        actT, sg_r, rings = work
        wg_d, wu_d, wd_d = self.dl("wfg"), self.dl("wfu"), self.dl("wfd")
        for j in range(6):
            w = 512 if j < 5 else 256
            wg = wr.next()
            wu = wr.next()
            P.dma("pool", wg.ap[:, :, 0:w], wg_d[:, j * 512:j * 512 + w].rearrange("(k p) n -> p k n", p=128), writes=[wg])
            P.dma("pool", wu.ap[:, :, 0:w], wu_d[:, j * 512:j * 512 + w].rearrange("(k p) n -> p k n", p=128), writes=[wu])
            for mm in range(w // 128):
                pg = self.proj_fm(wg, mm * 128, h2T, ntok)
                pu = self.proj_fm(wu, mm * 128, h2T, ntok)
                sg = sg_r.next()
                P.op("act", lambda e, pg=pg, sg=sg: e.activation(out=sg.ap[:, :ntok], in_=pg.ap[:, :ntok], func=AF.Silu), reads=[pg], writes=[sg])
                kc = j * 4 + mm
                P.op("dve", lambda e, sg=sg, pu=pu, kc=kc: e.tensor_tensor(out=actT.ap[:, kc, :ntok], in0=sg.ap[:, :ntok], in1=pu.ap[:, :ntok], op=ALU.mult),
                     reads=[sg, pu], writes=[actT])
            self.tick()
        nt = ntok // 128
        halves = []
        for hf in range(2):
            slots = []
            for sl in range(3):
                k0 = sl * 8
                nk = min(8, 22 - k0)
                wd = wr.next()
                P.dma("pool", wd.ap[:, 0:nk, :], wd_d[k0 * 128:(k0 + nk) * 128, hf * 512:(hf + 1) * 512].rearrange("(k p) n -> p k n", p=128), writes=[wd])
                slots.append((wd, k0, nk))
            halves.append(slots)

        def mm_fn(i, hf):
            pairs = []
            rd = [actT]
            for (wd, k0, nk) in halves[hf]:
                rd.append(wd)
                for k in range(nk):
                    pairs.append((actT.ap[:, k0 + k, i * 128:(i + 1) * 128], wd.ap[:, k, :]))
            return None, pairs, rd
        self.post_residual(mm_fn, nt, x_tiles_src, x_out_dst, self.gaG[s][1], rings, xout_buf, is_out)

    def ffn_moe_block(self, h2T, wr, work, x_tiles_src, x_out_dst, xout_buf):
        P, A = self.P, self.A
        ntok = 1024
        nt = 8
        acc, act_r, sg_r, gates, lg_r, rings, wrt, brt = work
        weg, weu, wed = self.dl("weg"), self.dl("weu"), self.dl("wed")
        for i in range(nt):
            pl = self.ps.next()
            for k in range(8):
                P.op("pe", lambda e, k=k, i=i, pl=pl: e.matmul(out=pl.ap[:, 0:8], lhsT=h2T.ap[:, k, i * 128:(i + 1) * 128], rhs=wrt.ap[:, k, :],
                                                           start=(k == 0), stop=(k == 7)), reads=[h2T, wrt], writes=[pl], signal=(k == 7))
            lg = lg_r.next()
            L, M1, L2, M2 = (lg.ap[:, j, :] for j in range(4))
            sm = lg.ap[:, 4, :]
            P.op("dve", lambda e, pl=pl, L=L: e.tensor_tensor(out=L, in0=pl.ap[:, 0:8], in1=brt.ap, op=ALU.add), reads=[pl, brt], writes=[lg])
            P.op("dve", lambda e, L=L, sm=sm: e.tensor_reduce(out=sm[:, 0:1], in_=L, axis=AX.X, op=ALU.max), reads=[lg], writes=[lg])
            P.op("dve", lambda e, L=L, M1=M1, sm=sm: e.tensor_scalar(out=M1, in0=L, scalar1=sm[:, 0:1], scalar2=None, op0=ALU.is_ge), reads=[lg], writes=[lg])
            P.op("dve", lambda e, L=L, M1=M1, L2=L2: e.scalar_tensor_tensor(out=L2, in0=M1, scalar=-1e30, in1=L, op0=ALU.mult, op1=ALU.add), reads=[lg], writes=[lg])
            P.op("dve", lambda e, L2=L2, sm=sm: e.tensor_reduce(out=sm[:, 1:2], in_=L2, axis=AX.X, op=ALU.max), reads=[lg], writes=[lg])
            P.op("dve", lambda e, L2=L2, M2=M2, sm=sm: e.tensor_scalar(out=M2, in0=L2, scalar1=sm[:, 1:2], scalar2=None, op0=ALU.is_ge), reads=[lg], writes=[lg])
            P.op("dve", lambda e, sm=sm: e.tensor_tensor(out=sm[:, 2:3], in0=sm[:, 1:2], in1=sm[:, 0:1], op=ALU.subtract), reads=[lg], writes=[lg])
            P.op("act", lambda e, sm=sm: e.activation(out=sm[:, 3:4], in_=sm[:, 2:3], func=AF.Exp), reads=[lg], writes=[lg])
            P.op("dve", lambda e, sm=sm: e.tensor_scalar(out=sm[:, 4:5], in0=sm[:, 3:4], scalar1=1.0, scalar2=None, op0=ALU.add), reads=[lg], writes=[lg])
            P.op("dve", lambda e, sm=sm: e.reciprocal(out=sm[:, 5:6], in_=sm[:, 4:5]), reads=[lg], writes=[lg])
            P.op("dve", lambda e, sm=sm: e.tensor_tensor(out=sm[:, 6:7], in0=sm[:, 3:4], in1=sm[:, 5:6], op=ALU.mult), reads=[lg], writes=[lg])
            P.op("dve", lambda e, M1=M1, sm=sm, i=i: e.tensor_scalar(out=gates.ap[:, i, :], in0=M1, scalar1=sm[:, 5:6], scalar2=None, op0=ALU.mult), reads=[lg], writes=[gates])
            P.op("dve", lambda e, M2=M2, sm=sm, i=i: e.scalar_tensor_tensor(out=gates.ap[:, i, :], in0=M2, scalar=sm[:, 6:7], in1=gates.ap[:, i, :], op0=ALU.mult, op1=ALU.add),
                 reads=[lg, gates], writes=[gates])
        first = True
        for ex in range(NEXP):
            for j in range(7):
                wg = wr.next()
                wu = wr.next()
                wd = wr.next()
                self.load(wg, weg[ex, :, j * 512:(j + 1) * 512].rearrange("(k p) n -> p k n", p=128), "pool")
                self.load(wu, weu[ex, :, j * 512:(j + 1) * 512].rearrange("(k p) n -> p k n", p=128), "pool")
                wdv = wd.ap.rearrange("p a b -> p (a b)").rearrange("p (k n) -> p k n", k=4)
                P.dma("pool", wdv, wed[ex, j * 512:(j + 1) * 512, :].rearrange("(k p) n -> p k n", p=128), writes=[wd])
                at = act_r.next()
                for mm in range(4):
                    for th in range(2):
                        ts = slice(th * 512, (th + 1) * 512)
                        pg = self.ps.next()
                        pu = self.ps.next()
                        for k in range(8):
                            P.op("pe", lambda e, k=k, mm=mm, ts=ts, pg=pg, wg=wg: e.matmul(out=pg.ap, lhsT=wg.ap[:, k, mm * 128:(mm + 1) * 128], rhs=h2T.ap[:, k, ts],
                                                                                  start=(k == 0), stop=(k == 7)), reads=[wg, h2T], writes=[pg], signal=(k == 7))
                        for k in range(8):
                            P.op("pe", lambda e, k=k, mm=mm, ts=ts, pu=pu, wu=wu: e.matmul(out=pu.ap, lhsT=wu.ap[:, k, mm * 128:(mm + 1) * 128], rhs=h2T.ap[:, k, ts],
                                                                                  start=(k == 0), stop=(k == 7)), reads=[wu, h2T], writes=[pu], signal=(k == 7))
                        sg = sg_r.next()
                        P.op("act", lambda e, pg=pg, sg=sg: e.activation(out=sg.ap, in_=pg.ap, func=AF.Silu), reads=[pg], writes=[sg])
                        P.op("dve", lambda e, sg=sg, pu=pu, at=at, mm=mm, ts=ts: e.tensor_tensor(out=at.ap[:, mm, ts], in0=sg.ap, in1=pu.ap, op=ALU.mult),
                             reads=[sg, pu], writes=[at])
                for i in range(nt):
                    for hf in range(2):
                        cs = slice(hf * 512, (hf + 1) * 512)
                        pd = self.ps.next()
                        for mm in range(4):
                            rhs = wdv[:, mm, cs]
                            P.op("pe", lambda e, mm=mm, i=i, pd=pd, at=at, rhs=rhs: e.matmul(out=pd.ap, lhsT=at.ap[:, mm, i * 128:(i + 1) * 128], rhs=rhs,
                                                                                    start=(mm == 0), stop=(mm == 3)), reads=[at, wd], writes=[pd], signal=(mm == 3))
                        if first:
                            P.op("dve", lambda e, pd=pd, i=i, cs=cs, ex=ex: e.tensor_scalar(out=acc.ap[:, i, cs], in0=pd.ap, scalar1=gates.ap[:, i, ex:ex + 1], scalar2=None, op0=ALU.mult),
                                 reads=[pd, gates], writes=[acc])
                        else:
                            P.op("dve", lambda e, pd=pd, i=i, cs=cs, ex=ex: e.scalar_tensor_tensor(out=acc.ap[:, i, cs], in0=pd.ap, scalar=gates.ap[:, i, ex:ex + 1], in1=acc.ap[:, i, cs],
                                                                                          op0=ALU.mult, op1=ALU.add), reads=[pd, gates, acc], writes=[acc])
                first = False
                self.tick()
        self.flush()
        self.post_residual(None, nt, x_tiles_src, x_out_dst, self.gaG[0][1], rings, xout_buf, True,
                           from_sbuf=lambda i: (acc.ap[:, i, :], acc))


def _decl_common(B, layer, main):
    B.inp("winP", [D, IN_DIM])
    if main:
        for nm, shp in (("wao", [512, D]), ("wfn", [512, D]), ("wco", [512, D]), ("wo", [D, D])):
            B.inp(nm, shp)
        B.inp("ropeC", [128, 34 * 128])
        B.inp("ropeS", [128, 34 * 128])
        if layer == 0:
            B.inp("wfg", [D, D_FF])
            B.inp("wfu", [D, D_FF])
            B.inp("wfd", [D_FF, D])
            B.inp("t256", [128, 2, 2, 256])
        else:
            B.inp("wrt", [128, 8, NEXP])
            B.inp("brt", [128, NEXP])
            B.inp("weg", [NEXP, D, D_EXP])
            B.inp("weu", [NEXP, D, D_EXP])
            B.inp("wed", [NEXP, D_EXP, D])


def build_pre(layer):
    B = Builder("pre", layer)
    P, A = B.P, B.A
    _decl_common(B, layer, False)
    xh = B.inp("xh", [34 * 128, D])
    zf = B.outp("zf", [4, TOK, 128], BF16)
    B.zf_b = P.buf("zf_d")
    B.emit_consts()
    B.emit_mod()
    B.emit_phaseA(xh, True, zf)
    P.finish()
    return B


def build_main(layer, stop_after=None, mix_stop=None):
    B = Builder("main", layer)
    B.mix_stop = mix_stop
    if isinstance(mix_stop, str) and mix_stop.startswith("pr"):
        B.pr_stop = int(mix_stop[2:])
    P, A, nc = B.P, B.A, B.nc
    last = layer == 1
    _decl_common(B, layer, True)
    xh = B.inp("xh", [34 * 128, D])
    hctx = B.inp("hctx", [CTX, D])
    zfg = B.inp("zfg", [4, 4, TOK, 128], BF16)
    xo = B.outp("xo", [TOK, D])
    if not last:
        hco = B.outp("hco", [CTX, D])
    B.u_d = B.scratch("u_d", [128, 4, 34 * 128], BF16)
    B.u_b = P.buf("u_d")
    B.ft_d = B.scratch("ft_d", [128, 4, TOK], BF16)
    B.ft_b = P.buf("ft_d")
    xmid = B.scratch("xmid_d", [TOK, D], F32)
    xmid_b = P.buf("xmid_d")
    xo_b = P.buf("xo")
    winP = B.din["winP"]
    B.emit_consts()
    B.emit_mod()
    m_mix = A.mark()
    B.kT = A.alloc("kT", [34 * 128], BF16)
    B.vaug = A.alloc("vaug", [34, 2, 65], BF16)
    P.op("dve", lambda e: e.memset(B.vaug.ap, 1.0), writes=[B.vaug])
    kcT = A.alloc("kcT", [CTX], BF16)
    vcaug = A.alloc("vcaug", [2, 2, 65], BF16)
    P.op("dve", lambda e: e.memset(vcaug.ap, 1.0), writes=[vcaug])
    B.wbr = []
    for nm in ("wao", "wfn", "wco"):
        w = A.alloc(nm, [4, D], BF16)
        B.load(w, B.dl(nm).rearrange("(k p) n -> p k n", p=128), "pool")
        B.wbr.append(w)
    B.wo2 = []
    for hf in range(2):
        w = A.alloc(f"wo{hf}", [8, 512], BF16)
        B.load(w, B.dl("wo")[:, hf * 512:(hf + 1) * 512].rearrange("(k p) n -> p k n", p=128), "pool")
        B.wo2.append(w)

    if stop_after == "mod":
        P.finish()
        return B
    m0 = A.mark()
    rings = B.norm_rings(2)
    hcT = A.alloc("hcT", [8, 256], BF16)
    B.make_hT([hctx[i * 128:(i + 1) * 128, :] for i in range(2)], hcT, 1, 0, rings)
    ncolA = PA_W
    wA = A.alloc("wAc", [8, ncolA], BF16)
    B.load(wA, winP[:, 0:ncolA].rearrange("(k p) n -> p k n", p=128), "pool")
    pr = B.proj_fm(wA, PA_K, hcT, CTX)
    P.op("act", lambda e: e.copy(out=kcT.ap, in_=pr.ap[:, :CTX]), reads=[pr], writes=[kcT])
    pv = B.ps.next()
    pvv = pv.ap.rearrange("p (i c) -> p i c", i=4)
    for i in range(2):
        for k in range(8):
            P.op("pe", lambda e, k=k, i=i: e.matmul(out=pvv[:, i, :], lhsT=hcT.ap[:, k, i * 128:(i + 1) * 128], rhs=wA.ap[:, k, PA_V:PA_V + 128],
                                                  start=(k == 0), stop=(k == 7)), reads=[hcT, wA], writes=[pv], signal=(k == 7 and i == 1))
    P.op("act", lambda e: e.copy(out=vcaug.ap[:, :, :, 0:64], in_=pvv[:, 0:2, :].rearrange("p i (g d) -> p i g d", g=2)), reads=[pv], writes=[vcaug])
    ctxkb = [(kcT.ap[:, i * 128:(i + 1) * 128], kcT, vcaug.ap[:, i, :, :], vcaug, None) for i in range(2)]
    if stop_after == "ctx_kv":
        P.finish()
        return B
    if not last:
        hmid = B.scratch("hmid_d", [CTX, D], F32)
        hmid_b = P.buf("hmid_d")
        hco_b = P.buf("hco")
        zcf = A.alloc("zcf", [2, 512], BF16)
        for i in range(2):
            pz = B.ps.next()
            for k in range(8):
                P.op("pe", lambda e, k=k, i=i, pz=pz: e.matmul(out=pz.ap, lhsT=hcT.ap[:, k, i * 128:(i + 1) * 128], rhs=wA.ap[:, k, PA_F:PA_F + 512],
                                                           start=(k == 0), stop=(k == 7)), reads=[hcT, wA], writes=[pz], signal=(k == 7))
            P.op("act", lambda e, i=i, pz=pz: e.copy(out=zcf.ap[:, i, :], in_=pz.ap), reads=[pz], writes=[zcf])
        t256 = B.const_tile("t256", [2, 2, 256], BF16, B.din["t256"])
        Gc = A.alloc("Gc", [2, 256], BF16)
        FTc = A.alloc("FTc", [4, 256], BF16)
        scl = 1.0 / math.sqrt(256.0 * 128.0)
        for g in range(4):
            pg_ = B.ps.next()
            for i in range(2):
                P.op("pe", lambda e, g=g, i=i, pg_=pg_: e.matmul(out=pg_.ap, lhsT=zcf.ap[:, i, g * 128:(g + 1) * 128],
                                                             rhs=t256.ap[:, i, :, :].rearrange("p r k -> p (r k)"), start=(i == 0), stop=(i == 1)),
                     reads=[zcf, t256], writes=[pg_], signal=(i == 1))
            P.op("act", lambda e, pg_=pg_: e.copy(out=Gc.ap.rearrange("p r k -> p (r k)"), in_=pg_.ap), reads=[pg_], writes=[Gc])
            pf = B.ps.next()
            P.op("pe", lambda e, pf=pf: e.matmul(out=pf.ap[:, 0:256], lhsT=B.cbsb.ap[:, 0, :], rhs=Gc.ap[:, 0, :], start=True, stop=False),
                 reads=[B.cbsb, Gc], writes=[pf], signal=False)
            P.op("pe", lambda e, pf=pf: e.matmul(out=pf.ap[:, 0:256], lhsT=B.cbsb.ap[:, 1, :], rhs=Gc.ap[:, 1, :], start=False, stop=True),
                 reads=[B.cbsb, Gc], writes=[pf])
            P.op("act", lambda e, g=g, pf=pf: e.activation(out=FTc.ap[:, g, :], in_=pf.ap[:, 0:256], func=AF.Copy, scale=scl), reads=[pf], writes=[FTc])
        if stop_after == "ctx_fft":
            P.finish()
            return B
        ucT = A.alloc("ucT", [4, 258], BF16)
        P.op("dve", lambda e: e.memset(ucT.ap, 0.0), writes=[ucT])
        cxs = A.alloc("cxs_c", [512], F32)
        for m in range(4):
            px = B.proj_fm(wA, PA_CX + m * 128, hcT, CTX)
            P.op("act", lambda e, px=px: e.copy(out=cxs.ap[:, :CTX], in_=px.ap[:, :CTX]), reads=[px], writes=[cxs])
            pc = B.proj_fm(wA, PA_CC + m * 128, hcT, CTX)
            P.op("dve", lambda e, m=m, pc=pc: e.tensor_tensor(out=ucT.ap[:, m, 1:257], in0=cxs.ap[:, :CTX], in1=pc.ap[:, :CTX], op=ALU.mult),
                 reads=[cxs, pc], writes=[ucT])
        wr = A.ring("wrc", 3, [8, 512], BF16)
        wq = wr.next()
        B.load(wq, winP[:, PM_Q:PM_Q + 512].rearrange("(k p) n -> p k n", p=128), "pool")
        qcT = A.alloc("qcT", [4, 256], BF16)
        for m in range(4):
            pq = B.proj_fm(wq, m * 128, hcT, CTX)
            P.op("act", lambda e, m=m, pq=pq: e.copy(out=qcT.ap[:, m, :], in_=pq.ap[:, :CTX]), reads=[pq], writes=[qcT])
        attnTc = A.alloc("attnTc", [4, 256], BF16)
        abufs = (A.ring("pTc", 1, [2, 2, 4, 128], BF16), A.ring("atokc", 1, [8, 64], BF16), A.ring("denc", 1, [2, 8], F32))
        for t in range(2):
            B.attention_tile(qcT, t * 128, ctxkb, attnTc, t * 128, abufs)
        if stop_after == "ctx_attn":
            dq = B.outp("dbg_qcT", [128, 4 * 256], BF16)
            da = B.outp("dbg_attnTc", [128, 4 * 256], BF16)
            dk = B.outp("dbg_kcT", [128, 256], BF16)
            dv = B.outp("dbg_vc", [128, 2 * 2 * 65], BF16)
            ob = P.buf("dbgo")
            P.dma("sp", dq, qcT.ap.rearrange("p a b -> p (a b)"), reads=[qcT], writes=[ob], is_output=True)
            P.dma("sp", da, attnTc.ap.rearrange("p a b -> p (a b)"), reads=[attnTc], writes=[ob], is_output=True)
            P.dma("sp", dk, kcT.ap, reads=[kcT], writes=[ob], is_output=True)
            P.dma("sp", dv, vcaug.ap.rearrange("p a b c -> p (a b c)"), reads=[vcaug], writes=[ob], is_output=True)
            P.finish()
            return B
        work = (A.alloc("yconvTc", [4, 256], BF16), A.ring("gsbc", 2, [512], BF16), A.ring("tmpc", 3, [512], F32), A.alloc("mergedTc", [8, 256], BF16),
                A.ring("mixtc", 1, [D], F32), A.ring("smc", 2, [4], F32), A.ring("xresc", 2, [D], F32), A.ring("sqc", 1, [512], BF16))
        B.mix_block(hcT, CTX, qcT, attnTc, ucT.ap, ucT, FTc.ap, FTc, wr, work,
                    [hctx[i * 128:(i + 1) * 128, :] for i in range(2)], [hmid[i * 128:(i + 1) * 128, :] for i in range(2)], 1, hmid_b)
        if stop_after == "ctx_mix":
            ob = P.buf("dbgo2")
            for nm_, t_, a_, n_ in (("dbg_FTc", FTc, 4, 256), ("dbg_ucT", ucT, 4, 258), ("dbg_yconv", work[0], 4, 256), ("dbg_merged", work[3], 8, 256), ("dbg_attnTc", attnTc, 4, 256)):
                do = B.outp(nm_, [128, a_, n_], BF16)
                P.dma("sp", do, t_.ap[:, :, 0:n_], reads=[t_], writes=[ob], is_output=True)
            P.finish()
            return B
        P.barrier()
        A.release(m0)
        rings = B.norm_rings(2)
        h2c = A.alloc("h2c", [8, 256], BF16)
        B.make_hT_dep([hmid[i * 128:(i + 1) * 128, :] for i in range(2)], h2c, 1, 1, rings, hmid_b)
        wr = A.ring("wrf", 8, [8, 512], BF16)
        work = (A.alloc("actTc", [22, 256], BF16), A.ring("sgc", 2, [512], BF16),
                (A.ring("mixtf", 1, [D], F32), A.ring("smf", 2, [4], F32), A.ring("xresf", 1, [D], F32), A.ring("sqf", 1, [512], BF16)))
        B.ffn_dense_block(h2c, CTX, wr, work, [(hmid[i * 128:(i + 1) * 128, :], hmid_b) for i in range(2)],
                          [hco[i * 128:(i + 1) * 128, :] for i in range(2)], 1, hco_b, True)
    P.barrier()
    A.release(m0)

    if stop_after == "ctx":
        P.finish()
        return B
    B.emit_phaseA(xh, False)
    if stop_after == "A":
        P.finish()
        return B
    B.emit_fft(zfg)
    if stop_after == "fft":
        P.finish()
        return B

    m0 = A.mark()
    rings = B.norm_rings(3)
    hTr = A.ring("hTm", 1, [8, 512], BF16)
    rope_r = A.ring("ropeM", 1, [2, 512], F32)
    qraw = A.alloc("qraw", [512], BF16)
    rtmp = (A.alloc("rt1m", [512], F32), A.alloc("rt2m", [512], F32))
    qT = A.alloc("qT", [4, 512], BF16)
    attnT = A.alloc("attnT", [4, 512], BF16)
    abufs = (A.ring("pT", 2, [5, 2, 4, 128], BF16), A.ring("atok", 2, [8, 64], BF16), A.ring("den", 2, [2, 8], F32))
    ubr = A.ring("ub", 1, [4, 514], BF16)
    ftr = A.ring("ftb", 1, [4, 512], BF16)
    wr = A.ring("wrm", 3, [8, 512], BF16)
    work = (A.alloc("yconvT", [4, 512], BF16), A.ring("gsb", 2, [512], BF16), A.ring("tmpm", 3, [512], F32), A.alloc("mergedT", [8, 512], BF16),
            A.ring("mixt", 1, [D], F32), A.ring("smm", 2, [4], F32), A.ring("xres", 1, [D], F32), A.ring("sqm", 1, [512], BF16))
    for bi in range(8):
        c0 = (4 * bi + 1) * 128
        hT = hTr.next()
        own = [xh[(4 * bi + 1 + i) * 128:(4 * bi + 2 + i) * 128, :] for i in range(4)]
        B.make_hT(own, hT, 0, 0, rings)
        rp = rope_r.next()
        P.dma("sp", rp.ap[:, 0, :], B.din["ropeC"][:, c0:c0 + 512], writes=[rp])
        P.dma("sp", rp.ap[:, 1, :], B.din["ropeS"][:, c0:c0 + 512], writes=[rp])
        B.ropeb = rp
        wq = wr.next()
        B.load(wq, winP[:, PM_Q:PM_Q + 512].rearrange("(k p) n -> p k n", p=128), "pool")
        for m in range(4):
            pq = B.proj_fm(wq, m * 128, hT, 512)
            B.rope(pq, qraw, qT.ap[:, m, :], qT, rp.ap[:, 0, :], rp.ap[:, 1, :], 512, rtmp)
        for t in range(4):
            T = 4 * bi + t
            kbs = []
            for d_, mi in ((0, 2 if T == 0 else 0), (1, None), (2, 3 if T == 31 else 1)):
                sl = T + d_
                kbs.append((B.kT.ap[:, sl * 128:(sl + 1) * 128], B.kT, B.vaug.ap[:, sl, :, :], B.vaug, mi))
            kbs += ctxkb
            B.attention_tile(qT, t * 128, kbs, attnT, t * 128, abufs)
        ub = ubr.next()
        P.dma("sp", ub.ap, B.u_d[:, :, c0 - 1:c0 + 513], reads=[B.u_b], writes=[ub])
        ftb = ftr.next()
        P.dma("sp", ftb.ap, B.ft_d[:, :, bi * 512:(bi + 1) * 512], reads=[B.ft_b], writes=[ftb])
        B.mix_block(hT, 512, qT, attnT, ub.ap, ub, ftb.ap, ftb, wr, work, own,
                    [xmid[(4 * bi + i) * 128:(4 * bi + i + 1) * 128, :] for i in range(4)], 0, xmid_b)
    P.barrier()
    A.release(m0)

    if stop_after == "mix":
        P.finish()
        return B
    A.release(m_mix)
    m0 = A.mark()
    if not last:
        rings = B.norm_rings(4)
        h2r = A.ring("h2T", 1, [8, 512], BF16)
        wr = A.ring("wrf2", 8, [8, 512], BF16)
        work = (A.alloc("actT", [22, 512], BF16), A.ring("sg", 2, [512], BF16),
                (A.ring("mixtF", 1, [D], F32), A.ring("smF", 2, [4], F32), A.ring("xresF", 2, [D], F32), A.ring("sqF", 1, [512], BF16)))
        for bi in range(8):
            h2T = h2r.next()
            src = [xmid[(4 * bi + i) * 128:(4 * bi + i + 1) * 128, :] for i in range(4)]
            B.make_hT_dep(src, h2T, 0, 1, rings, xmid_b)
            B.ffn_dense_block(h2T, 512, wr, work, [(a, xmid_b) for a in src],
                              [xo[(4 * bi + i) * 128:(4 * bi + i + 1) * 128, :] for i in range(4)], 0, xo_b, True)
    else:
        rings = B.norm_rings(4)
        h2r = A.ring("h2T", 1, [8, 1024], BF16)
        wr = A.ring("wre", 6, [8, 512], BF16)
        wrt = B.const_tile("wrt", [8, NEXP], BF16, B.din["wrt"])
        brt = B.const_tile("brt", [NEXP], F32, B.din["brt"])
        work = (A.alloc("acc", [8, D], F32), A.ring("actc", 2, [4, 1024], BF16), A.ring("sge", 2, [512], BF16), A.alloc("gates", [8, NEXP], F32),
                A.ring("lg", 2, [5, 8], F32),
                (A.ring("mixtE", 1, [D], F32), A.ring("smE", 2, [4], F32), A.ring("xresE", 2, [D], F32), A.ring("sqE", 1, [512], BF16)), wrt, brt)
        for bi in range(4):
            h2T = h2r.next()
            src = [xmid[(8 * bi + i) * 128:(8 * bi + i + 1) * 128, :] for i in range(8)]
            B.make_hT_dep(src, h2T, 0, 1, rings, xmid_b)
            B.ffn_moe_block(h2T, wr, work, [(a, xmid_b) for a in src], [xo[(8 * bi + i) * 128:(8 * bi + i + 1) * 128, :] for i in range(8)], xo_b)
    P.finish()
    return B


def _w_in_perm():
    cols = []
    cols += list(range(F_OFF, F_OFF + 512))
    cols += list(range(K_OFF, K_OFF + 128))
    cols += list(range(V_OFF, V_OFF + 128))
    cols += list(range(CX_OFF, CX_OFF + 512))
    cols += list(range(CC_OFF, CC_OFF + 512))
    for m in range(4):
        cols += list(range(Q_OFF + m * 64, Q_OFF + (m + 1) * 64))
        cols += list(range(Q_OFF + (4 + m) * 64, Q_OFF + (5 + m) * 64))
    cols += list(range(CB_OFF, CB_OFF + 512))
    for m in range(8):
        for r in range(3):
            cols += list(range(GATE_OFF + r * 1024 + m * 128, GATE_OFF + r * 1024 + (m + 1) * 128))
    assert len(cols) == IN_DIM and len(set(cols)) == IN_DIM
    return np.asarray(cols)


def _colform(v, n):
    return np.ascontiguousarray(np.asarray(v, np.float32).reshape(n, 128).T)


_CONST_CACHE = {}


def _static_tables():
    if "t" in _CONST_CACHE:
        return _CONST_CACHE["t"]
    t = {}
    t["ident"] = np.eye(128, dtype=np.float32)
    rt = np.zeros((128, 128), np.float32)
    for i in range(64):
        rt[2 * i + 1, 2 * i] = -1.0
        rt[2 * i, 2 * i + 1] = 1.0
    t["rt"] = rt
    n = np.arange(128, dtype=np.float64)
    k1 = np.arange(128, dtype=np.float64)
    ang = 2 * np.pi * np.outer(n, k1) / 128.0
    t1 = np.stack([np.cos(ang), -np.sin(ang)], axis=1).reshape(128, 2, 2, 64)
    t["t1"] = np.ascontiguousarray(t1.transpose(0, 2, 1, 3)).astype(np.float32)
    angc = 2 * np.pi * np.outer(n, n) / 128.0
    t["cbsb"] = np.ascontiguousarray(np.stack([np.cos(angc), np.sin(angc)], axis=1)).astype(np.float32)
    nn = np.arange(256, dtype=np.float64)
    a256 = 2 * np.pi * np.outer(nn, nn) / 256.0
    t256 = np.stack([np.cos(a256), -np.sin(a256)], axis=1)
    t["t256"] = np.ascontiguousarray(t256.reshape(2, 128, 2, 256).transpose(1, 0, 2, 3)).astype(np.float32)
    kk = np.arange(128)[:, None]
    qq = np.arange(128)[None, :]
    prev = (kk >= qq).astype(np.float32)
    nxt = (kk <= qq).astype(np.float32)
    half = 32
    inv_freq = 1.0 / (10000.0 ** (np.arange(0, half, 2, dtype=np.float64) / half))
    for j in range(4):
        m = np.stack([prev, nxt, prev * (1.0 if j != 0 else 0.0), nxt * (1.0 if j != 3 else 0.0)], axis=1)
        t[("masks", j)] = np.ascontiguousarray(m).astype(np.float32)
        t[("valid", j)] = np.tile(np.asarray([[1.0 if j != 0 else 0.0, 1.0 if j != 3 else 0.0]], np.float32), (128, 1))
        pos = TOK * j - 128 + np.arange(34 * 128)
        row = (pos // 64).astype(np.float64)
        col = (pos % 64).astype(np.float64)
        angp = np.concatenate([row[:, None] * inv_freq[None, :], col[:, None] * inv_freq[None, :]], axis=1)
        d = np.arange(128) % 64
        pi_ = d // 2
        t[("ropeC", j)] = np.ascontiguousarray(np.cos(angp)[:, pi_].T).astype(np.float32)
        t[("ropeS", j)] = np.ascontiguousarray(np.sin(angp)[:, pi_].T).astype(np.float32)
        n2 = np.arange(128, dtype=np.float64)[:, None, None]
        k1_ = np.arange(128, dtype=np.float64)[None, :, None]
        k2_ = (32 * j + np.arange(32, dtype=np.float64))[None, None, :]
        th = 2 * np.pi * n2 * (k1_ + 128.0 * k2_) / float(SEQ)
        et = np.concatenate([np.sin(th), np.cos(th), -np.sin(th)], axis=2)
        t[("etab", j)] = np.ascontiguousarray(et).astype(np.float32)
    _CONST_CACHE["t"] = t
    return t


def _layer_common(inp, l):
    perm = _w_in_perm()
    c = {}
    c["winP"] = np.ascontiguousarray(np.asarray(inp["w_in"][l], np.float32)[:, perm])
    c["wmod"] = np.ascontiguousarray(np.asarray(inp["w_mod"][l], np.float32))
    bm = np.asarray(inp["b_mod"][l], np.float32)
    c["bcols"] = _colform(bm, 48)
    c["brow"] = np.ascontiguousarray(np.broadcast_to(np.stack([bm[2 * D:3 * D], bm[5 * D:6 * D]])[None], (128, 2, D))).astype(np.float32)
    c["gpm_c"] = _colform(inp["g_pre_mix"][l], 8)
    c["gpf_c"] = _colform(inp["g_pre_ffn"][l], 8)
    c["gqm_r"] = np.ascontiguousarray(np.broadcast_to(np.asarray(inp["g_post_mix"][l], np.float32)[None], (128, D)))
    c["gqf_r"] = np.ascontiguousarray(np.broadcast_to(np.asarray(inp["g_post_ffn"][l], np.float32)[None], (128, D)))
    c["wao"] = np.ascontiguousarray(np.asarray(inp["w_attn_o"][l], np.float32))
    c["wfn"] = np.ascontiguousarray(np.asarray(inp["w_fnet"][l], np.float32))
    c["wco"] = np.ascontiguousarray(np.asarray(inp["w_conv_out"][l], np.float32))
    c["wo"] = np.ascontiguousarray(np.asarray(inp["w_o"][l], np.float32))
    c["sink_b"] = np.ascontiguousarray(np.broadcast_to(np.asarray(inp["attn_sink"][l], np.float32)[None], (128, 8)))
    wc = np.asarray(inp["w_conv"][l], np.float32)
    c["wconv_c"] = np.ascontiguousarray(wc.reshape(3, 4, 128).transpose(2, 1, 0))
    if l == 0:
        c["wfg"] = np.ascontiguousarray(np.asarray(inp["w_ff_gate"][0], np.float32))
        c["wfu"] = np.ascontiguousarray(np.asarray(inp["w_ff_up"][0], np.float32))
        c["wfd"] = np.ascontiguousarray(np.asarray(inp["w_ff_down"][0], np.float32))
    else:
        wr = np.asarray(inp["w_router"][0], np.float32)
        c["wrt"] = np.ascontiguousarray(wr.reshape(8, 128, NEXP).transpose(1, 0, 2))
        c["brt"] = np.ascontiguousarray(np.broadcast_to(np.asarray(inp["b_router"][0], np.float32)[None], (128, NEXP)))
        c["weg"] = np.ascontiguousarray(np.asarray(inp["w_exp_gate"][0], np.float32))
        c["weu"] = np.ascontiguousarray(np.asarray(inp["w_exp_up"][0], np.float32))
        c["wed"] = np.ascontiguousarray(np.asarray(inp["w_exp_down"][0], np.float32))
    return c


def _ccols(inp, b):
    cb = np.asarray(inp["c"][b], np.float32)
    cc = np.asarray(inp["c_ctx"], np.float32)
    return np.ascontiguousarray(np.stack([_colform(cb, 8), _colform(cc, 8)], axis=2))


def _xh(xfull, cid):
    b, j = cid // 4, cid % 4
    out = np.zeros((34 * 128, D), np.float32)
    lo = TOK * j - 128
    hi = TOK * (j + 1) + 128
    s0, s1 = max(lo, 0), min(hi, SEQ)
    out[s0 - lo:s1 - lo] = xfull[b, s0:s1]
    return out


_PROG_CACHE = {}


def _get_prog(kind, layer):
    key = (kind, layer)
    if key not in _PROG_CACHE:
        _PROG_CACHE[key] = build_pre(layer) if kind == "pre" else build_main(layer)
    return _PROG_CACHE[key]


def _run(B, maps):
    names = set(B.din.keys())
    in_maps = [{k: v for k, v in m.items() if k in names} for m in maps]
    for m in in_maps:
        missing = names - set(m.keys())
        assert not missing, missing
    res = run_bass_kernel_spmd(B.nc, in_maps, core_ids=list(range(8)))
    return res.results


def kernel_unfused(**inputs):
    tabs = _static_tables()
    x = np.asarray(inputs["x"], np.float32)
    hctx = [np.ascontiguousarray(np.asarray(inputs["ctx"][b], np.float32)) for b in range(NB)]
    for l in range(2):
        com = _layer_common(inputs, l)
        maps = []
        for cid in range(8):
            b, j = cid // 4, cid % 4
            m = dict(com)
            for nm in ("ident", "rt", "t1", "cbsb", "t256"):
                m[nm] = tabs[nm]
            for nm in ("masks", "valid", "ropeC", "ropeS", "etab"):
                m[nm] = tabs[(nm, j)]
            m["ccols"] = _ccols(inputs, b)
            m["xh"] = _xh(x, cid)
            m["hctx"] = hctx[b]
            maps.append(m)
        r = _run(_get_prog("pre", l), maps)
        for b in range(NB):
            zfg = np.ascontiguousarray(np.stack([np.asarray(r[4 * b + j]["zf"]) for j in range(4)]))
            for j in range(4):
                maps[4 * b + j]["zfg"] = zfg
        r = _run(_get_prog("main", l), maps)
        xn = np.empty_like(x)
        for cid in range(8):
            b, j = cid // 4, cid % 4
            xn[b, TOK * j:TOK * (j + 1)] = np.asarray(r[cid]["xo"], np.float32)
        x = xn
        if l == 0:
            hctx = [np.ascontiguousarray(np.asarray(r[4 * b]["hco"], np.float32)) for b in range(NB)]
    return x


GROUPS = [[0, 1, 2, 3], [4, 5, 6, 7]]


def _decl_layer(B, l):
    sfx = str(l)
    B.inp("winP" + sfx, [D, IN_DIM])
    for nm, shp in (("wao", [512, D]), ("wfn", [512, D]), ("wco", [512, D]), ("wo", [D, D])):
        B.inp(nm + sfx, shp)
    if l == 0:
        B.inp("wfg0", [D, D_FF])
        B.inp("wfu0", [D, D_FF])
        B.inp("wfd0", [D_FF, D])
    else:
        B.inp("wrt1", [128, 8, NEXP])
        B.inp("brt1", [128, NEXP])
        B.inp("weg1", [NEXP, D, D_EXP])
        B.inp("weu1", [NEXP, D, D_EXP])
        B.inp("wed1", [NEXP, D_EXP, D])


def build_fused(sim_cc=False, stop=None):
    B = Builder("main", 0)
    P, A, nc = B.P, B.A, B.nc
    B.sfx = "0"
    for l in range(2):
        if l == 1 and stop is not None and stop != "l1mix":
            continue
        _decl_layer(B, l)
    B.inp("ropeC", [128, 34 * 128])
    B.inp("ropeS", [128, 34 * 128])
    B.inp("t256", [128, 2, 2, 256])
    xh = B.inp("xh", [34 * 128, D])
    hctx_in = B.inp("hctx", [CTX, D])
    selh = B.inp("selh", [128, 8])
    xo = B.outp("xo", [TOK, D])
    xo_b = P.buf("xo")
    B.u_d = B.scratch("u_d", [128, 4, 34 * 128], BF16)
    B.u_b = P.buf("u_d")
    B.ft_d = B.scratch("ft_d", [128, 4, TOK], BF16)
    B.ft_b = P.buf("ft_d")
    xmid = B.scratch("xmid_d", [TOK, D], F32)
    xmid_b = P.buf("xmid_d")
    x1 = B.scratch("x1_d", [TOK, D], F32)
    x1_b = P.buf("x1_d")
    hmid = B.scratch("hmid_d", [CTX, D], F32)
    hmid_b = P.buf("hmid_d")
    hc1 = B.scratch("hc1_d", [CTX, D], F32)
    hc1_b = P.buf("hc1_d")
    xhalo = B.scratch("xhalo_d", [256, D], F32)
    xhalo_b = P.buf("xhalo_d")
    zf_l = [nc.dram_tensor(f"zf_cc{g}", [TOK, 128], BF16).ap() for g in range(4)]
    zfg_l = [nc.dram_tensor(f"zfg_cc{g}", [4 * TOK, 128], BF16).ap() for g in range(4)]
    xb_src = nc.dram_tensor("xb_cc", [256, D], F32).ap()
    xb_all = nc.dram_tensor("xball_cc", [4 * 256, D], F32).ap()
    B.zf_b = P.buf("zf_cc")
    zfg_b = P.buf("zfg_cc")
    xb_b = P.buf("xb_cc")
    xball_b = P.buf("xball_cc")
    def zsrc(r, g):
        return zfg_l[g][r * TOK:(r + 1) * TOK, :]

    B.emit_consts()
    B.alloc_mod_tiles()
    m_top = A.mark()
    for l in range(2):
        last = l == 1
        B.layer = l
        B.last = last
        B.sfx = str(l)
        winP = B.dl("winP")
        B.emit_layer_consts()
        B.emit_mod()
        m_mix = A.mark()
        B.kT = A.alloc("kT", [34 * 128], BF16)
        B.vaug = A.alloc("vaug", [34, 2, 65], BF16)
        P.op("dve", lambda e: e.memset(B.vaug.ap, 1.0), writes=[B.vaug])
        kcT = A.alloc("kcT", [CTX], BF16)
        vcaug = A.alloc("vcaug", [2, 2, 65], BF16)
        P.op("dve", lambda e: e.memset(vcaug.ap, 1.0), writes=[vcaug])
        B.wbr = []
        for nm in ("wao", "wfn", "wco"):
            w = A.alloc(nm, [4, D], BF16)
            B.load(w, B.dl(nm).rearrange("(k p) n -> p k n", p=128), "pool")
            B.wbr.append(w)
        B.wo2 = []
        for hf in range(2):
            w = A.alloc(f"wo{hf}", [8, 512], BF16)
            B.load(w, B.dl("wo")[:, hf * 512:(hf + 1) * 512].rearrange("(k p) n -> p k n", p=128), "pool")
            B.wo2.append(w)

        if l == 0:
            def xtile(t):
                return xh[(t + 1) * 128:(t + 2) * 128, :]
            hsrc = [hctx_in[i * 128:(i + 1) * 128, :] for i in range(2)]
        else:
            def xtile(t):
                if t < 0:
                    return (xhalo[0:128, :], xhalo_b)
                if t >= NTILE:
                    return (xhalo[128:256, :], xhalo_b)
                return (x1[t * 128:(t + 1) * 128, :], x1_b)
            hsrc = [(hc1[i * 128:(i + 1) * 128, :], hc1_b) for i in range(2)]
        B.xtile = xtile

        B.emit_phaseA(None, True, zf_l)
        m0 = A.mark()
        wA = A.alloc("wAc", [8, PA_W], BF16)
        B.load(wA, winP[:, 0:PA_W].rearrange("(k p) n -> p k n", p=128), "pool")
        if not last:
            wr_c = A.ring("wrc", 3, [8, 512], BF16)
            wq_c = wr_c.next()
            B.load(wq_c, winP[:, PM_Q:PM_Q + 512].rearrange("(k p) n -> p k n", p=128), "pool")
        for g in range(4):
            if sim_cc:
                for r in range(4):
                    P.dma("sp", zfg_l[g][r * TOK:(r + 1) * TOK, :], zf_l[g], reads=[B.zf_b], writes=[zfg_b])
            else:
                P.cc_allgather(zfg_l[g], zf_l[g], GROUPS, reads=[B.zf_b], writes=[zfg_b])

        rings = B.norm_rings(2)
        hcT = A.alloc("hcT", [8, 256], BF16)
        B.make_hT(hsrc, hcT, 1, 0, rings)
        pr = B.proj_fm(wA, PA_K, hcT, CTX)
        P.op("act", lambda e: e.copy(out=kcT.ap, in_=pr.ap[:, :CTX]), reads=[pr], writes=[kcT])
        pv = B.ps.next()
        pvv = pv.ap.rearrange("p (i c) -> p i c", i=4)
        for i in range(2):
            for k in range(8):
                P.op("pe", lambda e, k=k, i=i: e.matmul(out=pvv[:, i, :], lhsT=hcT.ap[:, k, i * 128:(i + 1) * 128], rhs=wA.ap[:, k, PA_V:PA_V + 128],
                                                      start=(k == 0), stop=(k == 7)), reads=[hcT, wA], writes=[pv], signal=(k == 7 and i == 1))
        P.op("act", lambda e: e.copy(out=vcaug.ap[:, :, :, 0:64], in_=pvv[:, 0:2, :].rearrange("p i (g d) -> p i g d", g=2)), reads=[pv], writes=[vcaug])
        ctxkb = [(kcT.ap[:, i * 128:(i + 1) * 128], kcT, vcaug.ap[:, i, :, :], vcaug, None) for i in range(2)]
        if not last:
            zcf = A.alloc("zcf", [2, 512], BF16)
            for i in range(2):
                pz = B.ps.next()
                for k in range(8):
                    P.op("pe", lambda e, k=k, i=i, pz=pz: e.matmul(out=pz.ap, lhsT=hcT.ap[:, k, i * 128:(i + 1) * 128], rhs=wA.ap[:, k, PA_F:PA_F + 512],
                                                               start=(k == 0), stop=(k == 7)), reads=[hcT, wA], writes=[pz], signal=(k == 7))
                P.op("act", lambda e, i=i, pz=pz: e.copy(out=zcf.ap[:, i, :], in_=pz.ap), reads=[pz], writes=[zcf])
            t256 = B.const_tile("t256", [2, 2, 256], BF16, B.din["t256"])
            Gc = A.alloc("Gc", [2, 256], BF16)
            FTc = A.alloc("FTc", [4, 256], BF16)
            scl = 1.0 / math.sqrt(256.0 * 128.0)
            for g in range(4):
                pg_ = B.ps.next()
                for i in range(2):
                    P.op("pe", lambda e, g=g, i=i, pg_=pg_: e.matmul(out=pg_.ap, lhsT=zcf.ap[:, i, g * 128:(g + 1) * 128],
                                                                 rhs=t256.ap[:, i, :, :].rearrange("p r k -> p (r k)"), start=(i == 0), stop=(i == 1)),
                         reads=[zcf, t256], writes=[pg_], signal=(i == 1))
                P.op("act", lambda e, pg_=pg_: e.copy(out=Gc.ap.rearrange("p r k -> p (r k)"), in_=pg_.ap), reads=[pg_], writes=[Gc])
                pf = B.ps.next()
                P.op("pe", lambda e, pf=pf: e.matmul(out=pf.ap[:, 0:256], lhsT=B.cbsb.ap[:, 0, :], rhs=Gc.ap[:, 0, :], start=True, stop=False),
                     reads=[B.cbsb, Gc], writes=[pf], signal=False)
                P.op("pe", lambda e, pf=pf: e.matmul(out=pf.ap[:, 0:256], lhsT=B.cbsb.ap[:, 1, :], rhs=Gc.ap[:, 1, :], start=False, stop=True),
                     reads=[B.cbsb, Gc], writes=[pf])
                P.op("act", lambda e, g=g, pf=pf: e.activation(out=FTc.ap[:, g, :], in_=pf.ap[:, 0:256], func=AF.Copy, scale=scl), reads=[pf], writes=[FTc])
            ucT = A.alloc("ucT", [4, 258], BF16)
            P.op("dve", lambda e: e.memset(ucT.ap, 0.0), writes=[ucT])
            cxs = A.alloc("cxs_c", [512], F32)
            for m in range(4):
                px = B.proj_fm(wA, PA_CX + m * 128, hcT, CTX)
                P.op("act", lambda e, px=px: e.copy(out=cxs.ap[:, :CTX], in_=px.ap[:, :CTX]), reads=[px], writes=[cxs])
                pc = B.proj_fm(wA, PA_CC + m * 128, hcT, CTX)
                P.op("dve", lambda e, m=m, pc=pc: e.tensor_tensor(out=ucT.ap[:, m, 1:257], in0=cxs.ap[:, :CTX], in1=pc.ap[:, :CTX], op=ALU.mult),
                     reads=[cxs, pc], writes=[ucT])
            wr = wr_c
            wq = wq_c
            qcT = A.alloc("qcT", [4, 256], BF16)
            for m in range(4):
                pq = B.proj_fm(wq, m * 128, hcT, CTX)
                P.op("act", lambda e, m=m, pq=pq: e.copy(out=qcT.ap[:, m, :], in_=pq.ap[:, :CTX]), reads=[pq], writes=[qcT])
            attnTc = A.alloc("attnTc", [4, 256], BF16)
            abufs = (A.ring("pTc", 1, [2, 2, 4, 128], BF16), A.ring("atokc", 1, [8, 64], BF16), A.ring("denc", 1, [2, 8], F32))
            for t in range(2):
                B.attention_tile(qcT, t * 128, ctxkb, attnTc, t * 128, abufs)
            work = (A.alloc("yconvTc", [4, 256], BF16), A.ring("gsbc", 2, [512], BF16), A.ring("tmpc", 3, [512], F32), A.alloc("mergedTc", [8, 256], BF16),
                    A.ring("mixtc", 1, [D], F32), A.ring("smc", 2, [4], F32), A.ring("xresc", 1, [D], F32), A.ring("sqc", 1, [512], BF16))
            B.mix_block(hcT, CTX, qcT, attnTc, ucT.ap, ucT, FTc.ap, FTc, wr, work, hsrc,
                        [hmid[i * 128:(i + 1) * 128, :] for i in range(2)], 1, hmid_b)
            P.barrier()
            A.release(m0)
            rings = B.norm_rings(2)
            h2c = A.alloc("h2c", [8, 256], BF16)
            B.make_hT([(hmid[i * 128:(i + 1) * 128, :], hmid_b) for i in range(2)], h2c, 1, 1, rings)
            wr = A.ring("wrf", 8, [8, 512], BF16)
            work = (A.alloc("actTc", [22, 256], BF16), A.ring("sgc", 2, [512], BF16),
                    (A.ring("mixtf", 1, [D], F32), A.ring("smf", 2, [4], F32), A.ring("xresf", 1, [D], F32), A.ring("sqf", 1, [512], BF16)))
            B.ffn_dense_block(h2c, CTX, wr, work, [(hmid[i * 128:(i + 1) * 128, :], hmid_b) for i in range(2)],
                              [hc1[i * 128:(i + 1) * 128, :] for i in range(2)], 1, hc1_b, False)
        P.barrier()
        A.release(m0)

        B.zfg_b = zfg_b
        B.emit_fft(zsrc)
        if stop == "l0fft":
            P.dma("sp", xo[0:128, :], xh[128:256, :], reads=[B.ft_b], writes=[xo_b], is_output=True)
            P.finish()
            return B

        m0 = A.mark()
        rings = B.norm_rings(2)
        hTr = A.ring("hTm", 2, [8, 512], BF16)
        rope_r = A.ring("ropeM", 1, [2, 512], F32)
        qraw = A.alloc("qraw", [512], BF16)
        tmpm_ring = A.ring("tmpm", 3, [512], F32)
        rtmp = (tmpm_ring.tiles[0], tmpm_ring.tiles[1])
        qT = A.alloc("qT", [4, 512], BF16)
        attnT = A.alloc("attnT", [4, 512], BF16)
        abufs = (A.ring("pT", 2, [5, 2, 4, 128], BF16), A.ring("atok", 2, [8, 64], BF16), A.ring("den", 2, [2, 8], F32))
        ubr = A.ring("ub", 1, [4, 514], BF16)
        ftr = A.ring("ftb", 1, [4, 512], BF16)
        wr = A.ring("wrm", 3, [8, 512], BF16)
        work = (A.alloc("yconvT", [4, 512], BF16), A.ring("gsb", 2, [512], BF16), tmpm_ring, A.alloc("mergedT", [8, 512], BF16),
                A.ring("mixt", 1, [D], F32), A.ring("smm", 2, [4], F32), A.ring("xres", 1, [D], F32), A.ring("sqm", 1, [512], BF16))

        def _mkm(bi_, defer):
            h_ = hTr.next()
            own_ = [xtile(4 * bi_ + i) for i in range(4)]
            B.make_hT(own_, h_, 0, 0, rings, defer=defer)
            return h_, own_
        nxtm = _mkm(0, False)
        for bi in range(8):
            c0 = (4 * bi + 1) * 128
            B.flush()
            hT, own = nxtm
            if bi + 1 < 8:
                nxtm = _mkm(bi + 1, True)
            rp = rope_r.next()
            P.dma("sp", rp.ap[:, 0, :], B.din["ropeC"][:, c0:c0 + 512], writes=[rp])
            P.dma("sp", rp.ap[:, 1, :], B.din["ropeS"][:, c0:c0 + 512], writes=[rp])
            B.ropeb = rp
            wq = wr.next()
            B.load(wq, winP[:, PM_Q:PM_Q + 512].rearrange("(k p) n -> p k n", p=128), "pool")
            for m in range(4):
                pq = B.proj_fm(wq, m * 128, hT, 512)
                B.rope(pq, qraw, qT.ap[:, m, :], qT, rp.ap[:, 0, :], rp.ap[:, 1, :], 512, rtmp)
            def _kbs(t_):
                T = 4 * bi + t_
                kbs = []
                for d_, mi in ((0, 2 if T == 0 else 0), (1, None), (2, 3 if T == 31 else 1)):
                    sl = T + d_
                    kbs.append((B.kT.ap[:, sl * 128:(sl + 1) * 128], B.kT, B.vaug.ap[:, sl, :, :], B.vaug, mi))
                return kbs + ctxkb
            kb_cur = _kbs(0)
            pT_cur = B.attention_scores(qT, 0, kb_cur, abufs)
            for t in range(4):
                if t + 1 < 4:
                    kb_nxt = _kbs(t + 1)
                    pT_nxt = B.attention_scores(qT, (t + 1) * 128, kb_nxt, abufs)
                B.attention_pv(pT_cur, kb_cur, attnT, t * 128, abufs)
                if t + 1 < 4:
                    kb_cur, pT_cur = kb_nxt, pT_nxt
                if t in (0, 2):
                    B.tick()
            ub = ubr.next()
            P.dma("sp", ub.ap, B.u_d[:, :, c0 - 1:c0 + 513], reads=[B.u_b], writes=[ub])
            ftb = ftr.next()
            P.dma("sp", ftb.ap, B.ft_d[:, :, bi * 512:(bi + 1) * 512], reads=[B.ft_b], writes=[ftb])
            B.mix_block(hT, 512, qT, attnT, ub.ap, ub, ftb.ap, ftb, wr, work, own,
                        [xmid[(4 * bi + i) * 128:(4 * bi + i + 1) * 128, :] for i in range(4)], 0, xmid_b)
        P.barrier()
        A.release(m_mix)

        m0 = A.mark()
        if not last:
            rings = B.norm_rings(4)
            h2r = A.ring("h2T", 2, [8, 512], BF16)
            wr = A.ring("wrf2", 8, [8, 512], BF16)
            work = (A.alloc("actT", [22, 512], BF16), A.ring("sg", 2, [512], BF16),
                    (A.ring("mixtF", 1, [D], F32), A.ring("smF", 2, [4], F32), A.ring("xresF", 2, [D], F32), A.ring("sqF", 1, [512], BF16)))
            def _mk2(bi_, defer):
                h_ = h2r.next()
                src_ = [(xmid[(4 * bi_ + i) * 128:(4 * bi_ + i + 1) * 128, :], xmid_b) for i in range(4)]
                B.make_hT(src_, h_, 0, 1, rings, defer=defer)
                return h_, src_
            nxt = _mk2(0, False)
            for bi in range(8):
                B.flush()
                h2T, src = nxt
                if bi + 1 < 8:
                    nxt = _mk2(bi + 1, True)
                B.ffn_dense_block(h2T, 512, wr, work, src,
                                  [x1[(4 * bi + i) * 128:(4 * bi + i + 1) * 128, :] for i in range(4)], 0, x1_b, False)
            P.dma("sp", xb_src[0:128, :], x1[0:128, :], reads=[x1_b], writes=[xb_b])
            P.dma("sp", xb_src[128:256, :], x1[TOK - 128:TOK, :], reads=[x1_b], writes=[xb_b])
            if sim_cc:
                for r in range(4):
                    P.dma("sp", xb_all[r * 256:(r + 1) * 256, :], xb_src, reads=[xb_b], writes=[xball_b])
            else:
                P.cc_allgather(xb_all, xb_src, GROUPS, reads=[xb_b], writes=[xball_b])
            P.barrier()
            A.release(m0)
            m0 = A.mark()
            sel = B.const_tile("selh", [8], F32, selh)
            hal = A.alloc("hal", [2, D], F32)
            gat = A.ring("gat", 2, [D], F32)
            for side in range(2):
                for r in range(4):
                    gt = gat.next()
                    row0 = r * 256 + (128 if side == 0 else 0)
                    P.dma("sp", gt.ap, xb_all[row0:row0 + 128, :], reads=[xball_b], writes=[gt])
                    sc_ap = sel.ap[:, side * 4 + r:side * 4 + r + 1]
                    if r == 0:
                        P.op("dve", lambda e, gt=gt, side=side, sc_ap=sc_ap: e.tensor_scalar(out=hal.ap[:, side, :], in0=gt.ap, scalar1=sc_ap, scalar2=None, op0=ALU.mult),
                             reads=[gt, sel], writes=[hal])
                    else:
                        P.op("dve", lambda e, gt=gt, side=side, sc_ap=sc_ap: e.scalar_tensor_tensor(out=hal.ap[:, side, :], in0=gt.ap, scalar=sc_ap, in1=hal.ap[:, side, :],
                                                                                              op0=ALU.mult, op1=ALU.add), reads=[gt, sel, hal], writes=[hal])
                P.dma("sp", xhalo[side * 128:(side + 1) * 128, :], hal.ap[:, side, :], reads=[hal], writes=[xhalo_b])
        else:
            rings = B.norm_rings(4)
            h2r = A.ring("h2T", 2, [8, 1024], BF16)
            wr = A.ring("wre", 6, [8, 512], BF16)
            wrt = B.const_tile("wrt", [8, NEXP], BF16, B.dl("wrt"))
            brt = B.const_tile("brt", [NEXP], F32, B.dl("brt"))
            work = (A.alloc("acc", [8, D], F32), A.ring("actc", 2, [4, 1024], BF16), A.ring("sge", 2, [512], BF16), A.alloc("gates", [8, NEXP], F32),
                    A.ring("lg", 2, [5, 8], F32),
                    (A.ring("mixtE", 1, [D], F32), A.ring("smE", 2, [4], F32), A.ring("xresE", 2, [D], F32), A.ring("sqE", 1, [512], BF16)), wrt, brt)
            def _mk3(bi_, defer):
                h_ = h2r.next()
                src_ = [(xmid[(8 * bi_ + i) * 128:(8 * bi_ + i + 1) * 128, :], xmid_b) for i in range(8)]
                B.make_hT(src_, h_, 0, 1, rings, defer=defer)
                return h_, src_
            nxt = _mk3(0, False)
            for bi in range(4):
                B.flush()
                h2T, src = nxt
                if bi + 1 < 4:
                    nxt = _mk3(bi + 1, True)
                B.ffn_moe_block(h2T, wr, work, src, [xo[(8 * bi + i) * 128:(8 * bi + i + 1) * 128, :] for i in range(8)], xo_b)
        P.barrier()
        A.release(m_top)
        if stop == "l0" and l == 0:
            P.dma("sp", xo[0:256, :], xhalo, reads=[xhalo_b], writes=[xo_b], is_output=True)
            P.dma("sp", xo[256:TOK, :], x1[256:TOK, :], reads=[x1_b], writes=[xo_b], is_output=True)
            P.finish()
            return B
    P.finish()
    return B


_FUSED = {}


def kernel(**inputs):
    tabs = _static_tables()
    x = np.asarray(inputs["x"], np.float32)
    com = {}
    for l in range(2):
        for k, v in _layer_common(inputs, l).items():
            com[k + str(l)] = v
    maps = []
    for cid in range(8):
        b, j = cid // 4, cid % 4
        m = dict(com)
        for nm in ("ident", "rt", "t1", "cbsb", "t256"):
            m[nm] = tabs[nm]
        for nm in ("masks", "valid", "ropeC", "ropeS", "etab"):
            m[nm] = tabs[(nm, j)]
        m["ccols"] = _ccols(inputs, b)
        m["xh"] = _xh(x, cid)
        m["hctx"] = np.ascontiguousarray(np.asarray(inputs["ctx"][b], np.float32))
        sel = np.zeros((128, 8), np.float32)
        if j - 1 >= 0:
            sel[:, j - 1] = 1.0
        if j + 1 <= 3:
            sel[:, 4 + j + 1] = 1.0
        m["selh"] = sel
        maps.append(m)
    if "B" not in _FUSED:
        _FUSED["B"] = build_fused()
    r = _run(_FUSED["B"], maps)
    out = np.empty_like(x)
    for cid in range(8):
        b, j = cid // 4, cid % 4
        out[b, TOK * j:TOK * (j + 1)] = np.asarray(r[cid]["xo"], np.float32)
    return out
```

```python
import contextlib
import math
import numpy as np
import ml_dtypes
import concourse.bass as bass
import concourse.mybir as mybir
from concourse.bass_utils import run_bass_kernel_spmd

F32 = mybir.dt.float32
BF16 = mybir.dt.bfloat16
AF = mybir.ActivationFunctionType
ALU = mybir.AluOpType
AX = mybir.AxisListType

ENGS = ("pe", "act", "dve", "pool", "sp")
SEM_EPOCH = 30000
SAME_ENGINE_SYNC = True
DEBUG_SCRATCH = False

D = 1024
SEQ = 16384
NB = 2
TOK = 4096
NTILE = 32
CTX = 256
Q_OFF, K_OFF, V_OFF, F_OFF, CX_OFF, CB_OFF, CC_OFF, GATE_OFF = 0, 512, 640, 768, 1280, 1792, 2304, 2816
IN_DIM = 5888
D_FF = 2816
NEXP = 8
D_EXP = 3584
EPS = 1e-6
PA_F, PA_K, PA_V, PA_CX, PA_CC = 0, 512, 640, 768, 1280
PA_W = 1792
PM_Q, PM_CB, PM_G = 1792, 2304, 2816


class Buf:
    __slots__ = ("name", "last_write", "reads", "dkey")

    def __init__(self, name):
        self.name = name
        self.last_write = None
        self.reads = {}
        self.dkey = None


class Tile:
    __slots__ = ("ap", "b")

    def __init__(self, ap, b):
        self.ap = ap
        self.b = b


class _Rec:
    def __init__(self):
        self.calls = []

    def __getattr__(self, name):
        def f(*a, **kw):
            self.calls.append((name, a, kw))
            return None
        return f


class Prog:
    def __init__(self, nc):
        self.nc = nc
        self.stack = contextlib.ExitStack()
        self.q = {e: [] for e in ENGS}
        self.cnt = {e: 0 for e in ENGS}
        self.epoch = {e: 0 for e in ENGS}
        self.pending = {e: False for e in ENGS}
        self.last_tok = {e: None for e in ENGS}
        self.dcnt = {}
        self.waited = {e: {} for e in ENGS}
        self.semkeys = []
        self.sems = {}
        self.nbuf = 0
        self.out_tokens = []
        self.free_dkeys = {"sp": [], "pool": [], "act": []}
        self.dma_bufs = []

    def sbuf(self, name, shape, dtype):
        return self.stack.enter_context(self.nc.sbuf_tensor(name, list(shape), dtype))

    def psum(self, name, shape, dtype=F32):
        return self.stack.enter_context(self.nc.psum_tensor(name, list(shape), dtype))

    def buf(self, name=None):
        self.nbuf += 1
        return Buf(name or f"b{self.nbuf}")

    def _semkey(self, k):
        if k not in self.sems:
            self.sems[k] = None
            self.semkeys.append(k)
        return k

    @staticmethod
    def _deps(reads, writes):
        deps = []
        for b in reads:
            if b.last_write is not None:
                deps.append(b.last_write)
        for b in writes:
            if b.last_write is not None:
                deps.append(b.last_write)
            deps.extend(b.reads.items())
        return deps

    def _emit_waits(self, eng, deps):
        need = {}
        for (k, v) in deps:
            if k[0] == eng and (eng == "pe" or not SAME_ENGINE_SYNC):
                continue
            if v > need.get(k, 0):
                need[k] = v
        w = self.waited[eng]
        for k, v in need.items():
            if w.get(k, 0) >= v:
                continue
            w[k] = v
            self._semkey(k)
            self.q[eng].append(("wait", k, v))

    @staticmethod
    def _mark(tok, reads, writes):
        k, v = tok
        for b in reads:
            if b.reads.get(k, 0) < v:
                b.reads[k] = v
        for b in writes:
            b.last_write = tok
            b.reads = {}

    def op(self, eng, fn, reads=(), writes=(), signal=True):
        reads = [t.b if isinstance(t, Tile) else t for t in reads]
        writes = [t.b if isinstance(t, Tile) else t for t in writes]
        self._emit_waits(eng, self._deps(reads, writes))
        if self.cnt[eng] >= SEM_EPOCH and signal and not self.pending[eng]:
            self.epoch[eng] += 1
            self.cnt[eng] = 0
        self.pending[eng] = not signal
        key = (eng, self.epoch[eng])
        self._semkey(key)
        if signal:
            self.cnt[eng] += 1
            tok = (key, self.cnt[eng])
        else:
            tok = (key, self.cnt[eng] + 1)
        rec = _Rec()
        fn(rec)
        assert len(rec.calls) == 1
        self.q[eng].append(("op", rec.calls[0], key if signal else None))
        self._mark(tok, reads, writes)
        self.last_tok[eng] = tok
        return tok

    def dma(self, qeng, out, in_, reads=(), writes=(), is_output=False):
        reads = [t.b if isinstance(t, Tile) else t for t in reads]
        writes = [t.b if isinstance(t, Tile) else t for t in writes]
        self._emit_waits(qeng, self._deps(reads, writes))
        sb = writes[0] if writes else reads[0]
        if sb.dkey is None or sb.dkey[2] != qeng or self.dcnt.get(sb.dkey, 0) >= SEM_EPOCH:
            fl = self.free_dkeys[qeng]
            while fl and self.dcnt.get(fl[-1], 0) >= SEM_EPOCH:
                fl.pop()
            if fl:
                sb.dkey = fl.pop()
            else:
                self.nbuf += 1
                sb.dkey = ("dma", self.nbuf, qeng)
            self.dma_bufs.append(sb)
        k = sb.dkey
        self._semkey(k)
        self.dcnt[k] = self.dcnt.get(k, 0) + 16
        tok = (k, self.dcnt[k])
        self.q[qeng].append(("dma", (out, in_), k))
        self._mark(tok, reads, writes)
        if is_output:
            self.out_tokens.append(tok)
        return tok

    def cc_allgather(self, out, in_, groups, reads=(), writes=()):
        reads = [t.b if isinstance(t, Tile) else t for t in reads]
        writes = [t.b if isinstance(t, Tile) else t for t in writes]
        self._emit_waits("pool", self._deps(reads, writes))
        self.nbuf += 1
        k = ("cc", self.nbuf)
        self._semkey(k)
        self.dcnt[k] = 1
        tok = (k, 1)
        self.q["pool"].append(("cc", (out, in_, groups), k))
        self._mark(tok, reads, writes)
        return tok

    def barrier(self):
        toks = [t for t in self.last_tok.values() if t is not None]
        toks += [(k, v) for k, v in self.dcnt.items()]
        for e in ENGS:
            self._emit_waits(e, [t for t in toks if not (t[0][0] == e and e in ("pe", "sp", "pool"))])
        seen = set()
        for k in list(self.dcnt.keys()):
            if k[0] == "dma" and k not in seen and all(k not in fl for fl in self.free_dkeys.values()):
                seen.add(k)
                self.free_dkeys[k[2]].append(k)
        for b in self.dma_bufs:
            b.dkey = None
        self.dma_bufs = []

    def finish(self):
        self._emit_waits("sp", self.out_tokens)
        nc = self.nc
        for i, k in enumerate(self.semkeys):
            self.sems[k] = self.stack.enter_context(nc.semaphore(f"s{i}_{k[0]}"))
        sems = self.sems
        q = self.q

        def replay(eng_name):
            def body(e):
                for item in q[eng_name]:
                    if item[0] == "wait":
                        e.wait_ge(sems[item[1]], item[2])
                    elif item[0] == "op":
                        nm, a_, kw_ = item[1]
                        ins = getattr(e, nm)(*a_, **kw_)
                        if item[2] is not None:
                            ins.then_inc(sems[item[2]], 1)
                    elif item[0] == "cc":
                        o, i_, grp = item[1]
                        e.collective_compute("AllGather", ALU.bypass, replica_groups=grp, ins=[i_.opt()], outs=[o.opt()]).then_inc(sems[item[2]], 1)
                    else:
                        o, i_ = item[1]
                        e.dma_start(out=o, in_=i_).then_inc(sems[item[2]], 16)
            return body

        with nc.Block() as block:
            block.tensor(replay("pe"))
            block.scalar(replay("act"))
            block.vector(replay("dve"))
            block.gpsimd(replay("pool"))
            block.sync(replay("sp"))
        self.stack.close()

    def stats(self):
        return {e: len(self.q[e]) for e in ENGS}, len(self.semkeys)


def _prod(s):
    r = 1
    for x in s:
        r *= x
    return r


class Arena:
    def __init__(self, P, nwords):
        self.P = P
        self.t = P.sbuf("arena", [128, nwords], F32)
        self.nwords = nwords
        self.top = 0
        self.peak = 0

    def alloc(self, name, shape, dtype):
        n = _prod(shape)
        nbytes = n * (4 if dtype == F32 else 2)
        words = ((nbytes + 31) // 32) * 8
        off = self.top
        self.top += words
        self.peak = max(self.peak, self.top)
        assert self.top <= self.nwords, f"arena overflow at {name}: {self.top} > {self.nwords}"
        v = self.t[:, off:off + words]
        if dtype != F32:
            v = v.bitcast(dtype)
        v = v[:, :n]
        if len(shape) == 2:
            v = v.rearrange("p (a b) -> p a b", a=shape[0])
        elif len(shape) == 3:
            v = v.rearrange("p (a b c) -> p a b c", a=shape[0], b=shape[1])
        elif len(shape) == 4:
            v = v.rearrange("p (a b c d) -> p a b c d", a=shape[0], b=shape[1], c=shape[2])
        return Tile(v, self.P.buf(name))

    def ring(self, name, n, shape, dtype):
        return Ring([self.alloc(f"{name}{i}", shape, dtype) for i in range(n)])

    def mark(self):
        return self.top

    def release(self, m):
        self.top = m


class Ring:
    def __init__(self, tiles):
        self.tiles = tiles
        self.i = 0

    def next(self):
        t = self.tiles[self.i % len(self.tiles)]
        self.i += 1
        return t


class Builder:
    def __init__(self, kind, layer):
        self.kind = kind
        self.layer = layer
        self.last = layer == 1
        self.sfx = ""
        self.nc = bass.Bass("TRN2", target_bir_lowering=False)
        self.P = Prog(self.nc)
        self.din = {}
        self.A = Arena(self.P, 52000)
        banks = [self.P.psum(f"ps{i}", [128, 512], F32) for i in range(8)]
        self.ps = Ring([Tile(b[:], self.P.buf(f"ps{i}")) for i, b in enumerate(banks)])
        self.pending = []

    def inp(self, name, shape, dtype=F32):
        if name in self.din:
            return self.din[name]
        t = self.nc.dram_tensor(name, list(shape), dtype, kind="ExternalInput").ap()
        self.din[name] = t
        return t

    def outp(self, name, shape, dtype=F32):
        return self.nc.dram_tensor(name, list(shape), dtype, kind="ExternalOutput").ap()

    def scratch(self, name, shape, dtype):
        kind = "ExternalOutput" if DEBUG_SCRATCH else "Internal"
        return self.nc.dram_tensor(name, list(shape), dtype, kind=kind).ap()

    def dl(self, name):
        return self.din[name + self.sfx]

    def load(self, tile, src, q="sp", dep=None):
        self.P.dma(q, tile.ap, src, reads=([dep] if dep is not None else []), writes=[tile])

    def const_tile(self, name, shape, dtype, src, q=None):
        t = self.A.alloc(name, shape, dtype)
        self.load(t, src, q or ("pool" if dtype != F32 else "sp"))
        return t

    def ps_bf16(self, bank, shape):
        v = bank.ap.bitcast(BF16)
        if len(shape) == 2:
            return v.rearrange("p (a b) -> p a b", a=shape[0])
        return v

    def emit_mod(self):
        P, A, nc = self.P, self.A, self.nc
        ccols = self.din["ccols"] if "ccols" in self.din else self.inp("ccols", [128, 8, 2])
        wmod = self.inp("wmod" + self.sfx, [D, 6 * D])
        bcols = self.inp("bcols" + self.sfx, [128, 48])
        brow = self.inp("brow" + self.sfx, [128, 2, D])
        gpm = self.inp("gpm_c" + self.sfx, [128, 8])
        gpf = self.inp("gpf_c" + self.sfx, [128, 8])
        gqm = self.inp("gqm_r" + self.sfx, [128, D])
        gqf = self.inp("gqf_r" + self.sfx, [128, D])
        self.alloc_mod_tiles()
        m0 = A.mark()
        cc = A.alloc("cc", [8, 2], F32)
        self.load(cc, ccols)
        bc = A.alloc("bc", [48], F32)
        self.load(bc, bcols)
        br = A.alloc("br", [2, D], F32)
        self.load(br, brow)
        gq = [A.alloc("gqm", [D], F32), A.alloc("gqf", [D], F32)]
        self.load(gq[0], gqm)
        self.load(gq[1], gqf)
        gp = [A.alloc("gpm", [8], F32), A.alloc("gpf", [8], F32)]
        self.load(gp[0], gpm)
        self.load(gp[1], gpf)
        sc = A.alloc("silu_c", [8, 2], F32)
        P.op("act", lambda e: e.activation(out=sc.ap, in_=cc.ap, func=AF.Silu), reads=[cc], writes=[sc])
        srep = A.alloc("srep", [8, 2, 128], F32)
        P.op("dve", lambda e: e.tensor_copy(out=srep.ap, in_=sc.ap.unsqueeze(3).broadcast_to([128, 8, 2, 128])),
             reads=[sc], writes=[srep])
        wring = A.ring("wm", 2, [8, 512], F32)
        modr = A.alloc("modr", [48, 2], F32)
        P.op("dve", lambda e: e.memset(modr.ap, 0.0), writes=[modr])
        for part in range(6):
            for hf in range(2):
                wm = wring.next()
                c0 = part * 1024 + hf * 512
                self.load(wm, wmod[:, c0:c0 + 512].rearrange("(k p) n -> p k n", p=128))
                if part in (2, 5):
                    gi = 0 if part == 2 else 1
                    for s in range(2):
                        pr = self.ps.next()
                        for k in range(8):
                            P.op("pe", lambda e, k=k, s=s, pr=pr, wm=wm: e.matmul(
                                out=pr.ap, lhsT=srep.ap[:, k, s, :], rhs=wm.ap[:, k, :], start=(k == 0), stop=(k == 7)),
                                reads=[srep, wm], writes=[pr], signal=(k == 7))
                        dst = self.gaG[s][gi]
                        cs = slice(hf * 512, hf * 512 + 512)
                        P.op("dve", lambda e, pr=pr, dst=dst, cs=cs, gi=gi: e.tensor_tensor(
                            out=dst.ap[:, cs], in0=pr.ap, in1=br.ap[:, gi, cs], op=ALU.add), reads=[pr, br], writes=[dst])
                        P.op("dve", lambda e, dst=dst, cs=cs, gi=gi: e.tensor_tensor(
                            out=dst.ap[:, cs], in0=dst.ap[:, cs], in1=gq[gi].ap[:, cs], op=ALU.mult),
                            reads=[dst, gq[gi]], writes=[dst])
                else:
                    pcb = self.ps.next()
                    pcbv = pcb.ap.rearrange("p (m n) -> p m n", m=4)
                    for m4 in range(4):
                        for k in range(8):
                            P.op("pe", lambda e, k=k, m4=m4, wm=wm, pcbv=pcbv: e.matmul(
                                out=pcbv[:, m4, :].rearrange("p (s n) -> p s n", s=2), lhsT=wm.ap[:, k, m4 * 128:(m4 + 1) * 128],
                                rhs=srep.ap[:, k, :, 0:64], start=(k == 0), stop=(k == 7)), reads=[wm, srep], writes=[pcb],
                                signal=(k == 7 and m4 == 3))
                    m_0 = part * 8 + hf * 4
                    P.op("dve", lambda e, m_0=m_0, pcbv=pcbv: e.tensor_copy(
                        out=modr.ap[:, m_0:m_0 + 4, :], in_=pcbv.rearrange("p m (s n) -> p m s n", s=2)[:, :, :, 0]),
                        reads=[pcb], writes=[modr])
        modc = A.alloc("modc", [48, 2], F32)
        self.dbg_modc = modc
        P.op("dve", lambda e: e.tensor_tensor(out=modc.ap, in0=modr.ap, in1=bc.ap.unsqueeze(2).broadcast_to([128, 48, 2]),
                                              op=ALU.add), reads=[modr, bc], writes=[modc])
        for s in range(2):
            for sub in range(2):
                shp, scp = (0, 1) if sub == 0 else (3, 4)
                P.op("dve", lambda e, s=s, sub=sub, shp=shp: e.tensor_copy(
                    out=self.shA.ap[:, s, sub, :], in_=modc.ap[:, shp * 8:shp * 8 + 8, s]), reads=[modc], writes=[self.shA])
                P.op("dve", lambda e, s=s, sub=sub, scp=scp: e.scalar_tensor_tensor(
                    out=self.scA.ap[:, s, sub, :], in0=modc.ap[:, scp * 8:scp * 8 + 8, s], scalar=1.0,
                    in1=gp[sub].ap, op0=ALU.add, op1=ALU.mult), reads=[modc, gp[sub]], writes=[self.scA])
        if getattr(self, "dbg_out", None) is not None:
            P.dma("sp", self.dbg_out, modc.ap.rearrange("p a b -> p (a b)"), reads=[modc], writes=[P.buf("dbgo")], is_output=True)
        P.barrier()
        A.release(m0)

    def emit_consts(self):
        A = self.A
        ident = self.inp("ident", [128, 128])
        self.ident = self.const_tile("ident", [128], BF16, ident)
        if self.kind == "main":
            self.rt = self.const_tile("rt", [128], BF16, self.inp("rt", [128, 128]))
            masks = self.inp("masks", [128, 4, 128])
            self.masks = self.const_tile("masks", [4, 128], BF16, masks)
            self.valid = self.const_tile("valid", [2], F32, self.inp("valid", [128, 2]))
            if self.sfx == "":
                self.emit_layer_consts()
            self.cbsb = self.const_tile("cbsb", [2, 128], BF16, self.inp("cbsb", [128, 2, 128]))

    def alloc_mod_tiles(self):
        A = self.A
        if not hasattr(self, "scA"):
            self.scA = A.alloc("scA", [2, 2, 8], F32)
            self.shA = A.alloc("shA", [2, 2, 8], F32)
            self.gaG = [[A.alloc(f"gaG{s}{g}", [D], F32) for g in range(2)] for s in range(2)]

    def emit_layer_consts(self):
        A = self.A
        es = self.const_tile("esink_src", [8], F32, self.inp("sink_b" + self.sfx, [128, 8]))
        self.esink = A.alloc("esink", [8], F32)
        self.P.op("act", lambda e: e.activation(out=self.esink.ap, in_=es.ap, func=AF.Exp), reads=[es], writes=[self.esink])
        self.wconv = self.const_tile("wconv", [4, 3], F32, self.inp("wconv_c" + self.sfx, [128, 4, 3]))

    def make_hT_dep(self, tiles_src, hT, s, sub, rings, dep):
        return self.make_hT(tiles_src, hT, s, sub, rings, dep)

    def make_hT(self, tiles_src, hT, s, sub, rings, dep=None, defer=False):
        for i, src in enumerate(tiles_src):
            fn = (lambda i=i, src=src: self._hT_tile(i, src, hT, s, sub, rings, dep))
            if defer:
                self.pending.append(fn)
            else:
                fn()

    def tick(self, n=1):
        for _ in range(n):
            if self.pending:
                self.pending.pop(0)()

    def flush(self):
        while self.pending:
            self.pending.pop(0)()

    def _hT_tile(self, i, src, hT, s, sub, rings, dep):
        P = self.P
        xr, xnr, sqr, smr = rings
        xt = xr.next()
        if isinstance(src, tuple):
            self.load(xt, src[0], dep=src[1])
        else:
            self.load(xt, src, dep=dep)
        sq = sqr.next()
        sm = smr.next()
        P.op("act", lambda e: e.activation(out=sq.ap, in_=xt.ap, func=AF.Square, accum_out=sm.ap[:, 0:1]), reads=[xt], writes=[sq, sm])
        P.op("act", lambda e: e.activation(out=sm.ap[:, 1:2], in_=sm.ap[:, 0:1], func=AF.Sqrt, scale=1.0 / D, bias=EPS), reads=[sm], writes=[sm])
        P.op("dve", lambda e: e.reciprocal(out=sm.ap[:, 1:2], in_=sm.ap[:, 1:2]), reads=[sm], writes=[sm])
        xn = xnr.next()
        P.op("dve", lambda e: e.tensor_scalar(out=xn.ap, in0=xt.ap, scalar1=sm.ap[:, 1:2], scalar2=None, op0=ALU.mult), reads=[xt, sm], writes=[xn])
        pt = self.ps.next()
        ptv = self.ps_bf16(pt, [8, 128])
        for k in range(8):
            P.op("pe", lambda e, k=k: e.transpose(out=ptv[:, k, :], in_=xn.ap[:, k * 128:(k + 1) * 128], identity=self.ident.ap),
                 reads=[xn, self.ident], writes=[pt], signal=(k == 7))
        for k in range(4):
            P.op("act", lambda e, k=k: e.activation(
                out=hT.ap[:, k, i * 128:(i + 1) * 128], in_=ptv[:, k, :], func=AF.Identity,
                bias=self.shA.ap[:, s, sub, k:k + 1], scale=self.scA.ap[:, s, sub, k:k + 1]),
                reads=[pt, self.shA, self.scA], writes=[hT])
        for k in range(4, 8):
            P.op("dve", lambda e, k=k: e.scalar_tensor_tensor(
                out=hT.ap[:, k, i * 128:(i + 1) * 128], in0=ptv[:, k, :], scalar=self.scA.ap[:, s, sub, k:k + 1],
                in1=self.shA.ap[:, s, sub, k:k + 1].broadcast_to([128, 128]), op0=ALU.mult, op1=ALU.add),
                reads=[pt, self.shA, self.scA], writes=[hT])

    def norm_rings(self, nx=4):
        A = self.A
        return (A.ring("xt", nx, [D], F32), A.ring("xn", 2, [D], BF16), A.ring("sq", 1, [D], BF16), A.ring("sm", 4, [2], F32))

    def proj_fm(self, w, col0, hT, ntok, nk=8):
        P = self.P
        pr = self.ps.next()
        for k in range(nk):
            P.op("pe", lambda e, k=k, pr=pr: e.matmul(out=pr.ap[:, :ntok], lhsT=w.ap[:, k, col0:col0 + 128], rhs=hT.ap[:, k, :ntok],
                                                   start=(k == 0), stop=(k == nk - 1)), reads=[w, hT], writes=[pr], signal=(k == nk - 1))
        return pr

    def rope(self, pr, raw, dst_ap, dst_tile, cosT, sinT, ntok, tmp):
        P = self.P
        P.op("act", lambda e: e.copy(out=raw.ap[:, :ntok], in_=pr.ap[:, :ntok]), reads=[pr], writes=[raw])
        p2 = self.ps.next()
        P.op("pe", lambda e: e.matmul(out=p2.ap[:, :ntok], lhsT=self.rt.ap, rhs=raw.ap[:, :ntok], start=True, stop=True),
             reads=[self.rt, raw], writes=[p2])
        t1, t2 = tmp
        P.op("dve", lambda e: e.tensor_tensor(out=t1.ap[:, :ntok], in0=pr.ap[:, :ntok], in1=cosT, op=ALU.mult), reads=[pr, self.ropeb, raw], writes=[t1])
        P.op("dve", lambda e: e.tensor_tensor(out=t2.ap[:, :ntok], in0=p2.ap[:, :ntok], in1=sinT, op=ALU.mult), reads=[p2, self.ropeb], writes=[t2])
        P.op("dve", lambda e: e.tensor_tensor(out=dst_ap, in0=t1.ap[:, :ntok], in1=t2.ap[:, :ntok], op=ALU.add), reads=[t1, t2], writes=[dst_tile])

    def emit_phaseA(self, xh, want_zf, zf_out=None):
        P, A = self.P, self.A
        main = self.kind == "main"
        winP = self.dl("winP")
        m0 = A.mark()
        ncolA = PA_W if main else 512
        wA = A.alloc("wA", [8, ncolA], BF16)
        self.load(wA, winP[:, 0:ncolA].rearrange("(k p) n -> p k n", p=128), "pool")
        rings = self.norm_rings(4)
        hTr = A.ring("hT", 2, [8, 512], BF16)
        zfr = A.ring("zfs", 2, [512], BF16)
        if main:
            ropeC = self.din["ropeC"]
            ropeS = self.din["ropeS"]
            rope_r = A.ring("ropeCS", 2, [2, 512], F32)
            kraw = A.alloc("kraw", [512], BF16)
            tmp = (A.alloc("rt1", [512], F32), A.alloc("rt2", [512], F32))
            cxs = A.ring("cxs", 2, [512], F32)
            ust = A.ring("ust", 2, [4, 512], BF16)
        blocks = []
        if main:
            blocks.append((-1, 1))
        for bi in range(8):
            blocks.append((bi * 4, 4))
        if main:
            blocks.append((32, 1))
        def _mk(bidx, defer):
            t0_, nt_ = blocks[bidx]
            hT_ = hTr.next()
            if xh is None:
                srcs_ = [self.xtile(t0_ + i) for i in range(nt_)]
            else:
                srcs_ = [xh[(t0_ + 1 + i) * 128:(t0_ + 2 + i) * 128, :] for i in range(nt_)]
            self.make_hT(srcs_, hT_, 0, 0, rings, defer=defer)
            return hT_
        hT_next = _mk(0, False)
        for bidx, (t0, nt) in enumerate(blocks):
            ntok = nt * 128
            halo = nt == 1
            self.flush()
            hT = hT_next
            if bidx + 1 < len(blocks):
                hT_next = _mk(bidx + 1, True)
            if want_zf and not halo:
                for i in range(nt):
                    pr = self.ps.next()
                    for k in range(8):
                        P.op("pe", lambda e, k=k, i=i, pr=pr, hT=hT: e.matmul(out=pr.ap, lhsT=hT.ap[:, k, i * 128:(i + 1) * 128],
                                                                     rhs=wA.ap[:, k, PA_F:PA_F + 512], start=(k == 0), stop=(k == 7)),
                             reads=[hT, wA], writes=[pr], signal=(k == 7))
                    zs = zfr.next()
                    P.op("act", lambda e, pr=pr, zs=zs: e.copy(out=zs.ap, in_=pr.ap), reads=[pr], writes=[zs])
                    tg = t0 + i
                    if i % 2 == 1:
                        self.tick()
                    if isinstance(zf_out, list):
                        for g_ in range(4):
                            P.dma("pool", zf_out[g_][tg * 128:(tg + 1) * 128, :], zs.ap[:, g_ * 128:(g_ + 1) * 128], reads=[zs], writes=[self.zf_b])
                    else:
                        P.dma("sp", zf_out[:, tg * 128:(tg + 1) * 128, :].rearrange("g t c -> t g c"),
                              zs.ap.rearrange("p (g c) -> p g c", g=4), reads=[zs], writes=[self.zf_b], is_output=(self.kind == "pre"))
            if not main:
                continue
            c0 = (t0 + 1) * 128
            rp = rope_r.next()
            P.dma("sp", rp.ap[:, 0, :ntok], ropeC[:, c0:c0 + ntok], writes=[rp])
            P.dma("sp", rp.ap[:, 1, :ntok], ropeS[:, c0:c0 + ntok], writes=[rp])
            self.ropeb = rp
            pr = self.proj_fm(wA, PA_K, hT, ntok)
            self.rope(pr, kraw, self.kT.ap[:, c0:c0 + ntok], self.kT, rp.ap[:, 0, :ntok], rp.ap[:, 1, :ntok], ntok, tmp)
            pv = self.ps.next()
            pvv = pv.ap.rearrange("p (i c) -> p i c", i=4)
            for i in range(nt):
                for k in range(8):
                    P.op("pe", lambda e, k=k, i=i, hT=hT: e.matmul(out=pvv[:, i, :], lhsT=hT.ap[:, k, i * 128:(i + 1) * 128],
                                                                 rhs=wA.ap[:, k, PA_V:PA_V + 128], start=(k == 0), stop=(k == 7)),
                         reads=[hT, wA], writes=[pv], signal=(k == 7 and i == nt - 1))
            P.op("act", lambda e, t0=t0, nt=nt: e.copy(
                out=self.vaug.ap[:, t0 + 1:t0 + 1 + nt, :, 0:64],
                in_=pvv[:, 0:nt, :].rearrange("p i (g d) -> p i g d", g=2)), reads=[pv], writes=[self.vaug])
            self.tick()
            us = ust.next()
            for m in range(4):
                if m == 2:
                    self.tick()
                px = self.proj_fm(wA, PA_CX + m * 128, hT, ntok)
                cx = cxs.next()
                P.op("act", lambda e, px=px, cx=cx: e.copy(out=cx.ap[:, :ntok], in_=px.ap[:, :ntok]), reads=[px], writes=[cx])
                pc = self.proj_fm(wA, PA_CC + m * 128, hT, ntok)
                if halo:
                    vi = 0 if t0 < 0 else 1
                    P.op("dve", lambda e, m=m, pc=pc, cx=cx, us=us, vi=vi: e.scalar_tensor_tensor(
                        out=us.ap[:, m, :ntok], in0=cx.ap[:, :ntok], scalar=self.valid.ap[:, vi:vi + 1], in1=pc.ap[:, :ntok],
                        op0=ALU.mult, op1=ALU.mult), reads=[cx, pc, self.valid], writes=[us])
                else:
                    P.op("dve", lambda e, m=m, pc=pc, cx=cx, us=us: e.tensor_tensor(
                        out=us.ap[:, m, :ntok], in0=cx.ap[:, :ntok], in1=pc.ap[:, :ntok], op=ALU.mult), reads=[cx, pc], writes=[us])
            P.dma("pool", self.u_d[:, :, c0:c0 + ntok], us.ap[:, :, :ntok], reads=[us], writes=[self.u_b])
        self.flush()
        P.barrier()
        A.release(m0)

    def emit_fft(self, zfg):
        P, A = self.P, self.A
        m0 = A.mark()
        t1 = self.const_tile("t1", [2, 2, 64], BF16, self.inp("t1", [128, 2, 2, 64]))
        etab_d = self.inp("etab", [128, 128, 96])
        et = A.alloc("etab", [128, 96], BF16)
        for q4 in range(4):
            P.dma("pool", et.ap[:, q4 * 32:(q4 + 1) * 32, :], etab_d[:, q4 * 32:(q4 + 1) * 32, :], writes=[et])
        Ur = A.ring("U", 1, [128, 128], BF16)
        Y = A.alloc("Y", [128, 2, 64], BF16)
        G = A.alloc("G", [2, 4096], BF16)
        FTs = A.ring("FTs", 2, [4096], BF16)
        scale = 1.0 / math.sqrt(SEQ * 128.0)
        ev = 0
        for g in range(4):
            U = Ur.next()
            for r in range(4):
                zsrc_ = zfg(r, g) if callable(zfg) else zfg[r, g]
                P.dma("sp", U.ap[r * 32:(r + 1) * 32, :, :], zsrc_.rearrange("(th tl) c -> th tl c", tl=128),
                      reads=([self.zfg_b] if getattr(self, "zfg_b", None) is not None else []), writes=[U])
            for hh in range(2):
                for c4 in range(32):
                    pr = self.ps.next()
                    prv = pr.ap.rearrange("p (c x) -> p c x", c=4)
                    for ci in range(4):
                        c = c4 * 4 + ci
                        P.op("pe", lambda e, c=c, ci=ci, hh=hh, prv=prv: e.matmul(
                            out=prv[:, ci, :], lhsT=U.ap[:, :, c], rhs=t1.ap[:, hh, :, :].rearrange("p r k -> p (r k)"),
                            start=True, stop=True), reads=[U, t1], writes=[pr], signal=(ci == 3))
                    eng = "act" if ev % 2 == 0 else "dve"
                    ev += 1
                    dst = Y.ap[:, c4 * 4:(c4 + 1) * 4, :, :].rearrange("p c r k -> p c (r k)")
                    if eng == "act":
                        P.op("act", lambda e, dst=dst, prv=prv: e.copy(out=dst, in_=prv), reads=[pr], writes=[Y])
                    else:
                        P.op("dve", lambda e, dst=dst, prv=prv: e.tensor_copy(out=dst, in_=prv), reads=[pr], writes=[Y])
                for k8 in range(8):
                    pr = self.ps.next()
                    prv = pr.ap.rearrange("p (k r x) -> p k r x", k=8, r=2)
                    for ki in range(8):
                        k1l = k8 * 8 + ki
                        k1 = hh * 64 + k1l
                        P.op("pe", lambda e, k1=k1, k1l=k1l, ki=ki, prv=prv: e.matmul(
                            out=prv[:, ki, :, :].rearrange("p r x -> p (r x)"), lhsT=Y.ap[:, :, 0, k1l], rhs=et.ap[:, k1, 32:96],
                            start=True, stop=False), reads=[Y, et], writes=[pr], signal=False)
                        P.op("pe", lambda e, k1=k1, k1l=k1l, ki=ki, prv=prv: e.matmul(
                            out=prv[:, ki, :, :].rearrange("p r x -> p (r x)"), lhsT=Y.ap[:, :, 1, k1l], rhs=et.ap[:, k1, 0:64],
                            start=False, stop=True), reads=[Y, et], writes=[pr], signal=(ki == 7))
                    k10 = hh * 64 + k8 * 8
                    for ri in range(2):
                        dst = G.ap[:, ri, :].rearrange("p (k2 k1) -> p k1 k2", k1=128)[:, k10:k10 + 8, :]
                        if ri == 0:
                            P.op("act", lambda e, dst=dst, prv=prv, ri=ri: e.copy(out=dst, in_=prv[:, :, ri, :]), reads=[pr], writes=[G])
                        else:
                            P.op("dve", lambda e, dst=dst, prv=prv, ri=ri: e.tensor_copy(out=dst, in_=prv[:, :, ri, :]), reads=[pr], writes=[G])
            ft = FTs.next()
            for cb in range(8):
                pr = self.ps.next()
                cs = slice(cb * 512, (cb + 1) * 512)
                P.op("pe", lambda e, pr=pr, cs=cs: e.matmul(out=pr.ap, lhsT=self.cbsb.ap[:, 0, :], rhs=G.ap[:, 0, cs], start=True, stop=False),
                     reads=[self.cbsb, G], writes=[pr], signal=False)
                P.op("pe", lambda e, pr=pr, cs=cs: e.matmul(out=pr.ap, lhsT=self.cbsb.ap[:, 1, :], rhs=G.ap[:, 1, cs], start=False, stop=True),
                     reads=[self.cbsb, G], writes=[pr])
                P.op("act", lambda e, pr=pr, cs=cs, ft=ft: e.activation(out=ft.ap[:, cs], in_=pr.ap, func=AF.Copy, scale=scale), reads=[pr], writes=[ft])
            P.dma("sp", self.ft_d[:, g, :], ft.ap, reads=[ft], writes=[self.ft_b])
        P.barrier()
        A.release(m0)

    def attention_tile(self, qT, qcol0, keyblocks, attnT, acol0, bufs):
        st = self.attention_scores(qT, qcol0, keyblocks, bufs)
        self.attention_pv(st, keyblocks, attnT, acol0, bufs)

    def attention_scores(self, qT, qcol0, keyblocks, bufs):
        P = self.P
        pTr, atok_r, den_r = bufs
        pT = pTr.next()
        for kb, (kap, kt, vap, vt, mi) in enumerate(keyblocks):
            for g in range(2):
                pr = self.ps.next()
                P.op("pe", lambda e, g=g, kap=kap, pr=pr: e.matmul(
                    out=pr.ap.rearrange("p (h q) -> p h q", h=4), lhsT=kap[g * 64:(g + 1) * 64, :],
                    rhs=qT.ap[g * 64:(g + 1) * 64, :, qcol0:qcol0 + 128], start=True, stop=True),
                    reads=[kt, qT], writes=[pr])
                P.op("act", lambda e, g=g, kb=kb, pr=pr, pT=pT: e.activation(
                    out=pT.ap[:, kb, g, :, :], in_=pr.ap.rearrange("p (h q) -> p h q", h=4), func=AF.Exp, scale=0.125),
                    reads=[pr], writes=[pT])
            if mi is not None:
                P.op("dve", lambda e, kb=kb, mi=mi, pT=pT: e.tensor_tensor(
                    out=pT.ap[:, kb, :, :, :].rearrange("p g h q -> p (g h) q"),
                    in0=pT.ap[:, kb, :, :, :].rearrange("p g h q -> p (g h) q"),
                    in1=self.masks.ap[:, mi, :].unsqueeze(1).broadcast_to([128, 8, 128]), op=ALU.mult),
                    reads=[pT, self.masks], writes=[pT])
        return pT

    def attention_pv(self, pT, keyblocks, attnT, acol0, bufs):
        P = self.P
        pTr, atok_r, den_r = bufs
        nkb = len(keyblocks)
        atok = atok_r.next()
        den = den_r.next()
        for b2 in range(2):
            po = self.ps.next()
            pov = po.ap[:, 0:260].rearrange("p (h x) -> p h x", h=4)
            for hh in range(4):
                for kb, (kap, kt, vap, vt, mi) in enumerate(keyblocks):
                    P.op("pe", lambda e, hh=hh, kb=kb, vap=vap, pov=pov, b2=b2, pT=pT: e.matmul(
                        out=pov[:, hh, :], lhsT=pT.ap[:, kb, b2, hh, :], rhs=vap[:, b2, :], start=(kb == 0), stop=(kb == nkb - 1)),
                        reads=[pT, vt], writes=[po], signal=(hh == 3 and kb == nkb - 1))
            hs = slice(b2 * 4, b2 * 4 + 4)
            P.op("dve", lambda e, pov=pov, hs=hs, den=den: e.tensor_tensor(out=den.ap[:, 0, hs], in0=pov[:, :, 64], in1=self.esink.ap[:, hs], op=ALU.add),
                 reads=[po, self.esink], writes=[den])
            P.op("dve", lambda e, hs=hs, den=den: e.reciprocal(out=den.ap[:, 1, hs], in_=den.ap[:, 0, hs]), reads=[den], writes=[den])
            P.op("dve", lambda e, pov=pov, hs=hs, den=den, atok=atok: e.tensor_tensor(
                out=atok.ap[:, hs, :], in0=pov[:, :, 0:64], in1=den.ap[:, 1, hs].unsqueeze(2).broadcast_to([128, 4, 64]), op=ALU.mult),
                reads=[po, den], writes=[atok])
        pt = self.ps.next()
        ptv = self.ps_bf16(pt, [8, 128])
        av = atok.ap.rearrange("p h d -> p (h d)")
        for kc in range(4):
            P.op("pe", lambda e, kc=kc, ptv=ptv: e.transpose(out=ptv[:, kc, :], in_=av[:, kc * 128:(kc + 1) * 128], identity=self.ident.ap),
                 reads=[atok, self.ident], writes=[pt], signal=(kc == 3))
        P.op("act", lambda e, ptv=ptv: e.copy(out=attnT.ap[:, 0:4, acol0:acol0 + 128], in_=ptv[:, 0:4, :]), reads=[pt], writes=[attnT])

    def mix_block(self, hT, ntok, qT, attnT, uap, u_tile, FTap, ft_tile, wr, work, x_tiles_src, x_out_dst, s, xout_buf, is_out=False):
        P = self.P
        winP = self.dl("winP")
        (yconvT, gsb_r, tmp_r, mergedT, mixt_r, sm_r, xres_r, sq_r) = work
        wcb = wr.next()
        self.load(wcb, winP[:, PM_CB:PM_CB + 512].rearrange("(k p) n -> p k n", p=128), "pool")
        for m in range(4):
            pb = self.proj_fm(wcb, m * 128, hT, ntok)
            t = tmp_r.next()
            P.op("dve", lambda e, m=m, t=t: e.tensor_scalar(out=t.ap[:, :ntok], in0=uap[:, m, 0:ntok], scalar1=self.wconv.ap[:, m, 0:1], scalar2=None, op0=ALU.mult),
                 reads=[u_tile, self.wconv], writes=[t])
            P.op("dve", lambda e, m=m, t=t: e.scalar_tensor_tensor(out=t.ap[:, :ntok], in0=uap[:, m, 1:ntok + 1], scalar=self.wconv.ap[:, m, 1:2],
                                                                 in1=t.ap[:, :ntok], op0=ALU.mult, op1=ALU.add), reads=[u_tile, self.wconv, t], writes=[t])
            P.op("dve", lambda e, m=m, t=t: e.scalar_tensor_tensor(out=t.ap[:, :ntok], in0=uap[:, m, 2:ntok + 2], scalar=self.wconv.ap[:, m, 2:3],
                                                                 in1=t.ap[:, :ntok], op0=ALU.mult, op1=ALU.add), reads=[u_tile, self.wconv, t], writes=[t])
            P.op("dve", lambda e, m=m, t=t, pb=pb: e.tensor_tensor(out=yconvT.ap[:, m, :ntok], in0=t.ap[:, :ntok], in1=pb.ap[:, :ntok], op=ALU.mult),
                 reads=[t, pb], writes=[yconvT])
        self.tick()
        if getattr(self, "mix_stop", None) == "conv":
            return
        wbr = self.wbr
        srcs = [(attnT.ap, attnT), (FTap, ft_tile), (yconvT.ap, yconvT)]
        for m in range(8):
            if m in (2, 5):
                self.tick()
            wg = wr.next()
            P.dma("pool", wg.ap[:, :, 0:384], winP[:, PM_G + m * 384:PM_G + (m + 1) * 384].rearrange("(k p) n -> p k n", p=128), writes=[wg])
            acc = None
            for r in range(3):
                pg = self.proj_fm(wg, r * 128, hT, ntok)
                gs = gsb_r.next()
                P.op("act", lambda e, pg=pg, gs=gs: e.activation(out=gs.ap[:, :ntok], in_=pg.ap[:, :ntok], func=AF.Sigmoid), reads=[pg], writes=[gs])
                sap, st = srcs[r]
                py = self.ps.next()
                for kc in range(4):
                    P.op("pe", lambda e, kc=kc, r=r, m=m, py=py, sap=sap: e.matmul(
                        out=py.ap[:, :ntok], lhsT=wbr[r].ap[:, kc, m * 128:(m + 1) * 128], rhs=sap[:, kc, 0:ntok], start=(kc == 0), stop=(kc == 3)),
                        reads=[wbr[r], st], writes=[py], signal=(kc == 3))
                if r == 0:
                    acc = tmp_r.next()
                    P.op("dve", lambda e, gs=gs, py=py, acc=acc: e.tensor_tensor(out=acc.ap[:, :ntok], in0=gs.ap[:, :ntok], in1=py.ap[:, :ntok], op=ALU.mult),
                         reads=[gs, py], writes=[acc])
                else:
                    t = tmp_r.next()
                    P.op("dve", lambda e, gs=gs, py=py, t=t: e.tensor_tensor(out=t.ap[:, :ntok], in0=gs.ap[:, :ntok], in1=py.ap[:, :ntok], op=ALU.mult),
                         reads=[gs, py], writes=[t])
                    if r == 1:
                        P.op("dve", lambda e, t=t, acc=acc: e.tensor_tensor(out=acc.ap[:, :ntok], in0=acc.ap[:, :ntok], in1=t.ap[:, :ntok], op=ALU.add),
                             reads=[acc, t], writes=[acc])
                    else:
                        P.op("dve", lambda e, t=t, acc=acc, m=m: e.tensor_tensor(out=mergedT.ap[:, m, :ntok], in0=acc.ap[:, :ntok], in1=t.ap[:, :ntok], op=ALU.add),
                             reads=[acc, t], writes=[mergedT])
        if getattr(self, "mix_stop", None) == "merge":
            return
        wo = self.wo2
        self.post_residual(lambda i, hf: (mergedT, [(mergedT.ap[:, k, i * 128:(i + 1) * 128], wo[hf].ap[:, k, :]) for k in range(8)], [mergedT, wo[hf]]),
                           ntok // 128, x_tiles_src, x_out_dst, self.gaG[s][0], (mixt_r, sm_r, xres_r, sq_r), xout_buf, is_out)

    def post_residual(self, mm_fn, nt, x_tiles_src, x_out_dst, gaG, rings, xout_buf, is_out, from_sbuf=None):
        P = self.P
        mixt_r, sm_r, xres_r, sq_r = rings
        for i in range(nt):
            xres = xres_r.next()
            xs = x_tiles_src[i]
            if isinstance(xs, tuple):
                self.load(xres, xs[0], dep=xs[1])
            else:
                self.load(xres, xs)
            sm = sm_r.next()
            mt = mixt_r.next()
            for hf in range(2):
                cs = slice(hf * 512, (hf + 1) * 512)
                if from_sbuf is None:
                    _, pairs, rd = mm_fn(i, hf)
                    pr = self.ps.next()
                    n = len(pairs)
                    for j, (l, r) in enumerate(pairs):
                        P.op("pe", lambda e, l=l, r=r, j=j, n=n, pr=pr: e.matmul(out=pr.ap, lhsT=l, rhs=r, start=(j == 0), stop=(j == n - 1)),
                             reads=rd, writes=[pr], signal=(j == n - 1))
                    src_ap, src_t = pr.ap, pr
                else:
                    src_ap, src_t = from_sbuf(i)[0][:, cs], from_sbuf(i)[1]
                sq = sq_r.next()
                P.op("act", lambda e, src_ap=src_ap, sq=sq, sm=sm, hf=hf: e.activation(out=sq.ap[:, 0:512], in_=src_ap, func=AF.Square, accum_out=sm.ap[:, hf:hf + 1]),
                     reads=[src_t], writes=[sq, sm])
                P.op("dve", lambda e, src_ap=src_ap, mt=mt, cs=cs: e.tensor_tensor(out=mt.ap[:, cs], in0=src_ap, in1=gaG.ap[:, cs], op=ALU.mult),
                     reads=[src_t, gaG, sq], writes=[mt])
            lvl = getattr(self, "pr_stop", 9)
            if lvl <= 1:
                continue
            P.op("dve", lambda e, sm=sm: e.tensor_tensor(out=sm.ap[:, 2:3], in0=sm.ap[:, 0:1], in1=sm.ap[:, 1:2], op=ALU.add), reads=[sm], writes=[sm])
            P.op("act", lambda e, sm=sm: e.activation(out=sm.ap[:, 3:4], in_=sm.ap[:, 2:3], func=AF.Sqrt, scale=1.0 / D, bias=EPS), reads=[sm], writes=[sm])
            P.op("dve", lambda e, sm=sm: e.reciprocal(out=sm.ap[:, 3:4], in_=sm.ap[:, 3:4]), reads=[sm], writes=[sm])
            if lvl <= 2:
                continue
            P.op("dve", lambda e, sm=sm, mt=mt, xres=xres: e.scalar_tensor_tensor(out=xres.ap, in0=mt.ap, scalar=sm.ap[:, 3:4], in1=xres.ap, op0=ALU.mult, op1=ALU.add),
                 reads=[mt, sm, xres], writes=[xres])
            if lvl <= 3:
                continue
            P.dma("sp", x_out_dst[i], xres.ap, reads=[xres], writes=[xout_buf], is_output=is_out)

    def ffn_dense_block(self, h2T, ntok, wr, work, x_tiles_src, x_out_dst, s, xout_buf, is_out):
        P = self.P
        actT, sg_r, rings = work
        wg_d, wu_d, wd_d = self.dl("wfg"), self.dl("wfu"), self.dl("wfd")
        for j in range(6):
            w = 512 if j < 5 else 256
            wg = wr.next()
            wu = wr.next()
            P.dma("pool", wg.ap[:, :, 0:w], wg_d[:, j * 512:j * 512 + w].rearrange("(k p) n -> p k n", p=128), writes=[wg])
            P.dma("pool", wu.ap[:, :, 0:w], wu_d[:, j * 512:j * 512 + w].rearrange("(k p) n -> p k n", p=128), writes=[wu])
            for mm in range(w // 128):
                pg = self.proj_fm(wg, mm * 128, h2T, ntok)
                pu = self.proj_fm(wu, mm * 128, h2T, ntok)
                sg = sg_r.next()
                P.op("act", lambda e, pg=pg, sg=sg: e.activation(out=sg.ap[:, :ntok], in_=pg.ap[:, :ntok], func=AF.Silu), reads=[pg], writes=[sg])
                kc = j * 4 + mm
                P.op("dve", lambda e, sg=sg, pu=pu, kc=kc: e.tensor_tensor(out=actT.ap[:, kc, :ntok], in0=sg.ap[:, :ntok], in1=pu.ap[:, :ntok], op=ALU.mult),
                     reads=[sg, pu], writes=[actT])
            self.tick()
        nt = ntok // 128
        halves = []
        for hf in range(2):
            slots = []
            for sl in range(3):
                k0 = sl * 8
                nk = min(8, 22 - k0)
                wd = wr.next()
                P.dma("pool", wd.ap[:, 0:nk, :], wd_d[k0 * 128:(k0 + nk) * 128, hf * 512:(hf + 1) * 512].rearrange("(k p) n -> p k n", p=128), writes=[wd])
                slots.append((wd, k0, nk))
            halves.append(slots)

        def mm_fn(i, hf):
            pairs = []
            rd = [actT]
            for (wd, k0, nk) in halves[hf]:
                rd.append(wd)
                for k in range(nk):
                    pairs.append((actT.ap[:, k0 + k, i * 128:(i + 1) * 128], wd.ap[:, k, :]))
            return None, pairs, rd
        self.post_residual(mm_fn, nt, x_tiles_src, x_out_dst, self.gaG[s][1], rings, xout_buf, is_out)

    def ffn_moe_block(self, h2T, wr, work, x_tiles_src, x_out_dst, xout_buf):
        P, A = self.P, self.A
        ntok = 1024
        nt = 8
        acc, act_r, sg_r, gates, lg_r, rings, wrt, brt = work
        weg, weu, wed = self.dl("weg"), self.dl("weu"), self.dl("wed")
        for i in range(nt):
            pl = self.ps.next()
            for k in range(8):
                P.op("pe", lambda e, k=k, i=i, pl=pl: e.matmul(out=pl.ap[:, 0:8], lhsT=h2T.ap[:, k, i * 128:(i + 1) * 128], rhs=wrt.ap[:, k, :],
                                                           start=(k == 0), stop=(k == 7)), reads=[h2T, wrt], writes=[pl], signal=(k == 7))
            lg = lg_r.next()
            L, M1, L2, M2 = (lg.ap[:, j, :] for j in range(4))
            sm = lg.ap[:, 4, :]
            P.op("dve", lambda e, pl=pl, L=L: e.tensor_tensor(out=L, in0=pl.ap[:, 0:8], in1=brt.ap, op=ALU.add), reads=[pl, brt], writes=[lg])
            P.op("dve", lambda e, L=L, sm=sm: e.tensor_reduce(out=sm[:, 0:1], in_=L, axis=AX.X, op=ALU.max), reads=[lg], writes=[lg])
            P.op("dve", lambda e, L=L, M1=M1, sm=sm: e.tensor_scalar(out=M1, in0=L, scalar1=sm[:, 0:1], scalar2=None, op0=ALU.is_ge), reads=[lg], writes=[lg])
            P.op("dve", lambda e, L=L, M1=M1, L2=L2: e.scalar_tensor_tensor(out=L2, in0=M1, scalar=-1e30, in1=L, op0=ALU.mult, op1=ALU.add), reads=[lg], writes=[lg])
            P.op("dve", lambda e, L2=L2, sm=sm: e.tensor_reduce(out=sm[:, 1:2], in_=L2, axis=AX.X, op=ALU.max), reads=[lg], writes=[lg])
            P.op("dve", lambda e, L2=L2, M2=M2, sm=sm: e.tensor_scalar(out=M2, in0=L2, scalar1=sm[:, 1:2], scalar2=None, op0=ALU.is_ge), reads=[lg], writes=[lg])
            P.op("dve", lambda e, sm=sm: e.tensor_tensor(out=sm[:, 2:3], in0=sm[:, 1:2], in1=sm[:, 0:1], op=ALU.subtract), reads=[lg], writes=[lg])
            P.op("act", lambda e, sm=sm: e.activation(out=sm[:, 3:4], in_=sm[:, 2:3], func=AF.Exp), reads=[lg], writes=[lg])
            P.op("dve", lambda e, sm=sm: e.tensor_scalar(out=sm[:, 4:5], in0=sm[:, 3:4], scalar1=1.0, scalar2=None, op0=ALU.add), reads=[lg], writes=[lg])
            P.op("dve", lambda e, sm=sm: e.reciprocal(out=sm[:, 5:6], in_=sm[:, 4:5]), reads=[lg], writes=[lg])
            P.op("dve", lambda e, sm=sm: e.tensor_tensor(out=sm[:, 6:7], in0=sm[:, 3:4], in1=sm[:, 5:6], op=ALU.mult), reads=[lg], writes=[lg])
            P.op("dve", lambda e, M1=M1, sm=sm, i=i: e.tensor_scalar(out=gates.ap[:, i, :], in0=M1, scalar1=sm[:, 5:6], scalar2=None, op0=ALU.mult), reads=[lg], writes=[gates])
            P.op("dve", lambda e, M2=M2, sm=sm, i=i: e.scalar_tensor_tensor(out=gates.ap[:, i, :], in0=M2, scalar=sm[:, 6:7], in1=gates.ap[:, i, :], op0=ALU.mult, op1=ALU.add),
                 reads=[lg, gates], writes=[gates])
        first = True
        for ex in range(NEXP):
            for j in range(7):
                wg = wr.next()
                wu = wr.next()
                wd = wr.next()
                self.load(wg, weg[ex, :, j * 512:(j + 1) * 512].rearrange("(k p) n -> p k n", p=128), "pool")
                self.load(wu, weu[ex, :, j * 512:(j + 1) * 512].rearrange("(k p) n -> p k n", p=128), "pool")
                wdv = wd.ap.rearrange("p a b -> p (a b)").rearrange("p (k n) -> p k n", k=4)
                P.dma("pool", wdv, wed[ex, j * 512:(j + 1) * 512, :].rearrange("(k p) n -> p k n", p=128), writes=[wd])
                at = act_r.next()
                for mm in range(4):
                    for th in range(2):
                        ts = slice(th * 512, (th + 1) * 512)
                        pg = self.ps.next()
                        pu = self.ps.next()
                        for k in range(8):
                            P.op("pe", lambda e, k=k, mm=mm, ts=ts, pg=pg, wg=wg: e.matmul(out=pg.ap, lhsT=wg.ap[:, k, mm * 128:(mm + 1) * 128], rhs=h2T.ap[:, k, ts],
                                                                                  start=(k == 0), stop=(k == 7)), reads=[wg, h2T], writes=[pg], signal=(k == 7))
                        for k in range(8):
                            P.op("pe", lambda e, k=k, mm=mm, ts=ts, pu=pu, wu=wu: e.matmul(out=pu.ap, lhsT=wu.ap[:, k, mm * 128:(mm + 1) * 128], rhs=h2T.ap[:, k, ts],
                                                                                  start=(k == 0), stop=(k == 7)), reads=[wu, h2T], writes=[pu], signal=(k == 7))
                        sg = sg_r.next()
                        P.op("act", lambda e, pg=pg, sg=sg: e.activation(out=sg.ap, in_=pg.ap, func=AF.Silu), reads=[pg], writes=[sg])
                        P.op("dve", lambda e, sg=sg, pu=pu, at=at, mm=mm, ts=ts: e.tensor_tensor(out=at.ap[:, mm, ts], in0=sg.ap, in1=pu.ap, op=ALU.mult),
                             reads=[sg, pu], writes=[at])
                for i in range(nt):
                    for hf in range(2):
                        cs = slice(hf * 512, (hf + 1) * 512)
                        pd = self.ps.next()
                        for mm in range(4):
                            rhs = wdv[:, mm, cs]
                            P.op("pe", lambda e, mm=mm, i=i, pd=pd, at=at, rhs=rhs: e.matmul(out=pd.ap, lhsT=at.ap[:, mm, i * 128:(i + 1) * 128], rhs=rhs,
                                                                                    start=(mm == 0), stop=(mm == 3)), reads=[at, wd], writes=[pd], signal=(mm == 3))
                        if first:
                            P.op("dve", lambda e, pd=pd, i=i, cs=cs, ex=ex: e.tensor_scalar(out=acc.ap[:, i, cs], in0=pd.ap, scalar1=gates.ap[:, i, ex:ex + 1], scalar2=None, op0=ALU.mult),
                                 reads=[pd, gates], writes=[acc])
                        else:
                            P.op("dve", lambda e, pd=pd, i=i, cs=cs, ex=ex: e.scalar_tensor_tensor(out=acc.ap[:, i, cs], in0=pd.ap, scalar=gates.ap[:, i, ex:ex + 1], in1=acc.ap[:, i, cs],
                                                                                          op0=ALU.mult, op1=ALU.add), reads=[pd, gates, acc], writes=[acc])
                first = False
                self.tick()
        self.flush()
        self.post_residual(None, nt, x_tiles_src, x_out_dst, self.gaG[0][1], rings, xout_buf, True,
                           from_sbuf=lambda i: (acc.ap[:, i, :], acc))


def _decl_common(B, layer, main):
    B.inp("winP", [D, IN_DIM])
    if main:
        for nm, shp in (("wao", [512, D]), ("wfn", [512, D]), ("wco", [512, D]), ("wo", [D, D])):
            B.inp(nm, shp)
        B.inp("ropeC", [128, 34 * 128])
        B.inp("ropeS", [128, 34 * 128])
        if layer == 0:
            B.inp("wfg", [D, D_FF])
            B.inp("wfu", [D, D_FF])
            B.inp("wfd", [D_FF, D])
            B.inp("t256", [128, 2, 2, 256])
        else:
            B.inp("wrt", [128, 8, NEXP])
            B.inp("brt", [128, NEXP])
            B.inp("weg", [NEXP, D, D_EXP])
            B.inp("weu", [NEXP, D, D_EXP])
            B.inp("wed", [NEXP, D_EXP, D])


def build_pre(layer):
    B = Builder("pre", layer)
    P, A = B.P, B.A
    _decl_common(B, layer, False)
    xh = B.inp("xh", [34 * 128, D])
    zf = B.outp("zf", [4, TOK, 128], BF16)
    B.zf_b = P.buf("zf_d")
    B.emit_consts()
    B.emit_mod()
    B.emit_phaseA(xh, True, zf)
    P.finish()
    return B


def build_main(layer, stop_after=None, mix_stop=None):
    B = Builder("main", layer)
    B.mix_stop = mix_stop
    if isinstance(mix_stop, str) and mix_stop.startswith("pr"):
        B.pr_stop = int(mix_stop[2:])
    P, A, nc = B.P, B.A, B.nc
    last = layer == 1
    _decl_common(B, layer, True)
    xh = B.inp("xh", [34 * 128, D])
    hctx = B.inp("hctx", [CTX, D])
    zfg = B.inp("zfg", [4, 4, TOK, 128], BF16)
    xo = B.outp("xo", [TOK, D])
    if not last:
        hco = B.outp("hco", [CTX, D])
    B.u_d = B.scratch("u_d", [128, 4, 34 * 128], BF16)
    B.u_b = P.buf("u_d")
    B.ft_d = B.scratch("ft_d", [128, 4, TOK], BF16)
    B.ft_b = P.buf("ft_d")
    xmid = B.scratch("xmid_d", [TOK, D], F32)
    xmid_b = P.buf("xmid_d")
    xo_b = P.buf("xo")
    winP = B.din["winP"]
    B.emit_consts()
    B.emit_mod()
    m_mix = A.mark()
    B.kT = A.alloc("kT", [34 * 128], BF16)
    B.vaug = A.alloc("vaug", [34, 2, 65], BF16)
    P.op("dve", lambda e: e.memset(B.vaug.ap, 1.0), writes=[B.vaug])
    kcT = A.alloc("kcT", [CTX], BF16)
    vcaug = A.alloc("vcaug", [2, 2, 65], BF16)
    P.op("dve", lambda e: e.memset(vcaug.ap, 1.0), writes=[vcaug])
    B.wbr = []
    for nm in ("wao", "wfn", "wco"):
        w = A.alloc(nm, [4, D], BF16)
        B.load(w, B.dl(nm).rearrange("(k p) n -> p k n", p=128), "pool")
        B.wbr.append(w)
    B.wo2 = []
    for hf in range(2):
        w = A.alloc(f"wo{hf}", [8, 512], BF16)
        B.load(w, B.dl("wo")[:, hf * 512:(hf + 1) * 512].rearrange("(k p) n -> p k n", p=128), "pool")
        B.wo2.append(w)

    if stop_after == "mod":
        P.finish()
        return B
    m0 = A.mark()
    rings = B.norm_rings(2)
    hcT = A.alloc("hcT", [8, 256], BF16)
    B.make_hT([hctx[i * 128:(i + 1) * 128, :] for i in range(2)], hcT, 1, 0, rings)
    ncolA = PA_W
    wA = A.alloc("wAc", [8, ncolA], BF16)
    B.load(wA, winP[:, 0:ncolA].rearrange("(k p) n -> p k n", p=128), "pool")
    pr = B.proj_fm(wA, PA_K, hcT, CTX)
    P.op("act", lambda e: e.copy(out=kcT.ap, in_=pr.ap[:, :CTX]), reads=[pr], writes=[kcT])
    pv = B.ps.next()
    pvv = pv.ap.rearrange("p (i c) -> p i c", i=4)
    for i in range(2):
        for k in range(8):
            P.op("pe", lambda e, k=k, i=i: e.matmul(out=pvv[:, i, :], lhsT=hcT.ap[:, k, i * 128:(i + 1) * 128], rhs=wA.ap[:, k, PA_V:PA_V + 128],
                                                  start=(k == 0), stop=(k == 7)), reads=[hcT, wA], writes=[pv], signal=(k == 7 and i == 1))
    P.op("act", lambda e: e.copy(out=vcaug.ap[:, :, :, 0:64], in_=pvv[:, 0:2, :].rearrange("p i (g d) -> p i g d", g=2)), reads=[pv], writes=[vcaug])
    ctxkb = [(kcT.ap[:, i * 128:(i + 1) * 128], kcT, vcaug.ap[:, i, :, :], vcaug, None) for i in range(2)]
    if stop_after == "ctx_kv":
        P.finish()
        return B
    if not last:
        hmid = B.scratch("hmid_d", [CTX, D], F32)
        hmid_b = P.buf("hmid_d")
        hco_b = P.buf("hco")
        zcf = A.alloc("zcf", [2, 512], BF16)
        for i in range(2):
            pz = B.ps.next()
            for k in range(8):
                P.op("pe", lambda e, k=k, i=i, pz=pz: e.matmul(out=pz.ap, lhsT=hcT.ap[:, k, i * 128:(i + 1) * 128], rhs=wA.ap[:, k, PA_F:PA_F + 512],
                                                           start=(k == 0), stop=(k == 7)), reads=[hcT, wA], writes=[pz], signal=(k == 7))
            P.op("act", lambda e, i=i, pz=pz: e.copy(out=zcf.ap[:, i, :], in_=pz.ap), reads=[pz], writes=[zcf])
        t256 = B.const_tile("t256", [2, 2, 256], BF16, B.din["t256"])
        Gc = A.alloc("Gc", [2, 256], BF16)
        FTc = A.alloc("FTc", [4, 256], BF16)
        scl = 1.0 / math.sqrt(256.0 * 128.0)
        for g in range(4):
            pg_ = B.ps.next()
            for i in range(2):
                P.op("pe", lambda e, g=g, i=i, pg_=pg_: e.matmul(out=pg_.ap, lhsT=zcf.ap[:, i, g * 128:(g + 1) * 128],
                                                             rhs=t256.ap[:, i, :, :].rearrange("p r k -> p (r k)"), start=(i == 0), stop=(i == 1)),
                     reads=[zcf, t256], writes=[pg_], signal=(i == 1))
            P.op("act", lambda e, pg_=pg_: e.copy(out=Gc.ap.rearrange("p r k -> p (r k)"), in_=pg_.ap), reads=[pg_], writes=[Gc])
            pf = B.ps.next()
            P.op("pe", lambda e, pf=pf: e.matmul(out=pf.ap[:, 0:256], lhsT=B.cbsb.ap[:, 0, :], rhs=Gc.ap[:, 0, :], start=True, stop=False),
                 reads=[B.cbsb, Gc], writes=[pf], signal=False)
            P.op("pe", lambda e, pf=pf: e.matmul(out=pf.ap[:, 0:256], lhsT=B.cbsb.ap[:, 1, :], rhs=Gc.ap[:, 1, :], start=False, stop=True),
                 reads=[B.cbsb, Gc], writes=[pf])
            P.op("act", lambda e, g=g, pf=pf: e.activation(out=FTc.ap[:, g, :], in_=pf.ap[:, 0:256], func=AF.Copy, scale=scl), reads=[pf], writes=[FTc])
        if stop_after == "ctx_fft":
            P.finish()
            return B
        ucT = A.alloc("ucT", [4, 258], BF16)
        P.op("dve", lambda e: e.memset(ucT.ap, 0.0), writes=[ucT])
        cxs = A.alloc("cxs_c", [512], F32)
        for m in range(4):
            px = B.proj_fm(wA, PA_CX + m * 128, hcT, CTX)
            P.op("act", lambda e, px=px: e.copy(out=cxs.ap[:, :CTX], in_=px.ap[:, :CTX]), reads=[px], writes=[cxs])
            pc = B.proj_fm(wA, PA_CC + m * 128, hcT, CTX)
            P.op("dve", lambda e, m=m, pc=pc: e.tensor_tensor(out=ucT.ap[:, m, 1:257], in0=cxs.ap[:, :CTX], in1=pc.ap[:, :CTX], op=ALU.mult),
                 reads=[cxs, pc], writes=[ucT])
        wr = A.ring("wrc", 3, [8, 512], BF16)
        wq = wr.next()
        B.load(wq, winP[:, PM_Q:PM_Q + 512].rearrange("(k p) n -> p k n", p=128), "pool")
        qcT = A.alloc("qcT", [4, 256], BF16)
        for m in range(4):
            pq = B.proj_fm(wq, m * 128, hcT, CTX)
            P.op("act", lambda e, m=m, pq=pq: e.copy(out=qcT.ap[:, m, :], in_=pq.ap[:, :CTX]), reads=[pq], writes=[qcT])
        attnTc = A.alloc("attnTc", [4, 256], BF16)
        abufs = (A.ring("pTc", 1, [2, 2, 4, 128], BF16), A.ring("atokc", 1, [8, 64], BF16), A.ring("denc", 1, [2, 8], F32))
        for t in range(2):
            B.attention_tile(qcT, t * 128, ctxkb, attnTc, t * 128, abufs)
        if stop_after == "ctx_attn":
            dq = B.outp("dbg_qcT", [128, 4 * 256], BF16)
            da = B.outp("dbg_attnTc", [128, 4 * 256], BF16)
            dk = B.outp("dbg_kcT", [128, 256], BF16)
            dv = B.outp("dbg_vc", [128, 2 * 2 * 65], BF16)
            ob = P.buf("dbgo")
            P.dma("sp", dq, qcT.ap.rearrange("p a b -> p (a b)"), reads=[qcT], writes=[ob], is_output=True)
            P.dma("sp", da, attnTc.ap.rearrange("p a b -> p (a b)"), reads=[attnTc], writes=[ob], is_output=True)
            P.dma("sp", dk, kcT.ap, reads=[kcT], writes=[ob], is_output=True)
            P.dma("sp", dv, vcaug.ap.rearrange("p a b c -> p (a b c)"), reads=[vcaug], writes=[ob], is_output=True)
            P.finish()
            return B
        work = (A.alloc("yconvTc", [4, 256], BF16), A.ring("gsbc", 2, [512], BF16), A.ring("tmpc", 3, [512], F32), A.alloc("mergedTc", [8, 256], BF16),
                A.ring("mixtc", 1, [D], F32), A.ring("smc", 2, [4], F32), A.ring("xresc", 2, [D], F32), A.ring("sqc", 1, [512], BF16))
        B.mix_block(hcT, CTX, qcT, attnTc, ucT.ap, ucT, FTc.ap, FTc, wr, work,
                    [hctx[i * 128:(i + 1) * 128, :] for i in range(2)], [hmid[i * 128:(i + 1) * 128, :] for i in range(2)], 1, hmid_b)
        if stop_after == "ctx_mix":
            ob = P.buf("dbgo2")
            for nm_, t_, a_, n_ in (("dbg_FTc", FTc, 4, 256), ("dbg_ucT", ucT, 4, 258), ("dbg_yconv", work[0], 4, 256), ("dbg_merged", work[3], 8, 256), ("dbg_attnTc", attnTc, 4, 256)):
                do = B.outp(nm_, [128, a_, n_], BF16)
                P.dma("sp", do, t_.ap[:, :, 0:n_], reads=[t_], writes=[ob], is_output=True)
            P.finish()
            return B
        P.barrier()
        A.release(m0)
        rings = B.norm_rings(2)
        h2c = A.alloc("h2c", [8, 256], BF16)
        B.make_hT_dep([hmid[i * 128:(i + 1) * 128, :] for i in range(2)], h2c, 1, 1, rings, hmid_b)
        wr = A.ring("wrf", 8, [8, 512], BF16)
        work = (A.alloc("actTc", [22, 256], BF16), A.ring("sgc", 2, [512], BF16),
                (A.ring("mixtf", 1, [D], F32), A.ring("smf", 2, [4], F32), A.ring("xresf", 1, [D], F32), A.ring("sqf", 1, [512], BF16)))
        B.ffn_dense_block(h2c, CTX, wr, work, [(hmid[i * 128:(i + 1) * 128, :], hmid_b) for i in range(2)],
                          [hco[i * 128:(i + 1) * 128, :] for i in range(2)], 1, hco_b, True)
    P.barrier()
    A.release(m0)

    if stop_after == "ctx":
        P.finish()
        return B
    B.emit_phaseA(xh, False)
    if stop_after == "A":
        P.finish()
        return B
    B.emit_fft(zfg)
    if stop_after == "fft":
        P.finish()
        return B

    m0 = A.mark()
    rings = B.norm_rings(3)
    hTr = A.ring("hTm", 1, [8, 512], BF16)
    rope_r = A.ring("ropeM", 1, [2, 512], F32)
    qraw = A.alloc("qraw", [512], BF16)
    rtmp = (A.alloc("rt1m", [512], F32), A.alloc("rt2m", [512], F32))
    qT = A.alloc("qT", [4, 512], BF16)
    attnT = A.alloc("attnT", [4, 512], BF16)
    abufs = (A.ring("pT", 2, [5, 2, 4, 128], BF16), A.ring("atok", 2, [8, 64], BF16), A.ring("den", 2, [2, 8], F32))
    ubr = A.ring("ub", 1, [4, 514], BF16)
    ftr = A.ring("ftb", 1, [4, 512], BF16)
    wr = A.ring("wrm", 3, [8, 512], BF16)
    work = (A.alloc("yconvT", [4, 512], BF16), A.ring("gsb", 2, [512], BF16), A.ring("tmpm", 3, [512], F32), A.alloc("mergedT", [8, 512], BF16),
            A.ring("mixt", 1, [D], F32), A.ring("smm", 2, [4], F32), A.ring("xres", 1, [D], F32), A.ring("sqm", 1, [512], BF16))
    for bi in range(8):
        c0 = (4 * bi + 1) * 128
        hT = hTr.next()
        own = [xh[(4 * bi + 1 + i) * 128:(4 * bi + 2 + i) * 128, :] for i in range(4)]
        B.make_hT(own, hT, 0, 0, rings)
        rp = rope_r.next()
        P.dma("sp", rp.ap[:, 0, :], B.din["ropeC"][:, c0:c0 + 512], writes=[rp])
        P.dma("sp", rp.ap[:, 1, :], B.din["ropeS"][:, c0:c0 + 512], writes=[rp])
        B.ropeb = rp
        wq = wr.next()
        B.load(wq, winP[:, PM_Q:PM_Q + 512].rearrange("(k p) n -> p k n", p=128), "pool")
        for m in range(4):
            pq = B.proj_fm(wq, m * 128, hT, 512)
            B.rope(pq, qraw, qT.ap[:, m, :], qT, rp.ap[:, 0, :], rp.ap[:, 1, :], 512, rtmp)
        for t in range(4):
            T = 4 * bi + t
            kbs = []
            for d_, mi in ((0, 2 if T == 0 else 0), (1, None), (2, 3 if T == 31 else 1)):
                sl = T + d_
                kbs.append((B.kT.ap[:, sl * 128:(sl + 1) * 128], B.kT, B.vaug.ap[:, sl, :, :], B.vaug, mi))
            kbs += ctxkb
            B.attention_tile(qT, t * 128, kbs, attnT, t * 128, abufs)
        ub = ubr.next()
        P.dma("sp", ub.ap, B.u_d[:, :, c0 - 1:c0 + 513], reads=[B.u_b], writes=[ub])
        ftb = ftr.next()
        P.dma("sp", ftb.ap, B.ft_d[:, :, bi * 512:(bi + 1) * 512], reads=[B.ft_b], writes=[ftb])
        B.mix_block(hT, 512, qT, attnT, ub.ap, ub, ftb.ap, ftb, wr, work, own,
                    [xmid[(4 * bi + i) * 128:(4 * bi + i + 1) * 128, :] for i in range(4)], 0, xmid_b)
    P.barrier()
    A.release(m0)

    if stop_after == "mix":
        P.finish()
        return B
    A.release(m_mix)
    m0 = A.mark()
    if not last:
        rings = B.norm_rings(4)
        h2r = A.ring("h2T", 1, [8, 512], BF16)
        wr = A.ring("wrf2", 8, [8, 512], BF16)
        work = (A.alloc("actT", [22, 512], BF16), A.ring("sg", 2, [512], BF16),
                (A.ring("mixtF", 1, [D], F32), A.ring("smF", 2, [4], F32), A.ring("xresF", 2, [D], F32), A.ring("sqF", 1, [512], BF16)))
        for bi in range(8):
            h2T = h2r.next()
            src = [xmid[(4 * bi + i) * 128:(4 * bi + i + 1) * 128, :] for i in range(4)]
            B.make_hT_dep(src, h2T, 0, 1, rings, xmid_b)
            B.ffn_dense_block(h2T, 512, wr, work, [(a, xmid_b) for a in src],
                              [xo[(4 * bi + i) * 128:(4 * bi + i + 1) * 128, :] for i in range(4)], 0, xo_b, True)
    else:
        rings = B.norm_rings(4)
        h2r = A.ring("h2T", 1, [8, 1024], BF16)
        wr = A.ring("wre", 6, [8, 512], BF16)
        wrt = B.const_tile("wrt", [8, NEXP], BF16, B.din["wrt"])
        brt = B.const_tile("brt", [NEXP], F32, B.din["brt"])
        work = (A.alloc("acc", [8, D], F32), A.ring("actc", 2, [4, 1024], BF16), A.ring("sge", 2, [512], BF16), A.alloc("gates", [8, NEXP], F32),
                A.ring("lg", 2, [5, 8], F32),
                (A.ring("mixtE", 1, [D], F32), A.ring("smE", 2, [4], F32), A.ring("xresE", 2, [D], F32), A.ring("sqE", 1, [512], BF16)), wrt, brt)
        for bi in range(4):
            h2T = h2r.next()
            src = [xmid[(8 * bi + i) * 128:(8 * bi + i + 1) * 128, :] for i in range(8)]
            B.make_hT_dep(src, h2T, 0, 1, rings, xmid_b)
            B.ffn_moe_block(h2T, wr, work, [(a, xmid_b) for a in src], [xo[(8 * bi + i) * 128:(8 * bi + i + 1) * 128, :] for i in range(8)], xo_b)
    P.finish()
    return B


def _w_in_perm():
    cols = []
    cols += list(range(F_OFF, F_OFF + 512))
    cols += list(range(K_OFF, K_OFF + 128))
    cols += list(range(V_OFF, V_OFF + 128))
    cols += list(range(CX_OFF, CX_OFF + 512))
    cols += list(range(CC_OFF, CC_OFF + 512))
    for m in range(4):
        cols += list(range(Q_OFF + m * 64, Q_OFF + (m + 1) * 64))
        cols += list(range(Q_OFF + (4 + m) * 64, Q_OFF + (5 + m) * 64))
    cols += list(range(CB_OFF, CB_OFF + 512))
    for m in range(8):
        for r in range(3):
            cols += list(range(GATE_OFF + r * 1024 + m * 128, GATE_OFF + r * 1024 + (m + 1) * 128))
    assert len(cols) == IN_DIM and len(set(cols)) == IN_DIM
    return np.asarray(cols)


def _colform(v, n):
    return np.ascontiguousarray(np.asarray(v, np.float32).reshape(n, 128).T)


_CONST_CACHE = {}


def _static_tables():
    if "t" in _CONST_CACHE:
        return _CONST_CACHE["t"]
    t = {}
    t["ident"] = np.eye(128, dtype=np.float32)
    rt = np.zeros((128, 128), np.float32)
    for i in range(64):
        rt[2 * i + 1, 2 * i] = -1.0
        rt[2 * i, 2 * i + 1] = 1.0
    t["rt"] = rt
    n = np.arange(128, dtype=np.float64)
    k1 = np.arange(128, dtype=np.float64)
    ang = 2 * np.pi * np.outer(n, k1) / 128.0
    t1 = np.stack([np.cos(ang), -np.sin(ang)], axis=1).reshape(128, 2, 2, 64)
    t["t1"] = np.ascontiguousarray(t1.transpose(0, 2, 1, 3)).astype(np.float32)
    angc = 2 * np.pi * np.outer(n, n) / 128.0
    t["cbsb"] = np.ascontiguousarray(np.stack([np.cos(angc), np.sin(angc)], axis=1)).astype(np.float32)
    nn = np.arange(256, dtype=np.float64)
    a256 = 2 * np.pi * np.outer(nn, nn) / 256.0
    t256 = np.stack([np.cos(a256), -np.sin(a256)], axis=1)
    t["t256"] = np.ascontiguousarray(t256.reshape(2, 128, 2, 256).transpose(1, 0, 2, 3)).astype(np.float32)
    kk = np.arange(128)[:, None]
    qq = np.arange(128)[None, :]
    prev = (kk >= qq).astype(np.float32)
    nxt = (kk <= qq).astype(np.float32)
    half = 32
    inv_freq = 1.0 / (10000.0 ** (np.arange(0, half, 2, dtype=np.float64) / half))
    for j in range(4):
        m = np.stack([prev, nxt, prev * (1.0 if j != 0 else 0.0), nxt * (1.0 if j != 3 else 0.0)], axis=1)
        t[("masks", j)] = np.ascontiguousarray(m).astype(np.float32)
        t[("valid", j)] = np.tile(np.asarray([[1.0 if j != 0 else 0.0, 1.0 if j != 3 else 0.0]], np.float32), (128, 1))
        pos = TOK * j - 128 + np.arange(34 * 128)
        row = (pos // 64).astype(np.float64)
        col = (pos % 64).astype(np.float64)
        angp = np.concatenate([row[:, None] * inv_freq[None, :], col[:, None] * inv_freq[None, :]], axis=1)
        d = np.arange(128) % 64
        pi_ = d // 2
        t[("ropeC", j)] = np.ascontiguousarray(np.cos(angp)[:, pi_].T).astype(np.float32)
        t[("ropeS", j)] = np.ascontiguousarray(np.sin(angp)[:, pi_].T).astype(np.float32)
        n2 = np.arange(128, dtype=np.float64)[:, None, None]
        k1_ = np.arange(128, dtype=np.float64)[None, :, None]
        k2_ = (32 * j + np.arange(32, dtype=np.float64))[None, None, :]
        th = 2 * np.pi * n2 * (k1_ + 128.0 * k2_) / float(SEQ)
        et = np.concatenate([np.sin(th), np.cos(th), -np.sin(th)], axis=2)
        t[("etab", j)] = np.ascontiguousarray(et).astype(np.float32)
    _CONST_CACHE["t"] = t
    return t


def _layer_common(inp, l):
    perm = _w_in_perm()
    c = {}
    c["winP"] = np.ascontiguousarray(np.asarray(inp["w_in"][l], np.float32)[:, perm])
    c["wmod"] = np.ascontiguousarray(np.asarray(inp["w_mod"][l], np.float32))
    bm = np.asarray(inp["b_mod"][l], np.float32)
    c["bcols"] = _colform(bm, 48)
    c["brow"] = np.ascontiguousarray(np.broadcast_to(np.stack([bm[2 * D:3 * D], bm[5 * D:6 * D]])[None], (128, 2, D))).astype(np.float32)
    c["gpm_c"] = _colform(inp["g_pre_mix"][l], 8)
    c["gpf_c"] = _colform(inp["g_pre_ffn"][l], 8)
    c["gqm_r"] = np.ascontiguousarray(np.broadcast_to(np.asarray(inp["g_post_mix"][l], np.float32)[None], (128, D)))
    c["gqf_r"] = np.ascontiguousarray(np.broadcast_to(np.asarray(inp["g_post_ffn"][l], np.float32)[None], (128, D)))
    c["wao"] = np.ascontiguousarray(np.asarray(inp["w_attn_o"][l], np.float32))
    c["wfn"] = np.ascontiguousarray(np.asarray(inp["w_fnet"][l], np.float32))
    c["wco"] = np.ascontiguousarray(np.asarray(inp["w_conv_out"][l], np.float32))
    c["wo"] = np.ascontiguousarray(np.asarray(inp["w_o"][l], np.float32))
    c["sink_b"] = np.ascontiguousarray(np.broadcast_to(np.asarray(inp["attn_sink"][l], np.float32)[None], (128, 8)))
    wc = np.asarray(inp["w_conv"][l], np.float32)
    c["wconv_c"] = np.ascontiguousarray(wc.reshape(3, 4, 128).transpose(2, 1, 0))
    if l == 0:
        c["wfg"] = np.ascontiguousarray(np.asarray(inp["w_ff_gate"][0], np.float32))
        c["wfu"] = np.ascontiguousarray(np.asarray(inp["w_ff_up"][0], np.float32))
        c["wfd"] = np.ascontiguousarray(np.asarray(inp["w_ff_down"][0], np.float32))
    else:
        wr = np.asarray(inp["w_router"][0], np.float32)
        c["wrt"] = np.ascontiguousarray(wr.reshape(8, 128, NEXP).transpose(1, 0, 2))
        c["brt"] = np.ascontiguousarray(np.broadcast_to(np.asarray(inp["b_router"][0], np.float32)[None], (128, NEXP)))
        c["weg"] = np.ascontiguousarray(np.asarray(inp["w_exp_gate"][0], np.float32))
        c["weu"] = np.ascontiguousarray(np.asarray(inp["w_exp_up"][0], np.float32))
        c["wed"] = np.ascontiguousarray(np.asarray(inp["w_exp_down"][0], np.float32))
    return c


def _ccols(inp, b):
    cb = np.asarray(inp["c"][b], np.float32)
    cc = np.asarray(inp["c_ctx"], np.float32)
    return np.ascontiguousarray(np.stack([_colform(cb, 8), _colform(cc, 8)], axis=2))


def _xh(xfull, cid):
    b, j = cid // 4, cid % 4
    out = np.zeros((34 * 128, D), np.float32)
    lo = TOK * j - 128
    hi = TOK * (j + 1) + 128
    s0, s1 = max(lo, 0), min(hi, SEQ)
    out[s0 - lo:s1 - lo] = xfull[b, s0:s1]
    return out


_PROG_CACHE = {}


def _get_prog(kind, layer):
    key = (kind, layer)
    if key not in _PROG_CACHE:
        _PROG_CACHE[key] = build_pre(layer) if kind == "pre" else build_main(layer)
    return _PROG_CACHE[key]


def _run(B, maps):
    names = set(B.din.keys())
    in_maps = [{k: v for k, v in m.items() if k in names} for m in maps]
    for m in in_maps:
        missing = names - set(m.keys())
        assert not missing, missing
    res = run_bass_kernel_spmd(B.nc, in_maps, core_ids=list(range(8)))
    return res.results


def kernel_unfused(**inputs):
    tabs = _static_tables()
    x = np.asarray(inputs["x"], np.float32)
    hctx = [np.ascontiguousarray(np.asarray(inputs["ctx"][b], np.float32)) for b in range(NB)]
    for l in range(2):
        com = _layer_common(inputs, l)
        maps = []
        for cid in range(8):
            b, j = cid // 4, cid % 4
            m = dict(com)
            for nm in ("ident", "rt", "t1", "cbsb", "t256"):
                m[nm] = tabs[nm]
            for nm in ("masks", "valid", "ropeC", "ropeS", "etab"):
                m[nm] = tabs[(nm, j)]
            m["ccols"] = _ccols(inputs, b)
            m["xh"] = _xh(x, cid)
            m["hctx"] = hctx[b]
            maps.append(m)
        r = _run(_get_prog("pre", l), maps)
        for b in range(NB):
            zfg = np.ascontiguousarray(np.stack([np.asarray(r[4 * b + j]["zf"]) for j in range(4)]))
            for j in range(4):
                maps[4 * b + j]["zfg"] = zfg
        r = _run(_get_prog("main", l), maps)
        xn = np.empty_like(x)
        for cid in range(8):
            b, j = cid // 4, cid % 4
            xn[b, TOK * j:TOK * (j + 1)] = np.asarray(r[cid]["xo"], np.float32)
        x = xn
        if l == 0:
            hctx = [np.ascontiguousarray(np.asarray(r[4 * b]["hco"], np.float32)) for b in range(NB)]
    return x


GROUPS = [[0, 1, 2, 3], [4, 5, 6, 7]]


def _decl_layer(B, l):
    sfx = str(l)
    B.inp("winP" + sfx, [D, IN_DIM])
    for nm, shp in (("wao", [512, D]), ("wfn", [512, D]), ("wco", [512, D]), ("wo", [D, D])):
        B.inp(nm + sfx, shp)
    if l == 0:
        B.inp("wfg0", [D, D_FF])
        B.inp("wfu0", [D, D_FF])
        B.inp("wfd0", [D_FF, D])
    else:
        B.inp("wrt1", [128, 8, NEXP])
        B.inp("brt1", [128, NEXP])
        B.inp("weg1", [NEXP, D, D_EXP])
        B.inp("weu1", [NEXP, D, D_EXP])
        B.inp("wed1", [NEXP, D_EXP, D])


def build_fused(sim_cc=False, stop=None):
    B = Builder("main", 0)
    P, A, nc = B.P, B.A, B.nc
    B.sfx = "0"
    for l in range(2):
        if l == 1 and stop is not None and stop != "l1mix":
            continue
        _decl_layer(B, l)
    B.inp("ropeC", [128, 34 * 128])
    B.inp("ropeS", [128, 34 * 128])
    B.inp("t256", [128, 2, 2, 256])
    xh = B.inp("xh", [34 * 128, D])
    hctx_in = B.inp("hctx", [CTX, D])
    selh = B.inp("selh", [128, 8])
    xo = B.outp("xo", [TOK, D])
    xo_b = P.buf("xo")
    B.u_d = B.scratch("u_d", [128, 4, 34 * 128], BF16)
    B.u_b = P.buf("u_d")
    B.ft_d = B.scratch("ft_d", [128, 4, TOK], BF16)
    B.ft_b = P.buf("ft_d")
    xmid = B.scratch("xmid_d", [TOK, D], F32)
    xmid_b = P.buf("xmid_d")
    x1 = B.scratch("x1_d", [TOK, D], F32)
    x1_b = P.buf("x1_d")
    hmid = B.scratch("hmid_d", [CTX, D], F32)
    hmid_b = P.buf("hmid_d")
    hc1 = B.scratch("hc1_d", [CTX, D], F32)
    hc1_b = P.buf("hc1_d")
    xhalo = B.scratch("xhalo_d", [256, D], F32)
    xhalo_b = P.buf("xhalo_d")
    zf_l = [nc.dram_tensor(f"zf_cc{g}", [TOK, 128], BF16).ap() for g in range(4)]
    zfg_l = [nc.dram_tensor(f"zfg_cc{g}", [4 * TOK, 128], BF16).ap() for g in range(4)]
    xb_src = nc.dram_tensor("xb_cc", [256, D], F32).ap()
    xb_all = nc.dram_tensor("xball_cc", [4 * 256, D], F32).ap()
    B.zf_b = P.buf("zf_cc")
    zfg_b = P.buf("zfg_cc")
    xb_b = P.buf("xb_cc")
    xball_b = P.buf("xball_cc")
    def zsrc(r, g):
        return zfg_l[g][r * TOK:(r + 1) * TOK, :]

    B.emit_consts()
    B.alloc_mod_tiles()
    m_top = A.mark()
    for l in range(2):
        last = l == 1
        B.layer = l
        B.last = last
        B.sfx = str(l)
        winP = B.dl("winP")
        B.emit_layer_consts()
        B.emit_mod()
        m_mix = A.mark()
        B.kT = A.alloc("kT", [34 * 128], BF16)
        B.vaug = A.alloc("vaug", [34, 2, 65], BF16)
        P.op("dve", lambda e: e.memset(B.vaug.ap, 1.0), writes=[B.vaug])
        kcT = A.alloc("kcT", [CTX], BF16)
        vcaug = A.alloc("vcaug", [2, 2, 65], BF16)
        P.op("dve", lambda e: e.memset(vcaug.ap, 1.0), writes=[vcaug])
        B.wbr = []
        for nm in ("wao", "wfn", "wco"):
            w = A.alloc(nm, [4, D], BF16)
            B.load(w, B.dl(nm).rearrange("(k p) n -> p k n", p=128), "pool")
            B.wbr.append(w)
        B.wo2 = []
        for hf in range(2):
            w = A.alloc(f"wo{hf}", [8, 512], BF16)
            B.load(w, B.dl("wo")[:, hf * 512:(hf + 1) * 512].rearrange("(k p) n -> p k n", p=128), "pool")
            B.wo2.append(w)

        if l == 0:
            def xtile(t):
                return xh[(t + 1) * 128:(t + 2) * 128, :]
            hsrc = [hctx_in[i * 128:(i + 1) * 128, :] for i in range(2)]
        else:
            def xtile(t):
                if t < 0:
                    return (xhalo[0:128, :], xhalo_b)
                if t >= NTILE:
                    return (xhalo[128:256, :], xhalo_b)
                return (x1[t * 128:(t + 1) * 128, :], x1_b)
            hsrc = [(hc1[i * 128:(i + 1) * 128, :], hc1_b) for i in range(2)]
        B.xtile = xtile

        B.emit_phaseA(None, True, zf_l)
        m0 = A.mark()
        wA = A.alloc("wAc", [8, PA_W], BF16)
        B.load(wA, winP[:, 0:PA_W].rearrange("(k p) n -> p k n", p=128), "pool")
        if not last:
            wr_c = A.ring("wrc", 3, [8, 512], BF16)
            wq_c = wr_c.next()
            B.load(wq_c, winP[:, PM_Q:PM_Q + 512].rearrange("(k p) n -> p k n", p=128), "pool")
        for g in range(4):
            if sim_cc:
                for r in range(4):
                    P.dma("sp", zfg_l[g][r * TOK:(r + 1) * TOK, :], zf_l[g], reads=[B.zf_b], writes=[zfg_b])
            else:
                P.cc_allgather(zfg_l[g], zf_l[g], GROUPS, reads=[B.zf_b], writes=[zfg_b])

        rings = B.norm_rings(2)
        hcT = A.alloc("hcT", [8, 256], BF16)
        B.make_hT(hsrc, hcT, 1, 0, rings)
        pr = B.proj_fm(wA, PA_K, hcT, CTX)
        P.op("act", lambda e: e.copy(out=kcT.ap, in_=pr.ap[:, :CTX]), reads=[pr], writes=[kcT])
        pv = B.ps.next()
        pvv = pv.ap.rearrange("p (i c) -> p i c", i=4)
        for i in range(2):
            for k in range(8):
                P.op("pe", lambda e, k=k, i=i: e.matmul(out=pvv[:, i, :], lhsT=hcT.ap[:, k, i * 128:(i + 1) * 128], rhs=wA.ap[:, k, PA_V:PA_V + 128],
                                                      start=(k == 0), stop=(k == 7)), reads=[hcT, wA], writes=[pv], signal=(k == 7 and i == 1))
        P.op("act", lambda e: e.copy(out=vcaug.ap[:, :, :, 0:64], in_=pvv[:, 0:2, :].rearrange("p i (g d) -> p i g d", g=2)), reads=[pv], writes=[vcaug])
        ctxkb = [(kcT.ap[:, i * 128:(i + 1) * 128], kcT, vcaug.ap[:, i, :, :], vcaug, None) for i in range(2)]
        if not last:
            zcf = A.alloc("zcf", [2, 512], BF16)
            for i in range(2):
                pz = B.ps.next()
                for k in range(8):
                    P.op("pe", lambda e, k=k, i=i, pz=pz: e.matmul(out=pz.ap, lhsT=hcT.ap[:, k, i * 128:(i + 1) * 128], rhs=wA.ap[:, k, PA_F:PA_F + 512],
                                                               start=(k == 0), stop=(k == 7)), reads=[hcT, wA], writes=[pz], signal=(k == 7))
                P.op("act", lambda e, i=i, pz=pz: e.copy(out=zcf.ap[:, i, :], in_=pz.ap), reads=[pz], writes=[zcf])
            t256 = B.const_tile("t256", [2, 2, 256], BF16, B.din["t256"])
            Gc = A.alloc("Gc", [2, 256], BF16)
            FTc = A.alloc("FTc", [4, 256], BF16)
            scl = 1.0 / math.sqrt(256.0 * 128.0)
            for g in range(4):
                pg_ = B.ps.next()
                for i in range(2):
                    P.op("pe", lambda e, g=g, i=i, pg_=pg_: e.matmul(out=pg_.ap, lhsT=zcf.ap[:, i, g * 128:(g + 1) * 128],
                                                                 rhs=t256.ap[:, i, :, :].rearrange("p r k -> p (r k)"), start=(i == 0), stop=(i == 1)),
                         reads=[zcf, t256], writes=[pg_], signal=(i == 1))
                P.op("act", lambda e, pg_=pg_: e.copy(out=Gc.ap.rearrange("p r k -> p (r k)"), in_=pg_.ap), reads=[pg_], writes=[Gc])
                pf = B.ps.next()
                P.op("pe", lambda e, pf=pf: e.matmul(out=pf.ap[:, 0:256], lhsT=B.cbsb.ap[:, 0, :], rhs=Gc.ap[:, 0, :], start=True, stop=False),
                     reads=[B.cbsb, Gc], writes=[pf], signal=False)
                P.op("pe", lambda e, pf=pf: e.matmul(out=pf.ap[:, 0:256], lhsT=B.cbsb.ap[:, 1, :], rhs=Gc.ap[:, 1, :], start=False, stop=True),
                     reads=[B.cbsb, Gc], writes=[pf])
                P.op("act", lambda e, g=g, pf=pf: e.activation(out=FTc.ap[:, g, :], in_=pf.ap[:, 0:256], func=AF.Copy, scale=scl), reads=[pf], writes=[FTc])
            ucT = A.alloc("ucT", [4, 258], BF16)
            P.op("dve", lambda e: e.memset(ucT.ap, 0.0), writes=[ucT])
            cxs = A.alloc("cxs_c", [512], F32)
            for m in range(4):
                px = B.proj_fm(wA, PA_CX + m * 128, hcT, CTX)
                P.op("act", lambda e, px=px: e.copy(out=cxs.ap[:, :CTX], in_=px.ap[:, :CTX]), reads=[px], writes=[cxs])
                pc = B.proj_fm(wA, PA_CC + m * 128, hcT, CTX)
                P.op("dve", lambda e, m=m, pc=pc: e.tensor_tensor(out=ucT.ap[:, m, 1:257], in0=cxs.ap[:, :CTX], in1=pc.ap[:, :CTX], op=ALU.mult),
                     reads=[cxs, pc], writes=[ucT])
            wr = wr_c
            wq = wq_c
            qcT = A.alloc("qcT", [4, 256], BF16)
            for m in range(4):
                pq = B.proj_fm(wq, m * 128, hcT, CTX)
                P.op("act", lambda e, m=m, pq=pq: e.copy(out=qcT.ap[:, m, :], in_=pq.ap[:, :CTX]), reads=[pq], writes=[qcT])
            attnTc = A.alloc("attnTc", [4, 256], BF16)
            abufs = (A.ring("pTc", 1, [2, 2, 4, 128], BF16), A.ring("atokc", 1, [8, 64], BF16), A.ring("denc", 1, [2, 8], F32))
            for t in range(2):
                B.attention_tile(qcT, t * 128, ctxkb, attnTc, t * 128, abufs)
            work = (A.alloc("yconvTc", [4, 256], BF16), A.ring("gsbc", 2, [512], BF16), A.ring("tmpc", 3, [512], F32), A.alloc("mergedTc", [8, 256], BF16),
                    A.ring("mixtc", 1, [D], F32), A.ring("smc", 2, [4], F32), A.ring("xresc", 1, [D], F32), A.ring("sqc", 1, [512], BF16))
            B.mix_block(hcT, CTX, qcT, attnTc, ucT.ap, ucT, FTc.ap, FTc, wr, work, hsrc,
                        [hmid[i * 128:(i + 1) * 128, :] for i in range(2)], 1, hmid_b)
            P.barrier()
            A.release(m0)
            rings = B.norm_rings(2)
            h2c = A.alloc("h2c", [8, 256], BF16)
            B.make_hT([(hmid[i * 128:(i + 1) * 128, :], hmid_b) for i in range(2)], h2c, 1, 1, rings)
            wr = A.ring("wrf", 8, [8, 512], BF16)
            work = (A.alloc("actTc", [22, 256], BF16), A.ring("sgc", 2, [512], BF16),
                    (A.ring("mixtf", 1, [D], F32), A.ring("smf", 2, [4], F32), A.ring("xresf", 1, [D], F32), A.ring("sqf", 1, [512], BF16)))
            B.ffn_dense_block(h2c, CTX, wr, work, [(hmid[i * 128:(i + 1) * 128, :], hmid_b) for i in range(2)],
                              [hc1[i * 128:(i + 1) * 128, :] for i in range(2)], 1, hc1_b, False)
        P.barrier()
        A.release(m0)

        B.zfg_b = zfg_b
        B.emit_fft(zsrc)
        if stop == "l0fft":
            P.dma("sp", xo[0:128, :], xh[128:256, :], reads=[B.ft_b], writes=[xo_b], is_output=True)
            P.finish()
            return B

        m0 = A.mark()
        rings = B.norm_rings(2)
        hTr = A.ring("hTm", 2, [8, 512], BF16)
        rope_r = A.ring("ropeM", 1, [2, 512], F32)
        qraw = A.alloc("qraw", [512], BF16)
        tmpm_ring = A.ring("tmpm", 3, [512], F32)
        rtmp = (tmpm_ring.tiles[0], tmpm_ring.tiles[1])
        qT = A.alloc("qT", [4, 512], BF16)
        attnT = A.alloc("attnT", [4, 512], BF16)
        abufs = (A.ring("pT", 2, [5, 2, 4, 128], BF16), A.ring("atok", 2, [8, 64], BF16), A.ring("den", 2, [2, 8], F32))
        ubr = A.ring("ub", 1, [4, 514], BF16)
        ftr = A.ring("ftb", 1, [4, 512], BF16)
        wr = A.ring("wrm", 3, [8, 512], BF16)
        work = (A.alloc("yconvT", [4, 512], BF16), A.ring("gsb", 2, [512], BF16), tmpm_ring, A.alloc("mergedT", [8, 512], BF16),
                A.ring("mixt", 1, [D], F32), A.ring("smm", 2, [4], F32), A.ring("xres", 1, [D], F32), A.ring("sqm", 1, [512], BF16))

        def _mkm(bi_, defer):
            h_ = hTr.next()
            own_ = [xtile(4 * bi_ + i) for i in range(4)]
            B.make_hT(own_, h_, 0, 0, rings, defer=defer)
            return h_, own_
        nxtm = _mkm(0, False)
        for bi in range(8):
            c0 = (4 * bi + 1) * 128
            B.flush()
            hT, own = nxtm
            if bi + 1 < 8:
                nxtm = _mkm(bi + 1, True)
            rp = rope_r.next()
            P.dma("sp", rp.ap[:, 0, :], B.din["ropeC"][:, c0:c0 + 512], writes=[rp])
            P.dma("sp", rp.ap[:, 1, :], B.din["ropeS"][:, c0:c0 + 512], writes=[rp])
            B.ropeb = rp
            wq = wr.next()
            B.load(wq, winP[:, PM_Q:PM_Q + 512].rearrange("(k p) n -> p k n", p=128), "pool")
            for m in range(4):
                pq = B.proj_fm(wq, m * 128, hT, 512)
                B.rope(pq, qraw, qT.ap[:, m, :], qT, rp.ap[:, 0, :], rp.ap[:, 1, :], 512, rtmp)
            def _kbs(t_):
                T = 4 * bi + t_
                kbs = []
                for d_, mi in ((0, 2 if T == 0 else 0), (1, None), (2, 3 if T == 31 else 1)):
                    sl = T + d_
                    kbs.append((B.kT.ap[:, sl * 128:(sl + 1) * 128], B.kT, B.vaug.ap[:, sl, :, :], B.vaug, mi))
                return kbs + ctxkb
            kb_cur = _kbs(0)
            pT_cur = B.attention_scores(qT, 0, kb_cur, abufs)
            for t in range(4):
                if t + 1 < 4:
                    kb_nxt = _kbs(t + 1)
                    pT_nxt = B.attention_scores(qT, (t + 1) * 128, kb_nxt, abufs)
                B.attention_pv(pT_cur, kb_cur, attnT, t * 128, abufs)
                if t + 1 < 4:
                    kb_cur, pT_cur = kb_nxt, pT_nxt
                if t in (0, 2):
                    B.tick()
            ub = ubr.next()
            P.dma("sp", ub.ap, B.u_d[:, :, c0 - 1:c0 + 513], reads=[B.u_b], writes=[ub])
            ftb = ftr.next()
            P.dma("sp", ftb.ap, B.ft_d[:, :, bi * 512:(bi + 1) * 512], reads=[B.ft_b], writes=[ftb])
            B.mix_block(hT, 512, qT, attnT, ub.ap, ub, ftb.ap, ftb, wr, work, own,
                        [xmid[(4 * bi + i) * 128:(4 * bi + i + 1) * 128, :] for i in range(4)], 0, xmid_b)
        P.barrier()
        A.release(m_mix)

        m0 = A.mark()
        if not last:
            rings = B.norm_rings(4)
            h2r = A.ring("h2T", 2, [8, 512], BF16)
            wr = A.ring("wrf2", 8, [8, 512], BF16)
            work = (A.alloc("actT", [22, 512], BF16), A.ring("sg", 2, [512], BF16),
                    (A.ring("mixtF", 1, [D], F32), A.ring("smF", 2, [4], F32), A.ring("xresF", 2, [D], F32), A.ring("sqF", 1, [512], BF16)))
            def _mk2(bi_, defer):
                h_ = h2r.next()
                src_ = [(xmid[(4 * bi_ + i) * 128:(4 * bi_ + i + 1) * 128, :], xmid_b) for i in range(4)]
                B.make_hT(src_, h_, 0, 1, rings, defer=defer)
                return h_, src_
            nxt = _mk2(0, False)
            for bi in range(8):
                B.flush()
                h2T, src = nxt
                if bi + 1 < 8:
                    nxt = _mk2(bi + 1, True)
                B.ffn_dense_block(h2T, 512, wr, work, src,
                                  [x1[(4 * bi + i) * 128:(4 * bi + i + 1) * 128, :] for i in range(4)], 0, x1_b, False)
            P.dma("sp", xb_src[0:128, :], x1[0:128, :], reads=[x1_b], writes=[xb_b])
            P.dma("sp", xb_src[128:256, :], x1[TOK - 128:TOK, :], reads=[x1_b], writes=[xb_b])
            if sim_cc:
                for r in range(4):
                    P.dma("sp", xb_all[r * 256:(r + 1) * 256, :], xb_src, reads=[xb_b], writes=[xball_b])
            else:
                P.cc_allgather(xb_all, xb_src, GROUPS, reads=[xb_b], writes=[xball_b])
            P.barrier()
            A.release(m0)
            m0 = A.mark()
            sel = B.const_tile("selh", [8], F32, selh)
            hal = A.alloc("hal", [2, D], F32)
            gat = A.ring("gat", 2, [D], F32)
            for side in range(2):
                for r in range(4):
                    gt = gat.next()
                    row0 = r * 256 + (128 if side == 0 else 0)
                    P.dma("sp", gt.ap, xb_all[row0:row0 + 128, :], reads=[xball_b], writes=[gt])
                    sc_ap = sel.ap[:, side * 4 + r:side * 4 + r + 1]
                    if r == 0:
                        P.op("dve", lambda e, gt=gt, side=side, sc_ap=sc_ap: e.tensor_scalar(out=hal.ap[:, side, :], in0=gt.ap, scalar1=sc_ap, scalar2=None, op0=ALU.mult),
                             reads=[gt, sel], writes=[hal])
                    else:
                        P.op("dve", lambda e, gt=gt, side=side, sc_ap=sc_ap: e.scalar_tensor_tensor(out=hal.ap[:, side, :], in0=gt.ap, scalar=sc_ap, in1=hal.ap[:, side, :],
                                                                                              op0=ALU.mult, op1=ALU.add), reads=[gt, sel, hal], writes=[hal])
                P.dma("sp", xhalo[side * 128:(side + 1) * 128, :], hal.ap[:, side, :], reads=[hal], writes=[xhalo_b])
        else:
            rings = B.norm_rings(4)
            h2r = A.ring("h2T", 2, [8, 1024], BF16)
            wr = A.ring("wre", 6, [8, 512], BF16)
            wrt = B.const_tile("wrt", [8, NEXP], BF16, B.dl("wrt"))
            brt = B.const_tile("brt", [NEXP], F32, B.dl("brt"))
            work = (A.alloc("acc", [8, D], F32), A.ring("actc", 2, [4, 1024], BF16), A.ring("sge", 2, [512], BF16), A.alloc("gates", [8, NEXP], F32),
                    A.ring("lg", 2, [5, 8], F32),
                    (A.ring("mixtE", 1, [D], F32), A.ring("smE", 2, [4], F32), A.ring("xresE", 2, [D], F32), A.ring("sqE", 1, [512], BF16)), wrt, brt)
            def _mk3(bi_, defer):
                h_ = h2r.next()
                src_ = [(xmid[(8 * bi_ + i) * 128:(8 * bi_ + i + 1) * 128, :], xmid_b) for i in range(8)]
                B.make_hT(src_, h_, 0, 1, rings, defer=defer)
                return h_, src_
            nxt = _mk3(0, False)
            for bi in range(4):
                B.flush()
                h2T, src = nxt
                if bi + 1 < 4:
                    nxt = _mk3(bi + 1, True)
                B.ffn_moe_block(h2T, wr, work, src, [xo[(8 * bi + i) * 128:(8 * bi + i + 1) * 128, :] for i in range(8)], xo_b)
        P.barrier()
        A.release(m_top)
        if stop == "l0" and l == 0:
            P.dma("sp", xo[0:256, :], xhalo, reads=[xhalo_b], writes=[xo_b], is_output=True)
            P.dma("sp", xo[256:TOK, :], x1[256:TOK, :], reads=[x1_b], writes=[xo_b], is_output=True)
            P.finish()
            return B
    P.finish()
    return B


_FUSED = {}


def kernel(**inputs):
    tabs = _static_tables()
    x = np.asarray(inputs["x"], np.float32)
    com = {}
    for l in range(2):
        for k, v in _layer_common(inputs, l).items():
            com[k + str(l)] = v
    maps = []
    for cid in range(8):
        b, j = cid // 4, cid % 4
        m = dict(com)
        for nm in ("ident", "rt", "t1", "cbsb", "t256"):
            m[nm] = tabs[nm]
        for nm in ("masks", "valid", "ropeC", "ropeS", "etab"):
            m[nm] = tabs[(nm, j)]
        m["ccols"] = _ccols(inputs, b)
        m["xh"] = _xh(x, cid)
        m["hctx"] = np.ascontiguousarray(np.asarray(inputs["ctx"][b], np.float32))
        sel = np.zeros((128, 8), np.float32)
        if j - 1 >= 0:
            sel[:, j - 1] = 1.0
        if j + 1 <= 3:
            sel[:, 4 + j + 1] = 1.0
        m["selh"] = sel
        maps.append(m)
    if "B" not in _FUSED:
        _FUSED["B"] = build_fused()
    r = _run(_FUSED["B"], maps)
    out = np.empty_like(x)
    for cid in range(8):
        b, j = cid // 4, cid % 4
        out[b, TOK * j:TOK * (j + 1)] = np.asarray(r[cid]["xo"], np.float32)
    return out
```

```python
import contextlib
import math
import numpy as np
import ml_dtypes
import concourse.bass as bass
import concourse.mybir as mybir
from concourse.bass_utils import run_bass_kernel_spmd

F32 = mybir.dt.float32
BF16 = mybir.dt.bfloat16
AF = mybir.ActivationFunctionType
ALU = mybir.AluOpType
AX = mybir.AxisListType

ENGS = ("pe", "act", "dve", "pool", "sp")
SEM_EPOCH = 30000
SAME_ENGINE_SYNC = True
DEBUG_SCRATCH = False

D = 1024
SEQ = 16384
NB = 2
TOK = 4096
NTILE = 32
CTX = 256
Q_OFF, K_OFF, V_OFF, F_OFF, CX_OFF, CB_OFF, CC_OFF, GATE_OFF = 0, 512, 640, 768, 1280, 1792, 2304, 2816
IN_DIM = 5888
D_FF = 2816
NEXP = 8
D_EXP = 3584
EPS = 1e-6
PA_F, PA_K, PA_V, PA_CX, PA_CC = 0, 512, 640, 768, 1280
PA_W = 1792
PM_Q, PM_CB, PM_G = 1792, 2304, 2816


class Buf:
    __slots__ = ("name", "last_write", "reads", "dkey")

    def __init__(self, name):
        self.name = name
        self.last_write = None
        self.reads = {}
        self.dkey = None


class Tile:
    __slots__ = ("ap", "b")

    def __init__(self, ap, b):
        self.ap = ap
        self.b = b


class _Rec:
    def __init__(self):
        self.calls = []

    def __getattr__(self, name):
        def f(*a, **kw):
            self.calls.append((name, a, kw))
            return None
        return f


class Prog:
    def __init__(self, nc):
        self.nc = nc
        self.stack = contextlib.ExitStack()
        self.q = {e: [] for e in ENGS}
        self.cnt = {e: 0 for e in ENGS}
        self.epoch = {e: 0 for e in ENGS}
        self.pending = {e: False for e in ENGS}
        self.last_tok = {e: None for e in ENGS}
        self.dcnt = {}
        self.waited = {e: {} for e in ENGS}
        self.semkeys = []
        self.sems = {}
        self.nbuf = 0
        self.out_tokens = []
        self.free_dkeys = {"sp": [], "pool": [], "act": []}
        self.dma_bufs = []

    def sbuf(self, name, shape, dtype):
        return self.stack.enter_context(self.nc.sbuf_tensor(name, list(shape), dtype))

    def psum(self, name, shape, dtype=F32):
        return self.stack.enter_context(self.nc.psum_tensor(name, list(shape), dtype))

    def buf(self, name=None):
        self.nbuf += 1
        return Buf(name or f"b{self.nbuf}")

    def _semkey(self, k):
        if k not in self.sems:
            self.sems[k] = None
            self.semkeys.append(k)
        return k

    @staticmethod
    def _deps(reads, writes):
        deps = []
        for b in reads:
            if b.last_write is not None:
                deps.append(b.last_write)
        for b in writes:
            if b.last_write is not None:
                deps.append(b.last_write)
            deps.extend(b.reads.items())
        return deps

    def _emit_waits(self, eng, deps):
        need = {}
        for (k, v) in deps:
            if k[0] == eng and (eng == "pe" or not SAME_ENGINE_SYNC):
                continue
            if v > need.get(k, 0):
                need[k] = v
        w = self.waited[eng]
        for k, v in need.items():
            if w.get(k, 0) >= v:
                continue
            w[k] = v
            self._semkey(k)
            self.q[eng].append(("wait", k, v))

    @staticmethod
    def _mark(tok, reads, writes):
        k, v = tok
        for b in reads:
            if b.reads.get(k, 0) < v:
                b.reads[k] = v
        for b in writes:
            b.last_write = tok
            b.reads = {}

    def op(self, eng, fn, reads=(), writes=(), signal=True):
        reads = [t.b if isinstance(t, Tile) else t for t in reads]
        writes = [t.b if isinstance(t, Tile) else t for t in writes]
        self._emit_waits(eng, self._deps(reads, writes))
        if self.cnt[eng] >= SEM_EPOCH and signal and not self.pending[eng]:
            self.epoch[eng] += 1
            self.cnt[eng] = 0
        self.pending[eng] = not signal
        key = (eng, self.epoch[eng])
        self._semkey(key)
        if signal:
            self.cnt[eng] += 1
            tok = (key, self.cnt[eng])
        else:
            tok = (key, self.cnt[eng] + 1)
        rec = _Rec()
        fn(rec)
        assert len(rec.calls) == 1
        self.q[eng].append(("op", rec.calls[0], key if signal else None))
        self._mark(tok, reads, writes)
        self.last_tok[eng] = tok
        return tok

    def dma(self, qeng, out, in_, reads=(), writes=(), is_output=False):
        reads = [t.b if isinstance(t, Tile) else t for t in reads]
        writes = [t.b if isinstance(t, Tile) else t for t in writes]
        self._emit_waits(qeng, self._deps(reads, writes))
        sb = writes[0] if writes else reads[0]
        if sb.dkey is None or sb.dkey[2] != qeng or self.dcnt.get(sb.dkey, 0) >= SEM_EPOCH:
            fl = self.free_dkeys[qeng]
            while fl and self.dcnt.get(fl[-1], 0) >= SEM_EPOCH:
                fl.pop()
            if fl:
                sb.dkey = fl.pop()
            else:
                self.nbuf += 1
                sb.dkey = ("dma", self.nbuf, qeng)
            self.dma_bufs.append(sb)
        k = sb.dkey
        self._semkey(k)
        self.dcnt[k] = self.dcnt.get(k, 0) + 16
        tok = (k, self.dcnt[k])
        self.q[qeng].append(("dma", (out, in_), k))
        self._mark(tok, reads, writes)
        if is_output:
            self.out_tokens.append(tok)
        return tok

    def cc_allgather(self, out, in_, groups, reads=(), writes=()):
        reads = [t.b if isinstance(t, Tile) else t for t in reads]
        writes = [t.b if isinstance(t, Tile) else t for t in writes]
        self._emit_waits("pool", self._deps(reads, writes))
        self.nbuf += 1
        k = ("cc", self.nbuf)
        self._semkey(k)
        self.dcnt[k] = 1
        tok = (k, 1)
        self.q["pool"].append(("cc", (out, in_, groups), k))
        self._mark(tok, reads, writes)
        return tok

    def barrier(self):
        toks = [t for t in self.last_tok.values() if t is not None]
        toks += [(k, v) for k, v in self.dcnt.items()]
        for e in ENGS:
            self._emit_waits(e, [t for t in toks if not (t[0][0] == e and e in ("pe", "sp", "pool"))])
        seen = set()
        for k in list(self.dcnt.keys()):
            if k[0] == "dma" and k not in seen and all(k not in fl for fl in self.free_dkeys.values()):
                seen.add(k)
                self.free_dkeys[k[2]].append(k)
        for b in self.dma_bufs:
            b.dkey = None
        self.dma_bufs = []

    def finish(self):
        self._emit_waits("sp", self.out_tokens)
        nc = self.nc
        for i, k in enumerate(self.semkeys):
            self.sems[k] = self.stack.enter_context(nc.semaphore(f"s{i}_{k[0]}"))
        sems = self.sems
        q = self.q

        def replay(eng_name):
            def body(e):
                for item in q[eng_name]:
                    if item[0] == "wait":
                        e.wait_ge(sems[item[1]], item[2])
                    elif item[0] == "op":
                        nm, a_, kw_ = item[1]
                        ins = getattr(e, nm)(*a_, **kw_)
                        if item[2] is not None:
                            ins.then_inc(sems[item[2]], 1)
                    elif item[0] == "cc":
                        o, i_, grp = item[1]
                        e.collective_compute("AllGather", ALU.bypass, replica_groups=grp, ins=[i_.opt()], outs=[o.opt()]).then_inc(sems[item[2]], 1)
                    else:
                        o, i_ = item[1]
                        e.dma_start(out=o, in_=i_).then_inc(sems[item[2]], 16)
            return body

        with nc.Block() as block:
            block.tensor(replay("pe"))
            block.scalar(replay("act"))
            block.vector(replay("dve"))
            block.gpsimd(replay("pool"))
            block.sync(replay("sp"))
        self.stack.close()

    def stats(self):
        return {e: len(self.q[e]) for e in ENGS}, len(self.semkeys)


def _prod(s):
    r = 1
    for x in s:
        r *= x
    return r


class Arena:
    def __init__(self, P, nwords):
        self.P = P
        self.t = P.sbuf("arena", [128, nwords], F32)
        self.nwords = nwords
        self.top = 0
        self.peak = 0

    def alloc(self, name, shape, dtype):
        n = _prod(shape)
        nbytes = n * (4 if dtype == F32 else 2)
        words = ((nbytes + 31) // 32) * 8
        off = self.top
        self.top += words
        self.peak = max(self.peak, self.top)
        assert self.top <= self.nwords, f"arena overflow at {name}: {self.top} > {self.nwords}"
        v = self.t[:, off:off + words]
        if dtype != F32:
            v = v.bitcast(dtype)
        v = v[:, :n]
        if len(shape) == 2:
            v = v.rearrange("p (a b) -> p a b", a=shape[0])
        elif len(shape) == 3:
            v = v.rearrange("p (a b c) -> p a b c", a=shape[0], b=shape[1])
        elif len(shape) == 4:
            v = v.rearrange("p (a b c d) -> p a b c d", a=shape[0], b=shape[1], c=shape[2])
        return Tile(v, self.P.buf(name))

    def ring(self, name, n, shape, dtype):
        return Ring([self.alloc(f"{name}{i}", shape, dtype) for i in range(n)])

    def mark(self):
        return self.top

    def release(self, m):
        self.top = m


class Ring:
    def __init__(self, tiles):
        self.tiles = tiles
        self.i = 0

    def next(self):
        t = self.tiles[self.i % len(self.tiles)]
        self.i += 1
        return t


class Builder:
    def __init__(self, kind, layer):
        self.kind = kind
        self.layer = layer
        self.last = layer == 1
        self.sfx = ""
        self.nc = bass.Bass("TRN2", target_bir_lowering=False)
        self.P = Prog(self.nc)
        self.din = {}
        self.A = Arena(self.P, 52000)
        banks = [self.P.psum(f"ps{i}", [128, 512], F32) for i in range(8)]
        self.ps = Ring([Tile(b[:], self.P.buf(f"ps{i}")) for i, b in enumerate(banks)])
        self.pending = []

    def inp(self, name, shape, dtype=F32):
        if name in self.din:
            return self.din[name]
        t = self.nc.dram_tensor(name, list(shape), dtype, kind="ExternalInput").ap()
        self.din[name] = t
        return t

    def outp(self, name, shape, dtype=F32):
        return self.nc.dram_tensor(name, list(shape), dtype, kind="ExternalOutput").ap()

    def scratch(self, name, shape, dtype):
        kind = "ExternalOutput" if DEBUG_SCRATCH else "Internal"
        return self.nc.dram_tensor(name, list(shape), dtype, kind=kind).ap()

    def dl(self, name):
        return self.din[name + self.sfx]

    def load(self, tile, src, q="sp", dep=None):
        self.P.dma(q, tile.ap, src, reads=([dep] if dep is not None else []), writes=[tile])

    def const_tile(self, name, shape, dtype, src, q=None):
        t = self.A.alloc(name, shape, dtype)
        self.load(t, src, q or ("pool" if dtype != F32 else "sp"))
        return t

    def ps_bf16(self, bank, shape):
        v = bank.ap.bitcast(BF16)
        if len(shape) == 2:
            return v.rearrange("p (a b) -> p a b", a=shape[0])
        return v

    def emit_mod(self):
        P, A, nc = self.P, self.A, self.nc
        ccols = self.din["ccols"] if "ccols" in self.din else self.inp("ccols", [128, 8, 2])
        wmod = self.inp("wmod" + self.sfx, [D, 6 * D])
        bcols = self.inp("bcols" + self.sfx, [128, 48])
        brow = self.inp("brow" + self.sfx, [128, 2, D])
        gpm = self.inp("gpm_c" + self.sfx, [128, 8])
        gpf = self.inp("gpf_c" + self.sfx, [128, 8])
        gqm = self.inp("gqm_r" + self.sfx, [128, D])
        gqf = self.inp("gqf_r" + self.sfx, [128, D])
        self.alloc_mod_tiles()
        m0 = A.mark()
        cc = A.alloc("cc", [8, 2], F32)
        self.load(cc, ccols)
        bc = A.alloc("bc", [48], F32)
        self.load(bc, bcols)
        br = A.alloc("br", [2, D], F32)
        self.load(br, brow)
        gq = [A.alloc("gqm", [D], F32), A.alloc("gqf", [D], F32)]
        self.load(gq[0], gqm)
        self.load(gq[1], gqf)
        gp = [A.alloc("gpm", [8], F32), A.alloc("gpf", [8], F32)]
        self.load(gp[0], gpm)
        self.load(gp[1], gpf)
        sc = A.alloc("silu_c", [8, 2], F32)
        P.op("act", lambda e: e.activation(out=sc.ap, in_=cc.ap, func=AF.Silu), reads=[cc], writes=[sc])
        srep = A.alloc("srep", [8, 2, 128], F32)
        P.op("dve", lambda e: e.tensor_copy(out=srep.ap, in_=sc.ap.unsqueeze(3).broadcast_to([128, 8, 2, 128])),
             reads=[sc], writes=[srep])
        wring = A.ring("wm", 2, [8, 512], F32)
        modr = A.alloc("modr", [48, 2], F32)
        P.op("dve", lambda e: e.memset(modr.ap, 0.0), writes=[modr])
        for part in range(6):
            for hf in range(2):
                wm = wring.next()
                c0 = part * 1024 + hf * 512
                self.load(wm, wmod[:, c0:c0 + 512].rearrange("(k p) n -> p k n", p=128))
                if part in (2, 5):
                    gi = 0 if part == 2 else 1
                    for s in range(2):
                        pr = self.ps.next()
                        for k in range(8):
                            P.op("pe", lambda e, k=k, s=s, pr=pr, wm=wm: e.matmul(
                                out=pr.ap, lhsT=srep.ap[:, k, s, :], rhs=wm.ap[:, k, :], start=(k == 0), stop=(k == 7)),
                                reads=[srep, wm], writes=[pr], signal=(k == 7))
                        dst = self.gaG[s][gi]
                        cs = slice(hf * 512, hf * 512 + 512)
                        P.op("dve", lambda e, pr=pr, dst=dst, cs=cs, gi=gi: e.tensor_tensor(
                            out=dst.ap[:, cs], in0=pr.ap, in1=br.ap[:, gi, cs], op=ALU.add), reads=[pr, br], writes=[dst])
                        P.op("dve", lambda e, dst=dst, cs=cs, gi=gi: e.tensor_tensor(
                            out=dst.ap[:, cs], in0=dst.ap[:, cs], in1=gq[gi].ap[:, cs], op=ALU.mult),
                            reads=[dst, gq[gi]], writes=[dst])
                else:
                    pcb = self.ps.next()
                    pcbv = pcb.ap.rearrange("p (m n) -> p m n", m=4)
                    for m4 in range(4):
                        for k in range(8):
                            P.op("pe", lambda e, k=k, m4=m4, wm=wm, pcbv=pcbv: e.matmul(
                                out=pcbv[:, m4, :].rearrange("p (s n) -> p s n", s=2), lhsT=wm.ap[:, k, m4 * 128:(m4 + 1) * 128],
                                rhs=srep.ap[:, k, :, 0:64], start=(k == 0), stop=(k == 7)), reads=[wm, srep], writes=[pcb],
                                signal=(k == 7 and m4 == 3))
                    m_0 = part * 8 + hf * 4
                    P.op("dve", lambda e, m_0=m_0, pcbv=pcbv: e.tensor_copy(
                        out=modr.ap[:, m_0:m_0 + 4, :], in_=pcbv.rearrange("p m (s n) -> p m s n", s=2)[:, :, :, 0]),
                        reads=[pcb], writes=[modr])
        modc = A.alloc("modc", [48, 2], F32)
        self.dbg_modc = modc
        P.op("dve", lambda e: e.tensor_tensor(out=modc.ap, in0=modr.ap, in1=bc.ap.unsqueeze(2).broadcast_to([128, 48, 2]),
                                              op=ALU.add), reads=[modr, bc], writes=[modc])
        for s in range(2):
            for sub in range(2):
                shp, scp = (0, 1) if sub == 0 else (3, 4)
                P.op("dve", lambda e, s=s, sub=sub, shp=shp: e.tensor_copy(
                    out=self.shA.ap[:, s, sub, :], in_=modc.ap[:, shp * 8:shp * 8 + 8, s]), reads=[modc], writes=[self.shA])
                P.op("dve", lambda e, s=s, sub=sub, scp=scp: e.scalar_tensor_tensor(
                    out=self.scA.ap[:, s, sub, :], in0=modc.ap[:, scp * 8:scp * 8 + 8, s], scalar=1.0,
                    in1=gp[sub].ap, op0=ALU.add, op1=ALU.mult), reads=[modc, gp[sub]], writes=[self.scA])
        if getattr(self, "dbg_out", None) is not None:
            P.dma("sp", self.dbg_out, modc.ap.rearrange("p a b -> p (a b)"), reads=[modc], writes=[P.buf("dbgo")], is_output=True)
        P.barrier()
        A.release(m0)

    def emit_consts(self):
        A = self.A
        ident = self.inp("ident", [128, 128])
        self.ident = self.const_tile("ident", [128], BF16, ident)
        if self.kind == "main":
            self.rt = self.const_tile("rt", [128], BF16, self.inp("rt", [128, 128]))
            masks = self.inp("masks", [128, 4, 128])
            self.masks = self.const_tile("masks", [4, 128], BF16, masks)
            self.valid = self.const_tile("valid", [2], F32, self.inp("valid", [128, 2]))
            if self.sfx == "":
                self.emit_layer_consts()
            self.cbsb = self.const_tile("cbsb", [2, 128], BF16, self.inp("cbsb", [128, 2, 128]))

    def alloc_mod_tiles(self):
        A = self.A
        if not hasattr(self, "scA"):
            self.scA = A.alloc("scA", [2, 2, 8], F32)
            self.shA = A.alloc("shA", [2, 2, 8], F32)
            self.gaG = [[A.alloc(f"gaG{s}{g}", [D], F32) for g in range(2)] for s in range(2)]

    def emit_layer_consts(self):
        A = self.A
        es = self.const_tile("esink_src", [8], F32, self.inp("sink_b" + self.sfx, [128, 8]))
        self.esink = A.alloc("esink", [8], F32)
        self.P.op("act", lambda e: e.activation(out=self.esink.ap, in_=es.ap, func=AF.Exp), reads=[es], writes=[self.esink])
        self.wconv = self.const_tile("wconv", [4, 3], F32, self.inp("wconv_c" + self.sfx, [128, 4, 3]))

    def make_hT_dep(self, tiles_src, hT, s, sub, rings, dep):
        return self.make_hT(tiles_src, hT, s, sub, rings, dep)

    def make_hT(self, tiles_src, hT, s, sub, rings, dep=None, defer=False):
        eager = defer and len(rings[0].tiles) >= len(tiles_src)
        for i, src in enumerate(tiles_src):
            xt = None
            if eager:
                xt = rings[0].next()
                if isinstance(src, tuple):
                    self.load(xt, src[0], dep=src[1])
                else:
                    self.load(xt, src, dep=dep)
            fn = (lambda i=i, src=src, xt=xt: self._hT_tile(i, src, hT, s, sub, rings, dep, xt))
            if defer:
                self.pending.append(fn)
            else:
                fn()

    def tick(self, n=1):
        for _ in range(n):
            if self.pending:
                self.pending.pop(0)()

    def flush(self):
        while self.pending:
            self.pending.pop(0)()

    def _hT_tile(self, i, src, hT, s, sub, rings, dep, xt=None):
        P = self.P
        xr, xnr, sqr, smr = rings
        if xt is None:
            xt = xr.next()
            if isinstance(src, tuple):
                self.load(xt, src[0], dep=src[1])
            else:
                self.load(xt, src, dep=dep)
        sq = sqr.next()
        sm = smr.next()
        P.op("act", lambda e: e.activation(out=sq.ap, in_=xt.ap, func=AF.Square, accum_out=sm.ap[:, 0:1]), reads=[xt], writes=[sq, sm])
        P.op("act", lambda e: e.activation(out=sm.ap[:, 1:2], in_=sm.ap[:, 0:1], func=AF.Sqrt, scale=1.0 / D, bias=EPS), reads=[sm], writes=[sm])
        P.op("dve", lambda e: e.reciprocal(out=sm.ap[:, 1:2], in_=sm.ap[:, 1:2]), reads=[sm], writes=[sm])
        xn = xnr.next()
        P.op("dve", lambda e: e.tensor_scalar(out=xn.ap, in0=xt.ap, scalar1=sm.ap[:, 1:2], scalar2=None, op0=ALU.mult), reads=[xt, sm], writes=[xn])
        pt = self.ps.next()
        ptv = self.ps_bf16(pt, [8, 128])
        for k in range(8):
            P.op("pe", lambda e, k=k: e.transpose(out=ptv[:, k, :], in_=xn.ap[:, k * 128:(k + 1) * 128], identity=self.ident.ap),
                 reads=[xn, self.ident], writes=[pt], signal=(k == 7))
        for k in range(4):
            P.op("act", lambda e, k=k: e.activation(
                out=hT.ap[:, k, i * 128:(i + 1) * 128], in_=ptv[:, k, :], func=AF.Identity,
                bias=self.shA.ap[:, s, sub, k:k + 1], scale=self.scA.ap[:, s, sub, k:k + 1]),
                reads=[pt, self.shA, self.scA], writes=[hT])
        for k in range(4, 8):
            P.op("dve", lambda e, k=k: e.scalar_tensor_tensor(
                out=hT.ap[:, k, i * 128:(i + 1) * 128], in0=ptv[:, k, :], scalar=self.scA.ap[:, s, sub, k:k + 1],
                in1=self.shA.ap[:, s, sub, k:k + 1].broadcast_to([128, 128]), op0=ALU.mult, op1=ALU.add),
                reads=[pt, self.shA, self.scA], writes=[hT])

    def norm_rings(self, nx=4):
        A = self.A
        return (A.ring("xt", nx, [D], F32), A.ring("xn", 2, [D], BF16), A.ring("sq", 1, [D], BF16), A.ring("sm", 4, [2], F32))

    def proj_fm(self, w, col0, hT, ntok, nk=8):
        P = self.P
        pr = self.ps.next()
        for k in range(nk):
            P.op("pe", lambda e, k=k, pr=pr: e.matmul(out=pr.ap[:, :ntok], lhsT=w.ap[:, k, col0:col0 + 128], rhs=hT.ap[:, k, :ntok],
                                                   start=(k == 0), stop=(k == nk - 1)), reads=[w, hT], writes=[pr], signal=(k == nk - 1))
        return pr

    def rope(self, pr, raw, dst_ap, dst_tile, cosT, sinT, ntok, tmp):
        P = self.P
        P.op("act", lambda e: e.copy(out=raw.ap[:, :ntok], in_=pr.ap[:, :ntok]), reads=[pr], writes=[raw])
        p2 = self.ps.next()
        P.op("pe", lambda e: e.matmul(out=p2.ap[:, :ntok], lhsT=self.rt.ap, rhs=raw.ap[:, :ntok], start=True, stop=True),
             reads=[self.rt, raw], writes=[p2])
        t1, t2 = tmp
        P.op("dve", lambda e: e.tensor_tensor(out=t1.ap[:, :ntok], in0=pr.ap[:, :ntok], in1=cosT, op=ALU.mult), reads=[pr, self.ropeb, raw], writes=[t1])
        P.op("dve", lambda e: e.tensor_tensor(out=t2.ap[:, :ntok], in0=p2.ap[:, :ntok], in1=sinT, op=ALU.mult), reads=[p2, self.ropeb], writes=[t2])
        P.op("dve", lambda e: e.tensor_tensor(out=dst_ap, in0=t1.ap[:, :ntok], in1=t2.ap[:, :ntok], op=ALU.add), reads=[t1, t2], writes=[dst_tile])

    def emit_phaseA(self, xh, want_zf, zf_out=None):
        P, A = self.P, self.A
        main = self.kind == "main"
        winP = self.dl("winP")
        m0 = A.mark()
        ncolA = PA_W if main else 512
        wA = A.alloc("wA", [8, ncolA], BF16)
        self.load(wA, winP[:, 0:ncolA].rearrange("(k p) n -> p k n", p=128), "pool")
        rings = self.norm_rings(4)
        hTr = A.ring("hT", 2, [8, 512], BF16)
        zfr = A.ring("zfs", 2, [512], BF16)
        if main:
            ropeC = self.din["ropeC"]
            ropeS = self.din["ropeS"]
            rope_r = A.ring("ropeCS", 2, [2, 512], F32)
            kraw = A.alloc("kraw", [512], BF16)
            tmp = (A.alloc("rt1", [512], F32), A.alloc("rt2", [512], F32))
            cxs = A.ring("cxs", 2, [512], F32)
            ust = A.ring("ust", 2, [4, 512], BF16)
        blocks = []
        if main:
            blocks.append((-1, 1))
        for bi in range(8):
            blocks.append((bi * 4, 4))
        if main:
            blocks.append((32, 1))
        def _mk(bidx, defer):
            t0_, nt_ = blocks[bidx]
            hT_ = hTr.next()
            if xh is None:
                srcs_ = [self.xtile(t0_ + i) for i in range(nt_)]
            else:
                srcs_ = [xh[(t0_ + 1 + i) * 128:(t0_ + 2 + i) * 128, :] for i in range(nt_)]
            self.make_hT(srcs_, hT_, 0, 0, rings, defer=defer)
            return hT_
        hT_next = _mk(0, False)
        for bidx, (t0, nt) in enumerate(blocks):
            ntok = nt * 128
            halo = nt == 1
            self.flush()
            hT = hT_next
            if bidx + 1 < len(blocks):
                hT_next = _mk(bidx + 1, True)
            if want_zf and not halo:
                for i in range(nt):
                    pr = self.ps.next()
                    for k in range(8):
                        P.op("pe", lambda e, k=k, i=i, pr=pr, hT=hT: e.matmul(out=pr.ap, lhsT=hT.ap[:, k, i * 128:(i + 1) * 128],
                                                                     rhs=wA.ap[:, k, PA_F:PA_F + 512], start=(k == 0), stop=(k == 7)),
                             reads=[hT, wA], writes=[pr], signal=(k == 7))
                    zs = zfr.next()
                    P.op("act", lambda e, pr=pr, zs=zs: e.copy(out=zs.ap, in_=pr.ap), reads=[pr], writes=[zs])
                    tg = t0 + i
                    if i % 2 == 1:
                        self.tick()
                    if isinstance(zf_out, list):
                        for g_ in range(4):
                            P.dma("pool", zf_out[g_][tg * 128:(tg + 1) * 128, :], zs.ap[:, g_ * 128:(g_ + 1) * 128], reads=[zs], writes=[self.zf_b])
                    else:
                        P.dma("sp", zf_out[:, tg * 128:(tg + 1) * 128, :].rearrange("g t c -> t g c"),
                              zs.ap.rearrange("p (g c) -> p g c", g=4), reads=[zs], writes=[self.zf_b], is_output=(self.kind == "pre"))
            if not main:
                continue
            c0 = (t0 + 1) * 128
            rp = rope_r.next()
            P.dma("sp", rp.ap[:, 0, :ntok], ropeC[:, c0:c0 + ntok], writes=[rp])
            P.dma("sp", rp.ap[:, 1, :ntok], ropeS[:, c0:c0 + ntok], writes=[rp])
            self.ropeb = rp
            pr = self.proj_fm(wA, PA_K, hT, ntok)
            self.rope(pr, kraw, self.kT.ap[:, c0:c0 + ntok], self.kT, rp.ap[:, 0, :ntok], rp.ap[:, 1, :ntok], ntok, tmp)
            pv = self.ps.next()
            pvv = pv.ap.rearrange("p (i c) -> p i c", i=4)
            for i in range(nt):
                for k in range(8):
                    P.op("pe", lambda e, k=k, i=i, hT=hT: e.matmul(out=pvv[:, i, :], lhsT=hT.ap[:, k, i * 128:(i + 1) * 128],
                                                                 rhs=wA.ap[:, k, PA_V:PA_V + 128], start=(k == 0), stop=(k == 7)),
                         reads=[hT, wA], writes=[pv], signal=(k == 7 and i == nt - 1))
            P.op("act", lambda e, t0=t0, nt=nt: e.copy(
                out=self.vaug.ap[:, t0 + 1:t0 + 1 + nt, :, 0:64],
                in_=pvv[:, 0:nt, :].rearrange("p i (g d) -> p i g d", g=2)), reads=[pv], writes=[self.vaug])
            self.tick()
            us = ust.next()
            for m in range(4):
                if m == 2:
                    self.tick()
                px = self.proj_fm(wA, PA_CX + m * 128, hT, ntok)
                cx = cxs.next()
                P.op("act", lambda e, px=px, cx=cx: e.copy(out=cx.ap[:, :ntok], in_=px.ap[:, :ntok]), reads=[px], writes=[cx])
                pc = self.proj_fm(wA, PA_CC + m * 128, hT, ntok)
                if halo:
                    vi = 0 if t0 < 0 else 1
                    P.op("dve", lambda e, m=m, pc=pc, cx=cx, us=us, vi=vi: e.scalar_tensor_tensor(
                        out=us.ap[:, m, :ntok], in0=cx.ap[:, :ntok], scalar=self.valid.ap[:, vi:vi + 1], in1=pc.ap[:, :ntok],
                        op0=ALU.mult, op1=ALU.mult), reads=[cx, pc, self.valid], writes=[us])
                else:
                    P.op("dve", lambda e, m=m, pc=pc, cx=cx, us=us: e.tensor_tensor(
                        out=us.ap[:, m, :ntok], in0=cx.ap[:, :ntok], in1=pc.ap[:, :ntok], op=ALU.mult), reads=[cx, pc], writes=[us])
            P.dma("pool", self.u_d[:, :, c0:c0 + ntok], us.ap[:, :, :ntok], reads=[us], writes=[self.u_b])
        self.flush()
        P.barrier()
        A.release(m0)

    def emit_fft(self, zfg):
        P, A = self.P, self.A
        m0 = A.mark()
        t1 = self.const_tile("t1", [2, 2, 64], BF16, self.inp("t1", [128, 2, 2, 64]))
        etab_d = self.inp("etab", [128, 128, 96])
        et = A.alloc("etab", [128, 96], BF16)
        for q4 in range(4):
            P.dma("pool", et.ap[:, q4 * 32:(q4 + 1) * 32, :], etab_d[:, q4 * 32:(q4 + 1) * 32, :], writes=[et])
        Ur = A.ring("U", 1, [128, 128], BF16)
        Y = A.alloc("Y", [128, 2, 64], BF16)
        G = A.alloc("G", [2, 4096], BF16)
        FTs = A.ring("FTs", 2, [4096], BF16)
        scale = 1.0 / math.sqrt(SEQ * 128.0)
        ev = 0
        for g in range(4):
            U = Ur.next()
            for r in range(4):
                zsrc_ = zfg(r, g) if callable(zfg) else zfg[r, g]
                P.dma("sp", U.ap[r * 32:(r + 1) * 32, :, :], zsrc_.rearrange("(th tl) c -> th tl c", tl=128),
                      reads=([self.zfg_b] if getattr(self, "zfg_b", None) is not None else []), writes=[U])
            for hh in range(2):
                for c4 in range(32):
                    pr = self.ps.next()
                    prv = pr.ap.rearrange("p (c x) -> p c x", c=4)
                    for ci in range(4):
                        c = c4 * 4 + ci
                        P.op("pe", lambda e, c=c, ci=ci, hh=hh, prv=prv: e.matmul(
                            out=prv[:, ci, :], lhsT=U.ap[:, :, c], rhs=t1.ap[:, hh, :, :].rearrange("p r k -> p (r k)"),
                            start=True, stop=True), reads=[U, t1], writes=[pr], signal=(ci == 3))
                    eng = "act" if ev % 2 == 0 else "dve"
                    ev += 1
                    dst = Y.ap[:, c4 * 4:(c4 + 1) * 4, :, :].rearrange("p c r k -> p c (r k)")
                    if eng == "act":
                        P.op("act", lambda e, dst=dst, prv=prv: e.copy(out=dst, in_=prv), reads=[pr], writes=[Y])
                    else:
                        P.op("dve", lambda e, dst=dst, prv=prv: e.tensor_copy(out=dst, in_=prv), reads=[pr], writes=[Y])
                for k8 in range(8):
                    pr = self.ps.next()
                    prv = pr.ap.rearrange("p (k r x) -> p k r x", k=8, r=2)
                    for ki in range(8):
                        k1l = k8 * 8 + ki
                        k1 = hh * 64 + k1l
                        P.op("pe", lambda e, k1=k1, k1l=k1l, ki=ki, prv=prv: e.matmul(
                            out=prv[:, ki, :, :].rearrange("p r x -> p (r x)"), lhsT=Y.ap[:, :, 0, k1l], rhs=et.ap[:, k1, 32:96],
                            start=True, stop=False), reads=[Y, et], writes=[pr], signal=False)
                        P.op("pe", lambda e, k1=k1, k1l=k1l, ki=ki, prv=prv: e.matmul(
                            out=prv[:, ki, :, :].rearrange("p r x -> p (r x)"), lhsT=Y.ap[:, :, 1, k1l], rhs=et.ap[:, k1, 0:64],
                            start=False, stop=True), reads=[Y, et], writes=[pr], signal=(ki == 7))
                    k10 = hh * 64 + k8 * 8
                    for ri in range(2):
                        dst = G.ap[:, ri, :].rearrange("p (k2 k1) -> p k1 k2", k1=128)[:, k10:k10 + 8, :]
                        if ri == 0:
                            P.op("act", lambda e, dst=dst, prv=prv, ri=ri: e.copy(out=dst, in_=prv[:, :, ri, :]), reads=[pr], writes=[G])
                        else:
                            P.op("dve", lambda e, dst=dst, prv=prv, ri=ri: e.tensor_copy(out=dst, in_=prv[:, :, ri, :]), reads=[pr], writes=[G])
            ft = FTs.next()
            for cb in range(8):
                pr = self.ps.next()
                cs = slice(cb * 512, (cb + 1) * 512)
                P.op("pe", lambda e, pr=pr, cs=cs: e.matmul(out=pr.ap, lhsT=self.cbsb.ap[:, 0, :], rhs=G.ap[:, 0, cs], start=True, stop=False),
                     reads=[self.cbsb, G], writes=[pr], signal=False)
                P.op("pe", lambda e, pr=pr, cs=cs: e.matmul(out=pr.ap, lhsT=self.cbsb.ap[:, 1, :], rhs=G.ap[:, 1, cs], start=False, stop=True),
                     reads=[self.cbsb, G], writes=[pr])
                P.op("act", lambda e, pr=pr, cs=cs, ft=ft: e.activation(out=ft.ap[:, cs], in_=pr.ap, func=AF.Copy, scale=scale), reads=[pr], writes=[ft])
            P.dma("sp", self.ft_d[:, g, :], ft.ap, reads=[ft], writes=[self.ft_b])
        P.barrier()
        A.release(m0)

    def attention_tile(self, qT, qcol0, keyblocks, attnT, acol0, bufs):
        st = self.attention_scores(qT, qcol0, keyblocks, bufs)
        self.attention_pv(st, keyblocks, attnT, acol0, bufs)

    def attention_scores(self, qT, qcol0, keyblocks, bufs):
        P = self.P
        pTr, atok_r, den_r = bufs
        pT = pTr.next()
        for kb, (kap, kt, vap, vt, mi) in enumerate(keyblocks):
            for g in range(2):
                pr = self.ps.next()
                P.op("pe", lambda e, g=g, kap=kap, pr=pr: e.matmul(
                    out=pr.ap.rearrange("p (h q) -> p h q", h=4), lhsT=kap[g * 64:(g + 1) * 64, :],
                    rhs=qT.ap[g * 64:(g + 1) * 64, :, qcol0:qcol0 + 128], start=True, stop=True),
                    reads=[kt, qT], writes=[pr])
                P.op("act", lambda e, g=g, kb=kb, pr=pr, pT=pT: e.activation(
                    out=pT.ap[:, kb, g, :, :], in_=pr.ap.rearrange("p (h q) -> p h q", h=4), func=AF.Exp, scale=0.125),
                    reads=[pr], writes=[pT])
            if mi is not None:
                P.op("dve", lambda e, kb=kb, mi=mi, pT=pT: e.tensor_tensor(
                    out=pT.ap[:, kb, :, :, :].rearrange("p g h q -> p (g h) q"),
                    in0=pT.ap[:, kb, :, :, :].rearrange("p g h q -> p (g h) q"),
                    in1=self.masks.ap[:, mi, :].unsqueeze(1).broadcast_to([128, 8, 128]), op=ALU.mult),
                    reads=[pT, self.masks], writes=[pT])
        return pT

    def attention_pv(self, pT, keyblocks, attnT, acol0, bufs):
        P = self.P
        pTr, atok_r, den_r = bufs
        nkb = len(keyblocks)
        atok = atok_r.next()
        den = den_r.next()
        for b2 in range(2):
            po = self.ps.next()
            pov = po.ap[:, 0:260].rearrange("p (h x) -> p h x", h=4)
            for hh in range(4):
                for kb, (kap, kt, vap, vt, mi) in enumerate(keyblocks):
                    P.op("pe", lambda e, hh=hh, kb=kb, vap=vap, pov=pov, b2=b2, pT=pT: e.matmul(
                        out=pov[:, hh, :], lhsT=pT.ap[:, kb, b2, hh, :], rhs=vap[:, b2, :], start=(kb == 0), stop=(kb == nkb - 1)),
                        reads=[pT, vt], writes=[po], signal=(hh == 3 and kb == nkb - 1))
            hs = slice(b2 * 4, b2 * 4 + 4)
            P.op("dve", lambda e, pov=pov, hs=hs, den=den: e.tensor_tensor(out=den.ap[:, 0, hs], in0=pov[:, :, 64], in1=self.esink.ap[:, hs], op=ALU.add),
                 reads=[po, self.esink], writes=[den])
            P.op("dve", lambda e, hs=hs, den=den: e.reciprocal(out=den.ap[:, 1, hs], in_=den.ap[:, 0, hs]), reads=[den], writes=[den])
            P.op("dve", lambda e, pov=pov, hs=hs, den=den, atok=atok: e.tensor_tensor(
                out=atok.ap[:, hs, :], in0=pov[:, :, 0:64], in1=den.ap[:, 1, hs].unsqueeze(2).broadcast_to([128, 4, 64]), op=ALU.mult),
                reads=[po, den], writes=[atok])
        pt = self.ps.next()
        ptv = self.ps_bf16(pt, [8, 128])
        av = atok.ap.rearrange("p h d -> p (h d)")
        for kc in range(4):
            P.op("pe", lambda e, kc=kc, ptv=ptv: e.transpose(out=ptv[:, kc, :], in_=av[:, kc * 128:(kc + 1) * 128], identity=self.ident.ap),
                 reads=[atok, self.ident], writes=[pt], signal=(kc == 3))
        P.op("act", lambda e, ptv=ptv: e.copy(out=attnT.ap[:, 0:4, acol0:acol0 + 128], in_=ptv[:, 0:4, :]), reads=[pt], writes=[attnT])

    def mix_block(self, hT, ntok, qT, attnT, uap, u_tile, FTap, ft_tile, wr, work, x_tiles_src, x_out_dst, s, xout_buf, is_out=False):
        P = self.P
        winP = self.dl("winP")
        (yconvT, gsb_r, tmp_r, mergedT, mixt_r, sm_r, xres_r, sq_r) = work
        wcb = wr.next()
        self.load(wcb, winP[:, PM_CB:PM_CB + 512].rearrange("(k p) n -> p k n", p=128), "pool")
        for m in range(4):
            pb = self.proj_fm(wcb, m * 128, hT, ntok)
            t = tmp_r.next()
            P.op("dve", lambda e, m=m, t=t: e.tensor_scalar(out=t.ap[:, :ntok], in0=uap[:, m, 0:ntok], scalar1=self.wconv.ap[:, m, 0:1], scalar2=None, op0=ALU.mult),
                 reads=[u_tile, self.wconv], writes=[t])
            P.op("dve", lambda e, m=m, t=t: e.scalar_tensor_tensor(out=t.ap[:, :ntok], in0=uap[:, m, 1:ntok + 1], scalar=self.wconv.ap[:, m, 1:2],
                                                                 in1=t.ap[:, :ntok], op0=ALU.mult, op1=ALU.add), reads=[u_tile, self.wconv, t], writes=[t])
            P.op("dve", lambda e, m=m, t=t: e.scalar_tensor_tensor(out=t.ap[:, :ntok], in0=uap[:, m, 2:ntok + 2], scalar=self.wconv.ap[:, m, 2:3],
                                                                 in1=t.ap[:, :ntok], op0=ALU.mult, op1=ALU.add), reads=[u_tile, self.wconv, t], writes=[t])
            P.op("dve", lambda e, m=m, t=t, pb=pb: e.tensor_tensor(out=yconvT.ap[:, m, :ntok], in0=t.ap[:, :ntok], in1=pb.ap[:, :ntok], op=ALU.mult),
                 reads=[t, pb], writes=[yconvT])
        self.tick()
        if getattr(self, "mix_stop", None) == "conv":
            return
        wbr = self.wbr
        srcs = [(attnT.ap, attnT), (FTap, ft_tile), (yconvT.ap, yconvT)]
        for m in range(8):
            if m in (2, 5):
                self.tick()
            wg = wr.next()
            P.dma("pool", wg.ap[:, :, 0:384], winP[:, PM_G + m * 384:PM_G + (m + 1) * 384].rearrange("(k p) n -> p k n", p=128), writes=[wg])
            acc = None
            for r in range(3):
                pg = self.proj_fm(wg, r * 128, hT, ntok)
                gs = gsb_r.next()
                P.op("act", lambda e, pg=pg, gs=gs: e.activation(out=gs.ap[:, :ntok], in_=pg.ap[:, :ntok], func=AF.Sigmoid), reads=[pg], writes=[gs])
                sap, st = srcs[r]
                py = self.ps.next()
                for kc in range(4):
                    P.op("pe", lambda e, kc=kc, r=r, m=m, py=py, sap=sap: e.matmul(
                        out=py.ap[:, :ntok], lhsT=wbr[r].ap[:, kc, m * 128:(m + 1) * 128], rhs=sap[:, kc, 0:ntok], start=(kc == 0), stop=(kc == 3)),
                        reads=[wbr[r], st], writes=[py], signal=(kc == 3))
                if r == 0:
                    acc = tmp_r.next()
                    P.op("dve", lambda e, gs=gs, py=py, acc=acc: e.tensor_tensor(out=acc.ap[:, :ntok], in0=gs.ap[:, :ntok], in1=py.ap[:, :ntok], op=ALU.mult),
                         reads=[gs, py], writes=[acc])
                else:
                    t = tmp_r.next()
                    P.op("dve", lambda e, gs=gs, py=py, t=t: e.tensor_tensor(out=t.ap[:, :ntok], in0=gs.ap[:, :ntok], in1=py.ap[:, :ntok], op=ALU.mult),
                         reads=[gs, py], writes=[t])
                    if r == 1:
                        P.op("dve", lambda e, t=t, acc=acc: e.tensor_tensor(out=acc.ap[:, :ntok], in0=acc.ap[:, :ntok], in1=t.ap[:, :ntok], op=ALU.add),
                             reads=[acc, t], writes=[acc])
                    else:
                        P.op("dve", lambda e, t=t, acc=acc, m=m: e.tensor_tensor(out=mergedT.ap[:, m, :ntok], in0=acc.ap[:, :ntok], in1=t.ap[:, :ntok], op=ALU.add),
                             reads=[acc, t], writes=[mergedT])
        if getattr(self, "mix_stop", None) == "merge":
            return
        wo = self.wo2
        self.post_residual(lambda i, hf: (mergedT, [(mergedT.ap[:, k, i * 128:(i + 1) * 128], wo[hf].ap[:, k, :]) for k in range(8)], [mergedT, wo[hf]]),
                           ntok // 128, x_tiles_src, x_out_dst, self.gaG[s][0], (mixt_r, sm_r, xres_r, sq_r), xout_buf, is_out)

    def post_residual(self, mm_fn, nt, x_tiles_src, x_out_dst, gaG, rings, xout_buf, is_out, from_sbuf=None):
        P = self.P
        mixt_r, sm_r, xres_r, sq_r = rings
        for i in range(nt):
            xres = xres_r.next()
            xs = x_tiles_src[i]
            if isinstance(xs, tuple):
                self.load(xres, xs[0], dep=xs[1])
            else:
                self.load(xres, xs)
            sm = sm_r.next()
            mt = mixt_r.next()
            for hf in range(2):
                cs = slice(hf * 512, (hf + 1) * 512)
                if from_sbuf is None:
                    _, pairs, rd = mm_fn(i, hf)
                    pr = self.ps.next()
                    n = len(pairs)
                    for j, (l, r) in enumerate(pairs):
                        P.op("pe", lambda e, l=l, r=r, j=j, n=n, pr=pr: e.matmul(out=pr.ap, lhsT=l, rhs=r, start=(j == 0), stop=(j == n - 1)),
                             reads=rd, writes=[pr], signal=(j == n - 1))
                    src_ap, src_t = pr.ap, pr
                else:
                    src_ap, src_t = from_sbuf(i)[0][:, cs], from_sbuf(i)[1]
                sq = sq_r.next()
                P.op("act", lambda e, src_ap=src_ap, sq=sq, sm=sm, hf=hf: e.activation(out=sq.ap[:, 0:512], in_=src_ap, func=AF.Square, accum_out=sm.ap[:, hf:hf + 1]),
                     reads=[src_t], writes=[sq, sm])
                P.op("dve", lambda e, src_ap=src_ap, mt=mt, cs=cs: e.tensor_tensor(out=mt.ap[:, cs], in0=src_ap, in1=gaG.ap[:, cs], op=ALU.mult),
                     reads=[src_t, gaG, sq], writes=[mt])
            lvl = getattr(self, "pr_stop", 9)
            if lvl <= 1:
                continue
            P.op("dve", lambda e, sm=sm: e.tensor_tensor(out=sm.ap[:, 2:3], in0=sm.ap[:, 0:1], in1=sm.ap[:, 1:2], op=ALU.add), reads=[sm], writes=[sm])
            P.op("act", lambda e, sm=sm: e.activation(out=sm.ap[:, 3:4], in_=sm.ap[:, 2:3], func=AF.Sqrt, scale=1.0 / D, bias=EPS), reads=[sm], writes=[sm])
            P.op("dve", lambda e, sm=sm: e.reciprocal(out=sm.ap[:, 3:4], in_=sm.ap[:, 3:4]), reads=[sm], writes=[sm])
            if lvl <= 2:
                continue
            P.op("dve", lambda e, sm=sm, mt=mt, xres=xres: e.scalar_tensor_tensor(out=xres.ap, in0=mt.ap, scalar=sm.ap[:, 3:4], in1=xres.ap, op0=ALU.mult, op1=ALU.add),
                 reads=[mt, sm, xres], writes=[xres])
            if lvl <= 3:
                continue
            P.dma("sp", x_out_dst[i], xres.ap, reads=[xres], writes=[xout_buf], is_output=is_out)

    def ffn_dense_block(self, h2T, ntok, wr, work, x_tiles_src, x_out_dst, s, xout_buf, is_out):
        P = self.P
        actT, sg_r, rings = work
        wg_d, wu_d, wd_d = self.dl("wfg"), self.dl("wfu"), self.dl("wfd")
        for j in range(6):
            w = 512 if j < 5 else 256
            wg = wr.next()
            wu = wr.next()
            P.dma("pool", wg.ap[:, :, 0:w], wg_d[:, j * 512:j * 512 + w].rearrange("(k p) n -> p k n", p=128), writes=[wg])
            P.dma("pool", wu.ap[:, :, 0:w], wu_d[:, j * 512:j * 512 + w].rearrange("(k p) n -> p k n", p=128), writes=[wu])
            for mm in range(w // 128):
                pg = self.proj_fm(wg, mm * 128, h2T, ntok)
                pu = self.proj_fm(wu, mm * 128, h2T, ntok)
                sg = sg_r.next()
                P.op("act", lambda e, pg=pg, sg=sg: e.activation(out=sg.ap[:, :ntok], in_=pg.ap[:, :ntok], func=AF.Silu), reads=[pg], writes=[sg])
                kc = j * 4 + mm
                P.op("dve", lambda e, sg=sg, pu=pu, kc=kc: e.tensor_tensor(out=actT.ap[:, kc, :ntok], in0=sg.ap[:, :ntok], in1=pu.ap[:, :ntok], op=ALU.mult),
                     reads=[sg, pu], writes=[actT])
            self.tick()
        nt = ntok // 128
        halves = []
        for hf in range(2):
            slots = []
            for sl in range(3):
                k0 = sl * 8
                nk = min(8, 22 - k0)
                wd = wr.next()
                P.dma("pool", wd.ap[:, 0:nk, :], wd_d[k0 * 128:(k0 + nk) * 128, hf * 512:(hf + 1) * 512].rearrange("(k p) n -> p k n", p=128), writes=[wd])
                slots.append((wd, k0, nk))
            halves.append(slots)

        def mm_fn(i, hf):
            pairs = []
            rd = [actT]
            for (wd, k0, nk) in halves[hf]:
                rd.append(wd)
                for k in range(nk):
                    pairs.append((actT.ap[:, k0 + k, i * 128:(i + 1) * 128], wd.ap[:, k, :]))
            return None, pairs, rd
        self.post_residual(mm_fn, nt, x_tiles_src, x_out_dst, self.gaG[s][1], rings, xout_buf, is_out)

    def ffn_moe_block(self, h2T, wr, work, x_tiles_src, x_out_dst, xout_buf):
        P, A = self.P, self.A
        ntok = 1024
        nt = 8
        acc, act_r, sg_r, gates, lg_r, rings, wrt, brt = work
        weg, weu, wed = self.dl("weg"), self.dl("weu"), self.dl("wed")
        for i in range(nt):
            pl = self.ps.next()
            for k in range(8):
                P.op("pe", lambda e, k=k, i=i, pl=pl: e.matmul(out=pl.ap[:, 0:8], lhsT=h2T.ap[:, k, i * 128:(i + 1) * 128], rhs=wrt.ap[:, k, :],
                                                           start=(k == 0), stop=(k == 7)), reads=[h2T, wrt], writes=[pl], signal=(k == 7))
            lg = lg_r.next()
            L, M1, L2, M2 = (lg.ap[:, j, :] for j in range(4))
            sm = lg.ap[:, 4, :]
            P.op("dve", lambda e, pl=pl, L=L: e.tensor_tensor(out=L, in0=pl.ap[:, 0:8], in1=brt.ap, op=ALU.add), reads=[pl, brt], writes=[lg])
            P.op("dve", lambda e, L=L, sm=sm: e.tensor_reduce(out=sm[:, 0:1], in_=L, axis=AX.X, op=ALU.max), reads=[lg], writes=[lg])
            P.op("dve", lambda e, L=L, M1=M1, sm=sm: e.tensor_scalar(out=M1, in0=L, scalar1=sm[:, 0:1], scalar2=None, op0=ALU.is_ge), reads=[lg], writes=[lg])
            P.op("dve", lambda e, L=L, M1=M1, L2=L2: e.scalar_tensor_tensor(out=L2, in0=M1, scalar=-1e30, in1=L, op0=ALU.mult, op1=ALU.add), reads=[lg], writes=[lg])
            P.op("dve", lambda e, L2=L2, sm=sm: e.tensor_reduce(out=sm[:, 1:2], in_=L2, axis=AX.X, op=ALU.max), reads=[lg], writes=[lg])
            P.op("dve", lambda e, L2=L2, M2=M2, sm=sm: e.tensor_scalar(out=M2, in0=L2, scalar1=sm[:, 1:2], scalar2=None, op0=ALU.is_ge), reads=[lg], writes=[lg])
            P.op("dve", lambda e, sm=sm: e.tensor_tensor(out=sm[:, 2:3], in0=sm[:, 1:2], in1=sm[:, 0:1], op=ALU.subtract), reads=[lg], writes=[lg])
            P.op("act", lambda e, sm=sm: e.activation(out=sm[:, 3:4], in_=sm[:, 2:3], func=AF.Exp), reads=[lg], writes=[lg])
            P.op("dve", lambda e, sm=sm: e.tensor_scalar(out=sm[:, 4:5], in0=sm[:, 3:4], scalar1=1.0, scalar2=None, op0=ALU.add), reads=[lg], writes=[lg])
            P.op("dve", lambda e, sm=sm: e.reciprocal(out=sm[:, 5:6], in_=sm[:, 4:5]), reads=[lg], writes=[lg])
            P.op("dve", lambda e, sm=sm: e.tensor_tensor(out=sm[:, 6:7], in0=sm[:, 3:4], in1=sm[:, 5:6], op=ALU.mult), reads=[lg], writes=[lg])
            P.op("dve", lambda e, M1=M1, sm=sm, i=i: e.tensor_scalar(out=gates.ap[:, i, :], in0=M1, scalar1=sm[:, 5:6], scalar2=None, op0=ALU.mult), reads=[lg], writes=[gates])
            P.op("dve", lambda e, M2=M2, sm=sm, i=i: e.scalar_tensor_tensor(out=gates.ap[:, i, :], in0=M2, scalar=sm[:, 6:7], in1=gates.ap[:, i, :], op0=ALU.mult, op1=ALU.add),
                 reads=[lg, gates], writes=[gates])
        first = True
        for ex in range(NEXP):
            for j in range(7):
                wg = wr.next()
                wu = wr.next()
                wd = wr.next()
                self.load(wg, weg[ex, :, j * 512:(j + 1) * 512].rearrange("(k p) n -> p k n", p=128), "pool")
                self.load(wu, weu[ex, :, j * 512:(j + 1) * 512].rearrange("(k p) n -> p k n", p=128), "pool")
                wdv = wd.ap.rearrange("p a b -> p (a b)").rearrange("p (k n) -> p k n", k=4)
                P.dma("pool", wdv, wed[ex, j * 512:(j + 1) * 512, :].rearrange("(k p) n -> p k n", p=128), writes=[wd])
                at = act_r.next()
                for mm in range(4):
                    for th in range(2):
                        ts = slice(th * 512, (th + 1) * 512)
                        pg = self.ps.next()
                        pu = self.ps.next()
                        for k in range(8):
                            P.op("pe", lambda e, k=k, mm=mm, ts=ts, pg=pg, wg=wg: e.matmul(out=pg.ap, lhsT=wg.ap[:, k, mm * 128:(mm + 1) * 128], rhs=h2T.ap[:, k, ts],
                                                                                  start=(k == 0), stop=(k == 7)), reads=[wg, h2T], writes=[pg], signal=(k == 7))
                        for k in range(8):
                            P.op("pe", lambda e, k=k, mm=mm, ts=ts, pu=pu, wu=wu: e.matmul(out=pu.ap, lhsT=wu.ap[:, k, mm * 128:(mm + 1) * 128], rhs=h2T.ap[:, k, ts],
                                                                                  start=(k == 0), stop=(k == 7)), reads=[wu, h2T], writes=[pu], signal=(k == 7))
                        sg = sg_r.next()
                        P.op("act", lambda e, pg=pg, sg=sg: e.activation(out=sg.ap, in_=pg.ap, func=AF.Silu), reads=[pg], writes=[sg])
                        P.op("dve", lambda e, sg=sg, pu=pu, at=at, mm=mm, ts=ts: e.tensor_tensor(out=at.ap[:, mm, ts], in0=sg.ap, in1=pu.ap, op=ALU.mult),
                             reads=[sg, pu], writes=[at])
                for i in range(nt):
                    for hf in range(2):
                        cs = slice(hf * 512, (hf + 1) * 512)
                        pd = self.ps.next()
                        for mm in range(4):
                            rhs = wdv[:, mm, cs]
                            P.op("pe", lambda e, mm=mm, i=i, pd=pd, at=at, rhs=rhs: e.matmul(out=pd.ap, lhsT=at.ap[:, mm, i * 128:(i + 1) * 128], rhs=rhs,
                                                                                    start=(mm == 0), stop=(mm == 3)), reads=[at, wd], writes=[pd], signal=(mm == 3))
                        if first:
                            P.op("dve", lambda e, pd=pd, i=i, cs=cs, ex=ex: e.tensor_scalar(out=acc.ap[:, i, cs], in0=pd.ap, scalar1=gates.ap[:, i, ex:ex + 1], scalar2=None, op0=ALU.mult),
                                 reads=[pd, gates], writes=[acc])
                        else:
                            P.op("dve", lambda e, pd=pd, i=i, cs=cs, ex=ex: e.scalar_tensor_tensor(out=acc.ap[:, i, cs], in0=pd.ap, scalar=gates.ap[:, i, ex:ex + 1], in1=acc.ap[:, i, cs],
                                                                                          op0=ALU.mult, op1=ALU.add), reads=[pd, gates, acc], writes=[acc])
                first = False
                self.tick()
        self.flush()
        self.post_residual(None, nt, x_tiles_src, x_out_dst, self.gaG[0][1], rings, xout_buf, True,
                           from_sbuf=lambda i: (acc.ap[:, i, :], acc))


def _decl_common(B, layer, main):
    B.inp("winP", [D, IN_DIM])
    if main:
        for nm, shp in (("wao", [512, D]), ("wfn", [512, D]), ("wco", [512, D]), ("wo", [D, D])):
            B.inp(nm, shp)
        B.inp("ropeC", [128, 34 * 128])
        B.inp("ropeS", [128, 34 * 128])
        if layer == 0:
            B.inp("wfg", [D, D_FF])
            B.inp("wfu", [D, D_FF])
            B.inp("wfd", [D_FF, D])
            B.inp("t256", [128, 2, 2, 256])
        else:
            B.inp("wrt", [128, 8, NEXP])
            B.inp("brt", [128, NEXP])
            B.inp("weg", [NEXP, D, D_EXP])
            B.inp("weu", [NEXP, D, D_EXP])
            B.inp("wed", [NEXP, D_EXP, D])


def build_pre(layer):
    B = Builder("pre", layer)
    P, A = B.P, B.A
    _decl_common(B, layer, False)
    xh = B.inp("xh", [34 * 128, D])
    zf = B.outp("zf", [4, TOK, 128], BF16)
    B.zf_b = P.buf("zf_d")
    B.emit_consts()
    B.emit_mod()
    B.emit_phaseA(xh, True, zf)
    P.finish()
    return B


def build_main(layer, stop_after=None, mix_stop=None):
    B = Builder("main", layer)
    B.mix_stop = mix_stop
    if isinstance(mix_stop, str) and mix_stop.startswith("pr"):
        B.pr_stop = int(mix_stop[2:])
    P, A, nc = B.P, B.A, B.nc
    last = layer == 1
    _decl_common(B, layer, True)
    xh = B.inp("xh", [34 * 128, D])
    hctx = B.inp("hctx", [CTX, D])
    zfg = B.inp("zfg", [4, 4, TOK, 128], BF16)
    xo = B.outp("xo", [TOK, D])
    if not last:
        hco = B.outp("hco", [CTX, D])
    B.u_d = B.scratch("u_d", [128, 4, 34 * 128], BF16)
    B.u_b = P.buf("u_d")
    B.ft_d = B.scratch("ft_d", [128, 4, TOK], BF16)
    B.ft_b = P.buf("ft_d")
    xmid = B.scratch("xmid_d", [TOK, D], F32)
    xmid_b = P.buf("xmid_d")
    xo_b = P.buf("xo")
    winP = B.din["winP"]
    B.emit_consts()
    B.emit_mod()
    m_mix = A.mark()
    B.kT = A.alloc("kT", [34 * 128], BF16)
    B.vaug = A.alloc("vaug", [34, 2, 65], BF16)
    P.op("dve", lambda e: e.memset(B.vaug.ap, 1.0), writes=[B.vaug])
    kcT = A.alloc("kcT", [CTX], BF16)
    vcaug = A.alloc("vcaug", [2, 2, 65], BF16)
    P.op("dve", lambda e: e.memset(vcaug.ap, 1.0), writes=[vcaug])
    B.wbr = []
    for nm in ("wao", "wfn", "wco"):
        w = A.alloc(nm, [4, D], BF16)
        B.load(w, B.dl(nm).rearrange("(k p) n -> p k n", p=128), "pool")
        B.wbr.append(w)
    B.wo2 = []
    for hf in range(2):
        w = A.alloc(f"wo{hf}", [8, 512], BF16)
        B.load(w, B.dl("wo")[:, hf * 512:(hf + 1) * 512].rearrange("(k p) n -> p k n", p=128), "pool")
        B.wo2.append(w)

    if stop_after == "mod":
        P.finish()
        return B
    m0 = A.mark()
    rings = B.norm_rings(2)
    hcT = A.alloc("hcT", [8, 256], BF16)
    B.make_hT([hctx[i * 128:(i + 1) * 128, :] for i in range(2)], hcT, 1, 0, rings)
    ncolA = PA_W
    wA = A.alloc("wAc", [8, ncolA], BF16)
    B.load(wA, winP[:, 0:ncolA].rearrange("(k p) n -> p k n", p=128), "pool")
    pr = B.proj_fm(wA, PA_K, hcT, CTX)
    P.op("act", lambda e: e.copy(out=kcT.ap, in_=pr.ap[:, :CTX]), reads=[pr], writes=[kcT])
    pv = B.ps.next()
    pvv = pv.ap.rearrange("p (i c) -> p i c", i=4)
    for i in range(2):
        for k in range(8):
            P.op("pe", lambda e, k=k, i=i: e.matmul(out=pvv[:, i, :], lhsT=hcT.ap[:, k, i * 128:(i + 1) * 128], rhs=wA.ap[:, k, PA_V:PA_V + 128],
                                                  start=(k == 0), stop=(k == 7)), reads=[hcT, wA], writes=[pv], signal=(k == 7 and i == 1))
    P.op("act", lambda e: e.copy(out=vcaug.ap[:, :, :, 0:64], in_=pvv[:, 0:2, :].rearrange("p i (g d) -> p i g d", g=2)), reads=[pv], writes=[vcaug])
    ctxkb = [(kcT.ap[:, i * 128:(i + 1) * 128], kcT, vcaug.ap[:, i, :, :], vcaug, None) for i in range(2)]
    if stop_after == "ctx_kv":
        P.finish()
        return B
    if not last:
        hmid = B.scratch("hmid_d", [CTX, D], F32)
        hmid_b = P.buf("hmid_d")
        hco_b = P.buf("hco")
        zcf = A.alloc("zcf", [2, 512], BF16)
        for i in range(2):
            pz = B.ps.next()
            for k in range(8):
                P.op("pe", lambda e, k=k, i=i, pz=pz: e.matmul(out=pz.ap, lhsT=hcT.ap[:, k, i * 128:(i + 1) * 128], rhs=wA.ap[:, k, PA_F:PA_F + 512],
                                                           start=(k == 0), stop=(k == 7)), reads=[hcT, wA], writes=[pz], signal=(k == 7))
            P.op("act", lambda e, i=i, pz=pz: e.copy(out=zcf.ap[:, i, :], in_=pz.ap), reads=[pz], writes=[zcf])
        t256 = B.const_tile("t256", [2, 2, 256], BF16, B.din["t256"])
        Gc = A.alloc("Gc", [2, 256], BF16)
        FTc = A.alloc("FTc", [4, 256], BF16)
        scl = 1.0 / math.sqrt(256.0 * 128.0)
        for g in range(4):
            pg_ = B.ps.next()
            for i in range(2):
                P.op("pe", lambda e, g=g, i=i, pg_=pg_: e.matmul(out=pg_.ap, lhsT=zcf.ap[:, i, g * 128:(g + 1) * 128],
                                                             rhs=t256.ap[:, i, :, :].rearrange("p r k -> p (r k)"), start=(i == 0), stop=(i == 1)),
                     reads=[zcf, t256], writes=[pg_], signal=(i == 1))
            P.op("act", lambda e, pg_=pg_: e.copy(out=Gc.ap.rearrange("p r k -> p (r k)"), in_=pg_.ap), reads=[pg_], writes=[Gc])
            pf = B.ps.next()
            P.op("pe", lambda e, pf=pf: e.matmul(out=pf.ap[:, 0:256], lhsT=B.cbsb.ap[:, 0, :], rhs=Gc.ap[:, 0, :], start=True, stop=False),
                 reads=[B.cbsb, Gc], writes=[pf], signal=False)
            P.op("pe", lambda e, pf=pf: e.matmul(out=pf.ap[:, 0:256], lhsT=B.cbsb.ap[:, 1, :], rhs=Gc.ap[:, 1, :], start=False, stop=True),
                 reads=[B.cbsb, Gc], writes=[pf])
            P.op("act", lambda e, g=g, pf=pf: e.activation(out=FTc.ap[:, g, :], in_=pf.ap[:, 0:256], func=AF.Copy, scale=scl), reads=[pf], writes=[FTc])
        if stop_after == "ctx_fft":
            P.finish()
            return B
        ucT = A.alloc("ucT", [4, 258], BF16)
        P.op("dve", lambda e: e.memset(ucT.ap, 0.0), writes=[ucT])
        cxs = A.alloc("cxs_c", [512], F32)
        for m in range(4):
            px = B.proj_fm(wA, PA_CX + m * 128, hcT, CTX)
            P.op("act", lambda e, px=px: e.copy(out=cxs.ap[:, :CTX], in_=px.ap[:, :CTX]), reads=[px], writes=[cxs])
            pc = B.proj_fm(wA, PA_CC + m * 128, hcT, CTX)
            P.op("dve", lambda e, m=m, pc=pc: e.tensor_tensor(out=ucT.ap[:, m, 1:257], in0=cxs.ap[:, :CTX], in1=pc.ap[:, :CTX], op=ALU.mult),
                 reads=[cxs, pc], writes=[ucT])
        wr = A.ring("wrc", 3, [8, 512], BF16)
        wq = wr.next()
        B.load(wq, winP[:, PM_Q:PM_Q + 512].rearrange("(k p) n -> p k n", p=128), "pool")
        qcT = A.alloc("qcT", [4, 256], BF16)
        for m in range(4):
            pq = B.proj_fm(wq, m * 128, hcT, CTX)
            P.op("act", lambda e, m=m, pq=pq: e.copy(out=qcT.ap[:, m, :], in_=pq.ap[:, :CTX]), reads=[pq], writes=[qcT])
        attnTc = A.alloc("attnTc", [4, 256], BF16)
        abufs = (A.ring("pTc", 1, [2, 2, 4, 128], BF16), A.ring("atokc", 1, [8, 64], BF16), A.ring("denc", 1, [2, 8], F32))
        for t in range(2):
            B.attention_tile(qcT, t * 128, ctxkb, attnTc, t * 128, abufs)
        if stop_after == "ctx_attn":
            dq = B.outp("dbg_qcT", [128, 4 * 256], BF16)
            da = B.outp("dbg_attnTc", [128, 4 * 256], BF16)
            dk = B.outp("dbg_kcT", [128, 256], BF16)
            dv = B.outp("dbg_vc", [128, 2 * 2 * 65], BF16)
            ob = P.buf("dbgo")
            P.dma("sp", dq, qcT.ap.rearrange("p a b -> p (a b)"), reads=[qcT], writes=[ob], is_output=True)
            P.dma("sp", da, attnTc.ap.rearrange("p a b -> p (a b)"), reads=[attnTc], writes=[ob], is_output=True)
            P.dma("sp", dk, kcT.ap, reads=[kcT], writes=[ob], is_output=True)
            P.dma("sp", dv, vcaug.ap.rearrange("p a b c -> p (a b c)"), reads=[vcaug], writes=[ob], is_output=True)
            P.finish()
            return B
        work = (A.alloc("yconvTc", [4, 256], BF16), A.ring("gsbc", 2, [512], BF16), A.ring("tmpc", 3, [512], F32), A.alloc("mergedTc", [8, 256], BF16),
                A.ring("mixtc", 1, [D], F32), A.ring("smc", 2, [4], F32), A.ring("xresc", 2, [D], F32), A.ring("sqc", 1, [512], BF16))
        B.mix_block(hcT, CTX, qcT, attnTc, ucT.ap, ucT, FTc.ap, FTc, wr, work,
                    [hctx[i * 128:(i + 1) * 128, :] for i in range(2)], [hmid[i * 128:(i + 1) * 128, :] for i in range(2)], 1, hmid_b)
        if stop_after == "ctx_mix":
            ob = P.buf("dbgo2")
            for nm_, t_, a_, n_ in (("dbg_FTc", FTc, 4, 256), ("dbg_ucT", ucT, 4, 258), ("dbg_yconv", work[0], 4, 256), ("dbg_merged", work[3], 8, 256), ("dbg_attnTc", attnTc, 4, 256)):
                do = B.outp(nm_, [128, a_, n_], BF16)
                P.dma("sp", do, t_.ap[:, :, 0:n_], reads=[t_], writes=[ob], is_output=True)
            P.finish()
            return B
        P.barrier()
        A.release(m0)
        rings = B.norm_rings(2)
        h2c = A.alloc("h2c", [8, 256], BF16)
        B.make_hT_dep([hmid[i * 128:(i + 1) * 128, :] for i in range(2)], h2c, 1, 1, rings, hmid_b)
        wr = A.ring("wrf", 8, [8, 512], BF16)
        work = (A.alloc("actTc", [22, 256], BF16), A.ring("sgc", 2, [512], BF16),
                (A.ring("mixtf", 1, [D], F32), A.ring("smf", 2, [4], F32), A.ring("xresf", 1, [D], F32), A.ring("sqf", 1, [512], BF16)))
        B.ffn_dense_block(h2c, CTX, wr, work, [(hmid[i * 128:(i + 1) * 128, :], hmid_b) for i in range(2)],
                          [hco[i * 128:(i + 1) * 128, :] for i in range(2)], 1, hco_b, True)
    P.barrier()
    A.release(m0)

    if stop_after == "ctx":
        P.finish()
        return B
    B.emit_phaseA(xh, False)
    if stop_after == "A":
        P.finish()
        return B
    B.emit_fft(zfg)
    if stop_after == "fft":
        P.finish()
        return B

    m0 = A.mark()
    rings = B.norm_rings(3)
    hTr = A.ring("hTm", 1, [8, 512], BF16)
    rope_r = A.ring("ropeM", 1, [2, 512], F32)
    qraw = A.alloc("qraw", [512], BF16)
    rtmp = (A.alloc("rt1m", [512], F32), A.alloc("rt2m", [512], F32))
    qT = A.alloc("qT", [4, 512], BF16)
    attnT = A.alloc("attnT", [4, 512], BF16)
    abufs = (A.ring("pT", 2, [5, 2, 4, 128], BF16), A.ring("atok", 2, [8, 64], BF16), A.ring("den", 2, [2, 8], F32))
    ubr = A.ring("ub", 1, [4, 514], BF16)
    ftr = A.ring("ftb", 1, [4, 512], BF16)
    wr = A.ring("wrm", 3, [8, 512], BF16)
    work = (A.alloc("yconvT", [4, 512], BF16), A.ring("gsb", 2, [512], BF16), A.ring("tmpm", 3, [512], F32), A.alloc("mergedT", [8, 512], BF16),
            A.ring("mixt", 1, [D], F32), A.ring("smm", 2, [4], F32), A.ring("xres", 1, [D], F32), A.ring("sqm", 1, [512], BF16))
    for bi in range(8):
        c0 = (4 * bi + 1) * 128
        hT = hTr.next()
        own = [xh[(4 * bi + 1 + i) * 128:(4 * bi + 2 + i) * 128, :] for i in range(4)]
        B.make_hT(own, hT, 0, 0, rings)
        rp = rope_r.next()
        P.dma("sp", rp.ap[:, 0, :], B.din["ropeC"][:, c0:c0 + 512], writes=[rp])
        P.dma("sp", rp.ap[:, 1, :], B.din["ropeS"][:, c0:c0 + 512], writes=[rp])
        B.ropeb = rp
        wq = wr.next()
        B.load(wq, winP[:, PM_Q:PM_Q + 512].rearrange("(k p) n -> p k n", p=128), "pool")
        for m in range(4):
            pq = B.proj_fm(wq, m * 128, hT, 512)
            B.rope(pq, qraw, qT.ap[:, m, :], qT, rp.ap[:, 0, :], rp.ap[:, 1, :], 512, rtmp)
        for t in range(4):
            T = 4 * bi + t
            kbs = []
            for d_, mi in ((0, 2 if T == 0 else 0), (1, None), (2, 3 if T == 31 else 1)):
                sl = T + d_
                kbs.append((B.kT.ap[:, sl * 128:(sl + 1) * 128], B.kT, B.vaug.ap[:, sl, :, :], B.vaug, mi))
            kbs += ctxkb
            B.attention_tile(qT, t * 128, kbs, attnT, t * 128, abufs)
        ub = ubr.next()
        P.dma("sp", ub.ap, B.u_d[:, :, c0 - 1:c0 + 513], reads=[B.u_b], writes=[ub])
        ftb = ftr.next()
        P.dma("sp", ftb.ap, B.ft_d[:, :, bi * 512:(bi + 1) * 512], reads=[B.ft_b], writes=[ftb])
        B.mix_block(hT, 512, qT, attnT, ub.ap, ub, ftb.ap, ftb, wr, work, own,
                    [xmid[(4 * bi + i) * 128:(4 * bi + i + 1) * 128, :] for i in range(4)], 0, xmid_b)
    P.barrier()
    A.release(m0)

    if stop_after == "mix":
        P.finish()
        return B
    A.release(m_mix)
    m0 = A.mark()
    if not last:
        rings = B.norm_rings(4)
        h2r = A.ring("h2T", 1, [8, 512], BF16)
        wr = A.ring("wrf2", 8, [8, 512], BF16)
        work = (A.alloc("actT", [22, 512], BF16), A.ring("sg", 2, [512], BF16),
                (A.ring("mixtF", 1, [D], F32), A.ring("smF", 2, [4], F32), A.ring("xresF", 2, [D], F32), A.ring("sqF", 1, [512], BF16)))
        for bi in range(8):
            h2T = h2r.next()
            src = [xmid[(4 * bi + i) * 128:(4 * bi + i + 1) * 128, :] for i in range(4)]
            B.make_hT_dep(src, h2T, 0, 1, rings, xmid_b)
            B.ffn_dense_block(h2T, 512, wr, work, [(a, xmid_b) for a in src],
                              [xo[(4 * bi + i) * 128:(4 * bi + i + 1) * 128, :] for i in range(4)], 0, xo_b, True)
    else:
        rings = B.norm_rings(4)
        h2r = A.ring("h2T", 1, [8, 1024], BF16)
        wr = A.ring("wre", 6, [8, 512], BF16)
        wrt = B.const_tile("wrt", [8, NEXP], BF16, B.din["wrt"])
        brt = B.const_tile("brt", [NEXP], F32, B.din["brt"])
        work = (A.alloc("acc", [8, D], F32), A.ring("actc", 2, [4, 1024], BF16), A.ring("sge", 2, [512], BF16), A.alloc("gates", [8, NEXP], F32),
                A.ring("lg", 2, [5, 8], F32),
                (A.ring("mixtE", 1, [D], F32), A.ring("smE", 2, [4], F32), A.ring("xresE", 2, [D], F32), A.ring("sqE", 1, [512], BF16)), wrt, brt)
        for bi in range(4):
            h2T = h2r.next()
            src = [xmid[(8 * bi + i) * 128:(8 * bi + i + 1) * 128, :] for i in range(8)]
            B.make_hT_dep(src, h2T, 0, 1, rings, xmid_b)
            B.ffn_moe_block(h2T, wr, work, [(a, xmid_b) for a in src], [xo[(8 * bi + i) * 128:(8 * bi + i + 1) * 128, :] for i in range(8)], xo_b)
    P.finish()
    return B


def _w_in_perm():
    cols = []
    cols += list(range(F_OFF, F_OFF + 512))
    cols += list(range(K_OFF, K_OFF + 128))
    cols += list(range(V_OFF, V_OFF + 128))
    cols += list(range(CX_OFF, CX_OFF + 512))
    cols += list(range(CC_OFF, CC_OFF + 512))
    for m in range(4):
        cols += list(range(Q_OFF + m * 64, Q_OFF + (m + 1) * 64))
        cols += list(range(Q_OFF + (4 + m) * 64, Q_OFF + (5 + m) * 64))
    cols += list(range(CB_OFF, CB_OFF + 512))
    for m in range(8):
        for r in range(3):
            cols += list(range(GATE_OFF + r * 1024 + m * 128, GATE_OFF + r * 1024 + (m + 1) * 128))
    assert len(cols) == IN_DIM and len(set(cols)) == IN_DIM
    return np.asarray(cols)


def _colform(v, n):
    return np.ascontiguousarray(np.asarray(v, np.float32).reshape(n, 128).T)


_CONST_CACHE = {}


def _static_tables():
    if "t" in _CONST_CACHE:
        return _CONST_CACHE["t"]
    t = {}
    t["ident"] = np.eye(128, dtype=np.float32)
    rt = np.zeros((128, 128), np.float32)
    for i in range(64):
        rt[2 * i + 1, 2 * i] = -1.0
        rt[2 * i, 2 * i + 1] = 1.0
    t["rt"] = rt
    n = np.arange(128, dtype=np.float64)
    k1 = np.arange(128, dtype=np.float64)
    ang = 2 * np.pi * np.outer(n, k1) / 128.0
    t1 = np.stack([np.cos(ang), -np.sin(ang)], axis=1).reshape(128, 2, 2, 64)
    t["t1"] = np.ascontiguousarray(t1.transpose(0, 2, 1, 3)).astype(np.float32)
    angc = 2 * np.pi * np.outer(n, n) / 128.0
    t["cbsb"] = np.ascontiguousarray(np.stack([np.cos(angc), np.sin(angc)], axis=1)).astype(np.float32)
    nn = np.arange(256, dtype=np.float64)
    a256 = 2 * np.pi * np.outer(nn, nn) / 256.0
    t256 = np.stack([np.cos(a256), -np.sin(a256)], axis=1)
    t["t256"] = np.ascontiguousarray(t256.reshape(2, 128, 2, 256).transpose(1, 0, 2, 3)).astype(np.float32)
    kk = np.arange(128)[:, None]
    qq = np.arange(128)[None, :]
    prev = (kk >= qq).astype(np.float32)
    nxt = (kk <= qq).astype(np.float32)
    half = 32
    inv_freq = 1.0 / (10000.0 ** (np.arange(0, half, 2, dtype=np.float64) / half))
    for j in range(4):
        m = np.stack([prev, nxt, prev * (1.0 if j != 0 else 0.0), nxt * (1.0 if j != 3 else 0.0)], axis=1)
        t[("masks", j)] = np.ascontiguousarray(m).astype(np.float32)
        t[("valid", j)] = np.tile(np.asarray([[1.0 if j != 0 else 0.0, 1.0 if j != 3 else 0.0]], np.float32), (128, 1))
        pos = TOK * j - 128 + np.arange(34 * 128)
        row = (pos // 64).astype(np.float64)
        col = (pos % 64).astype(np.float64)
        angp = np.concatenate([row[:, None] * inv_freq[None, :], col[:, None] * inv_freq[None, :]], axis=1)
        d = np.arange(128) % 64
        pi_ = d // 2
        t[("ropeC", j)] = np.ascontiguousarray(np.cos(angp)[:, pi_].T).astype(np.float32)
        t[("ropeS", j)] = np.ascontiguousarray(np.sin(angp)[:, pi_].T).astype(np.float32)
        n2 = np.arange(128, dtype=np.float64)[:, None, None]
        k1_ = np.arange(128, dtype=np.float64)[None, :, None]
        k2_ = (32 * j + np.arange(32, dtype=np.float64))[None, None, :]
        th = 2 * np.pi * n2 * (k1_ + 128.0 * k2_) / float(SEQ)
        et = np.concatenate([np.sin(th), np.cos(th), -np.sin(th)], axis=2)
        t[("etab", j)] = np.ascontiguousarray(et).astype(np.float32)
    _CONST_CACHE["t"] = t
    return t


def _layer_common(inp, l):
    perm = _w_in_perm()
    c = {}
    c["winP"] = np.ascontiguousarray(np.asarray(inp["w_in"][l], np.float32)[:, perm])
    c["wmod"] = np.ascontiguousarray(np.asarray(inp["w_mod"][l], np.float32))
    bm = np.asarray(inp["b_mod"][l], np.float32)
    c["bcols"] = _colform(bm, 48)
    c["brow"] = np.ascontiguousarray(np.broadcast_to(np.stack([bm[2 * D:3 * D], bm[5 * D:6 * D]])[None], (128, 2, D))).astype(np.float32)
    c["gpm_c"] = _colform(inp["g_pre_mix"][l], 8)
    c["gpf_c"] = _colform(inp["g_pre_ffn"][l], 8)
    c["gqm_r"] = np.ascontiguousarray(np.broadcast_to(np.asarray(inp["g_post_mix"][l], np.float32)[None], (128, D)))
    c["gqf_r"] = np.ascontiguousarray(np.broadcast_to(np.asarray(inp["g_post_ffn"][l], np.float32)[None], (128, D)))
    c["wao"] = np.ascontiguousarray(np.asarray(inp["w_attn_o"][l], np.float32))
    c["wfn"] = np.ascontiguousarray(np.asarray(inp["w_fnet"][l], np.float32))
    c["wco"] = np.ascontiguousarray(np.asarray(inp["w_conv_out"][l], np.float32))
    c["wo"] = np.ascontiguousarray(np.asarray(inp["w_o"][l], np.float32))
    c["sink_b"] = np.ascontiguousarray(np.broadcast_to(np.asarray(inp["attn_sink"][l], np.float32)[None], (128, 8)))
    wc = np.asarray(inp["w_conv"][l], np.float32)
    c["wconv_c"] = np.ascontiguousarray(wc.reshape(3, 4, 128).transpose(2, 1, 0))
    if l == 0:
        c["wfg"] = np.ascontiguousarray(np.asarray(inp["w_ff_gate"][0], np.float32))
        c["wfu"] = np.ascontiguousarray(np.asarray(inp["w_ff_up"][0], np.float32))
        c["wfd"] = np.ascontiguousarray(np.asarray(inp["w_ff_down"][0], np.float32))
    else:
        wr = np.asarray(inp["w_router"][0], np.float32)
        c["wrt"] = np.ascontiguousarray(wr.reshape(8, 128, NEXP).transpose(1, 0, 2))
        c["brt"] = np.ascontiguousarray(np.broadcast_to(np.asarray(inp["b_router"][0], np.float32)[None], (128, NEXP)))
        c["weg"] = np.ascontiguousarray(np.asarray(inp["w_exp_gate"][0], np.float32))
        c["weu"] = np.ascontiguousarray(np.asarray(inp["w_exp_up"][0], np.float32))
        c["wed"] = np.ascontiguousarray(np.asarray(inp["w_exp_down"][0], np.float32))
    return c


def _ccols(inp, b):
    cb = np.asarray(inp["c"][b], np.float32)
    cc = np.asarray(inp["c_ctx"], np.float32)
    return np.ascontiguousarray(np.stack([_colform(cb, 8), _colform(cc, 8)], axis=2))


def _xh(xfull, cid):
    b, j = cid // 4, cid % 4
    out = np.zeros((34 * 128, D), np.float32)
    lo = TOK * j - 128
    hi = TOK * (j + 1) + 128
    s0, s1 = max(lo, 0), min(hi, SEQ)
    out[s0 - lo:s1 - lo] = xfull[b, s0:s1]
    return out


_PROG_CACHE = {}


def _get_prog(kind, layer):
    key = (kind, layer)
    if key not in _PROG_CACHE:
        _PROG_CACHE[key] = build_pre(layer) if kind == "pre" else build_main(layer)
    return _PROG_CACHE[key]


def _run(B, maps):
    names = set(B.din.keys())
    in_maps = [{k: v for k, v in m.items() if k in names} for m in maps]
    for m in in_maps:
        missing = names - set(m.keys())
        assert not missing, missing
    res = run_bass_kernel_spmd(B.nc, in_maps, core_ids=list(range(8)))
    return res.results


def kernel_unfused(**inputs):
    tabs = _static_tables()
    x = np.asarray(inputs["x"], np.float32)
    hctx = [np.ascontiguousarray(np.asarray(inputs["ctx"][b], np.float32)) for b in range(NB)]
    for l in range(2):
        com = _layer_common(inputs, l)
        maps = []
        for cid in range(8):
            b, j = cid // 4, cid % 4
            m = dict(com)
            for nm in ("ident", "rt", "t1", "cbsb", "t256"):
                m[nm] = tabs[nm]
            for nm in ("masks", "valid", "ropeC", "ropeS", "etab"):
                m[nm] = tabs[(nm, j)]
            m["ccols"] = _ccols(inputs, b)
            m["xh"] = _xh(x, cid)
            m["hctx"] = hctx[b]
            maps.append(m)
        r = _run(_get_prog("pre", l), maps)
        for b in range(NB):
            zfg = np.ascontiguousarray(np.stack([np.asarray(r[4 * b + j]["zf"]) for j in range(4)]))
            for j in range(4):
                maps[4 * b + j]["zfg"] = zfg
        r = _run(_get_prog("main", l), maps)
        xn = np.empty_like(x)
        for cid in range(8):
            b, j = cid // 4, cid % 4
            xn[b, TOK * j:TOK * (j + 1)] = np.asarray(r[cid]["xo"], np.float32)
        x = xn
        if l == 0:
            hctx = [np.ascontiguousarray(np.asarray(r[4 * b]["hco"], np.float32)) for b in range(NB)]
    return x


GROUPS = [[0, 1, 2, 3], [4, 5, 6, 7]]


def _decl_layer(B, l):
    sfx = str(l)
    B.inp("winP" + sfx, [D, IN_DIM])
    for nm, shp in (("wao", [512, D]), ("wfn", [512, D]), ("wco", [512, D]), ("wo", [D, D])):
        B.inp(nm + sfx, shp)
    if l == 0:
        B.inp("wfg0", [D, D_FF])
        B.inp("wfu0", [D, D_FF])
        B.inp("wfd0", [D_FF, D])
    else:
        B.inp("wrt1", [128, 8, NEXP])
        B.inp("brt1", [128, NEXP])
        B.inp("weg1", [NEXP, D, D_EXP])
        B.inp("weu1", [NEXP, D, D_EXP])
        B.inp("wed1", [NEXP, D_EXP, D])


def build_fused(sim_cc=False, stop=None):
    B = Builder("main", 0)
    P, A, nc = B.P, B.A, B.nc
    B.sfx = "0"
    for l in range(2):
        if l == 1 and stop is not None and stop != "l1mix":
            continue
        _decl_layer(B, l)
    B.inp("ropeC", [128, 34 * 128])
    B.inp("ropeS", [128, 34 * 128])
    B.inp("t256", [128, 2, 2, 256])
    xh = B.inp("xh", [34 * 128, D])
    hctx_in = B.inp("hctx", [CTX, D])
    selh = B.inp("selh", [128, 8])
    xo = B.outp("xo", [TOK, D])
    xo_b = P.buf("xo")
    B.u_d = B.scratch("u_d", [128, 4, 34 * 128], BF16)
    B.u_b = P.buf("u_d")
    B.ft_d = B.scratch("ft_d", [128, 4, TOK], BF16)
    B.ft_b = P.buf("ft_d")
    xmid = B.scratch("xmid_d", [TOK, D], F32)
    xmid_b = P.buf("xmid_d")
    x1 = B.scratch("x1_d", [TOK, D], F32)
    x1_b = P.buf("x1_d")
    hmid = B.scratch("hmid_d", [CTX, D], F32)
    hmid_b = P.buf("hmid_d")
    hc1 = B.scratch("hc1_d", [CTX, D], F32)
    hc1_b = P.buf("hc1_d")
    xhalo = B.scratch("xhalo_d", [256, D], F32)
    xhalo_b = P.buf("xhalo_d")
    zf_l = [nc.dram_tensor(f"zf_cc{g}", [TOK, 128], BF16).ap() for g in range(4)]
    zfg_l = [nc.dram_tensor(f"zfg_cc{g}", [4 * TOK, 128], BF16).ap() for g in range(4)]
    xb_src = nc.dram_tensor("xb_cc", [256, D], F32).ap()
    xb_all = nc.dram_tensor("xball_cc", [4 * 256, D], F32).ap()
    B.zf_b = P.buf("zf_cc")
    zfg_b = P.buf("zfg_cc")
    xb_b = P.buf("xb_cc")
    xball_b = P.buf("xball_cc")
    def zsrc(r, g):
        return zfg_l[g][r * TOK:(r + 1) * TOK, :]

    B.emit_consts()
    B.alloc_mod_tiles()
    m_top = A.mark()
    for l in range(2):
        last = l == 1
        B.layer = l
        B.last = last
        B.sfx = str(l)
        winP = B.dl("winP")
        B.emit_layer_consts()
        B.emit_mod()
        m_mix = A.mark()
        B.kT = A.alloc("kT", [34 * 128], BF16)
        B.vaug = A.alloc("vaug", [34, 2, 65], BF16)
        P.op("dve", lambda e: e.memset(B.vaug.ap, 1.0), writes=[B.vaug])
        kcT = A.alloc("kcT", [CTX], BF16)
        vcaug = A.alloc("vcaug", [2, 2, 65], BF16)
        P.op("dve", lambda e: e.memset(vcaug.ap, 1.0), writes=[vcaug])
        B.wbr = []
        for nm in ("wao", "wfn", "wco"):
            w = A.alloc(nm, [4, D], BF16)
            B.load(w, B.dl(nm).rearrange("(k p) n -> p k n", p=128), "pool")
            B.wbr.append(w)
        B.wo2 = []
        for hf in range(2):
            w = A.alloc(f"wo{hf}", [8, 512], BF16)
            B.load(w, B.dl("wo")[:, hf * 512:(hf + 1) * 512].rearrange("(k p) n -> p k n", p=128), "pool")
            B.wo2.append(w)

        if l == 0:
            def xtile(t):
                return xh[(t + 1) * 128:(t + 2) * 128, :]
            hsrc = [hctx_in[i * 128:(i + 1) * 128, :] for i in range(2)]
        else:
            def xtile(t):
                if t < 0:
                    return (xhalo[0:128, :], xhalo_b)
                if t >= NTILE:
                    return (xhalo[128:256, :], xhalo_b)
                return (x1[t * 128:(t + 1) * 128, :], x1_b)
            hsrc = [(hc1[i * 128:(i + 1) * 128, :], hc1_b) for i in range(2)]
        B.xtile = xtile

        B.emit_phaseA(None, True, zf_l)
        m0 = A.mark()
        wA = A.alloc("wAc", [8, PA_W], BF16)
        B.load(wA, winP[:, 0:PA_W].rearrange("(k p) n -> p k n", p=128), "pool")
        if not last:
            wr_c = A.ring("wrc", 3, [8, 512], BF16)
            wq_c = wr_c.next()
            B.load(wq_c, winP[:, PM_Q:PM_Q + 512].rearrange("(k p) n -> p k n", p=128), "pool")
        for g in range(4):
            if sim_cc:
                for r in range(4):
                    P.dma("sp", zfg_l[g][r * TOK:(r + 1) * TOK, :], zf_l[g], reads=[B.zf_b], writes=[zfg_b])
            else:
                P.cc_allgather(zfg_l[g], zf_l[g], GROUPS, reads=[B.zf_b], writes=[zfg_b])

        rings = B.norm_rings(2)
        hcT = A.alloc("hcT", [8, 256], BF16)
        B.make_hT(hsrc, hcT, 1, 0, rings)
        pr = B.proj_fm(wA, PA_K, hcT, CTX)
        P.op("act", lambda e: e.copy(out=kcT.ap, in_=pr.ap[:, :CTX]), reads=[pr], writes=[kcT])
        pv = B.ps.next()
        pvv = pv.ap.rearrange("p (i c) -> p i c", i=4)
        for i in range(2):
            for k in range(8):
                P.op("pe", lambda e, k=k, i=i: e.matmul(out=pvv[:, i, :], lhsT=hcT.ap[:, k, i * 128:(i + 1) * 128], rhs=wA.ap[:, k, PA_V:PA_V + 128],
                                                      start=(k == 0), stop=(k == 7)), reads=[hcT, wA], writes=[pv], signal=(k == 7 and i == 1))
        P.op("act", lambda e: e.copy(out=vcaug.ap[:, :, :, 0:64], in_=pvv[:, 0:2, :].rearrange("p i (g d) -> p i g d", g=2)), reads=[pv], writes=[vcaug])
        ctxkb = [(kcT.ap[:, i * 128:(i + 1) * 128], kcT, vcaug.ap[:, i, :, :], vcaug, None) for i in range(2)]
        if not last:
            zcf = A.alloc("zcf", [2, 512], BF16)
            for i in range(2):
                pz = B.ps.next()
                for k in range(8):
                    P.op("pe", lambda e, k=k, i=i, pz=pz: e.matmul(out=pz.ap, lhsT=hcT.ap[:, k, i * 128:(i + 1) * 128], rhs=wA.ap[:, k, PA_F:PA_F + 512],
                                                               start=(k == 0), stop=(k == 7)), reads=[hcT, wA], writes=[pz], signal=(k == 7))
                P.op("act", lambda e, i=i, pz=pz: e.copy(out=zcf.ap[:, i, :], in_=pz.ap), reads=[pz], writes=[zcf])
            t256 = B.const_tile("t256", [2, 2, 256], BF16, B.din["t256"])
            Gc = A.alloc("Gc", [2, 256], BF16)
            FTc = A.alloc("FTc", [4, 256], BF16)
            scl = 1.0 / math.sqrt(256.0 * 128.0)
            for g in range(4):
                pg_ = B.ps.next()
                for i in range(2):
                    P.op("pe", lambda e, g=g, i=i, pg_=pg_: e.matmul(out=pg_.ap, lhsT=zcf.ap[:, i, g * 128:(g + 1) * 128],
                                                                 rhs=t256.ap[:, i, :, :].rearrange("p r k -> p (r k)"), start=(i == 0), stop=(i == 1)),
                         reads=[zcf, t256], writes=[pg_], signal=(i == 1))
                P.op("act", lambda e, pg_=pg_: e.copy(out=Gc.ap.rearrange("p r k -> p (r k)"), in_=pg_.ap), reads=[pg_], writes=[Gc])
                pf = B.ps.next()
                P.op("pe", lambda e, pf=pf: e.matmul(out=pf.ap[:, 0:256], lhsT=B.cbsb.ap[:, 0, :], rhs=Gc.ap[:, 0, :], start=True, stop=False),
                     reads=[B.cbsb, Gc], writes=[pf], signal=False)
                P.op("pe", lambda e, pf=pf: e.matmul(out=pf.ap[:, 0:256], lhsT=B.cbsb.ap[:, 1, :], rhs=Gc.ap[:, 1, :], start=False, stop=True),
                     reads=[B.cbsb, Gc], writes=[pf])
                P.op("act", lambda e, g=g, pf=pf: e.activation(out=FTc.ap[:, g, :], in_=pf.ap[:, 0:256], func=AF.Copy, scale=scl), reads=[pf], writes=[FTc])
            ucT = A.alloc("ucT", [4, 258], BF16)
            P.op("dve", lambda e: e.memset(ucT.ap, 0.0), writes=[ucT])
            cxs = A.alloc("cxs_c", [512], F32)
            for m in range(4):
                px = B.proj_fm(wA, PA_CX + m * 128, hcT, CTX)
                P.op("act", lambda e, px=px: e.copy(out=cxs.ap[:, :CTX], in_=px.ap[:, :CTX]), reads=[px], writes=[cxs])
                pc = B.proj_fm(wA, PA_CC + m * 128, hcT, CTX)
                P.op("dve", lambda e, m=m, pc=pc: e.tensor_tensor(out=ucT.ap[:, m, 1:257], in0=cxs.ap[:, :CTX], in1=pc.ap[:, :CTX], op=ALU.mult),
                     reads=[cxs, pc], writes=[ucT])
            wr = wr_c
            wq = wq_c
            qcT = A.alloc("qcT", [4, 256], BF16)
            for m in range(4):
                pq = B.proj_fm(wq, m * 128, hcT, CTX)
                P.op("act", lambda e, m=m, pq=pq: e.copy(out=qcT.ap[:, m, :], in_=pq.ap[:, :CTX]), reads=[pq], writes=[qcT])
            attnTc = A.alloc("attnTc", [4, 256], BF16)
            abufs = (A.ring("pTc", 1, [2, 2, 4, 128], BF16), A.ring("atokc", 1, [8, 64], BF16), A.ring("denc", 1, [2, 8], F32))
            for t in range(2):
                B.attention_tile(qcT, t * 128, ctxkb, attnTc, t * 128, abufs)
            work = (A.alloc("yconvTc", [4, 256], BF16), A.ring("gsbc", 2, [512], BF16), A.ring("tmpc", 3, [512], F32), A.alloc("mergedTc", [8, 256], BF16),
                    A.ring("mixtc", 1, [D], F32), A.ring("smc", 2, [4], F32), A.ring("xresc", 1, [D], F32), A.ring("sqc", 1, [512], BF16))
            B.mix_block(hcT, CTX, qcT, attnTc, ucT.ap, ucT, FTc.ap, FTc, wr, work, hsrc,
                        [hmid[i * 128:(i + 1) * 128, :] for i in range(2)], 1, hmid_b)
            P.barrier()
            A.release(m0)
            rings = B.norm_rings(2)
            h2c = A.alloc("h2c", [8, 256], BF16)
            B.make_hT([(hmid[i * 128:(i + 1) * 128, :], hmid_b) for i in range(2)], h2c, 1, 1, rings)
            wr = A.ring("wrf", 8, [8, 512], BF16)
            work = (A.alloc("actTc", [22, 256], BF16), A.ring("sgc", 2, [512], BF16),
                    (A.ring("mixtf", 1, [D], F32), A.ring("smf", 2, [4], F32), A.ring("xresf", 1, [D], F32), A.ring("sqf", 1, [512], BF16)))
            B.ffn_dense_block(h2c, CTX, wr, work, [(hmid[i * 128:(i + 1) * 128, :], hmid_b) for i in range(2)],
                              [hc1[i * 128:(i + 1) * 128, :] for i in range(2)], 1, hc1_b, False)
        P.barrier()
        A.release(m0)

        B.zfg_b = zfg_b
        B.emit_fft(zsrc)
        if stop == "l0fft":
            P.dma("sp", xo[0:128, :], xh[128:256, :], reads=[B.ft_b], writes=[xo_b], is_output=True)
            P.finish()
            return B

        m0 = A.mark()
        rings = B.norm_rings(2)
        hTr = A.ring("hTm", 2, [8, 512], BF16)
        rope_r = A.ring("ropeM", 1, [2, 512], F32)
        qraw = A.alloc("qraw", [512], BF16)
        tmpm_ring = A.ring("tmpm", 3, [512], F32)
        rtmp = (tmpm_ring.tiles[0], tmpm_ring.tiles[1])
        qT = A.alloc("qT", [4, 512], BF16)
        attnT = A.alloc("attnT", [4, 512], BF16)
        abufs = (A.ring("pT", 2, [5, 2, 4, 128], BF16), A.ring("atok", 2, [8, 64], BF16), A.ring("den", 2, [2, 8], F32))
        ubr = A.ring("ub", 1, [4, 514], BF16)
        ftr = A.ring("ftb", 1, [4, 512], BF16)
        wr = A.ring("wrm", 3, [8, 512], BF16)
        work = (A.alloc("yconvT", [4, 512], BF16), A.ring("gsb", 2, [512], BF16), tmpm_ring, A.alloc("mergedT", [8, 512], BF16),
                A.ring("mixt", 1, [D], F32), A.ring("smm", 2, [4], F32), A.ring("xres", 1, [D], F32), A.ring("sqm", 1, [512], BF16))

        def _mkm(bi_, defer):
            h_ = hTr.next()
            own_ = [xtile(4 * bi_ + i) for i in range(4)]
            B.make_hT(own_, h_, 0, 0, rings, defer=defer)
            return h_, own_
        nxtm = _mkm(0, False)
        for bi in range(8):
            c0 = (4 * bi + 1) * 128
            B.flush()
            hT, own = nxtm
            if bi + 1 < 8:
                nxtm = _mkm(bi + 1, True)
            rp = rope_r.next()
            P.dma("sp", rp.ap[:, 0, :], B.din["ropeC"][:, c0:c0 + 512], writes=[rp])
            P.dma("sp", rp.ap[:, 1, :], B.din["ropeS"][:, c0:c0 + 512], writes=[rp])
            B.ropeb = rp
            wq = wr.next()
            B.load(wq, winP[:, PM_Q:PM_Q + 512].rearrange("(k p) n -> p k n", p=128), "pool")
            for m in range(4):
                pq = B.proj_fm(wq, m * 128, hT, 512)
                B.rope(pq, qraw, qT.ap[:, m, :], qT, rp.ap[:, 0, :], rp.ap[:, 1, :], 512, rtmp)
            def _kbs(t_):
                T = 4 * bi + t_
                kbs = []
                for d_, mi in ((0, 2 if T == 0 else 0), (1, None), (2, 3 if T == 31 else 1)):
                    sl = T + d_
                    kbs.append((B.kT.ap[:, sl * 128:(sl + 1) * 128], B.kT, B.vaug.ap[:, sl, :, :], B.vaug, mi))
                return kbs + ctxkb
            kb_cur = _kbs(0)
            pT_cur = B.attention_scores(qT, 0, kb_cur, abufs)
            for t in range(4):
                if t + 1 < 4:
                    kb_nxt = _kbs(t + 1)
                    pT_nxt = B.attention_scores(qT, (t + 1) * 128, kb_nxt, abufs)
                B.attention_pv(pT_cur, kb_cur, attnT, t * 128, abufs)
                if t + 1 < 4:
                    kb_cur, pT_cur = kb_nxt, pT_nxt
                if t in (0, 2):
                    B.tick()
            ub = ubr.next()
            P.dma("sp", ub.ap, B.u_d[:, :, c0 - 1:c0 + 513], reads=[B.u_b], writes=[ub])
            ftb = ftr.next()
            P.dma("sp", ftb.ap, B.ft_d[:, :, bi * 512:(bi + 1) * 512], reads=[B.ft_b], writes=[ftb])
            B.mix_block(hT, 512, qT, attnT, ub.ap, ub, ftb.ap, ftb, wr, work, own,
                        [xmid[(4 * bi + i) * 128:(4 * bi + i + 1) * 128, :] for i in range(4)], 0, xmid_b)
        P.barrier()
        A.release(m_mix)

        m0 = A.mark()
        if not last:
            rings = B.norm_rings(4)
            h2r = A.ring("h2T", 2, [8, 512], BF16)
            wr = A.ring("wrf2", 8, [8, 512], BF16)
            work = (A.alloc("actT", [22, 512], BF16), A.ring("sg", 2, [512], BF16),
                    (A.ring("mixtF", 1, [D], F32), A.ring("smF", 2, [4], F32), A.ring("xresF", 2, [D], F32), A.ring("sqF", 1, [512], BF16)))
            def _mk2(bi_, defer):
                h_ = h2r.next()
                src_ = [(xmid[(4 * bi_ + i) * 128:(4 * bi_ + i + 1) * 128, :], xmid_b) for i in range(4)]
                B.make_hT(src_, h_, 0, 1, rings, defer=defer)
                return h_, src_
            nxt = _mk2(0, False)
            for bi in range(8):
                B.flush()
                h2T, src = nxt
                if bi + 1 < 8:
                    nxt = _mk2(bi + 1, True)
                B.ffn_dense_block(h2T, 512, wr, work, src,
                                  [x1[(4 * bi + i) * 128:(4 * bi + i + 1) * 128, :] for i in range(4)], 0, x1_b, False)
            P.dma("sp", xb_src[0:128, :], x1[0:128, :], reads=[x1_b], writes=[xb_b])
            P.dma("sp", xb_src[128:256, :], x1[TOK - 128:TOK, :], reads=[x1_b], writes=[xb_b])
            if sim_cc:
                for r in range(4):
                    P.dma("sp", xb_all[r * 256:(r + 1) * 256, :], xb_src, reads=[xb_b], writes=[xball_b])
            else:
                P.cc_allgather(xb_all, xb_src, GROUPS, reads=[xb_b], writes=[xball_b])
            P.barrier()
            A.release(m0)
            m0 = A.mark()
            sel = B.const_tile("selh", [8], F32, selh)
            hal = A.alloc("hal", [2, D], F32)
            gat = A.ring("gat", 2, [D], F32)
            for side in range(2):
                for r in range(4):
                    gt = gat.next()
                    row0 = r * 256 + (128 if side == 0 else 0)
                    P.dma("sp", gt.ap, xb_all[row0:row0 + 128, :], reads=[xball_b], writes=[gt])
                    sc_ap = sel.ap[:, side * 4 + r:side * 4 + r + 1]
                    if r == 0:
                        P.op("dve", lambda e, gt=gt, side=side, sc_ap=sc_ap: e.tensor_scalar(out=hal.ap[:, side, :], in0=gt.ap, scalar1=sc_ap, scalar2=None, op0=ALU.mult),
                             reads=[gt, sel], writes=[hal])
                    else:
                        P.op("dve", lambda e, gt=gt, side=side, sc_ap=sc_ap: e.scalar_tensor_tensor(out=hal.ap[:, side, :], in0=gt.ap, scalar=sc_ap, in1=hal.ap[:, side, :],
                                                                                              op0=ALU.mult, op1=ALU.add), reads=[gt, sel, hal], writes=[hal])
                P.dma("sp", xhalo[side * 128:(side + 1) * 128, :], hal.ap[:, side, :], reads=[hal], writes=[xhalo_b])
        else:
            rings = B.norm_rings(4)
            h2r = A.ring("h2T", 2, [8, 1024], BF16)
            wr = A.ring("wre", 6, [8, 512], BF16)
            wrt = B.const_tile("wrt", [8, NEXP], BF16, B.dl("wrt"))
            brt = B.const_tile("brt", [NEXP], F32, B.dl("brt"))
            work = (A.alloc("acc", [8, D], F32), A.ring("actc", 2, [4, 1024], BF16), A.ring("sge", 2, [512], BF16), A.alloc("gates", [8, NEXP], F32),
                    A.ring("lg", 2, [5, 8], F32),
                    (A.ring("mixtE", 1, [D], F32), A.ring("smE", 2, [4], F32), A.ring("xresE", 2, [D], F32), A.ring("sqE", 1, [512], BF16)), wrt, brt)
            def _mk3(bi_, defer):
                h_ = h2r.next()
                src_ = [(xmid[(8 * bi_ + i) * 128:(8 * bi_ + i + 1) * 128, :], xmid_b) for i in range(8)]
                B.make_hT(src_, h_, 0, 1, rings, defer=defer)
                return h_, src_
            nxt = _mk3(0, False)
            for bi in range(4):
                B.flush()
                h2T, src = nxt
                if bi + 1 < 4:
                    nxt = _mk3(bi + 1, True)
                B.ffn_moe_block(h2T, wr, work, src, [xo[(8 * bi + i) * 128:(8 * bi + i + 1) * 128, :] for i in range(8)], xo_b)
        P.barrier()
        A.release(m_top)
        if stop == "l0" and l == 0:
            P.dma("sp", xo[0:256, :], xhalo, reads=[xhalo_b], writes=[xo_b], is_output=True)
            P.dma("sp", xo[256:TOK, :], x1[256:TOK, :], reads=[x1_b], writes=[xo_b], is_output=True)
            P.finish()
            return B
    P.finish()
    return B


_FUSED = {}


def kernel(**inputs):
    tabs = _static_tables()
    x = np.asarray(inputs["x"], np.float32)
    com = {}
    for l in range(2):
        for k, v in _layer_common(inputs, l).items():
            com[k + str(l)] = v
    maps = []
    for cid in range(8):
        b, j = cid // 4, cid % 4
        m = dict(com)
        for nm in ("ident", "rt", "t1", "cbsb", "t256"):
            m[nm] = tabs[nm]
        for nm in ("masks", "valid", "ropeC", "ropeS", "etab"):
            m[nm] = tabs[(nm, j)]
        m["ccols"] = _ccols(inputs, b)
        m["xh"] = _xh(x, cid)
        m["hctx"] = np.ascontiguousarray(np.asarray(inputs["ctx"][b], np.float32))
        sel = np.zeros((128, 8), np.float32)
        if j - 1 >= 0:
            sel[:, j - 1] = 1.0
        if j + 1 <= 3:
            sel[:, 4 + j + 1] = 1.0
        m["selh"] = sel
        maps.append(m)
    if "B" not in _FUSED:
        _FUSED["B"] = build_fused()
    r = _run(_FUSED["B"], maps)
    out = np.empty_like(x)
    for cid in range(8):
        b, j = cid // 4, cid % 4
        out[b, TOK * j:TOK * (j + 1)] = np.asarray(r[cid]["xo"], np.float32)
    return out
```
